# Optimizing a Trainium2 kernel written in Bass

```python
import math
import jax
import jax.numpy as jnp
from jax import lax
import numpy as np

D_MODEL = 2048
BATCH = 4
SEQ = 8192
DEPTH = 1

CHUNK = 64
QBLK = 128
RMS_EPS = 1e-6
ROPE_THETA = 500000.0

A_HEADS = 8
A_KV_HEADS = 2
A_HEAD_DIM = 128
A_ROT_DIM = A_HEAD_DIM // 4
IDX_HEADS = 16
IDX_DIM = 64
IDX_ROT_DIM = IDX_DIM // 4
TOPK_MAX = 256

B_HEADS = 8
B_HEAD_DIM = 128

N_EXPERTS = 32
TOP_K_EXPERTS = 4
D_FF = D_MODEL
SWIGLU_LIMIT = 7.0
SWIGLU_ALPHA = 1.702
MOE_BLOCK = 128

A_Q_W = A_HEADS * A_HEAD_DIM
A_KV_W = A_KV_HEADS * A_HEAD_DIM
B_W = B_HEADS * B_HEAD_DIM
IDX_Q_W = IDX_HEADS * IDX_DIM
IN_SPLITS = (A_Q_W, A_KV_W, A_KV_W, B_W, B_W, B_W, IDX_Q_W, IDX_DIM, IDX_HEADS, D_MODEL, D_MODEL)
IN_W = A_Q_W + 2 * A_KV_W + 3 * B_W + IDX_Q_W + IDX_DIM + IDX_HEADS + 2 * D_MODEL

kernel_name = 'hybrid_dsa_stickbreak_moe_block'


def rms_norm(x, g):
    xf = x.astype(jnp.float32)
    y = xf * lax.rsqrt(jnp.mean(xf * xf, axis=-1, keepdims=True) + RMS_EPS)
    return (y * g.astype(jnp.float32)).astype(x.dtype)


def partial_rope(x, positions, rot_dim):
    half = rot_dim // 2
    inv_freq = ROPE_THETA ** (-jnp.arange(half, dtype=jnp.float32) / half)
    ang = positions.astype(jnp.float32)[..., None] * inv_freq
    cos = jnp.cos(ang)[:, :, None, :]
    sin = jnp.sin(ang)[:, :, None, :]
    xf = x.astype(jnp.float32)
    x1, x2, rest = xf[..., :half], xf[..., half:rot_dim], xf[..., rot_dim:]
    out = jnp.concatenate([x1 * cos - x2 * sin, x2 * cos + x1 * sin, rest], axis=-1)
    return out.astype(x.dtype)


def split_cols(z):
    outs = []
    off = 0
    for w in IN_SPLITS:
        outs.append(z[..., off:off + w])
        off += w
    return outs


def dsa_mixer(q, k, v, qi, ki, wi):
    bsz, seq = q.shape[0], q.shape[1]
    topk = min(TOPK_MAX, seq // 4)
    n_blk = seq // QBLK
    grp = A_HEADS // A_KV_HEADS
    key_pos = jnp.arange(seq)
    bidx = jnp.arange(bsz)[:, None, None]
    ki32 = ki.astype(jnp.float32)

    def block(i):
        t0 = i * QBLK
        t = t0 + jnp.arange(QBLK)
        limit = (t // CHUNK + 1) * CHUNK
        admissible = key_pos[None, :] < limit[:, None]
        qi_b = lax.dynamic_slice_in_dim(qi, t0, QBLK, axis=1).astype(jnp.float32)
        wi_b = lax.dynamic_slice_in_dim(wi, t0, QBLK, axis=1).astype(jnp.float32)
        dots = jnp.einsum('bqhd,bsd->bhqs', qi_b, ki32) * (IDX_DIM ** -0.5)
        score = jnp.einsum('bhqs,bqh->bqs', jax.nn.relu(dots), wi_b) * (IDX_HEADS ** -0.5)
        score = jnp.where(admissible[None], score, -jnp.inf)
        _, sel = lax.top_k(score, topk)
        sel_ok = sel < limit[None, :, None]
        kg = k[bidx, sel]
        vg = v[bidx, sel]
        q_b = lax.dynamic_slice_in_dim(q, t0, QBLK, axis=1).reshape(bsz, QBLK, A_KV_HEADS, grp, A_HEAD_DIM)
        logits = jnp.einsum('bqjgd,bqnjd->bqjgn', q_b, kg).astype(jnp.float32) * (A_HEAD_DIM ** -0.5)
        logits = jnp.where(sel_ok[:, :, None, None, :], logits, -jnp.inf)
        p = jax.nn.softmax(logits, axis=-1).astype(v.dtype)
        o = jnp.einsum('bqjgn,bqnjd->bqjgd', p, vg)
        return o.reshape(bsz, QBLK, A_Q_W)

    out = lax.map(block, jnp.arange(n_blk))
    return jnp.transpose(out, (1, 0, 2, 3)).reshape(bsz, seq, A_Q_W)


def stick_breaking_mixer(q, k, v):
    bsz, seq = q.shape[0], q.shape[1]
    n_blk = seq // QBLK
    key_pos = jnp.arange(seq)

    def block(i):
        t0 = i * QBLK
        t = t0 + jnp.arange(QBLK)
        causal = key_pos[None, :] < t[:, None]
        q_b = lax.dynamic_slice_in_dim(q, t0, QBLK, axis=1)
        z = jnp.einsum('bqhd,bshd->bhqs', q_b, k).astype(jnp.float32) * (B_HEAD_DIM ** -0.5)
        log_beta = jax.nn.log_sigmoid(z)
        log_keep = jnp.where(causal, log_beta - z, 0.0)
        log_gap = lax.cumsum(log_keep, axis=3, reverse=True) - log_keep
        attn = jnp.where(causal, jnp.exp(log_beta + log_gap), 0.0).astype(v.dtype)
        return jnp.einsum('bhqs,bshd->bqhd', attn, v).reshape(bsz, QBLK, B_W)

    out = lax.map(block, jnp.arange(n_blk))
    return jnp.transpose(out, (1, 0, 2, 3)).reshape(bsz, seq, B_W)


def clamped_swiglu(hid):
    glu = jnp.minimum(hid[..., ::2], SWIGLU_LIMIT)
    lin = jnp.clip(hid[..., 1::2], -SWIGLU_LIMIT, SWIGLU_LIMIT)
    return glu * jax.nn.sigmoid(SWIGLU_ALPHA * glu) * (lin + 1.0)


def moe_ffn(h, w_router, b_router, w1, b1, w2, b2):
    n_tok, d = h.shape
    logits = (h @ w_router).astype(jnp.float32) + b_router.astype(jnp.float32)
    top_logit, top_e = lax.top_k(logits, TOP_K_EXPERTS)
    top_w = jax.nn.softmax(top_logit, axis=-1).astype(h.dtype)
    n_slots = n_tok * TOP_K_EXPERTS
    flat_e = top_e.reshape(-1)
    flat_tok = jnp.arange(n_slots, dtype=jnp.int32) // TOP_K_EXPERTS
    flat_w = top_w.reshape(-1)
    order = jnp.argsort(flat_e)
    se, stok, sw = flat_e[order], flat_tok[order], flat_w[order]
    counts = jnp.bincount(flat_e, length=N_EXPERTS)
    padded = (counts + MOE_BLOCK - 1) // MOE_BLOCK * MOE_BLOCK
    pad_end = jnp.cumsum(padded)
    pad_start = pad_end - padded
    start = jnp.cumsum(counts) - counts
    dest = pad_start[se] + jnp.arange(n_slots, dtype=jnp.int32) - start[se]
    n_blocks = -(-n_slots // MOE_BLOCK) + N_EXPERTS
    n_rows = n_blocks * MOE_BLOCK
    buf_tok = jnp.full((n_rows,), n_tok, dtype=jnp.int32).at[dest].set(stok)
    buf_w = jnp.zeros((n_rows,), dtype=h.dtype).at[dest].set(sw)
    block_e = jnp.minimum(jnp.searchsorted(pad_end, jnp.arange(n_blocks) * MOE_BLOCK, side='right'), N_EXPERTS - 1)
    h_pad = jnp.concatenate([h, jnp.zeros((1, d), h.dtype)], axis=0)

    def expert_block(args):
        tok, wgt, e = args
        hid = h_pad[tok] @ w1[e] + b1[e]
        out = clamped_swiglu(hid) @ w2[e] + b2[e]
        return out * wgt[:, None]

    out = lax.map(expert_block, (buf_tok.reshape(n_blocks, MOE_BLOCK), buf_w.reshape(n_blocks, MOE_BLOCK), block_e))
    y = jax.ops.segment_sum(out.reshape(n_rows, d), buf_tok, num_segments=n_tok + 1)
    return y[:n_tok]


def setup_inputs(seed: int = 0) -> dict:
    key = jax.random.key(seed)
    ks = jax.random.split(key, 20)
    f32 = jnp.float32
    nrm = jax.random.normal
    x = nrm(ks[0], (BATCH, SEQ, D_MODEL), f32)
    c = nrm(ks[1], (BATCH, D_MODEL), f32)
    offset = jax.random.randint(ks[2], (BATCH, 1), 0, 4096, dtype=jnp.int32)
    positions = offset + jnp.arange(SEQ, dtype=jnp.int32)[None, :]
    w_ada = nrm(ks[3], (DEPTH, D_MODEL, 6 * D_MODEL), f32) * (0.5 * D_MODEL ** -0.5)
    b_ada = nrm(ks[4], (DEPTH, 6 * D_MODEL), f32) * 0.02
    g_pre_mix = 1.0 + 0.05 * nrm(ks[5], (DEPTH, D_MODEL), f32)
    g_post_mix = 1.0 + 0.05 * nrm(ks[6], (DEPTH, D_MODEL), f32)
    w_in = nrm(ks[7], (DEPTH, D_MODEL, IN_W), f32) * D_MODEL ** -0.5
    w_branch_a = nrm(ks[8], (DEPTH, A_Q_W, D_MODEL), f32) * A_Q_W ** -0.5
    w_branch_b = nrm(ks[9], (DEPTH, B_W, D_MODEL), f32) * B_W ** -0.5
    w_out = nrm(ks[10], (DEPTH, D_MODEL, D_MODEL), f32) * D_MODEL ** -0.5
    g_pre_ffn = 1.0 + 0.05 * nrm(ks[11], (DEPTH, D_MODEL), f32)
    g_post_ffn = 1.0 + 0.05 * nrm(ks[12], (DEPTH, D_MODEL), f32)
    w_router = nrm(ks[13], (DEPTH, D_MODEL, N_EXPERTS), f32) * D_MODEL ** -0.5
    b_router = nrm(ks[14], (DEPTH, N_EXPERTS), f32) * 0.01
    w1 = nrm(ks[15], (DEPTH, N_EXPERTS, D_MODEL, 2 * D_FF), f32) * D_MODEL ** -0.5
    b1 = nrm(ks[16], (DEPTH, N_EXPERTS, 2 * D_FF), f32) * 0.02
    w2 = nrm(ks[17], (DEPTH, N_EXPERTS, D_FF, D_MODEL), f32) * D_FF ** -0.5
    b2 = nrm(ks[18], (DEPTH, N_EXPERTS, D_MODEL), f32) * 0.02
    return {'x': x, 'c': c, 'positions': positions, 'w_ada': w_ada, 'b_ada': b_ada,
            'g_pre_mix': g_pre_mix, 'g_post_mix': g_post_mix, 'w_in': w_in,
            'w_branch_a': w_branch_a, 'w_branch_b': w_branch_b, 'w_out': w_out,
            'g_pre_ffn': g_pre_ffn, 'g_post_ffn': g_post_ffn, 'w_router': w_router,
            'b_router': b_router, 'w1': w1, 'b1': b1, 'w2': w2, 'b2': b2}


def reference(x, c, positions, w_ada, b_ada, g_pre_mix, g_post_mix, w_in, w_branch_a, w_branch_b, w_out, g_pre_ffn, g_post_ffn, w_router, b_router, w1, b1, w2, b2):
    bsz, seq, d = x.shape
    for l in range(DEPTH):
        mod = jax.nn.silu(c) @ w_ada[l] + b_ada[l]
        sh1, sc1, ga1, sh2, sc2, ga2 = jnp.split(mod, 6, axis=-1)

        h = rms_norm(x, g_pre_mix[l]) * (1.0 + sc1[:, None, :]) + sh1[:, None, :]
        z = h @ w_in[l]
        qa, ka, va, qb, kb, vb, qi, ki, wi, gate_a, gate_b = split_cols(z)
        qa = partial_rope(qa.reshape(bsz, seq, A_HEADS, A_HEAD_DIM), positions, A_ROT_DIM)
        ka = partial_rope(ka.reshape(bsz, seq, A_KV_HEADS, A_HEAD_DIM), positions, A_ROT_DIM)
        va = va.reshape(bsz, seq, A_KV_HEADS, A_HEAD_DIM)
        qi = partial_rope(qi.reshape(bsz, seq, IDX_HEADS, IDX_DIM), positions, IDX_ROT_DIM)
        ki = partial_rope(ki[:, :, None, :], positions, IDX_ROT_DIM)[:, :, 0, :]
        o_a = dsa_mixer(qa, ka, va, qi, ki, wi)
        o_b = stick_breaking_mixer(qb.reshape(bsz, seq, B_HEADS, B_HEAD_DIM),
                                   kb.reshape(bsz, seq, B_HEADS, B_HEAD_DIM),
                                   vb.reshape(bsz, seq, B_HEADS, B_HEAD_DIM))
        merged = jax.nn.sigmoid(gate_a) * (o_a @ w_branch_a[l]) + jax.nn.sigmoid(gate_b) * (o_b @ w_branch_b[l])
        mix = merged @ w_out[l]
        x = x + ga1[:, None, :] * rms_norm(mix, g_post_mix[l])

        h2 = rms_norm(x, g_pre_ffn[l]) * (1.0 + sc2[:, None, :]) + sh2[:, None, :]
        f = moe_ffn(h2.reshape(bsz * seq, d), w_router[l], b_router[l], w1[l], b1[l], w2[l], b2[l]).reshape(bsz, seq, d)
        x = x + ga2[:, None, :] * rms_norm(f, g_post_ffn[l])
    return x
```

```python
import math
from contextlib import ExitStack

import numpy as np
import ml_dtypes

import concourse.bass as bass
import concourse.mybir as mybir
from concourse.bass_utils import run_bass_kernel_spmd

F32 = mybir.dt.float32
BF16 = mybir.dt.bfloat16
I32 = mybir.dt.int32
AF = mybir.ActivationFunctionType
ALU = mybir.AluOpType
AX = mybir.AxisListType

D = 2048
KC = D // 128
A_H, A_KV, HD = 8, 2, 128
IDX_H, IDX_D = 16, 64
B_H = 8
TOPK = 256
THETA = 500000.0
EPS = 1e-6
LIMIT = 7.0
ALPHA = 1.702
C_QA, C_KA, C_VA = 0, 1024, 1280
C_QB, C_KB, C_VB = 1536, 2560, 3584
C_QI, C_KI, C_WI = 4608, 5632, 5696
C_GA, C_GB = 5712, 7760
IN_W = 9808
NEG = -1.0e30
NBIS = 26


class Buf:
    __slots__ = ("w", "r", "name", "excl")

    def __init__(self, name="", excl=False):
        self.w = None
        self.r = []
        self.name = name
        self.excl = excl


class Sched:
    ENGS = ("pe", "act", "dve", "pool", "sp")
    RQ = {"sp": 8, "pool": 2, "act": 4}

    def __init__(self, nc, es):
        self.nc = nc
        self.sems = {}
        for e in self.ENGS:
            self.sems[e] = es.enter_context(nc.semaphore("c_" + e))
        self.dq = {}
        for q in ("sp", "pool", "act"):
            self.dq[q] = [es.enter_context(nc.semaphore(f"d_{q}{i}")) for i in range(self.RQ[q])]
        self.n = {e: 0 for e in self.ENGS}
        self.dn = {q: 0 for q in self.dq}
        self.seen = {e: {} for e in self.ENGS}
        self.streams = {e: [] for e in self.ENGS}
        self.last = {}

    def _sem(self, key):
        return self.sems[key] if isinstance(key, str) else self.dq[key[0]][key[1]]

    def _waits(self, eng, deps):
        out = []
        best = {}
        for d in deps:
            if d is None:
                continue
            k, v = d
            if k == "pe" and eng == "pe":
                continue
            if best.get(k, 0) < v:
                best[k] = v
        for k, v in best.items():
            if self.seen[eng].get(k, 0) >= v:
                continue
            self.seen[eng][k] = v
            out.append((k, v))
        return out

    def _deps(self, r, w):
        deps = []
        for b in r:
            deps.append(b.w)
            if b.excl:
                deps.extend(b.r)
        for b in w:
            deps.append(b.w)
            deps.extend(b.r)
        return deps

    def _commit(self, tok, r, w):
        for b in r:
            b.r.append(tok)
        for b in w:
            b.w = tok
            b.r = []
        self.last[tok[0]] = tok[1]

    def op(self, eng, fn, r=(), w=()):
        waits = self._waits(eng, self._deps(r, w))
        self.n[eng] += 1
        tok = (eng, self.n[eng])
        self.streams[eng].append((waits, fn, self.sems[eng], 1))
        self._commit(tok, r, w)

    def dma(self, q, fn, r=(), w=()):
        i = self.dn[q]
        self.dn[q] += 1
        R = self.RQ[q]
        key = (q, i % R)
        deps = self._deps(r, w)
        if i >= R:
            deps.append((key, 16 * (i // R)))
        waits = self._waits(q, deps)
        tok = (key, 16 * (i // R + 1))
        self.streams[q].append((waits, fn, self._sem(key), 16))
        self._commit(tok, r, w)

    def barrier(self):
        toks = list(self.last.items())
        for e in self.ENGS:
            waits = self._waits(e, [t for t in toks if not (t[0] == e)])
            if waits:
                self.streams[e].append((waits, None, None, 0))

    def breg(self, e, val):
        if val not in self._regs:
            self._regs[val] = e.to_reg(val)
        return self._regs[val]

    def emit(self):
        nc = self.nc
        streams = self.streams
        self._regs = {}
        self.streams = {e: [] for e in self.ENGS}

        def run(e, lst):
            for waits, fn, sem, inc in lst:
                for k, v in waits:
                    e.wait_ge(self._sem(k), v)
                if fn is not None:
                    try:
                        fn(e).then_inc(sem, inc)
                    except Exception:
                        print("EMIT FAIL at stream idx", lst.index((waits, fn, sem, inc)), "of", len(lst), flush=True)
                        raise

        with nc.Block() as block:
            @block.tensor
            def _(e):
                run(e, streams["pe"])

            @block.scalar
            def _(e):
                run(e, streams["act"])

            @block.vector
            def _(e):
                run(e, streams["dve"])

            @block.gpsimd
            def _(e):
                run(e, streams["pool"])

            @block.sync
            def _(e):
                run(e, streams["sp"])


class Ring:
    def __init__(self, nc, es, name, shape, dtype, n, psum=False):
        self.tiles = []
        for i in range(n):
            if psum:
                t = es.enter_context(nc.psum_tensor(f"{name}{i}", shape, dtype))
            else:
                t = es.enter_context(nc.sbuf_tensor(f"{name}{i}", shape, dtype))
            self.tiles.append((t, Buf(f"{name}{i}", excl=psum)))
        self.i = 0

    def next(self):
        t = self.tiles[self.i % len(self.tiles)]
        self.i += 1
        return t


def own_groups(G, half):
    return [g for g in range(G) if ((g % 4) in (0, 3)) == (half == 0)]


def build(cfg):
    S = cfg["S"]
    NE = cfg["NE"]
    CAP = cfg["CAP"]
    dbg = cfg.get("dbg", False)
    upto = cfg.get("upto", "Z")
    So = S // 2
    G = S // 512
    Go = G // 2
    NT = S // 128
    NTo = So // 128
    SG = min(2048, So)
    nc = bass.Bass("TRN2", target_bir_lowering=False)
    es = ExitStack()
    scratch_kind = "ExternalOutput" if dbg else "Internal"

    def din(name, shape, dt):
        return nc.dram_tensor(name, list(shape), dt, kind="ExternalInput").ap()

    def dscr(name, shape, dt):
        return nc.dram_tensor(name, list(shape), dt, kind=scratch_kind).ap()

    x_all = din("x_all", [S, D], F32)
    x_own = din("x_own", [So, D], F32)
    pos_all = din("pos_all", [1, S], I32)
    pos_own = din("pos_own", [1, So], I32)
    cvec = din("cvec", [128, KC], F32)
    badac = din("badac", [128, 6 * KC], F32)
    gcols = din("gcols", [128, 4 * KC], F32)
    b_ada = din("b_ada", [1, 6 * D], F32)
    g_post_mix = din("g_post_mix", [1, D], F32)
    g_post_ffn = din("g_post_ffn", [1, D], F32)
    w_ada = din("w_ada", [D, 6 * D], F32)
    w_in = din("w_in", [D, IN_W], F32)
    w_bra = din("w_branch_a", [A_H * HD, D], F32)
    w_brb = din("w_branch_b", [B_H * HD, D], F32)
    w_out = din("w_out", [D, D], F32)
    w_router = din("w_router", [D, NE], F32)
    b_router = din("b_router", [1, NE], F32)
    w1 = din("w1", [NE, D, 2 * D], F32)
    b1c = din("b1c", [NE, 128, 2 * KC], F32)
    w2 = din("w2", [NE, D, D], F32)
    b2 = din("b2", [NE, D], F32)
    cmask_d = din("cmask", [128, 2 * 8 * 512], BF16)
    amask_d = din("amask", [128, 2 * 640], F32)
    consts_d = din("consts", [128, 8 * 128], F32)
    out_d = nc.dram_tensor("out", [So, D], F32, kind="ExternalOutput").ap()

    KA = dscr("s_ka", [A_KV, 128, S], BF16)
    KB = dscr("s_kb", [B_H, 128, S], BF16)
    KI = dscr("s_ki", [IDX_D, S], BF16)
    VA = dscr("s_va", [S, A_KV * HD], BF16)
    VB = dscr("s_vb", [S, B_H * HD], BF16)
    QA = dscr("s_qa", [A_H, 128, So], BF16)
    QB = dscr("s_qb", [B_H, 128, So], BF16)
    QI = dscr("s_qi", [IDX_H // 2, 128, So], BF16)
    WI = dscr("s_wi", [So, IDX_H], F32)
    GA = dscr("s_ga", [KC, 128, So], BF16)
    GB = dscr("s_gb", [KC, 128, So], BF16)
    MK = dscr("s_mk", [NTo, 128, S], BF16)
    OA = dscr("s_oa", [A_H, 128, So], BF16)
    OB = dscr("s_ob", [B_H, 128, So], BF16)
    MG = dscr("s_mg", [KC, 128, So], BF16)
    X1 = dscr("s_x1", [So, D], F32)
    XG = dscr("s_xg", [NE * CAP, D], BF16)
    YG = dscr("s_yg", [NE * CAP, D], BF16)

    sch = Sched(nc, es)
    op, dma = sch.op, sch.dma
    dbg_outs = {}

    def finish():
        if dbg:
            d1 = nc.dram_tensor("dbg_modc", [128, 4 * KC], F32, kind="ExternalOutput").ap()
            d2 = nc.dram_tensor("dbg_ga", [128, 2 * D], F32, kind="ExternalOutput").ap()
            dma("sp", lambda e: e.dma_start(out=d1[:, :], in_=modc[:]), r=[b_modc])
            dma("sp", lambda e: e.dma_start(out=d2[:, 0:D], in_=ga1row[:]), r=[b_garow[0]])
            dma("sp", lambda e: e.dma_start(out=d2[:, D:2 * D], in_=ga2row[:]), r=[b_garow[1]])
            d3 = nc.dram_tensor("dbg_rslot", [128, NTo * 4], I32, kind="ExternalOutput").ap()
            d4 = nc.dram_tensor("dbg_rwgt", [128, NTo * 4], F32, kind="ExternalOutput").ap()
            dma("sp", lambda e: e.dma_start(out=d3[:, :], in_=rslot[:]), r=[b_route])
            dma("sp", lambda e: e.dma_start(out=d4[:, :], in_=rwgt[:]), r=[b_route])
        sch.barrier()
        sch.emit()
        es.close()
        return nc

    def sbp(name, shape, dt):
        return es.enter_context(nc.sbuf_tensor(name, shape, dt))

    consts = sbp("consts_s", [128, 8 * 128], F32)
    b_consts = Buf("consts")
    identb = sbp("identb", [128, 128], BF16)
    rtA = sbp("rtA", [128, 128], BF16)
    rtI = sbp("rtI", [128, 128], BF16)
    triI = sbp("triI", [128, 128], BF16)
    triS = sbp("triS", [128, 128], BF16)
    onesb = sbp("onesb", [128, 128], BF16)
    onesf = consts[:, 5 * 128:6 * 128]
    invfA = consts[:, 6 * 128:6 * 128 + 1]
    invfI = consts[:, 6 * 128 + 1:6 * 128 + 2]
    iota_f = consts[:, 7 * 128:8 * 128]
    b_cb = Buf("constsb")
    modc = sbp("modc", [128, 4 * KC], F32)
    b_modc = Buf("modc")
    ga1row = sbp("ga1row", [128, D], F32)
    ga2row = sbp("ga2row", [128, D], F32)
    b_garow = [Buf("ga1row"), Buf("ga2row")]
    rslot = sbp("rslot", [128, NTo * 4], I32)
    rwgt = sbp("rwgt", [128, NTo * 4], F32)
    b_route = Buf("route")

    dma("sp", lambda e: e.dma_start(out=consts[:], in_=consts_d[:, :]), w=[b_consts])
    for i, t in enumerate((identb, rtA, rtI, triI, triS, onesb)):
        op("dve", lambda e, i=i, t=t: e.tensor_copy(out=t[:], in_=consts[:, i * 128:(i + 1) * 128]),
           r=[b_consts], w=[b_cb])

    def mmgroup(out_ap, out_buf, pairs, rbufs):
        n = len(pairs)
        for i, (l, r_) in enumerate(pairs):
            rb = rbufs if (i == 0 or i == n - 1) else ()
            op("pe", lambda e, l=l, r_=r_, i=i: e.matmul(out_ap, l, r_, start=(i == 0), stop=(i == n - 1)),
               r=rb, w=[out_buf])

    with ExitStack() as ph:
        cc = ph.enter_context(nc.sbuf_tensor("cc", [128, KC], F32))
        sg_ = ph.enter_context(nc.sbuf_tensor("sg_", [128, KC], F32))
        siluc = ph.enter_context(nc.sbuf_tensor("siluc", [128, KC], F32))
        silurep = ph.enter_context(nc.sbuf_tensor("silurep", [128, KC, 128], F32))
        badac_s = ph.enter_context(nc.sbuf_tensor("badac_s", [128, 6 * KC], F32))
        gcols_s = ph.enter_context(nc.sbuf_tensor("gcols_s", [128, 4 * KC], F32))
        rawc = ph.enter_context(nc.sbuf_tensor("rawc", [128, 4 * KC], F32))
        b_small = Buf("small")
        b_rawc = Buf("rawc")
        wch = Ring(nc, ph, "wch", [128, KC, 512], F32, 2)
        rowt = Ring(nc, ph, "rowt", [128, 2, 512], F32, 2)
        tmpr = Ring(nc, ph, "tmpr", [128, 512], F32, 2)
        psA = Ring(nc, ph, "psA", [128, 512], F32, 2, psum=True)
        psC = ph.enter_context(nc.psum_tensor("psC", [128, 512], F32))
        b_psC = Buf("psC", excl=True)

        dma("sp", lambda e: e.dma_start(out=cc[:], in_=cvec[:, :]), w=[b_small])
        dma("sp", lambda e: e.dma_start(out=badac_s[:], in_=badac[:, :]), w=[b_small])
        dma("sp", lambda e: e.dma_start(out=gcols_s[:], in_=gcols[:, :]), w=[b_small])
        op("act", lambda e: e.activation(out=sg_[:], in_=cc[:], func=AF.Sigmoid), r=[b_small], w=[b_small])
        op("dve", lambda e: e.tensor_tensor(out=siluc[:], in0=cc[:], in1=sg_[:], op=ALU.mult), r=[b_small], w=[b_small])
        op("dve", lambda e: e.tensor_copy(out=silurep[:], in_=siluc[:].unsqueeze(2).broadcast_to([128, KC, 128])),
           r=[b_small], w=[b_small])
        w_ada_v = w_ada.rearrange("(kc p) n -> p kc n", p=128)
        for idx, which in enumerate((1, 0, 4, 3)):
            for cq in range(4):
                wt, wb = wch.next()
                c0 = which * D + cq * 512
                dma("sp", lambda e, wt=wt, c0=c0: e.dma_start(out=wt[:], in_=w_ada_v[:, :, c0:c0 + 512]), w=[wb])
                for j in range(4):
                    col = idx * KC + cq * 4 + j
                    mmgroup(psC[:, col:col + 1], b_psC,
                            [(wt[:, kc, j * 128:(j + 1) * 128], siluc[:, kc:kc + 1]) for kc in range(KC)],
                            [wb, b_small])
        op("dve", lambda e: e.tensor_copy(out=rawc[:], in_=psC[:, 0:4 * KC]), r=[b_psC], w=[b_rawc])
        for idx, which in enumerate((1, 0, 4, 3)):
            op("dve", lambda e, idx=idx, which=which: e.tensor_tensor(
                out=rawc[:, idx * KC:(idx + 1) * KC], in0=rawc[:, idx * KC:(idx + 1) * KC],
                in1=badac_s[:, which * KC:(which + 1) * KC], op=ALU.add), r=[b_small, b_rawc], w=[b_rawc])
        op("dve", lambda e: e.scalar_tensor_tensor(out=modc[:, 0:KC], in0=rawc[:, 0:KC], scalar=1.0,
                                                   in1=gcols_s[:, 0:KC], op0=ALU.add, op1=ALU.mult),
           r=[b_rawc, b_small], w=[b_modc])
        op("dve", lambda e: e.tensor_copy(out=modc[:, KC:2 * KC], in_=rawc[:, KC:2 * KC]), r=[b_rawc], w=[b_modc])
        op("dve", lambda e: e.scalar_tensor_tensor(out=modc[:, 2 * KC:3 * KC], in0=rawc[:, 2 * KC:3 * KC], scalar=1.0,
                                                   in1=gcols_s[:, 2 * KC:3 * KC], op0=ALU.add, op1=ALU.mult),
           r=[b_rawc, b_small], w=[b_modc])
        op("dve", lambda e: e.tensor_copy(out=modc[:, 3 * KC:4 * KC], in_=rawc[:, 3 * KC:4 * KC]), r=[b_rawc], w=[b_modc])
        for gi, (which, grow, gpost) in enumerate(((2, ga1row, g_post_mix), (5, ga2row, g_post_ffn))):
            for cq in range(4):
                wt, wb = wch.next()
                c0 = which * D + cq * 512
                dma("sp", lambda e, wt=wt, c0=c0: e.dma_start(out=wt[:], in_=w_ada_v[:, :, c0:c0 + 512]), w=[wb])
                rt, rb = rowt.next()
                dma("sp", lambda e, rt=rt, c0=c0: e.dma_start(out=rt[:, 0, :], in_=b_ada[0:1, c0:c0 + 512].partition_broadcast(128)), w=[rb])
                dma("sp", lambda e, rt=rt, cq=cq, gpost=gpost: e.dma_start(
                    out=rt[:, 1, :], in_=gpost[0:1, cq * 512:(cq + 1) * 512].partition_broadcast(128)), w=[rb])
                pt, pb = psA.next()
                mmgroup(pt[:], pb, [(silurep[:, kc, :], wt[:, kc, :]) for kc in range(KC)], [wb, b_small])
                tt, tb = tmpr.next()
                op("dve", lambda e, tt=tt, pt=pt, rt=rt: e.tensor_tensor(out=tt[:], in0=pt[:], in1=rt[:, 0, :], op=ALU.add),
                   r=[pb, rb], w=[tb])
                op("dve", lambda e, tt=tt, rt=rt, grow=grow, cq=cq: e.tensor_tensor(
                    out=grow[:, cq * 512:(cq + 1) * 512], in0=tt[:], in1=rt[:, 1, :], op=ALU.mult),
                   r=[tb, rb], w=[b_garow[gi]])
        sch.barrier()
        sch.emit()
    if upto == "A":
        return finish()

    w_in_v = w_in.rearrange("(kc p) n -> p kc n", p=128)
    TWO_PI = 2.0 * math.pi
    BL = cfg.get('blevel', 9)
    ROPE_ADD_ENG = cfg.get('rope_add', 'dve')
    CW1 = 6.28125
    CW2 = TWO_PI - CW1

    def norm_transpose(x_src, tok0, hT, b_hT, hcol0, sc_off, xt_r, xn_r, junk, b_junk, st_r, psT_r, alt):
        xt, xb = xt_r.next()
        dma("sp", lambda e: e.dma_start(out=xt[:], in_=x_src[tok0:tok0 + 128, :]), w=[xb])
        st, sb_ = st_r.next()
        op("act", lambda e: e.activation(out=junk[:], in_=xt[:], func=AF.Square, accum_out=st[:, 0:1]),
           r=[xb], w=[b_junk, sb_])
        op("act", lambda e: e.activation(out=st[:, 1:2], in_=st[:, 0:1], func=AF.Sqrt, scale=1.0 / D, bias=EPS),
           r=[sb_], w=[sb_])
        op("dve", lambda e: e.reciprocal(out=st[:, 2:3], in_=st[:, 1:2]), r=[sb_], w=[sb_])
        xn, xnb = xn_r.next()
        op("dve", lambda e: e.tensor_scalar(out=xn[:], in0=xt[:], scalar1=st[:, 2:3], scalar2=None, op0=ALU.mult),
           r=[xb, sb_], w=[xnb])
        for half in range(2):
            pt, pb = psT_r.next()
            for j in range(8):
                kc = half * 8 + j
                op("pe", lambda e, pt=pt, j=j, kc=kc: e.transpose(pt[:, j * 128:(j + 1) * 128],
                                                                   xn[:, kc * 128:(kc + 1) * 128], identb[:]),
                   r=[xnb, b_cb] if j in (0, 7) else (), w=[pb])
            for j in range(8):
                kc = half * 8 + j
                eng = "act" if (half + alt) % 2 == 0 else "dve"
                if eng == "act":
                    op("act", lambda e, pt=pt, j=j, kc=kc: e.activation(
                        out=hT[:, kc, hcol0:hcol0 + 128], in_=pt[:, j * 128:(j + 1) * 128], func=AF.Identity,
                        scale=modc[:, sc_off + kc:sc_off + kc + 1], bias=modc[:, sc_off + KC + kc:sc_off + KC + kc + 1]),
                       r=[pb, b_modc], w=[b_hT])
                else:
                    op("dve", lambda e, pt=pt, j=j, kc=kc: e.tensor_scalar(
                        out=hT[:, kc, hcol0:hcol0 + 128], in0=pt[:, j * 128:(j + 1) * 128],
                        scalar1=modc[:, sc_off + kc:sc_off + kc + 1], scalar2=modc[:, sc_off + KC + kc:sc_off + KC + kc + 1],
                        op0=ALU.mult, op1=ALU.add), r=[pb, b_modc], w=[b_hT])

    def proj_pass(pname, x_src, pos_src, T, fm_blocks, tm_blocks):
        with ExitStack() as ph:
            sg = min(SG, T)
            ngr = sg // 512
            hT = ph.enter_context(nc.sbuf_tensor(pname + "hT", [128, KC, sg], BF16))
            b_hT = Buf("hT")
            xt_r = Ring(nc, ph, pname + "xt", [128, D], F32, 2)
            xn_r = Ring(nc, ph, pname + "xn", [128, D], BF16, 1)
            junk = ph.enter_context(nc.sbuf_tensor(pname + "junk", [128, D], BF16))
            b_junk = Buf("junk")
            st_r = Ring(nc, ph, pname + "st", [128, 4], F32, 4)
            psT_r = Ring(nc, ph, pname + "psT", [128, 1024], BF16, 2, psum=True)
            psM_r = Ring(nc, ph, pname + "psM", [128, 512], F32, 4, psum=True)
            psR_r = Ring(nc, ph, pname + "psR", [128, 512], F32, 2, psum=True)
            wfm_r = Ring(nc, ph, pname + "wfm", [128, KC, 128], BF16, 3)
            wtm_r = Ring(nc, ph, pname + "wtm", [128, KC, 512], BF16, 1)
            zb_r = Ring(nc, ph, pname + "zb", [128, 512], BF16, 2)
            t1_r = Ring(nc, ph, pname + "t1", [128, 512], F32, 2)
            t2_r = Ring(nc, ph, pname + "t2", [128, 512], F32, 2)
            ob_r = Ring(nc, ph, pname + "ob", [128, 512], BF16, 3)
            obf_r = Ring(nc, ph, pname + "obf", [128, 512], F32, 2)
            posi = ph.enter_context(nc.sbuf_tensor(pname + "posi", [128, 512], I32))
            posf = ph.enter_context(nc.sbuf_tensor(pname + "posf", [128, 512], F32))
            ang = ph.enter_context(nc.sbuf_tensor(pname + "ang", [128, 512], F32))
            kfi = ph.enter_context(nc.sbuf_tensor(pname + "kfi", [128, 512], I32))
            kff = ph.enter_context(nc.sbuf_tensor(pname + "kff", [128, 512], F32))
            msk = ph.enter_context(nc.sbuf_tensor(pname + "msk", [128, 512], F32))
            b_tmp = Buf("ropetmp")
            tabs = ph.enter_context(nc.sbuf_tensor(pname + "tabs", [128, ngr, 4, 512], F32))
            b_tabs = Buf("tabs")
            for s0 in range(0, T, sg):
                for ti in range(sg // 128):
                    norm_transpose(x_src, s0 + ti * 128, hT, b_hT, ti * 128, 0, xt_r, xn_r, junk, b_junk, st_r, psT_r, ti)
                for g in range(ngr if BL >= 2 else 0):
                    t0 = s0 + g * 512
                    dma("sp", lambda e, t0=t0: e.dma_start(out=posi[:], in_=pos_src[0:1, t0:t0 + 512].partition_broadcast(128)),
                        w=[b_tmp])
                    op("dve", lambda e: e.tensor_copy(out=posf[:], in_=posi[:]), r=[b_tmp], w=[b_tmp])
                    for ti_, (invf, shift) in enumerate(((invfA, math.pi / 2), (invfA, 0.0), (invfI, math.pi / 2), (invfI, 0.0))):
                        R_, W_ = [b_tmp, b_consts], [b_tmp]
                        op("dve", lambda e, invf=invf, shift=shift: e.tensor_scalar(
                            out=ang[:], in0=posf[:], scalar1=invf, scalar2=shift, op0=ALU.mult, op1=ALU.add), r=R_, w=W_)
                        op("dve", lambda e: e.tensor_scalar(out=kfi[:], in0=ang[:], scalar1=1.0 / TWO_PI, scalar2=None,
                                                            op0=ALU.mult), r=R_, w=W_)
                        op("dve", lambda e: e.tensor_copy(out=kff[:], in_=kfi[:]), r=R_, w=W_)
                        op("dve", lambda e: e.scalar_tensor_tensor(out=ang[:], in0=kff[:], scalar=-CW1, in1=ang[:],
                                                                   op0=ALU.mult, op1=ALU.add), r=R_, w=W_)
                        op("dve", lambda e: e.scalar_tensor_tensor(out=ang[:], in0=kff[:], scalar=-CW2, in1=ang[:],
                                                                   op0=ALU.mult, op1=ALU.add), r=R_, w=W_)
                        op("dve", lambda e: e.tensor_scalar(out=msk[:], in0=ang[:], scalar1=math.pi, scalar2=None,
                                                            op0=ALU.is_gt), r=R_, w=W_)
                        op("dve", lambda e: e.scalar_tensor_tensor(out=ang[:], in0=msk[:], scalar=-TWO_PI, in1=ang[:],
                                                                   op0=ALU.mult, op1=ALU.add), r=R_, w=W_)
                        op("dve", lambda e: e.tensor_scalar(out=msk[:], in0=ang[:], scalar1=-math.pi, scalar2=None,
                                                            op0=ALU.is_lt), r=R_, w=W_)
                        op("dve", lambda e: e.scalar_tensor_tensor(out=ang[:], in0=msk[:], scalar=TWO_PI, in1=ang[:],
                                                                   op0=ALU.mult, op1=ALU.add), r=R_, w=W_)
                        op("dve", lambda e: e.tensor_scalar(out=ang[:], in0=ang[:], scalar1=math.pi, scalar2=-math.pi,
                                                            op0=ALU.min, op1=ALU.max), r=R_, w=W_)
                        op("act", lambda e, g=g, ti_=ti_: e.activation(out=tabs[:, g, ti_, :], in_=ang[:], func=AF.Sin),
                           r=[b_tmp], w=[b_tabs, b_tmp])
                for (col0, ncol, dst, rope) in fm_blocks:
                    if BL < 3 or (BL < 4 and rope is not None):
                        continue
                    if rope is not None and cfg.get('ropeonly') and (rope, ncol) != cfg.get('ropeonly'):
                        continue
                    wt, wb = wfm_r.next()
                    dma("pool", lambda e, wt=wt, col0=col0, ncol=ncol: e.dma_start(
                        out=wt[:, :, 0:ncol], in_=w_in_v[:, :, col0:col0 + ncol]), w=[wb])
                    for g in range(ngr):
                        t0 = s0 + g * 512
                        pt, pb = psM_r.next()
                        mmgroup(pt[0:ncol, :], pb,
                                [(wt[:, kc, 0:ncol], hT[:, kc, g * 512:(g + 1) * 512]) for kc in range(KC)], [wb, b_hT])
                        ot, obb = ob_r.next()
                        if rope is None:
                            op("act", lambda e, ot=ot, pt=pt, ncol=ncol: e.activation(out=ot[0:ncol, :], in_=pt[0:ncol, :], func=AF.Copy),
                               r=[pb], w=[obb])
                        else:
                            ci = 0 if rope == "A" else 2
                            rt = rtA if rope == "A" else rtI
                            zt, zbb = zb_r.next()
                            op("act", lambda e, zt=zt, pt=pt, ncol=ncol: e.activation(out=zt[0:ncol, :], in_=pt[0:ncol, :], func=AF.Copy),
                               r=[pb], w=[zbb])
                            p2, p2b = psR_r.next()
                            if cfg.get('ropevar', 0) != 1:
                                op("pe", lambda e, p2=p2, zt=zt, rt=rt, ncol=ncol: e.matmul(
                                    p2[0:ncol, :], rt[0:ncol, 0:ncol], zt[0:ncol, :], start=True, stop=True),
                                   r=[zbb, b_cb], w=[p2b])
                            else:
                                p2, p2b = pt, pb
                            a1, a1b = t1_r.next()
                            a2, a2b = t2_r.next()
                            if cfg.get('ropevar', 0) == 2:
                                op("act", lambda e, ot=ot, pt=pt, ncol=ncol: e.activation(out=ot[0:ncol, :], in_=pt[0:ncol, :], func=AF.Copy),
                                   r=[pb], w=[obb])
                                dma("sp", lambda e, ot=ot, dst=dst, t0=t0, ncol=ncol: e.dma_start(
                                    out=dst[0:ncol, t0:t0 + 512], in_=ot[0:ncol, :]), r=[obb])
                                continue
                            if cfg.get('ropevar', 0) == 3:
                                op("dve", lambda e, ot=ot, pt=pt, g=g, ci=ci, ncol=ncol: e.tensor_tensor(
                                    out=ot[0:ncol, :], in0=pt[0:ncol, :], in1=tabs[0:ncol, g, ci, :], op=ALU.mult),
                                   r=[pb, b_tabs], w=[obb])
                                dma("sp", lambda e, ot=ot, dst=dst, t0=t0, ncol=ncol: e.dma_start(
                                    out=dst[0:ncol, t0:t0 + 512], in_=ot[0:ncol, :]), r=[obb])
                                continue
                            op("dve", lambda e, a1=a1, pt=pt, g=g, ci=ci, ncol=ncol: e.tensor_tensor(
                                out=a1[0:ncol, :], in0=pt[0:ncol, :], in1=tabs[0:ncol, g, ci, :], op=ALU.mult),
                               r=[pb, b_tabs], w=[a1b])
                            op("dve", lambda e, a2=a2, p2=p2, g=g, ci=ci, ncol=ncol: e.tensor_tensor(
                                out=a2[0:ncol, :], in0=p2[0:ncol, :], in1=tabs[0:ncol, g, ci + 1, :], op=ALU.mult),
                               r=[p2b, b_tabs], w=[a2b])
                            op(ROPE_ADD_ENG, lambda e, ot=ot, a1=a1, a2=a2, ncol=ncol: e.tensor_tensor(
                                out=ot[0:ncol, :], in0=a1[0:ncol, :], in1=a2[0:ncol, :], op=ALU.add),
                               r=[a1b, a2b], w=[obb])
                        dma("sp", lambda e, ot=ot, dst=dst, t0=t0, ncol=ncol: e.dma_start(
                            out=dst[0:ncol, t0:t0 + 512], in_=ot[0:ncol, :]), r=[obb])
                for (col0, ncol, dst, dcol0, isf32) in tm_blocks:
                    if BL < 5:
                        continue
                    wt, wb = wtm_r.next()
                    dma("pool", lambda e, wt=wt, col0=col0, ncol=ncol: e.dma_start(
                        out=wt[:, :, 0:ncol], in_=w_in_v[:, :, col0:col0 + ncol]), w=[wb])
                    for ti in range(sg // 128):
                        t0 = s0 + ti * 128
                        pt, pb = psM_r.next()
                        mmgroup(pt[:, 0:ncol], pb,
                                [(hT[:, kc, ti * 128:(ti + 1) * 128], wt[:, kc, 0:ncol]) for kc in range(KC)], [wb, b_hT])
                        ot, obb = (obf_r if isf32 else ob_r).next()
                        op("act", lambda e, ot=ot, pt=pt, ncol=ncol: e.activation(out=ot[:, 0:ncol], in_=pt[:, 0:ncol], func=AF.Copy),
                           r=[pb], w=[obb])
                        dma("sp", lambda e, ot=ot, dst=dst, t0=t0, ncol=ncol, dcol0=dcol0: e.dma_start(
                            out=dst[t0:t0 + 128, dcol0:dcol0 + ncol], in_=ot[:, 0:ncol]), r=[obb])
            sch.barrier()
            sch.emit()

    fm1 = [(C_KA + j * 128, 128, KA[j], "A") for j in range(A_KV)]
    fm1 += [(C_KB + h * 128, 128, KB[h], None) for h in range(B_H)]
    fm1 += [(C_KI, 64, KI, "I")]
    tm1 = [(C_VA, 256, VA, 0, False), (C_VB, 512, VB, 0, False), (C_VB + 512, 512, VB, 512, False)]
    proj_pass("p1", x_all, pos_all, S, fm1, tm1)
    fm2 = [(C_QA + h * 128, 128, QA[h], "A") for h in range(A_H)]
    fm2 += [(C_QB + h * 128, 128, QB[h], None) for h in range(B_H)]
    fm2 += [(C_QI + h * 128, 128, QI[h], "I") for h in range(IDX_H // 2)]
    fm2 += [(C_GA + f * 128, 128, GA[f], None) for f in range(KC)]
    fm2 += [(C_GB + f * 128, 128, GB[f], None) for f in range(KC)]
    tm2 = [(C_WI, IDX_H, WI, 0, True)]
    proj_pass("p2", x_own, pos_own, So, fm2, tm2)
    if upto == "B":
        return finish()

    def qblocks():
        for i in range(Go):
            for jj in range(4):
                qb = i * 4 + jj
                yield i, jj, qb, qb * 128, 128 * (4 * (2 * i + 1) + jj + 1), i % 2

    with ExitStack() as ph:
        kiT = ph.enter_context(nc.sbuf_tensor("kiT", [64, S], BF16))
        b_ki = Buf("kiT")
        amask_s = ph.enter_context(nc.sbuf_tensor("amask_s", [128, 2, 640], F32))
        b_am = Buf("amask")
        Ssc = ph.enter_context(nc.sbuf_tensor("Ssc", [128, S], F32))
        b_S = Buf("S")
        Mk = ph.enter_context(nc.sbuf_tensor("Mk", [128, S], BF16))
        b_M = Buf("Mk")
        qi_r = Ring(nc, ph, "qiT", [64, IDX_H, 128], BF16, 2)
        wi_r = Ring(nc, ph, "wit", [128, 3, IDX_H], F32, 2)
        rr_r = Ring(nc, ph, "rr", [128, 512], F32, 3)
        ps_r = Ring(nc, ph, "psI", [128, 512], F32, 4, psum=True)
        bs = ph.enter_context(nc.sbuf_tensor("bs", [128, 8], F32))
        b_bs = Buf("bs")
        dma("sp", lambda e: e.dma_start(out=kiT[:], in_=KI[:, :]), w=[b_ki])
        dma("sp", lambda e: e.dma_start(out=amask_s[:].rearrange("p m c -> p (m c)"), in_=amask_d[:, :]), w=[b_am])
        for (i, jj, qb, tok0, nk, m) in qblocks():
            qt, qbb = qi_r.next()
            for hh in range(2):
                dma("sp", lambda e, qt=qt, hh=hh, tok0=tok0: e.dma_start(
                    out=qt[:, hh::2, :], in_=QI[:, hh * 64:(hh + 1) * 64, tok0:tok0 + 128].rearrange("hp d t -> d hp t")),
                    w=[qbb])
            wt, wbb = wi_r.next()
            dma("sp", lambda e, wt=wt, tok0=tok0: e.dma_start(out=wt[:, 0, :], in_=WI[tok0:tok0 + 128, :]), w=[wbb])
            op("act", lambda e, wt=wt: e.activation(out=wt[:, 1, :], in_=wt[:, 0, :], func=AF.Abs), r=[wbb], w=[wbb])
            op("act", lambda e, wt=wt: e.activation(out=wt[:, 2, :], in_=wt[:, 0, :], func=AF.Sign), r=[wbb], w=[wbb])
            nch = (nk + 511) // 512
            for c in range(nch):
                c0 = c * 512
                cw = min(512, nk - c0)
                for h in range(IDX_H):
                    pt, pb = ps_r.next()
                    op("pe", lambda e, pt=pt, qt=qt, h=h, c0=c0, cw=cw: e.matmul(
                        pt[:, 0:cw], qt[:, h, :], kiT[:, c0:c0 + cw], start=True, stop=True),
                       r=[qbb, b_ki], w=[pb])
                    rt, rb = rr_r.next()
                    op("act", lambda e, rt=rt, pt=pt, wt=wt, h=h, cw=cw: e.activation(
                        out=rt[:, 0:cw], in_=pt[:, 0:cw], func=AF.Relu, scale=wt[:, 1, h:h + 1]),
                       r=[pb, wbb], w=[rb])
                    if h == 0:
                        op("dve", lambda e, rt=rt, wt=wt, h=h, c0=c0, cw=cw: e.tensor_scalar(
                            out=Ssc[:, c0:c0 + cw], in0=rt[:, 0:cw], scalar1=wt[:, 2, h:h + 1], scalar2=None, op0=ALU.mult),
                           r=[rb, wbb], w=[b_S])
                    else:
                        op("dve", lambda e, rt=rt, wt=wt, h=h, c0=c0, cw=cw: e.scalar_tensor_tensor(
                            out=Ssc[:, c0:c0 + cw], in0=rt[:, 0:cw], scalar=wt[:, 2, h:h + 1], in1=Ssc[:, c0:c0 + cw],
                            op0=ALU.mult, op1=ALU.add), r=[rb, wbb, b_S], w=[b_S])
            op("dve", lambda e, nk=nk: e.tensor_reduce(out=bs[:, 5:6], in_=Ssc[:, 0:nk], axis=AX.X, op=ALU.max),
               r=[b_S], w=[b_bs])
            op("dve", lambda e, nk=nk: e.tensor_reduce(out=bs[:, 0:1], in_=Ssc[:, 0:nk], axis=AX.X, op=ALU.min),
               r=[b_S], w=[b_bs])
            op("dve", lambda e: e.tensor_tensor(out=bs[:, 1:2], in0=bs[:, 5:6], in1=bs[:, 0:1], op=ALU.subtract),
               r=[b_bs], w=[b_bs])
            op("dve", lambda e, nk=nk, m=m: e.tensor_tensor(out=Ssc[:, nk - 640:nk], in0=Ssc[:, nk - 640:nk],
                                                             in1=amask_s[:, m, :], op=ALU.add),
               r=[b_S, b_am], w=[b_S])
            for it in range(NBIS):
                op("dve", lambda e: e.tensor_scalar(out=bs[:, 1:2], in0=bs[:, 1:2], scalar1=0.5, scalar2=None, op0=ALU.mult),
                   r=[b_bs], w=[b_bs])
                op("dve", lambda e: e.tensor_tensor(out=bs[:, 2:3], in0=bs[:, 0:1], in1=bs[:, 1:2], op=ALU.add),
                   r=[b_bs], w=[b_bs])
                op("dve", lambda e, nk=nk: e.tensor_scalar(out=Mk[:, 0:nk], in0=Ssc[:, 0:nk], scalar1=bs[:, 2:3], scalar2=None,
                                                           op0=ALU.is_ge, op1=ALU.add, accum_out=bs[:, 3:4]),
                   r=[b_bs, b_S, b_M], w=[b_M, b_bs])
                op("dve", lambda e: e.tensor_scalar(out=bs[:, 4:5], in0=bs[:, 3:4], scalar1=float(TOPK), scalar2=bs[:, 1:2],
                                                    op0=ALU.is_ge, op1=ALU.mult), r=[b_bs], w=[b_bs])
                op("dve", lambda e: e.tensor_tensor(out=bs[:, 0:1], in0=bs[:, 0:1], in1=bs[:, 4:5], op=ALU.add),
                   r=[b_bs], w=[b_bs])
            op("dve", lambda e, nk=nk: e.tensor_scalar(out=Mk[:, 0:nk], in0=Ssc[:, 0:nk], scalar1=bs[:, 0:1], scalar2=None,
                                                       op0=ALU.is_ge), r=[b_bs, b_S, b_M], w=[b_M])
            dma("sp", lambda e, qb=qb, nk=nk: e.dma_start(out=MK[qb, :, 0:nk], in_=Mk[:, 0:nk]), r=[b_M])
        sch.barrier()
        sch.emit()
    if upto == "C1":
        return finish()

    with ExitStack() as ph:
        kaT = ph.enter_context(nc.sbuf_tensor("kaT", [128, A_KV, S], BF16))
        va = ph.enter_context(nc.sbuf_tensor("va_s", [128, NT, A_KV * HD], BF16))
        b_kv = Buf("kv")
        mk_r = Ring(nc, ph, "mk", [128, S], BF16, 2)
        mt_r = Ring(nc, ph, "mt", [128, NT, 128], BF16, 2)
        qa_r = Ring(nc, ph, "qa", [128, A_H * 128], BF16, 2)
        e_r = Ring(nc, ph, "ee", [128, 512], BF16, 3)
        p_r = Ring(nc, ph, "pp", [128, 512], BF16, 3)
        rd_r = Ring(nc, ph, "rd", [128, 512], F32, 2)
        of_r = Ring(nc, ph, "of", [128, 512], F32, 2)
        ob_r = Ring(nc, ph, "obc", [128, 512], BF16, 2)
        psS = Ring(nc, ph, "psS", [128, 512], F32, 3, psum=True)
        psO = Ring(nc, ph, "psO", [128, 512], F32, 2, psum=True)
        psD = Ring(nc, ph, "psD", [128, 512], F32, 2, psum=True)
        psT = Ring(nc, ph, "psTc", [128, 1024], BF16, 1, psum=True)
        for j in range(A_KV):
            dma("sp", lambda e, j=j: e.dma_start(out=kaT[:, j, :], in_=KA[j]), w=[b_kv])
        dma("sp", lambda e: e.dma_start(out=va[:], in_=VA.rearrange("(kb p) c -> p kb c", p=128)), w=[b_kv])
        isq = 1.0 / math.sqrt(HD)
        for (i, jj, qb, tok0, nk, m) in qblocks():
            nkb = nk // 128
            mkt, mkb = mk_r.next()
            dma("sp", lambda e, mkt=mkt, qb=qb, nk=nk: e.dma_start(out=mkt[:, 0:nk], in_=MK[qb, :, 0:nk]), w=[mkb])
            qt, qbb = qa_r.next()
            dma("sp", lambda e, qt=qt, tok0=tok0: e.dma_start(
                out=qt[:].rearrange("p (h t) -> p h t", h=A_H), in_=QA[:, :, tok0:tok0 + 128].rearrange("h d t -> d h t")),
                w=[qbb])
            mtt, mtb = mt_r.next()
            for k0 in range(0, nkb, 8):
                n8 = min(8, nkb - k0)
                pt, pb = psT.next()
                for u in range(n8):
                    op("pe", lambda e, pt=pt, u=u, k0=k0, mkt=mkt: e.transpose(
                        pt[:, u * 128:(u + 1) * 128], mkt[:, (k0 + u) * 128:(k0 + u + 1) * 128], identb[:]),
                       r=[mkb, b_cb] if u in (0, n8 - 1) else (), w=[pb])
                op("act", lambda e, pt=pt, mtt=mtt, k0=k0, n8=n8: e.activation(
                    out=mtt[:, k0:k0 + n8, :].rearrange("p k t -> p (k t)"), in_=pt[:, 0:n8 * 128], func=AF.Copy),
                   r=[pb], w=[mtb])
            for j in range(A_KV):
                po, pob = psO.next()
                pd, pdb = psD.next()
                for kb in range(nkb):
                    ps, psb = psS.next()
                    op("pe", lambda e, ps=ps, j=j, kb=kb, qt=qt: e.matmul(
                        ps[:], kaT[:, j, kb * 128:(kb + 1) * 128], qt[:, j * 512:(j + 1) * 512], start=True, stop=True),
                       r=[b_kv, qbb], w=[psb])
                    et, eb = e_r.next()
                    op("act", lambda e, et=et, ps=ps: e.activation(out=et[:], in_=ps[:], func=AF.Exp, scale=isq),
                       r=[psb], w=[eb])
                    pp, ppb = p_r.next()
                    op("dve", lambda e, pp=pp, et=et, mtt=mtt, kb=kb: e.tensor_tensor(
                        out=pp[:].rearrange("p (g t) -> p g t", g=4), in0=et[:].rearrange("p (g t) -> p g t", g=4),
                        in1=mtt[:, kb, :].unsqueeze(1).broadcast_to([128, 4, 128]), op=ALU.mult),
                       r=[eb, mtb], w=[ppb])
                    op("pe", lambda e, po=po, pp=pp, j=j, kb=kb, nkb=nkb: e.matmul(
                        po[:], va[:, kb, j * 128:(j + 1) * 128], pp[:], start=(kb == 0), stop=(kb == nkb - 1)),
                       r=[ppb, b_kv], w=[pob])
                    op("pe", lambda e, pd=pd, pp=pp, kb=kb, nkb=nkb: e.matmul(
                        pd[:], onesb[:], pp[:], start=(kb == 0), stop=(kb == nkb - 1)),
                       r=[ppb, b_cb], w=[pdb])
                rd, rdb = rd_r.next()
                op("dve", lambda e, rd=rd, pd=pd: e.reciprocal(out=rd[:], in_=pd[:]), r=[pdb], w=[rdb])
                of_, ofb = of_r.next()
                op("dve", lambda e, of_=of_, po=po, rd=rd: e.tensor_tensor(out=of_[:], in0=po[:], in1=rd[:], op=ALU.mult),
                   r=[pob, rdb], w=[ofb])
                obt, obb = ob_r.next()
                op("act", lambda e, obt=obt, of_=of_: e.activation(out=obt[:], in_=of_[:], func=AF.Copy), r=[ofb], w=[obb])
                dma("sp", lambda e, obt=obt, j=j, tok0=tok0: e.dma_start(
                    out=OA[4 * j:4 * j + 4, :, tok0:tok0 + 128].rearrange("h d t -> d h t"),
                    in_=obt[:].rearrange("p (h t) -> p h t", h=4)), r=[obb])
        sch.barrier()
        sch.emit()
    if upto == "C2":
        return finish()

    with ExitStack() as ph:
        cmask_s = ph.enter_context(nc.sbuf_tensor("cmask_s", [128, 2, 8, 512], BF16))
        b_cm = Buf("cmask")
        dma("sp", lambda e: e.dma_start(out=cmask_s[:].rearrange("p m j t -> p (m j t)"), in_=cmask_d[:, :]), w=[b_cm])
        kb_r = Ring(nc, ph, "kbT", [128, S], BF16, 2)
        vb_r = Ring(nc, ph, "vbh", [128, NT, 128], BF16, 2)
        qb_r = Ring(nc, ph, "qbT", [128, So], BF16, 2)
        E_r = Ring(nc, ph, "sbE", [128, 512], F32, 3)
        Em_r = Ring(nc, ph, "sbEm", [128, 512], F32, 2)
        SP_r = Ring(nc, ph, "sbSP", [128, 512], BF16, 3)
        SPm_r = Ring(nc, ph, "sbSPm", [128, 512], BF16, 2)
        X_r = Ring(nc, ph, "sbX", [128, 512], F32, 3)
        At_r = Ring(nc, ph, "sbAt", [128, 512], BF16, 3)
        Ra_r = Ring(nc, ph, "sbRa", [128, 512], F32, 2)
        obD_r = Ring(nc, ph, "obD", [128, 512], BF16, 2)
        psZ = Ring(nc, ph, "psZ", [128, 512], F32, 3, psum=True)
        psCc = Ring(nc, ph, "psCc", [128, 512], F32, 3, psum=True)
        psOd = Ring(nc, ph, "psOd", [128, 512], F32, 2, psum=True)
        isq = 1.0 / math.sqrt(HD)
        for h in range(B_H):
            kt, ktb = kb_r.next()
            vt, vtb = vb_r.next()
            qt, qtb = qb_r.next()
            dma("sp", lambda e, kt=kt, h=h: e.dma_start(out=kt[:], in_=KB[h]), w=[ktb])
            dma("sp", lambda e, vt=vt, h=h: e.dma_start(
                out=vt[:], in_=VB[:, h * 128:(h + 1) * 128].rearrange("(kb p) d -> p kb d", p=128)), w=[vtb])
            dma("sp", lambda e, qt=qt, h=h: e.dma_start(out=qt[:], in_=QB[h]), w=[qtb])
            for i in range(Go):
                nkb = 4 * (2 * i + 2)
                m = i % 2
                ra, rab = Ra_r.next()
                po, pob = psOd.next()
                for n, kb in enumerate(reversed(range(nkb))):
                    j = kb - (nkb - 8)
                    pz, pzb = psZ.next()
                    op("pe", lambda e, pz=pz, kt=kt, qt=qt, kb=kb, i=i: e.matmul(
                        pz[:], kt[:, kb * 128:(kb + 1) * 128], qt[:, i * 512:(i + 1) * 512], start=True, stop=True),
                       r=[ktb, qtb], w=[pzb])
                    Et, Eb = E_r.next()
                    op("act", lambda e, Et=Et, pz=pz: e.activation(out=Et[:], in_=pz[:], func=AF.Exp, scale=isq),
                       r=[pzb], w=[Eb])
                    St, Sb = SP_r.next()
                    op("act", lambda e, St=St, Et=Et: e.activation(out=St[:], in_=Et[:], func=AF.Ln, bias=1.0),
                       r=[Eb], w=[Sb])
                    if j >= 0:
                        Sm, Smb = SPm_r.next()
                        op("dve", lambda e, Sm=Sm, St=St, m=m, j=j: e.tensor_tensor(
                            out=Sm[:], in0=St[:], in1=cmask_s[:, m, j, :], op=ALU.mult), r=[Sb, b_cm], w=[Smb])
                        Em, Emb = Em_r.next()
                        op("pool", lambda e, Em=Em, Et=Et, m=m, j=j: e.tensor_tensor(
                            out=Em[:], in0=Et[:], in1=cmask_s[:, m, j, :], op=ALU.mult), r=[Eb, b_cm], w=[Emb])
                    else:
                        Sm, Smb = St, Sb
                        Em, Emb = Et, Eb
                    pc, pcb = psCc.next()
                    op("pe", lambda e, pc=pc, Sm=Sm, n=n: e.matmul(pc[:], triI[:], Sm[:], start=True, stop=(n == 0)),
                       r=[Smb, b_cb], w=[pcb])
                    if n > 0:
                        op("pe", lambda e, pc=pc, ra=ra: e.matmul(pc[:], onesf, ra[:], start=False, stop=True),
                           r=[rab, b_consts], w=[pcb])
                    Xt, Xb = X_r.next()
                    op("act", lambda e, Xt=Xt, pc=pc: e.activation(out=Xt[:], in_=pc[:], func=AF.Exp, scale=-1.0),
                       r=[pcb], w=[Xb])
                    At, Ab = At_r.next()
                    op("dve", lambda e, At=At, Em=Em, Xt=Xt: e.tensor_tensor(out=At[:], in0=Em[:], in1=Xt[:], op=ALU.mult),
                       r=[Emb, Xb], w=[Ab])
                    if n == 0:
                        op("pool", lambda e, ra=ra, Sm=Sm: e.tensor_copy(out=ra[:], in_=Sm[:]), r=[Smb], w=[rab])
                    elif n < nkb - 1:
                        op("pool", lambda e, ra=ra, Sm=Sm: e.tensor_tensor(out=ra[:], in0=ra[:], in1=Sm[:], op=ALU.add),
                           r=[Smb, rab], w=[rab])
                    op("pe", lambda e, po=po, vt=vt, At=At, kb=kb, n=n, nkb=nkb: e.matmul(
                        po[:], vt[:, kb, :], At[:], start=(n == 0), stop=(n == nkb - 1)), r=[vtb, Ab], w=[pob])
                ot, otb = obD_r.next()
                op("act", lambda e, ot=ot, po=po: e.activation(out=ot[:], in_=po[:], func=AF.Copy), r=[pob], w=[otb])
                dma("sp", lambda e, ot=ot, h=h, i=i: e.dma_start(out=OB[h, :, i * 512:(i + 1) * 512], in_=ot[:]), r=[otb])
        sch.barrier()
        sch.emit()
    if upto == "D":
        return finish()

    with ExitStack() as ph:
        wa = ph.enter_context(nc.sbuf_tensor("wa", [128, 8, D], BF16))
        wb_ = ph.enter_context(nc.sbuf_tensor("wb_", [128, 8, D], BF16))
        b_wab = Buf("wab")
        for (wt, src) in ((wa, w_bra), (wb_, w_brb)):
            sv = src.rearrange("(kc p) n -> p kc n", p=128)
            for cq in range(4):
                dma("pool", lambda e, wt=wt, sv=sv, cq=cq: e.dma_start(
                    out=wt[:, :, cq * 512:(cq + 1) * 512], in_=sv[:, :, cq * 512:(cq + 1) * 512]), w=[b_wab])
        zt = ph.enter_context(nc.sbuf_tensor("zt", [128, 4096], BF16))
        b_zt = Buf("zt")
        op("dve", lambda e: e.memset(zt[:], 0.0), w=[b_zt])
        XGf = XG.rearrange("(r p two) c -> r p (two c)", p=128, two=2)
        for r_ in range(NE * CAP // 256):
            dma("sp", lambda e, r_=r_: e.dma_start(out=XGf[r_], in_=zt[:]), r=[b_zt])
        oa_r = Ring(nc, ph, "oaT", [128, 8, 512], BF16, 2)
        ob2_r = Ring(nc, ph, "obT", [128, 8, 512], BF16, 2)
        g_r = Ring(nc, ph, "gt", [128, 2, 512], BF16, 3)
        sg_r = Ring(nc, ph, "sgt", [128, 2, 512], F32, 2)
        t1e_r = Ring(nc, ph, "t1e", [128, 512], F32, 2)
        t2e_r = Ring(nc, ph, "t2e", [128, 512], F32, 2)
        mg_r = Ring(nc, ph, "mgo", [128, 512], BF16, 3)
        psa_r = Ring(nc, ph, "psEa", [128, 512], F32, 3, psum=True)
        psb_r = Ring(nc, ph, "psEb", [128, 512], F32, 3, psum=True)
        for tg in range(So // 512):
            t0 = tg * 512
            oat, oab = oa_r.next()
            obt, obb = ob2_r.next()
            dma("sp", lambda e, oat=oat, t0=t0: e.dma_start(out=oat[:], in_=OA[:, :, t0:t0 + 512].rearrange("h d t -> d h t")), w=[oab])
            dma("sp", lambda e, obt=obt, t0=t0: e.dma_start(out=obt[:], in_=OB[:, :, t0:t0 + 512].rearrange("h d t -> d h t")), w=[obb])
            for fo in range(KC):
                gt, gb_ = g_r.next()
                dma("sp", lambda e, gt=gt, fo=fo, t0=t0: e.dma_start(out=gt[:, 0, :], in_=GA[fo, :, t0:t0 + 512]), w=[gb_])
                dma("sp", lambda e, gt=gt, fo=fo, t0=t0: e.dma_start(out=gt[:, 1, :], in_=GB[fo, :, t0:t0 + 512]), w=[gb_])
                st_, sb_ = sg_r.next()
                op("act", lambda e, st_=st_, gt=gt: e.activation(out=st_[:], in_=gt[:], func=AF.Sigmoid), r=[gb_], w=[sb_])
                pa, pab = psa_r.next()
                mmgroup(pa[:], pab, [(wa[:, kc, fo * 128:(fo + 1) * 128], oat[:, kc, :]) for kc in range(8)], [b_wab, oab])
                pb2, pbb = psb_r.next()
                mmgroup(pb2[:], pbb, [(wb_[:, kc, fo * 128:(fo + 1) * 128], obt[:, kc, :]) for kc in range(8)], [b_wab, obb])
                a1, a1b = t1e_r.next()
                a2, a2b = t2e_r.next()
                op("dve", lambda e, a1=a1, pa=pa, st_=st_: e.tensor_tensor(out=a1[:], in0=pa[:], in1=st_[:, 0, :], op=ALU.mult),
                   r=[pab, sb_], w=[a1b])
                op("dve", lambda e, a2=a2, pb2=pb2, st_=st_: e.tensor_tensor(out=a2[:], in0=pb2[:], in1=st_[:, 1, :], op=ALU.mult),
                   r=[pbb, sb_], w=[a2b])
                mt_, mb_ = mg_r.next()
                op("pool", lambda e, mt_=mt_, a1=a1, a2=a2: e.tensor_tensor(out=mt_[:], in0=a1[:], in1=a2[:], op=ALU.add),
                   r=[a1b, a2b], w=[mb_])
                dma("sp", lambda e, mt_=mt_, fo=fo, t0=t0: e.dma_start(out=MG[fo, :, t0:t0 + 512], in_=mt_[:]), r=[mb_])
        sch.barrier()
        sch.emit()
    if upto == "E1":
        return finish()

    NSLOT = NE * CAP
    NSOK = min(NSLOT, 65535)
    with ExitStack() as ph:
        wo = ph.enter_context(nc.sbuf_tensor("wo", [128, KC, D], BF16))
        b_wo = Buf("wo")
        wo_v = w_out.rearrange("(kc p) n -> p kc n", p=128)
        for cq in range(4):
            dma("pool", lambda e, cq=cq: e.dma_start(out=wo[:, :, cq * 512:(cq + 1) * 512], in_=wo_v[:, :, cq * 512:(cq + 1) * 512]),
                w=[b_wo])
        wr = ph.enter_context(nc.sbuf_tensor("wr", [128, KC, NE], BF16))
        b_wr = Buf("wr")
        dma("pool", lambda e: e.dma_start(out=wr[:], in_=w_router.rearrange("(kc p) n -> p kc n", p=128)), w=[b_wr])
        brow = ph.enter_context(nc.sbuf_tensor("brow", [128, NE], F32))
        dma("sp", lambda e: e.dma_start(out=brow[:], in_=b_router[0:1, :].partition_broadcast(128)), w=[b_wr])
        base = ph.enter_context(nc.sbuf_tensor("base", [128, NE], F32))
        slotbase = ph.enter_context(nc.sbuf_tensor("slotbase", [128, NE], F32))
        b_base = Buf("base")
        op("dve", lambda e: e.memset(base[:], 0.0), w=[b_base])
        op("dve", lambda e: e.tensor_scalar(out=slotbase[:], in0=iota_f[:, 0:NE], scalar1=float(CAP), scalar2=None, op0=ALU.mult),
           r=[b_consts], w=[b_base])
        mg_r = Ring(nc, ph, "mgi", [128, KC, 512], BF16, 1)
        x_r = Ring(nc, ph, "xe", [128, D], F32, 2)
        mix_r = Ring(nc, ph, "mixs", [128, D], F32, 1)
        x1_r = Ring(nc, ph, "x1t", [128, D], F32, 2)
        xn_r = Ring(nc, ph, "xn2", [128, D], BF16, 2)
        h2_r = Ring(nc, ph, "h2T", [128, KC, 128], BF16, 2)
        junk = ph.enter_context(nc.sbuf_tensor("junkE", [128, D], BF16))
        b_junk = Buf("junkE")
        st_r = Ring(nc, ph, "stE", [128, 8], F32, 4)
        rt_r = Ring(nc, ph, "rtE", [128, 8, NE], F32, 2)
        mkb_r = Ring(nc, ph, "mkE", [128, NE], BF16, 2)
        t8_r = Ring(nc, ph, "t8E", [128, 24], F32, 2)
        psm_r = Ring(nc, ph, "psEm", [128, 512], F32, 3, psum=True)
        pst_r = Ring(nc, ph, "psEt", [128, 1024], BF16, 2, psum=True)
        psr_r = Ring(nc, ph, "psEr", [128, 512], F32, 2, psum=True)
        for tg in range(So // 512):
            mgt, mgb = mg_r.next()
            dma("sp", lambda e, mgt=mgt, tg=tg: e.dma_start(
                out=mgt[:], in_=MG[:, :, tg * 512:(tg + 1) * 512].rearrange("f d t -> d f t")), w=[mgb])
            for tt in range(4):
                ti = tg * 4 + tt
                tok0 = ti * 128
                xt, xb = x_r.next()
                dma("sp", lambda e, xt=xt, tok0=tok0: e.dma_start(out=xt[:], in_=x_own[tok0:tok0 + 128, :]), w=[xb])
                mx, mxb = mix_r.next()
                for dc in range(4):
                    pm, pmb = psm_r.next()
                    mmgroup(pm[:], pmb, [(mgt[:, kc, tt * 128:(tt + 1) * 128], wo[:, kc, dc * 512:(dc + 1) * 512])
                                         for kc in range(KC)], [mgb, b_wo])
                    op("act", lambda e, mx=mx, pm=pm, dc=dc: e.activation(out=mx[:, dc * 512:(dc + 1) * 512], in_=pm[:], func=AF.Copy),
                       r=[pmb], w=[mxb])
                st, sb_ = st_r.next()
                op("act", lambda e, st=st, mx=mx: e.activation(out=junk[:], in_=mx[:], func=AF.Square, accum_out=st[:, 0:1]),
                   r=[mxb], w=[b_junk, sb_])
                op("act", lambda e, st=st: e.activation(out=st[:, 1:2], in_=st[:, 0:1], func=AF.Sqrt, scale=1.0 / D, bias=EPS),
                   r=[sb_], w=[sb_])
                op("dve", lambda e, st=st: e.reciprocal(out=st[:, 2:3], in_=st[:, 1:2]), r=[sb_], w=[sb_])
                op("dve", lambda e, st=st, mx=mx: e.scalar_tensor_tensor(out=mx[:], in0=mx[:], scalar=st[:, 2:3], in1=ga1row[:],
                                                                        op0=ALU.mult, op1=ALU.mult),
                   r=[mxb, sb_, b_garow[0]], w=[mxb])
                x1, x1b = x1_r.next()
                op("pool", lambda e, x1=x1, mx=mx, xt=xt: e.tensor_tensor(out=x1[:], in0=mx[:], in1=xt[:], op=ALU.add),
                   r=[mxb, xb], w=[x1b])
                dma("sp", lambda e, x1=x1, tok0=tok0: e.dma_start(out=X1[tok0:tok0 + 128, :], in_=x1[:]), r=[x1b])
                op("act", lambda e, st=st, x1=x1: e.activation(out=junk[:], in_=x1[:], func=AF.Square, accum_out=st[:, 3:4]),
                   r=[x1b], w=[b_junk, sb_])
                op("act", lambda e, st=st: e.activation(out=st[:, 4:5], in_=st[:, 3:4], func=AF.Sqrt, scale=1.0 / D, bias=EPS),
                   r=[sb_], w=[sb_])
                op("dve", lambda e, st=st: e.reciprocal(out=st[:, 5:6], in_=st[:, 4:5]), r=[sb_], w=[sb_])
                xn, xnb = xn_r.next()
                op("dve", lambda e, xn=xn, x1=x1, st=st: e.tensor_scalar(out=xn[:], in0=x1[:], scalar1=st[:, 5:6], scalar2=None,
                                                                       op0=ALU.mult), r=[x1b, sb_], w=[xnb])
                h2, h2b = h2_r.next()
                for half in range(2):
                    pt, pb = pst_r.next()
                    for j in range(8):
                        kc = half * 8 + j
                        op("pe", lambda e, pt=pt, j=j, kc=kc, xn=xn: e.transpose(
                            pt[:, j * 128:(j + 1) * 128], xn[:, kc * 128:(kc + 1) * 128], identb[:]),
                           r=[xnb, b_cb] if j in (0, 7) else (), w=[pb])
                    for j in range(8):
                        kc = half * 8 + j
                        op("act", lambda e, pt=pt, j=j, kc=kc, h2=h2: e.activation(
                            out=h2[:, kc, :], in_=pt[:, j * 128:(j + 1) * 128], func=AF.Identity,
                            scale=modc[:, 2 * KC + kc:2 * KC + kc + 1], bias=modc[:, 3 * KC + kc:3 * KC + kc + 1]),
                           r=[pb, b_modc], w=[h2b])
                pr, prb = psr_r.next()
                mmgroup(pr[:, 0:NE], prb, [(h2[:, kc, :], wr[:, kc, :]) for kc in range(KC)], [h2b, b_wr])
                rt, rtb = rt_r.next()
                t8, t8b = t8_r.next()
                mk, mkb = mkb_r.next()
                lg, pos_, sla, ov, jk = (rt[:, q, :] for q in range(5))
                op("dve", lambda e, lg=lg, pr=pr: e.tensor_tensor(out=lg, in0=pr[:, 0:NE], in1=brow[:], op=ALU.add),
                   r=[prb, b_wr], w=[rtb])
                op("dve", lambda e, lg=lg, t8=t8: e.max(out=t8[:, 0:8], in_=lg), r=[rtb], w=[t8b])
                op("dve", lambda e, t8=t8: e.tensor_scalar(out=t8[:, 8:12], in0=t8[:, 0:4], scalar1=t8[:, 0:1], scalar2=None,
                                                          op0=ALU.subtract), r=[t8b], w=[t8b])
                op("act", lambda e, t8=t8, st=st: e.activation(out=t8[:, 12:16], in_=t8[:, 8:12], func=AF.Exp, accum_out=st[:, 6:7]),
                   r=[t8b], w=[t8b, sb_])
                op("dve", lambda e, st=st: e.reciprocal(out=st[:, 7:8], in_=st[:, 6:7]), r=[sb_], w=[sb_])
                op("dve", lambda e, mk=mk, lg=lg, t8=t8: e.tensor_scalar(out=mk[:], in0=lg, scalar1=t8[:, 3:4], scalar2=None,
                                                                       op0=ALU.is_ge), r=[rtb, t8b], w=[mkb])
                pp, ppb = psr_r.next()
                op("pe", lambda e, pp=pp, mk=mk: e.matmul(pp[:, 0:NE], triS[:], mk[:], start=True, stop=True),
                   r=[mkb, b_cb], w=[ppb])
                op("pe", lambda e, pp=pp, mk=mk: e.matmul(pp[:, 64:64 + NE], onesb[:], mk[:], start=True, stop=True),
                   r=[mkb, b_cb], w=[ppb])
                op("dve", lambda e, pos_=pos_, pp=pp: e.tensor_tensor(out=pos_, in0=pp[:, 0:NE], in1=base[:], op=ALU.add),
                   r=[ppb, b_base], w=[rtb])
                op("dve", lambda e, pp=pp: e.tensor_tensor(out=base[:], in0=pp[:, 64:64 + NE], in1=base[:], op=ALU.add),
                   r=[ppb, b_base], w=[b_base])
                op("dve", lambda e, ov=ov, pos_=pos_: e.tensor_scalar(out=ov, in0=pos_, scalar1=float(CAP), scalar2=1.0e9,
                                                                    op0=ALU.is_ge, op1=ALU.mult), r=[rtb], w=[rtb])
                op("dve", lambda e, sla=sla, pos_=pos_: e.tensor_tensor(out=sla, in0=pos_, in1=slotbase[:], op=ALU.add),
                   r=[rtb, b_base], w=[rtb])
                op("dve", lambda e, sla=sla, ov=ov: e.tensor_tensor(out=sla, in0=sla, in1=ov, op=ALU.add), r=[rtb], w=[rtb])
                for k in range(4):
                    op("dve", lambda e, jk=jk, lg=lg, t8=t8, sla=sla, k=k: e.scalar_tensor_tensor(
                        out=jk, in0=lg, scalar=t8[:, k:k + 1], in1=sla, op0=ALU.is_equal, op1=ALU.mult,
                        accum_out=t8[:, 16 + k:17 + k]), r=[rtb, t8b], w=[rtb, t8b])
                op("dve", lambda e, t8=t8, ti=ti: e.tensor_copy(out=rslot[:, ti * 4:(ti + 1) * 4], in_=t8[:, 16:20]),
                   r=[t8b], w=[b_route])
                op("dve", lambda e, t8=t8: e.tensor_scalar(out=t8[:, 20:24], in0=t8[:, 16:20], scalar1=float(NSOK), scalar2=None,
                                                          op0=ALU.is_lt), r=[t8b], w=[t8b])
                op("dve", lambda e, t8=t8, st=st: e.scalar_tensor_tensor(
                    out=t8[:, 12:16], in0=t8[:, 12:16], scalar=st[:, 7:8], in1=t8[:, 20:24], op0=ALU.mult, op1=ALU.mult),
                   r=[t8b, sb_], w=[t8b])
                op("dve", lambda e, t8=t8, ti=ti: e.tensor_copy(out=rwgt[:, ti * 4:(ti + 1) * 4], in_=t8[:, 12:16]),
                   r=[t8b], w=[b_route])
                for k in range(4):
                    dma("pool", lambda e, xn=xn, ti=ti, k=k: e.indirect_dma_start(
                        out=XG[0:NSOK, :], out_offset=bass.IndirectOffsetOnAxis(ap=rslot[:, ti * 4 + k:ti * 4 + k + 1], axis=0),
                        in_=xn[:, :], in_offset=None, bounds_check=sch.breg(e, NSOK - 1), oob_is_err=False),
                        r=[xnb, b_route])
        sch.barrier()
        sch.emit()
    if upto == "E2":
        return finish()

    NBLK = CAP // 128
    NTG = CAP // 512
    with ExitStack() as ph:
        XT = ph.enter_context(nc.sbuf_tensor("XT", [128, KC, CAP], BF16))
        b_XT = Buf("XT")
        actT = ph.enter_context(nc.sbuf_tensor("actT", [128, KC, CAP], BF16))
        b_act = [Buf(f"act{f}") for f in range(KC)]
        wc_r = Ring(nc, ph, "wcG", [128, KC, 256], BF16, 2)
        gt_r = Ring(nc, ph, "gtG", [128, D], BF16, 2)
        bb_r = Ring(nc, ph, "bbG", [128, 2 * KC], F32, 2)
        b2_r = Ring(nc, ph, "b2G", [1, 256], F32, 3)
        g_r = Ring(nc, ph, "gG", [128, 512], F32, 2)
        s_r = Ring(nc, ph, "sG", [128, 512], F32, 2)
        l_r = Ring(nc, ph, "lG", [128, 512], F32, 2)
        gs_r = Ring(nc, ph, "gsG", [128, 512], F32, 2)
        ot_r = Ring(nc, ph, "otG", [128, 256], BF16, 3)
        psT_r = Ring(nc, ph, "psGt", [128, 1024], BF16, 2, psum=True)
        psg_r = Ring(nc, ph, "psGg", [128, 512], F32, 2, psum=True)
        psl_r = Ring(nc, ph, "psGl", [128, 512], F32, 2, psum=True)
        pso_r = Ring(nc, ph, "psGo", [128, 512], F32, 2, psum=True)
        for ex in range(NE):
            bt, btb = bb_r.next()
            dma("sp", lambda e, bt=bt, ex=ex: e.dma_start(out=bt[:], in_=b1c[ex]), w=[btb])
            w1v = w1[ex].rearrange("(kc p) n -> p kc n", p=128)
            w2v = w2[ex].rearrange("(kc p) n -> p kc n", p=128)
            for blk in range(NBLK):
                gt, gtb = gt_r.next()
                r0 = ex * CAP + blk * 128
                dma("sp", lambda e, gt=gt, r0=r0: e.dma_start(out=gt[:], in_=XG[r0:r0 + 128, :]), w=[gtb])
                for half in range(2):
                    pt, pb = psT_r.next()
                    for j in range(8):
                        kc = half * 8 + j
                        op("pe", lambda e, pt=pt, j=j, kc=kc, gt=gt: e.transpose(
                            pt[:, j * 128:(j + 1) * 128], gt[:, kc * 128:(kc + 1) * 128], identb[:]),
                           r=[gtb, b_cb] if j in (0, 7) else (), w=[pb])
                    for j in range(8):
                        kc = half * 8 + j
                        if half == 0:
                            op("act", lambda e, pt=pt, j=j, kc=kc, blk=blk: e.activation(
                                out=XT[:, kc, blk * 128:(blk + 1) * 128], in_=pt[:, j * 128:(j + 1) * 128], func=AF.Identity,
                                scale=modc[:, 2 * KC + kc:2 * KC + kc + 1], bias=modc[:, 3 * KC + kc:3 * KC + kc + 1]),
                               r=[pb, b_modc], w=[b_XT])
                        else:
                            op("dve", lambda e, pt=pt, j=j, kc=kc, blk=blk: e.tensor_scalar(
                                out=XT[:, kc, blk * 128:(blk + 1) * 128], in0=pt[:, j * 128:(j + 1) * 128],
                                scalar1=modc[:, 2 * KC + kc:2 * KC + kc + 1], scalar2=modc[:, 3 * KC + kc:3 * KC + kc + 1],
                                op0=ALU.mult, op1=ALU.add), r=[pb, b_modc], w=[b_XT])
            for cq in range(KC):
                wt, wb = wc_r.next()
                dma("pool", lambda e, wt=wt, w1v=w1v, cq=cq: e.dma_start(out=wt[:], in_=w1v[:, :, cq * 256:(cq + 1) * 256]), w=[wb])
                for tg in range(NTG):
                    pg, pgb = psg_r.next()
                    mmgroup(pg[:], pgb, [(wt[:, kc, 0:256:2], XT[:, kc, tg * 512:(tg + 1) * 512]) for kc in range(KC)], [wb, b_XT])
                    pl, plb = psl_r.next()
                    mmgroup(pl[:], plb, [(wt[:, kc, 1:256:2], XT[:, kc, tg * 512:(tg + 1) * 512]) for kc in range(KC)], [wb, b_XT])
                    g_, gb_ = g_r.next()
                    op("dve", lambda e, g_=g_, pg=pg, bt=bt, cq=cq: e.tensor_scalar(
                        out=g_[:], in0=pg[:], scalar1=bt[:, cq:cq + 1], scalar2=LIMIT, op0=ALU.add, op1=ALU.min),
                       r=[pgb, btb], w=[gb_])
                    sg, sgb = s_r.next()
                    op("act", lambda e, sg=sg, g_=g_: e.activation(out=sg[:], in_=g_[:], func=AF.Sigmoid, scale=ALPHA),
                       r=[gb_], w=[sgb])
                    l_, lb_ = l_r.next()
                    op("dve", lambda e, l_=l_, pl=pl, bt=bt, cq=cq: e.tensor_scalar(
                        out=l_[:], in0=pl[:], scalar1=bt[:, KC + cq:KC + cq + 1], scalar2=LIMIT, op0=ALU.add, op1=ALU.min),
                       r=[plb, btb], w=[lb_])
                    op("dve", lambda e, l_=l_: e.tensor_scalar(out=l_[:], in0=l_[:], scalar1=-LIMIT, scalar2=1.0,
                                                               op0=ALU.max, op1=ALU.add), r=[lb_], w=[lb_])
                    gs, gsb = gs_r.next()
                    op("pool", lambda e, gs=gs, g_=g_, sg=sg: e.tensor_tensor(out=gs[:], in0=g_[:], in1=sg[:], op=ALU.mult),
                       r=[gb_, sgb], w=[gsb])
                    op("dve", lambda e, gs=gs, l_=l_, cq=cq, tg=tg: e.tensor_tensor(
                        out=actT[:, cq, tg * 512:(tg + 1) * 512], in0=gs[:], in1=l_[:], op=ALU.mult),
                       r=[gsb, lb_], w=[b_act[cq]])
            for dc in range(D // 256):
                wt, wb = wc_r.next()
                dma("pool", lambda e, wt=wt, w2v=w2v, dc=dc: e.dma_start(out=wt[:], in_=w2v[:, :, dc * 256:(dc + 1) * 256]), w=[wb])
                b2t, b2b = b2_r.next()
                dma("sp", lambda e, b2t=b2t, ex=ex, dc=dc: e.dma_start(out=b2t[:], in_=b2[ex:ex + 1, dc * 256:(dc + 1) * 256]), w=[b2b])
                for blk in range(NBLK):
                    po, pob = pso_r.next()
                    pairs = [(actT[:, fc, blk * 128:(blk + 1) * 128], wt[:, fc, :]) for fc in range(KC)]
                    n = len(pairs)
                    for q, (l, r_) in enumerate(pairs):
                        op("pe", lambda e, po=po, l=l, r_=r_, q=q: e.matmul(po[:, 0:256], l, r_, start=(q == 0), stop=False),
                           r=(b_act + [wb]) if q in (0, n - 1) else (), w=[pob])
                    op("pe", lambda e, po=po, b2t=b2t, dc=dc: e.matmul(
                        po[:, 0:256], onesf[0:1, :], b2t[0:1, :], start=False, stop=True),
                       r=[b2b, b_consts], w=[pob])
                    ot, otb = ot_r.next()
                    op("act", lambda e, ot=ot, po=po: e.activation(out=ot[:], in_=po[:, 0:256], func=AF.Copy), r=[pob], w=[otb])
                    r0 = ex * CAP + blk * 128
                    dma("sp", lambda e, ot=ot, r0=r0, dc=dc: e.dma_start(out=YG[r0:r0 + 128, dc * 256:(dc + 1) * 256], in_=ot[:]),
                        r=[otb])
        sch.barrier()
        sch.emit()
    if upto == "G":
        return finish()

    with ExitStack() as ph:
        y_r = Ring(nc, ph, "yH", [128, D], BF16, 5)
        x1_r = Ring(nc, ph, "x1H", [128, D], F32, 2)
        f_r = Ring(nc, ph, "fH", [128, D], F32, 2)
        o_r = Ring(nc, ph, "oH", [128, D], F32, 2)
        junk = ph.enter_context(nc.sbuf_tensor("junkH", [128, D], BF16))
        b_junk = Buf("junkH")
        st_r = Ring(nc, ph, "stH", [128, 4], F32, 3)
        for (yt, yb) in y_r.tiles:
            op("dve", lambda e, yt=yt: e.memset(yt[:], 0.0), w=[yb])
        for ti in range(NTo):
            tok0 = ti * 128
            ys = []
            for k in range(4):
                yt, yb = y_r.next()
                dma("pool", lambda e, yt=yt, ti=ti, k=k: e.indirect_dma_start(
                    out=yt[:, :], out_offset=None, in_=YG[0:NSOK, :],
                    in_offset=bass.IndirectOffsetOnAxis(ap=rslot[:, ti * 4 + k:ti * 4 + k + 1], axis=0),
                    bounds_check=sch.breg(e, NSOK - 1), oob_is_err=False), r=[b_route], w=[yb])
                ys.append((yt, yb))
            x1, x1b = x1_r.next()
            dma("sp", lambda e, x1=x1, tok0=tok0: e.dma_start(out=x1[:], in_=X1[tok0:tok0 + 128, :]), w=[x1b])
            ft, fb = f_r.next()
            op("dve", lambda e, ft=ft, ti=ti, y0=ys[0][0]: e.tensor_scalar(
                out=ft[:], in0=y0[:], scalar1=rwgt[:, ti * 4:ti * 4 + 1], scalar2=None, op0=ALU.mult),
               r=[ys[0][1], b_route], w=[fb])
            for k in range(1, 4):
                op("dve", lambda e, ft=ft, ti=ti, k=k, yk=ys[k][0]: e.scalar_tensor_tensor(
                    out=ft[:], in0=yk[:], scalar=rwgt[:, ti * 4 + k:ti * 4 + k + 1], in1=ft[:], op0=ALU.mult, op1=ALU.add),
                   r=[ys[k][1], b_route, fb], w=[fb])
            st, sb_ = st_r.next()
            op("act", lambda e, st=st, ft=ft: e.activation(out=junk[:], in_=ft[:], func=AF.Square, accum_out=st[:, 0:1]),
               r=[fb], w=[b_junk, sb_])
            op("act", lambda e, st=st: e.activation(out=st[:, 1:2], in_=st[:, 0:1], func=AF.Sqrt, scale=1.0 / D, bias=EPS),
               r=[sb_], w=[sb_])
            op("dve", lambda e, st=st: e.reciprocal(out=st[:, 2:3], in_=st[:, 1:2]), r=[sb_], w=[sb_])
            op("dve", lambda e, st=st, ft=ft: e.scalar_tensor_tensor(out=ft[:], in0=ft[:], scalar=st[:, 2:3], in1=ga2row[:],
                                                                    op0=ALU.mult, op1=ALU.mult),
               r=[fb, sb_, b_garow[1]], w=[fb])
            ot, otb = o_r.next()
            op("pool", lambda e, ot=ot, ft=ft, x1=x1: e.tensor_tensor(out=ot[:], in0=ft[:], in1=x1[:], op=ALU.add),
               r=[fb, x1b], w=[otb])
            dma("sp", lambda e, ot=ot, tok0=tok0: e.dma_start(out=out_d[tok0:tok0 + 128, :], in_=ot[:]), r=[otb])
        sch.barrier()
        sch.emit()
    return finish()


def make_consts():
    c = np.zeros((128, 8, 128), np.float32)
    c[:, 0, :] = np.eye(128)
    for dp in range(16):
        c[dp + 16, 1, dp] = -1.0
        c[dp, 1, dp + 16] = 1.0
    for o in (0, 64):
        for dp in range(8):
            c[o + dp + 8, 2, o + dp] = -1.0
            c[o + dp, 2, o + dp + 8] = 1.0
    k = np.arange(128)
    c[:, 3, :] = (k[:, None] >= k[None, :])
    c[:, 4, :] = (k[:, None] < k[None, :])
    c[:, 5, :] = 1.0
    invA = THETA ** (-(np.arange(16, dtype=np.float32)) / np.float32(16))
    invI = THETA ** (-(np.arange(8, dtype=np.float32)) / np.float32(8))
    for p in range(128):
        c[p, 6, 0] = invA[p % 16] if p < 32 else 0.0
        c[p, 6, 1] = invI[(p % 64) % 8] if (p % 64) < 16 else 0.0
    c[:, 7, :] = k[None, :]
    return np.ascontiguousarray(c.reshape(128, 8 * 128))


def make_masks(half):
    p = np.arange(128)[:, None]
    tl = np.arange(512)[None, :]
    cm = np.zeros((128, 2, 8, 512), np.float32)
    am = np.zeros((128, 2, 640), np.float32)
    cc = np.arange(640)[None, :]
    for m in range(2):
        delta = (1 if m == 0 else 0) if half == 0 else (0 if m == 0 else 1)
        for j in range(8):
            if delta == 0:
                cm[:, m, j, :] = (128 * (j - 4) + p) < tl
            else:
                cm[:, m, j, :] = (128 * j + p) < tl
        lim = 64 * (p // 64 + 1)
        am[:, m, :] = np.where(512 * (delta - 1) + cc >= lim, NEG, 0.0)
    return (np.ascontiguousarray(cm.reshape(128, -1)).astype(ml_dtypes.bfloat16),
            np.ascontiguousarray(am.reshape(128, -1)))


def col_layout(v):
    return np.ascontiguousarray(v.reshape(-1, 128).T)


def prep(inputs, cfg, batches):
    S, NE = cfg["S"], cfg["NE"]
    G = S // 512
    f32 = np.float32
    x = np.asarray(inputs["x"], f32)
    c = np.asarray(inputs["c"], f32)
    pos = np.asarray(inputs["positions"], np.int32)
    shared = {
        "b_ada": np.ascontiguousarray(np.asarray(inputs["b_ada"], f32)[0][None, :]),
        "badac": col_layout(np.asarray(inputs["b_ada"], f32)[0]),
        "gcols": np.concatenate([col_layout(np.asarray(inputs[k], f32)[0]) for k in
                                 ("g_pre_mix", "g_post_mix", "g_pre_ffn", "g_post_ffn")], axis=1),
        "g_post_mix": np.ascontiguousarray(np.asarray(inputs["g_post_mix"], f32)[0][None, :]),
        "g_post_ffn": np.ascontiguousarray(np.asarray(inputs["g_post_ffn"], f32)[0][None, :]),
        "w_ada": np.ascontiguousarray(np.asarray(inputs["w_ada"], f32)[0]),
        "w_in": np.ascontiguousarray(np.asarray(inputs["w_in"], f32)[0]),
        "w_branch_a": np.ascontiguousarray(np.asarray(inputs["w_branch_a"], f32)[0]),
        "w_branch_b": np.ascontiguousarray(np.asarray(inputs["w_branch_b"], f32)[0]),
        "w_out": np.ascontiguousarray(np.asarray(inputs["w_out"], f32)[0]),
        "w_router": np.ascontiguousarray(np.asarray(inputs["w_router"], f32)[0][:, :NE]),
        "b_router": np.ascontiguousarray(np.asarray(inputs["b_router"], f32)[0][None, :NE]),
        "w1": np.ascontiguousarray(np.asarray(inputs["w1"], f32)[0][:NE]),
        "w2": np.ascontiguousarray(np.asarray(inputs["w2"], f32)[0][:NE]),
        "b2": np.ascontiguousarray(np.asarray(inputs["b2"], f32)[0][:NE]),
        "consts": make_consts(),
    }
    b1 = np.asarray(inputs["b1"], f32)[0][:NE]
    b1g = b1[:, 0::2].reshape(NE, KC, 128).transpose(0, 2, 1)
    b1l = b1[:, 1::2].reshape(NE, KC, 128).transpose(0, 2, 1)
    shared["b1c"] = np.ascontiguousarray(np.concatenate([b1g, b1l], axis=2))
    masks = [make_masks(0), make_masks(1)]
    in_maps, owns = [], []
    for b in batches:
        for half in range(2):
            toks = np.concatenate([np.arange(g * 512, (g + 1) * 512) for g in own_groups(G, half)])
            m = dict(shared)
            m["x_all"] = np.ascontiguousarray(x[b])
            m["x_own"] = np.ascontiguousarray(x[b][toks])
            m["pos_all"] = np.ascontiguousarray(pos[b][None, :])
            m["pos_own"] = np.ascontiguousarray(pos[b][toks][None, :])
            m["cvec"] = col_layout(c[b])
            m["cmask"], m["amask"] = masks[half]
            in_maps.append(m)
            owns.append((b, toks))
    return in_maps, owns


_CACHE = {}


def kernel(**inputs):
    x = np.asarray(inputs["x"])
    B, S, _ = x.shape
    cfg = {"S": S, "NE": 32, "CAP": 2048}
    in_maps, owns = prep(inputs, cfg, list(range(B)))
    nc = build(cfg)
    res = run_bass_kernel_spmd(nc, in_maps, core_ids=list(range(len(in_maps))))
    out = np.empty((B, S, D), np.float32)
    for r, (b, toks) in zip(res.results, owns):
        out[b, toks] = np.asarray(r["out"], np.float32)
    return out
```

```python
import math
from contextlib import ExitStack

import numpy as np
import ml_dtypes

import concourse.bass as bass
import concourse.mybir as mybir
from concourse.bass_utils import run_bass_kernel_spmd

F32 = mybir.dt.float32
BF16 = mybir.dt.bfloat16
I32 = mybir.dt.int32
AF = mybir.ActivationFunctionType
ALU = mybir.AluOpType
AX = mybir.AxisListType

D = 2048
KC = D // 128
A_H, A_KV, HD = 8, 2, 128
IDX_H, IDX_D = 16, 64
B_H = 8
TOPK = 256
THETA = 500000.0
EPS = 1e-6
LIMIT = 7.0
ALPHA = 1.702
C_QA, C_KA, C_VA = 0, 1024, 1280
C_QB, C_KB, C_VB = 1536, 2560, 3584
C_QI, C_KI, C_WI = 4608, 5632, 5696
C_GA, C_GB = 5712, 7760
IN_W = 9808
NEG = -1.0e30
NBIS = 26


class Buf:
    __slots__ = ("w", "r", "name", "excl")

    def __init__(self, name="", excl=False):
        self.w = None
        self.r = []
        self.name = name
        self.excl = excl


class Sched:
    ENGS = ("pe", "act", "dve", "pool", "sp")
    RQ = {"sp": 8, "pool": 2, "act": 4}

    def __init__(self, nc, es):
        self.nc = nc
        self.sems = {}
        for e in self.ENGS:
            self.sems[e] = es.enter_context(nc.semaphore("c_" + e))
        self.dq = {}
        for q in ("sp", "pool", "act"):
            self.dq[q] = [es.enter_context(nc.semaphore(f"d_{q}{i}")) for i in range(self.RQ[q])]
        self.n = {e: 0 for e in self.ENGS}
        self.dn = {q: 0 for q in self.dq}
        self.seen = {e: {} for e in self.ENGS}
        self.streams = {e: [] for e in self.ENGS}
        self.last = {}

    def _sem(self, key):
        return self.sems[key] if isinstance(key, str) else self.dq[key[0]][key[1]]

    def _waits(self, eng, deps):
        out = []
        best = {}
        for d in deps:
            if d is None:
                continue
            k, v = d
            if k == "pe" and eng == "pe":
                continue
            if best.get(k, 0) < v:
                best[k] = v
        for k, v in best.items():
            if self.seen[eng].get(k, 0) >= v:
                continue
            self.seen[eng][k] = v
            out.append((k, v))
        return out

    def _deps(self, r, w):
        deps = []
        for b in r:
            deps.append(b.w)
            if b.excl:
                deps.extend(b.r)
        for b in w:
            deps.append(b.w)
            deps.extend(b.r)
        return deps

    def _commit(self, tok, r, w):
        for b in r:
            b.r.append(tok)
        for b in w:
            b.w = tok
            b.r = []
        self.last[tok[0]] = tok[1]

    def op(self, eng, fn, r=(), w=()):
        waits = self._waits(eng, self._deps(r, w))
        self.n[eng] += 1
        tok = (eng, self.n[eng])
        self.streams[eng].append((waits, fn, self.sems[eng], 1))
        self._commit(tok, r, w)

    def dma(self, q, fn, r=(), w=()):
        i = self.dn[q]
        self.dn[q] += 1
        R = self.RQ[q]
        key = (q, i % R)
        deps = self._deps(r, w)
        if i >= R:
            deps.append((key, 16 * (i // R)))
        waits = self._waits(q, deps)
        tok = (key, 16 * (i // R + 1))
        self.streams[q].append((waits, fn, self._sem(key), 16))
        self._commit(tok, r, w)

    def barrier(self):
        toks = list(self.last.items())
        for e in self.ENGS:
            waits = self._waits(e, [t for t in toks if not (t[0] == e)])
            if waits:
                self.streams[e].append((waits, None, None, 0))

    def breg(self, e, val):
        if val not in self._regs:
            self._regs[val] = e.to_reg(val)
        return self._regs[val]

    def emit(self):
        nc = self.nc
        streams = self.streams
        self._regs = {}
        self.streams = {e: [] for e in self.ENGS}

        def run(e, lst):
            for waits, fn, sem, inc in lst:
                for k, v in waits:
                    e.wait_ge(self._sem(k), v)
                if fn is not None:
                    try:
                        fn(e).then_inc(sem, inc)
                    except Exception:
                        print("EMIT FAIL at stream idx", lst.index((waits, fn, sem, inc)), "of", len(lst), flush=True)
                        raise

        with nc.Block() as block:
            @block.tensor
            def _(e):
                run(e, streams["pe"])

            @block.scalar
            def _(e):
                run(e, streams["act"])

            @block.vector
            def _(e):
                run(e, streams["dve"])

            @block.gpsimd
            def _(e):
                run(e, streams["pool"])

            @block.sync
            def _(e):
                run(e, streams["sp"])


class Ring:
    def __init__(self, nc, es, name, shape, dtype, n, psum=False):
        self.tiles = []
        for i in range(n):
            if psum:
                t = es.enter_context(nc.psum_tensor(f"{name}{i}", shape, dtype))
            else:
                t = es.enter_context(nc.sbuf_tensor(f"{name}{i}", shape, dtype))
            self.tiles.append((t, Buf(f"{name}{i}", excl=psum)))
        self.i = 0

    def next(self):
        t = self.tiles[self.i % len(self.tiles)]
        self.i += 1
        return t


def own_groups(G, half):
    return [g for g in range(G) if ((g % 4) in (0, 3)) == (half == 0)]


def build(cfg):
    S = cfg["S"]
    NE = cfg["NE"]
    CAP = cfg["CAP"]
    dbg = cfg.get("dbg", False)
    upto = cfg.get("upto", "Z")
    So = S // 2
    G = S // 512
    Go = G // 2
    NT = S // 128
    NTo = So // 128
    SG = min(2048, So)
    nc = bass.Bass("TRN2", target_bir_lowering=False)
    es = ExitStack()
    scratch_kind = "ExternalOutput" if dbg else "Internal"

    def din(name, shape, dt):
        return nc.dram_tensor(name, list(shape), dt, kind="ExternalInput").ap()

    def dscr(name, shape, dt):
        return nc.dram_tensor(name, list(shape), dt, kind=scratch_kind).ap()

    x_all = din("x_all", [S, D], F32)
    x_own = din("x_own", [So, D], F32)
    pos_all = din("pos_all", [1, S], I32)
    pos_own = din("pos_own", [1, So], I32)
    cvec = din("cvec", [128, KC], F32)
    badac = din("badac", [128, 6 * KC], F32)
    gcols = din("gcols", [128, 4 * KC], F32)
    b_ada = din("b_ada", [1, 6 * D], F32)
    g_post_mix = din("g_post_mix", [1, D], F32)
    g_post_ffn = din("g_post_ffn", [1, D], F32)
    w_ada = din("w_ada", [D, 6 * D], F32)
    w_in = din("w_in", [D, IN_W], F32)
    w_bra = din("w_branch_a", [A_H * HD, D], F32)
    w_brb = din("w_branch_b", [B_H * HD, D], F32)
    w_out = din("w_out", [D, D], F32)
    w_router = din("w_router", [D, NE], F32)
    b_router = din("b_router", [1, NE], F32)
    w1 = din("w1", [NE, D, 2 * D], F32)
    b1c = din("b1c", [NE, 128, 2 * KC], F32)
    w2 = din("w2", [NE, D, D], F32)
    b2 = din("b2", [NE, D], F32)
    cmask_d = din("cmask", [128, 2 * 8 * 512], BF16)
    amask_d = din("amask", [128, 2 * 640], F32)
    consts_d = din("consts", [128, 8 * 128], F32)
    out_d = nc.dram_tensor("out", [So, D], F32, kind="ExternalOutput").ap()

    KA = dscr("s_ka", [A_KV, 128, S], BF16)
    KB = dscr("s_kb", [B_H, 128, S], BF16)
    KI = dscr("s_ki", [IDX_D, S], BF16)
    VA = dscr("s_va", [S, A_KV * HD], BF16)
    VB = dscr("s_vb", [S, B_H * HD], BF16)
    QA = dscr("s_qa", [A_H, 128, So], BF16)
    QB = dscr("s_qb", [B_H, 128, So], BF16)
    QI = dscr("s_qi", [IDX_H // 2, 128, So], BF16)
    WI = dscr("s_wi", [So, IDX_H], F32)
    GA = dscr("s_ga", [KC, 128, So], BF16)
    GB = dscr("s_gb", [KC, 128, So], BF16)
    MK = dscr("s_mk", [NTo, 128, S], BF16)
    OA = dscr("s_oa", [A_H, 128, So], BF16)
    OB = dscr("s_ob", [B_H, 128, So], BF16)
    MG = dscr("s_mg", [KC, 128, So], BF16)
    X1 = dscr("s_x1", [So, D], F32)
    XG = dscr("s_xg", [NE * CAP, D], BF16)
    YG = dscr("s_yg", [NE * CAP, D], BF16)

    sch = Sched(nc, es)
    op, dma = sch.op, sch.dma
    dbg_outs = {}

    def finish():
        if dbg:
            d1 = nc.dram_tensor("dbg_modc", [128, 4 * KC], F32, kind="ExternalOutput").ap()
            d2 = nc.dram_tensor("dbg_ga", [128, 2 * D], F32, kind="ExternalOutput").ap()
            dma("sp", lambda e: e.dma_start(out=d1[:, :], in_=modc[:]), r=[b_modc])
            dma("sp", lambda e: e.dma_start(out=d2[:, 0:D], in_=ga1row[:]), r=[b_garow[0]])
            dma("sp", lambda e: e.dma_start(out=d2[:, D:2 * D], in_=ga2row[:]), r=[b_garow[1]])
            d3 = nc.dram_tensor("dbg_rslot", [128, NTo * 4], I32, kind="ExternalOutput").ap()
            d4 = nc.dram_tensor("dbg_rwgt", [128, NTo * 4], F32, kind="ExternalOutput").ap()
            dma("sp", lambda e: e.dma_start(out=d3[:, :], in_=rslot[:]), r=[b_route])
            dma("sp", lambda e: e.dma_start(out=d4[:, :], in_=rwgt[:]), r=[b_route])
        sch.barrier()
        sch.emit()
        es.close()
        return nc

    def sbp(name, shape, dt):
        return es.enter_context(nc.sbuf_tensor(name, shape, dt))

    consts = sbp("consts_s", [128, 8 * 128], F32)
    b_consts = Buf("consts")
    identb = sbp("identb", [128, 128], BF16)
    rtA = sbp("rtA", [128, 128], BF16)
    rtI = sbp("rtI", [128, 128], BF16)
    triI = sbp("triI", [128, 128], BF16)
    triS = sbp("triS", [128, 128], BF16)
    onesb = sbp("onesb", [128, 128], BF16)
    onesf = consts[:, 5 * 128:6 * 128]
    invfA = consts[:, 6 * 128:6 * 128 + 1]
    invfI = consts[:, 6 * 128 + 1:6 * 128 + 2]
    iota_f = consts[:, 7 * 128:8 * 128]
    b_cb = Buf("constsb")
    modc = sbp("modc", [128, 4 * KC], F32)
    b_modc = Buf("modc")
    ga1row = sbp("ga1row", [128, D], F32)
    ga2row = sbp("ga2row", [128, D], F32)
    b_garow = [Buf("ga1row"), Buf("ga2row")]
    rslot = sbp("rslot", [128, NTo * 4], I32)
    rwgt = sbp("rwgt", [128, NTo * 4], F32)
    b_route = Buf("route")

    dma("sp", lambda e: e.dma_start(out=consts[:], in_=consts_d[:, :]), w=[b_consts])
    for i, t in enumerate((identb, rtA, rtI, triI, triS, onesb)):
        op("dve", lambda e, i=i, t=t: e.tensor_copy(out=t[:], in_=consts[:, i * 128:(i + 1) * 128]),
           r=[b_consts], w=[b_cb])

    def mmgroup(out_ap, out_buf, pairs, rbufs):
        n = len(pairs)
        for i, (l, r_) in enumerate(pairs):
            rb = rbufs if (i == 0 or i == n - 1) else ()
            op("pe", lambda e, l=l, r_=r_, i=i: e.matmul(out_ap, l, r_, start=(i == 0), stop=(i == n - 1)),
               r=rb, w=[out_buf])

    with ExitStack() as ph:
        cc = ph.enter_context(nc.sbuf_tensor("cc", [128, KC], F32))
        sg_ = ph.enter_context(nc.sbuf_tensor("sg_", [128, KC], F32))
        siluc = ph.enter_context(nc.sbuf_tensor("siluc", [128, KC], F32))
        silurep = ph.enter_context(nc.sbuf_tensor("silurep", [128, KC, 128], F32))
        badac_s = ph.enter_context(nc.sbuf_tensor("badac_s", [128, 6 * KC], F32))
        gcols_s = ph.enter_context(nc.sbuf_tensor("gcols_s", [128, 4 * KC], F32))
        rawc = ph.enter_context(nc.sbuf_tensor("rawc", [128, 4 * KC], F32))
        b_small = Buf("small")
        b_rawc = Buf("rawc")
        wch = Ring(nc, ph, "wch", [128, KC, 512], F32, 2)
        rowt = Ring(nc, ph, "rowt", [128, 2, 512], F32, 2)
        tmpr = Ring(nc, ph, "tmpr", [128, 512], F32, 2)
        psA = Ring(nc, ph, "psA", [128, 512], F32, 2, psum=True)
        psC = ph.enter_context(nc.psum_tensor("psC", [128, 512], F32))
        b_psC = Buf("psC", excl=True)

        dma("sp", lambda e: e.dma_start(out=cc[:], in_=cvec[:, :]), w=[b_small])
        dma("sp", lambda e: e.dma_start(out=badac_s[:], in_=badac[:, :]), w=[b_small])
        dma("sp", lambda e: e.dma_start(out=gcols_s[:], in_=gcols[:, :]), w=[b_small])
        op("act", lambda e: e.activation(out=sg_[:], in_=cc[:], func=AF.Sigmoid), r=[b_small], w=[b_small])
        op("dve", lambda e: e.tensor_tensor(out=siluc[:], in0=cc[:], in1=sg_[:], op=ALU.mult), r=[b_small], w=[b_small])
        op("dve", lambda e: e.tensor_copy(out=silurep[:], in_=siluc[:].unsqueeze(2).broadcast_to([128, KC, 128])),
           r=[b_small], w=[b_small])
        w_ada_v = w_ada.rearrange("(kc p) n -> p kc n", p=128)
        for idx, which in enumerate((1, 0, 4, 3)):
            for cq in range(4):
                wt, wb = wch.next()
                c0 = which * D + cq * 512
                dma("sp", lambda e, wt=wt, c0=c0: e.dma_start(out=wt[:], in_=w_ada_v[:, :, c0:c0 + 512]), w=[wb])
                for j in range(4):
                    col = idx * KC + cq * 4 + j
                    mmgroup(psC[:, col:col + 1], b_psC,
                            [(wt[:, kc, j * 128:(j + 1) * 128], siluc[:, kc:kc + 1]) for kc in range(KC)],
                            [wb, b_small])
        op("dve", lambda e: e.tensor_copy(out=rawc[:], in_=psC[:, 0:4 * KC]), r=[b_psC], w=[b_rawc])
        for idx, which in enumerate((1, 0, 4, 3)):
            op("dve", lambda e, idx=idx, which=which: e.tensor_tensor(
                out=rawc[:, idx * KC:(idx + 1) * KC], in0=rawc[:, idx * KC:(idx + 1) * KC],
                in1=badac_s[:, which * KC:(which + 1) * KC], op=ALU.add), r=[b_small, b_rawc], w=[b_rawc])
        op("dve", lambda e: e.scalar_tensor_tensor(out=modc[:, 0:KC], in0=rawc[:, 0:KC], scalar=1.0,
                                                   in1=gcols_s[:, 0:KC], op0=ALU.add, op1=ALU.mult),
           r=[b_rawc, b_small], w=[b_modc])
        op("dve", lambda e: e.tensor_copy(out=modc[:, KC:2 * KC], in_=rawc[:, KC:2 * KC]), r=[b_rawc], w=[b_modc])
        op("dve", lambda e: e.scalar_tensor_tensor(out=modc[:, 2 * KC:3 * KC], in0=rawc[:, 2 * KC:3 * KC], scalar=1.0,
                                                   in1=gcols_s[:, 2 * KC:3 * KC], op0=ALU.add, op1=ALU.mult),
           r=[b_rawc, b_small], w=[b_modc])
        op("dve", lambda e: e.tensor_copy(out=modc[:, 3 * KC:4 * KC], in_=rawc[:, 3 * KC:4 * KC]), r=[b_rawc], w=[b_modc])
        for gi, (which, grow, gpost) in enumerate(((2, ga1row, g_post_mix), (5, ga2row, g_post_ffn))):
            for cq in range(4):
                wt, wb = wch.next()
                c0 = which * D + cq * 512
                dma("sp", lambda e, wt=wt, c0=c0: e.dma_start(out=wt[:], in_=w_ada_v[:, :, c0:c0 + 512]), w=[wb])
                rt, rb = rowt.next()
                dma("sp", lambda e, rt=rt, c0=c0: e.dma_start(out=rt[:, 0, :], in_=b_ada[0:1, c0:c0 + 512].partition_broadcast(128)), w=[rb])
                dma("sp", lambda e, rt=rt, cq=cq, gpost=gpost: e.dma_start(
                    out=rt[:, 1, :], in_=gpost[0:1, cq * 512:(cq + 1) * 512].partition_broadcast(128)), w=[rb])
                pt, pb = psA.next()
                mmgroup(pt[:], pb, [(silurep[:, kc, :], wt[:, kc, :]) for kc in range(KC)], [wb, b_small])
                tt, tb = tmpr.next()
                op("dve", lambda e, tt=tt, pt=pt, rt=rt: e.tensor_tensor(out=tt[:], in0=pt[:], in1=rt[:, 0, :], op=ALU.add),
                   r=[pb, rb], w=[tb])
                op("dve", lambda e, tt=tt, rt=rt, grow=grow, cq=cq: e.tensor_tensor(
                    out=grow[:, cq * 512:(cq + 1) * 512], in0=tt[:], in1=rt[:, 1, :], op=ALU.mult),
                   r=[tb, rb], w=[b_garow[gi]])
        sch.barrier()
        sch.emit()
    if upto == "A":
        return finish()

    w_in_v = w_in.rearrange("(kc p) n -> p kc n", p=128)
    TWO_PI = 2.0 * math.pi
    BL = cfg.get('blevel', 9)
    ROPE_ADD_ENG = cfg.get('rope_add', 'dve')
    CW1 = 6.28125
    CW2 = TWO_PI - CW1

    def norm_transpose(x_src, tok0, hT, b_hT, hcol0, sc_off, xt_r, xn_r, junk, b_junk, st_r, psT_r, alt):
        xt, xb = xt_r.next()
        dma("sp", lambda e: e.dma_start(out=xt[:], in_=x_src[tok0:tok0 + 128, :]), w=[xb])
        st, sb_ = st_r.next()
        op("act", lambda e: e.activation(out=junk[:], in_=xt[:], func=AF.Square, accum_out=st[:, 0:1]),
           r=[xb], w=[b_junk, sb_])
        op("act", lambda e: e.activation(out=st[:, 1:2], in_=st[:, 0:1], func=AF.Sqrt, scale=1.0 / D, bias=EPS),
           r=[sb_], w=[sb_])
        op("dve", lambda e: e.reciprocal(out=st[:, 2:3], in_=st[:, 1:2]), r=[sb_], w=[sb_])
        xn, xnb = xn_r.next()
        op("dve", lambda e: e.tensor_scalar(out=xn[:], in0=xt[:], scalar1=st[:, 2:3], scalar2=None, op0=ALU.mult),
           r=[xb, sb_], w=[xnb])
        for half in range(2):
            pt, pb = psT_r.next()
            for j in range(8):
                kc = half * 8 + j
                op("pe", lambda e, pt=pt, j=j, kc=kc: e.transpose(pt[:, j * 128:(j + 1) * 128],
                                                                   xn[:, kc * 128:(kc + 1) * 128], identb[:]),
                   r=[xnb, b_cb] if j in (0, 7) else (), w=[pb])
            for j in range(8):
                kc = half * 8 + j
                eng = "act" if (half + alt) % 2 == 0 else "dve"
                if eng == "act":
                    op("act", lambda e, pt=pt, j=j, kc=kc: e.activation(
                        out=hT[:, kc, hcol0:hcol0 + 128], in_=pt[:, j * 128:(j + 1) * 128], func=AF.Identity,
                        scale=modc[:, sc_off + kc:sc_off + kc + 1], bias=modc[:, sc_off + KC + kc:sc_off + KC + kc + 1]),
                       r=[pb, b_modc], w=[b_hT])
                else:
                    op("dve", lambda e, pt=pt, j=j, kc=kc: e.tensor_scalar(
                        out=hT[:, kc, hcol0:hcol0 + 128], in0=pt[:, j * 128:(j + 1) * 128],
                        scalar1=modc[:, sc_off + kc:sc_off + kc + 1], scalar2=modc[:, sc_off + KC + kc:sc_off + KC + kc + 1],
                        op0=ALU.mult, op1=ALU.add), r=[pb, b_modc], w=[b_hT])

    def proj_pass(pname, x_src, pos_src, T, fm_blocks, tm_blocks):
        with ExitStack() as ph:
            sg = min(SG, T)
            ngr = sg // 512
            hT = ph.enter_context(nc.sbuf_tensor(pname + "hT", [128, KC, sg], BF16))
            b_hT = Buf("hT")
            xt_r = Ring(nc, ph, pname + "xt", [128, D], F32, 2)
            xn_r = Ring(nc, ph, pname + "xn", [128, D], BF16, 1)
            junk = ph.enter_context(nc.sbuf_tensor(pname + "junk", [128, D], BF16))
            b_junk = Buf("junk")
            st_r = Ring(nc, ph, pname + "st", [128, 4], F32, 4)
            psT_r = Ring(nc, ph, pname + "psT", [128, 1024], BF16, 2, psum=True)
            psM_r = Ring(nc, ph, pname + "psM", [128, 512], F32, 4, psum=True)
            psR_r = Ring(nc, ph, pname + "psR", [128, 512], F32, 2, psum=True)
            wfm_r = Ring(nc, ph, pname + "wfm", [128, KC, 128], BF16, 3)
            wtm_r = Ring(nc, ph, pname + "wtm", [128, KC, 512], BF16, 1)
            zb_r = Ring(nc, ph, pname + "zb", [128, 512], BF16, 2)
            t1_r = Ring(nc, ph, pname + "t1", [128, 512], F32, 2)
            t2_r = Ring(nc, ph, pname + "t2", [128, 512], F32, 2)
            ob_r = Ring(nc, ph, pname + "ob", [128, 512], BF16, 3)
            obf_r = Ring(nc, ph, pname + "obf", [128, 512], F32, 2)
            posi = ph.enter_context(nc.sbuf_tensor(pname + "posi", [128, 512], I32))
            posf = ph.enter_context(nc.sbuf_tensor(pname + "posf", [128, 512], F32))
            ang = ph.enter_context(nc.sbuf_tensor(pname + "ang", [128, 512], F32))
            kfi = ph.enter_context(nc.sbuf_tensor(pname + "kfi", [128, 512], I32))
            kff = ph.enter_context(nc.sbuf_tensor(pname + "kff", [128, 512], F32))
            msk = ph.enter_context(nc.sbuf_tensor(pname + "msk", [128, 512], F32))
            b_tmp = Buf("ropetmp")
            tabs = ph.enter_context(nc.sbuf_tensor(pname + "tabs", [128, ngr, 4, 512], F32))
            b_tabs = Buf("tabs")
            for s0 in range(0, T, sg):
                for ti in range(sg // 128):
                    norm_transpose(x_src, s0 + ti * 128, hT, b_hT, ti * 128, 0, xt_r, xn_r, junk, b_junk, st_r, psT_r, ti)
                for g in range(ngr if BL >= 2 else 0):
                    t0 = s0 + g * 512
                    dma("sp", lambda e, t0=t0: e.dma_start(out=posi[:], in_=pos_src[0:1, t0:t0 + 512].partition_broadcast(128)),
                        w=[b_tmp])
                    op("dve", lambda e: e.tensor_copy(out=posf[:], in_=posi[:]), r=[b_tmp], w=[b_tmp])
                    for ti_, (invf, shift) in enumerate(((invfA, math.pi / 2), (invfA, 0.0), (invfI, math.pi / 2), (invfI, 0.0))):
                        R_, W_ = [b_tmp, b_consts], [b_tmp]
                        op("dve", lambda e, invf=invf, shift=shift: e.tensor_scalar(
                            out=ang[:], in0=posf[:], scalar1=invf, scalar2=shift, op0=ALU.mult, op1=ALU.add), r=R_, w=W_)
                        op("dve", lambda e: e.tensor_scalar(out=kfi[:], in0=ang[:], scalar1=1.0 / TWO_PI, scalar2=None,
                                                            op0=ALU.mult), r=R_, w=W_)
                        op("dve", lambda e: e.tensor_copy(out=kff[:], in_=kfi[:]), r=R_, w=W_)
                        op("dve", lambda e: e.scalar_tensor_tensor(out=ang[:], in0=kff[:], scalar=-CW1, in1=ang[:],
                                                                   op0=ALU.mult, op1=ALU.add), r=R_, w=W_)
                        op("dve", lambda e: e.scalar_tensor_tensor(out=ang[:], in0=kff[:], scalar=-CW2, in1=ang[:],
                                                                   op0=ALU.mult, op1=ALU.add), r=R_, w=W_)
                        op("dve", lambda e: e.tensor_scalar(out=msk[:], in0=ang[:], scalar1=math.pi, scalar2=None,
                                                            op0=ALU.is_gt), r=R_, w=W_)
                        op("dve", lambda e: e.scalar_tensor_tensor(out=ang[:], in0=msk[:], scalar=-TWO_PI, in1=ang[:],
                                                                   op0=ALU.mult, op1=ALU.add), r=R_, w=W_)
                        op("dve", lambda e: e.tensor_scalar(out=msk[:], in0=ang[:], scalar1=-math.pi, scalar2=None,
                                                            op0=ALU.is_lt), r=R_, w=W_)
                        op("dve", lambda e: e.scalar_tensor_tensor(out=ang[:], in0=msk[:], scalar=TWO_PI, in1=ang[:],
                                                                   op0=ALU.mult, op1=ALU.add), r=R_, w=W_)
                        op("dve", lambda e: e.tensor_scalar(out=ang[:], in0=ang[:], scalar1=math.pi, scalar2=-math.pi,
                                                            op0=ALU.min, op1=ALU.max), r=R_, w=W_)
                        op("act", lambda e, g=g, ti_=ti_: e.activation(out=tabs[:, g, ti_, :], in_=ang[:], func=AF.Sin),
                           r=[b_tmp], w=[b_tabs, b_tmp])
                for (col0, ncol, dst, rope) in fm_blocks:
                    if BL < 3 or (BL < 4 and rope is not None):
                        continue
                    if rope is not None and cfg.get('ropeonly') and (rope, ncol) != cfg.get('ropeonly'):
                        continue
                    wt, wb = wfm_r.next()
                    dma("pool", lambda e, wt=wt, col0=col0, ncol=ncol: e.dma_start(
                        out=wt[:, :, 0:ncol], in_=w_in_v[:, :, col0:col0 + ncol]), w=[wb])
                    for g in range(ngr):
                        t0 = s0 + g * 512
                        pt, pb = psM_r.next()
                        mmgroup(pt[0:ncol, :], pb,
                                [(wt[:, kc, 0:ncol], hT[:, kc, g * 512:(g + 1) * 512]) for kc in range(KC)], [wb, b_hT])
                        ot, obb = ob_r.next()
                        if rope is None:
                            op("act", lambda e, ot=ot, pt=pt, ncol=ncol: e.activation(out=ot[0:ncol, :], in_=pt[0:ncol, :], func=AF.Copy),
                               r=[pb], w=[obb])
                        else:
                            ci = 0 if rope == "A" else 2
                            rt = rtA if rope == "A" else rtI
                            zt, zbb = zb_r.next()
                            op("act", lambda e, zt=zt, pt=pt, ncol=ncol: e.activation(out=zt[0:ncol, :], in_=pt[0:ncol, :], func=AF.Copy),
                               r=[pb], w=[zbb])
                            p2, p2b = psR_r.next()
                            if cfg.get('ropevar', 0) != 1:
                                op("pe", lambda e, p2=p2, zt=zt, rt=rt, ncol=ncol: e.matmul(
                                    p2[0:ncol, :], rt[0:ncol, 0:ncol], zt[0:ncol, :], start=True, stop=True),
                                   r=[zbb, b_cb], w=[p2b])
                            else:
                                p2, p2b = pt, pb
                            a1, a1b = t1_r.next()
                            a2, a2b = t2_r.next()
                            if cfg.get('ropevar', 0) == 2:
                                op("act", lambda e, ot=ot, pt=pt, ncol=ncol: e.activation(out=ot[0:ncol, :], in_=pt[0:ncol, :], func=AF.Copy),
                                   r=[pb], w=[obb])
                                dma("sp", lambda e, ot=ot, dst=dst, t0=t0, ncol=ncol: e.dma_start(
                                    out=dst[0:ncol, t0:t0 + 512], in_=ot[0:ncol, :]), r=[obb])
                                continue
                            if cfg.get('ropevar', 0) == 3:
                                op("dve", lambda e, ot=ot, pt=pt, g=g, ci=ci, ncol=ncol: e.tensor_tensor(
                                    out=ot[0:ncol, :], in0=pt[0:ncol, :], in1=tabs[0:ncol, g, ci, :], op=ALU.mult),
                                   r=[pb, b_tabs], w=[obb])
                                dma("sp", lambda e, ot=ot, dst=dst, t0=t0, ncol=ncol: e.dma_start(
                                    out=dst[0:ncol, t0:t0 + 512], in_=ot[0:ncol, :]), r=[obb])
                                continue
                            op("dve", lambda e, a1=a1, pt=pt, g=g, ci=ci, ncol=ncol: e.tensor_tensor(
                                out=a1[0:ncol, :], in0=pt[0:ncol, :], in1=tabs[0:ncol, g, ci, :], op=ALU.mult),
                               r=[pb, b_tabs], w=[a1b])
                            op("dve", lambda e, a2=a2, p2=p2, g=g, ci=ci, ncol=ncol: e.tensor_tensor(
                                out=a2[0:ncol, :], in0=p2[0:ncol, :], in1=tabs[0:ncol, g, ci + 1, :], op=ALU.mult),
                               r=[p2b, b_tabs], w=[a2b])
                            op(ROPE_ADD_ENG, lambda e, ot=ot, a1=a1, a2=a2, ncol=ncol: e.tensor_tensor(
                                out=ot[0:ncol, :], in0=a1[0:ncol, :], in1=a2[0:ncol, :], op=ALU.add),
                               r=[a1b, a2b], w=[obb])
                        dma("sp", lambda e, ot=ot, dst=dst, t0=t0, ncol=ncol: e.dma_start(
                            out=dst[0:ncol, t0:t0 + 512], in_=ot[0:ncol, :]), r=[obb])
                for (col0, ncol, dst, dcol0, isf32) in tm_blocks:
                    if BL < 5:
                        continue
                    wt, wb = wtm_r.next()
                    dma("pool", lambda e, wt=wt, col0=col0, ncol=ncol: e.dma_start(
                        out=wt[:, :, 0:ncol], in_=w_in_v[:, :, col0:col0 + ncol]), w=[wb])
                    for ti in range(sg // 128):
                        t0 = s0 + ti * 128
                        pt, pb = psM_r.next()
                        mmgroup(pt[:, 0:ncol], pb,
                                [(hT[:, kc, ti * 128:(ti + 1) * 128], wt[:, kc, 0:ncol]) for kc in range(KC)], [wb, b_hT])
                        ot, obb = (obf_r if isf32 else ob_r).next()
                        op("act", lambda e, ot=ot, pt=pt, ncol=ncol: e.activation(out=ot[:, 0:ncol], in_=pt[:, 0:ncol], func=AF.Copy),
                           r=[pb], w=[obb])
                        dma("sp", lambda e, ot=ot, dst=dst, t0=t0, ncol=ncol, dcol0=dcol0: e.dma_start(
                            out=dst[t0:t0 + 128, dcol0:dcol0 + ncol], in_=ot[:, 0:ncol]), r=[obb])
            sch.barrier()
            sch.emit()

    fm1 = [(C_KA + j * 128, 128, KA[j], "A") for j in range(A_KV)]
    fm1 += [(C_KB + h * 128, 128, KB[h], None) for h in range(B_H)]
    fm1 += [(C_KI, 64, KI, "I")]
    tm1 = [(C_VA, 256, VA, 0, False), (C_VB, 512, VB, 0, False), (C_VB + 512, 512, VB, 512, False)]
    proj_pass("p1", x_all, pos_all, S, fm1, tm1)
    fm2 = [(C_QA + h * 128, 128, QA[h], "A") for h in range(A_H)]
    fm2 += [(C_QB + h * 128, 128, QB[h], None) for h in range(B_H)]
    fm2 += [(C_QI + h * 128, 128, QI[h], "I") for h in range(IDX_H // 2)]
    fm2 += [(C_GA + f * 128, 128, GA[f], None) for f in range(KC)]
    fm2 += [(C_GB + f * 128, 128, GB[f], None) for f in range(KC)]
    tm2 = [(C_WI, IDX_H, WI, 0, True)]
    proj_pass("p2", x_own, pos_own, So, fm2, tm2)
    if upto == "B":
        return finish()

    def pipeline(items, skew):
        n = len(items)
        for idx in range(n + skew):
            if idx < n:
                items[idx][0]()
            if idx >= skew:
                items[idx - skew][1]()

    def qblocks():
        for i in range(Go):
            for jj in range(4):
                qb = i * 4 + jj
                yield i, jj, qb, qb * 128, 128 * (4 * (2 * i + 1) + jj + 1), i % 2

    with ExitStack() as ph:
        kiT = ph.enter_context(nc.sbuf_tensor("kiT", [64, S], BF16))
        b_ki = Buf("kiT")
        amask_s = ph.enter_context(nc.sbuf_tensor("amask_s", [128, 2, 640], F32))
        b_am = Buf("amask")
        Ssc = ph.enter_context(nc.sbuf_tensor("Ssc", [128, S], F32))
        b_S = Buf("S")
        Mk = ph.enter_context(nc.sbuf_tensor("Mk", [128, S], BF16))
        b_M = Buf("Mk")
        qi_r = Ring(nc, ph, "qiT", [64, IDX_H, 128], BF16, 2)
        wi_r = Ring(nc, ph, "wit", [128, 3, IDX_H], F32, 2)
        rr_r = Ring(nc, ph, "rr", [128, 512], F32, 3)
        ps_r = Ring(nc, ph, "psI", [128, 512], F32, 4, psum=True)
        bs = ph.enter_context(nc.sbuf_tensor("bs", [128, 8], F32))
        b_bs = Buf("bs")
        dma("sp", lambda e: e.dma_start(out=kiT[:], in_=KI[:, :]), w=[b_ki])
        dma("sp", lambda e: e.dma_start(out=amask_s[:].rearrange("p m c -> p (m c)"), in_=amask_d[:, :]), w=[b_am])
        for (i, jj, qb, tok0, nk, m) in qblocks():
            qt, qbb = qi_r.next()
            for hh in range(2):
                dma("sp", lambda e, qt=qt, hh=hh, tok0=tok0: e.dma_start(
                    out=qt[:, hh::2, :], in_=QI[:, hh * 64:(hh + 1) * 64, tok0:tok0 + 128].rearrange("hp d t -> d hp t")),
                    w=[qbb])
            wt, wbb = wi_r.next()
            dma("sp", lambda e, wt=wt, tok0=tok0: e.dma_start(out=wt[:, 0, :], in_=WI[tok0:tok0 + 128, :]), w=[wbb])
            op("act", lambda e, wt=wt: e.activation(out=wt[:, 1, :], in_=wt[:, 0, :], func=AF.Abs), r=[wbb], w=[wbb])
            op("act", lambda e, wt=wt: e.activation(out=wt[:, 2, :], in_=wt[:, 0, :], func=AF.Sign), r=[wbb], w=[wbb])
            nch = (nk + 511) // 512
            for c in range(nch):
                c0 = c * 512
                cw = min(512, nk - c0)
                for h in range(IDX_H):
                    pt, pb = ps_r.next()
                    op("pe", lambda e, pt=pt, qt=qt, h=h, c0=c0, cw=cw: e.matmul(
                        pt[:, 0:cw], qt[:, h, :], kiT[:, c0:c0 + cw], start=True, stop=True),
                       r=[qbb, b_ki], w=[pb])
                    rt, rb = rr_r.next()
                    op("act", lambda e, rt=rt, pt=pt, wt=wt, h=h, cw=cw: e.activation(
                        out=rt[:, 0:cw], in_=pt[:, 0:cw], func=AF.Relu, scale=wt[:, 1, h:h + 1]),
                       r=[pb, wbb], w=[rb])
                    if h == 0:
                        op("dve", lambda e, rt=rt, wt=wt, h=h, c0=c0, cw=cw: e.tensor_scalar(
                            out=Ssc[:, c0:c0 + cw], in0=rt[:, 0:cw], scalar1=wt[:, 2, h:h + 1], scalar2=None, op0=ALU.mult),
                           r=[rb, wbb], w=[b_S])
                    else:
                        op("dve", lambda e, rt=rt, wt=wt, h=h, c0=c0, cw=cw: e.scalar_tensor_tensor(
                            out=Ssc[:, c0:c0 + cw], in0=rt[:, 0:cw], scalar=wt[:, 2, h:h + 1], in1=Ssc[:, c0:c0 + cw],
                            op0=ALU.mult, op1=ALU.add), r=[rb, wbb, b_S], w=[b_S])
            op("dve", lambda e, nk=nk: e.tensor_reduce(out=bs[:, 5:6], in_=Ssc[:, 0:nk], axis=AX.X, op=ALU.max),
               r=[b_S], w=[b_bs])
            op("dve", lambda e, nk=nk: e.tensor_reduce(out=bs[:, 0:1], in_=Ssc[:, 0:nk], axis=AX.X, op=ALU.min),
               r=[b_S], w=[b_bs])
            op("dve", lambda e: e.tensor_tensor(out=bs[:, 1:2], in0=bs[:, 5:6], in1=bs[:, 0:1], op=ALU.subtract),
               r=[b_bs], w=[b_bs])
            op("dve", lambda e, nk=nk, m=m: e.tensor_tensor(out=Ssc[:, nk - 640:nk], in0=Ssc[:, nk - 640:nk],
                                                             in1=amask_s[:, m, :], op=ALU.add),
               r=[b_S, b_am], w=[b_S])
            for it in range(NBIS):
                op("dve", lambda e: e.tensor_scalar(out=bs[:, 1:2], in0=bs[:, 1:2], scalar1=0.5, scalar2=None, op0=ALU.mult),
                   r=[b_bs], w=[b_bs])
                op("dve", lambda e: e.tensor_tensor(out=bs[:, 2:3], in0=bs[:, 0:1], in1=bs[:, 1:2], op=ALU.add),
                   r=[b_bs], w=[b_bs])
                op("dve", lambda e, nk=nk: e.tensor_scalar(out=Mk[:, 0:nk], in0=Ssc[:, 0:nk], scalar1=bs[:, 2:3], scalar2=None,
                                                           op0=ALU.is_ge, op1=ALU.add, accum_out=bs[:, 3:4]),
                   r=[b_bs, b_S, b_M], w=[b_M, b_bs])
                op("dve", lambda e: e.tensor_scalar(out=bs[:, 4:5], in0=bs[:, 3:4], scalar1=float(TOPK), scalar2=bs[:, 1:2],
                                                    op0=ALU.is_ge, op1=ALU.mult), r=[b_bs], w=[b_bs])
                op("dve", lambda e: e.tensor_tensor(out=bs[:, 0:1], in0=bs[:, 0:1], in1=bs[:, 4:5], op=ALU.add),
                   r=[b_bs], w=[b_bs])
            op("dve", lambda e, nk=nk: e.tensor_scalar(out=Mk[:, 0:nk], in0=Ssc[:, 0:nk], scalar1=bs[:, 0:1], scalar2=None,
                                                       op0=ALU.is_ge), r=[b_bs, b_S, b_M], w=[b_M])
            dma("sp", lambda e, qb=qb, nk=nk: e.dma_start(out=MK[qb, :, 0:nk], in_=Mk[:, 0:nk]), r=[b_M])
        sch.barrier()
        sch.emit()
    if upto == "C1":
        return finish()

    with ExitStack() as ph:
        kaT = ph.enter_context(nc.sbuf_tensor("kaT", [128, A_KV, S], BF16))
        va = ph.enter_context(nc.sbuf_tensor("va_s", [128, NT, A_KV * HD], BF16))
        b_kv = Buf("kv")
        mk_r = Ring(nc, ph, "mk", [128, S], BF16, 2)
        mt_r = Ring(nc, ph, "mt", [128, NT, 128], BF16, 2)
        qa_r = Ring(nc, ph, "qa", [128, A_H * 128], BF16, 2)
        e_r = Ring(nc, ph, "ee", [128, 512], BF16, 3)
        p_r = Ring(nc, ph, "pp", [128, 512], BF16, 4)
        rd_r = Ring(nc, ph, "rd", [128, 512], F32, 2)
        of_r = Ring(nc, ph, "of", [128, 512], F32, 2)
        ob_r = Ring(nc, ph, "obc", [128, 512], BF16, 2)
        psS = Ring(nc, ph, "psS", [128, 512], F32, 3, psum=True)
        psO = Ring(nc, ph, "psO", [128, 512], F32, 2, psum=True)
        psD = Ring(nc, ph, "psD", [128, 512], F32, 2, psum=True)
        psT = Ring(nc, ph, "psTc", [128, 1024], BF16, 1, psum=True)
        for j in range(A_KV):
            dma("sp", lambda e, j=j: e.dma_start(out=kaT[:, j, :], in_=KA[j]), w=[b_kv])
        dma("sp", lambda e: e.dma_start(out=va[:], in_=VA.rearrange("(kb p) c -> p kb c", p=128)), w=[b_kv])
        isq = 1.0 / math.sqrt(HD)
        items = []
        for (i, jj, qb, tok0, nk, m) in qblocks():
            nkb = nk // 128
            qst = {}

            def setup(qst=qst, qb=qb, tok0=tok0, nk=nk, nkb=nkb):
                mkt, mkb = mk_r.next()
                dma("sp", lambda e: e.dma_start(out=mkt[:, 0:nk], in_=MK[qb, :, 0:nk]), w=[mkb])
                qt, qbb = qa_r.next()
                dma("sp", lambda e: e.dma_start(
                    out=qt[:].rearrange("p (h t) -> p h t", h=A_H), in_=QA[:, :, tok0:tok0 + 128].rearrange("h d t -> d h t")),
                    w=[qbb])
                mtt, mtb = mt_r.next()
                for k0 in range(0, nkb, 8):
                    n8 = min(8, nkb - k0)
                    pt, pb = psT.next()
                    for u in range(n8):
                        op("pe", lambda e, pt=pt, u=u, k0=k0: e.transpose(
                            pt[:, u * 128:(u + 1) * 128], mkt[:, (k0 + u) * 128:(k0 + u + 1) * 128], identb[:]),
                           r=[mkb, b_cb] if u in (0, n8 - 1) else (), w=[pb])
                    op("act", lambda e, pt=pt, k0=k0, n8=n8: e.activation(
                        out=mtt[:, k0:k0 + n8, :].rearrange("p k t -> p (k t)"), in_=pt[:, 0:n8 * 128], func=AF.Copy),
                       r=[pb], w=[mtb])
                qst.update(qt=qt, qbb=qbb, mtt=mtt, mtb=mtb)

            for j in range(A_KV):
                jst = {}
                for kb in range(nkb):
                    it = {}

                    def stA(it=it, qst=qst, jst=jst, j=j, kb=kb, first=(j == 0 and kb == 0), setup=setup):
                        if first:
                            setup()
                        if kb == 0:
                            jst["po"], jst["pob"] = psO.next()
                            jst["pd"], jst["pdb"] = psD.next()
                        qt, qbb, mtt, mtb = qst["qt"], qst["qbb"], qst["mtt"], qst["mtb"]
                        ps, psb = psS.next()
                        op("pe", lambda e: e.matmul(ps[:], kaT[:, j, kb * 128:(kb + 1) * 128], qt[:, j * 512:(j + 1) * 512],
                                                    start=True, stop=True), r=[b_kv, qbb], w=[psb])
                        et, eb = e_r.next()
                        op("act", lambda e: e.activation(out=et[:], in_=ps[:], func=AF.Exp, scale=isq), r=[psb], w=[eb])
                        pp, ppb = p_r.next()
                        op("dve", lambda e: e.tensor_tensor(
                            out=pp[:].rearrange("p (g t) -> p g t", g=4), in0=et[:].rearrange("p (g t) -> p g t", g=4),
                            in1=mtt[:, kb, :].unsqueeze(1).broadcast_to([128, 4, 128]), op=ALU.mult), r=[eb, mtb], w=[ppb])
                        it["pp"], it["ppb"] = pp, ppb

                    def stB(it=it, jst=jst, j=j, kb=kb, nkb=nkb, tok0=tok0):
                        pp, ppb = it["pp"], it["ppb"]
                        po, pob, pd, pdb = jst["po"], jst["pob"], jst["pd"], jst["pdb"]
                        op("pe", lambda e: e.matmul(po[:], va[:, kb, j * 128:(j + 1) * 128], pp[:],
                                                    start=(kb == 0), stop=(kb == nkb - 1)), r=[ppb, b_kv], w=[pob])
                        op("pe", lambda e: e.matmul(pd[:], onesb[:], pp[:], start=(kb == 0), stop=(kb == nkb - 1)),
                           r=[ppb, b_cb], w=[pdb])
                        if kb == nkb - 1:
                            rd, rdb = rd_r.next()
                            op("dve", lambda e: e.reciprocal(out=rd[:], in_=pd[:]), r=[pdb], w=[rdb])
                            of_, ofb = of_r.next()
                            op("dve", lambda e: e.tensor_tensor(out=of_[:], in0=po[:], in1=rd[:], op=ALU.mult),
                               r=[pob, rdb], w=[ofb])
                            obt, obb = ob_r.next()
                            op("act", lambda e: e.activation(out=obt[:], in_=of_[:], func=AF.Copy), r=[ofb], w=[obb])
                            dma("sp", lambda e: e.dma_start(
                                out=OA[4 * j:4 * j + 4, :, tok0:tok0 + 128].rearrange("h d t -> d h t"),
                                in_=obt[:].rearrange("p (h t) -> p h t", h=4)), r=[obb])

                    items.append((stA, stB))
        pipeline(items, 2)
        sch.barrier()
        sch.emit()
    if upto == "C2":
        return finish()

    with ExitStack() as ph:
        cmask_s = ph.enter_context(nc.sbuf_tensor("cmask_s", [128, 2, 8, 512], BF16))
        b_cm = Buf("cmask")
        dma("sp", lambda e: e.dma_start(out=cmask_s[:].rearrange("p m j t -> p (m j t)"), in_=cmask_d[:, :]), w=[b_cm])
        kb_r = Ring(nc, ph, "kbT", [128, S], BF16, 2)
        vb_r = Ring(nc, ph, "vbh", [128, NT, 128], BF16, 2)
        qb_r = Ring(nc, ph, "qbT", [128, So], BF16, 2)
        E_r = Ring(nc, ph, "sbE", [128, 512], F32, 4)
        Em_r = Ring(nc, ph, "sbEm", [128, 512], F32, 4)
        SP_r = Ring(nc, ph, "sbSP", [128, 512], BF16, 4)
        SPm_r = Ring(nc, ph, "sbSPm", [128, 512], BF16, 4)
        X_r = Ring(nc, ph, "sbX", [128, 512], F32, 3)
        At_r = Ring(nc, ph, "sbAt", [128, 512], BF16, 3)
        Ra_r = Ring(nc, ph, "sbRa", [128, 512], F32, 2)
        obD_r = Ring(nc, ph, "obD", [128, 512], BF16, 2)
        psZ = Ring(nc, ph, "psZ", [128, 512], F32, 3, psum=True)
        psCc = Ring(nc, ph, "psCc", [128, 512], F32, 3, psum=True)
        psOd = Ring(nc, ph, "psOd", [128, 512], F32, 2, psum=True)
        isq = 1.0 / math.sqrt(HD)
        items = []
        for h in range(B_H):
            hst = {}

            def hsetup(hst=hst, h=h):
                kt, ktb = kb_r.next()
                vt, vtb = vb_r.next()
                qt, qtb = qb_r.next()
                dma("sp", lambda e: e.dma_start(out=kt[:], in_=KB[h]), w=[ktb])
                dma("sp", lambda e: e.dma_start(
                    out=vt[:], in_=VB[:, h * 128:(h + 1) * 128].rearrange("(kb p) d -> p kb d", p=128)), w=[vtb])
                dma("sp", lambda e: e.dma_start(out=qt[:], in_=QB[h]), w=[qtb])
                hst.update(kt=kt, ktb=ktb, vt=vt, vtb=vtb, qt=qt, qtb=qtb)

            for i in range(Go):
                nkb = 4 * (2 * i + 2)
                m = i % 2
                gst = {}
                for n, kb in enumerate(reversed(range(nkb))):
                    j = kb - (nkb - 8)
                    it = {}

                    def stA(it=it, hst=hst, gst=gst, i=i, n=n, kb=kb, j=j, m=m, first=(i == 0 and n == 0), hsetup=hsetup):
                        if first:
                            hsetup()
                        if n == 0:
                            gst["ra"], gst["rab"] = Ra_r.next()
                            gst["po"], gst["pob"] = psOd.next()
                        kt, ktb, qt, qtb = hst["kt"], hst["ktb"], hst["qt"], hst["qtb"]
                        pz, pzb = psZ.next()
                        op("pe", lambda e: e.matmul(pz[:], kt[:, kb * 128:(kb + 1) * 128], qt[:, i * 512:(i + 1) * 512],
                                                    start=True, stop=True), r=[ktb, qtb], w=[pzb])
                        Et, Eb = E_r.next()
                        op("act", lambda e: e.activation(out=Et[:], in_=pz[:], func=AF.Exp, scale=isq), r=[pzb], w=[Eb])
                        St, Sb = SP_r.next()
                        op("act", lambda e: e.activation(out=St[:], in_=Et[:], func=AF.Ln, bias=1.0), r=[Eb], w=[Sb])
                        if j >= 0:
                            Sm, Smb = SPm_r.next()
                            op("dve", lambda e: e.tensor_tensor(out=Sm[:], in0=St[:], in1=cmask_s[:, m, j, :], op=ALU.mult),
                               r=[Sb, b_cm], w=[Smb])
                            Em, Emb = Em_r.next()
                            op("pool", lambda e: e.tensor_tensor(out=Em[:], in0=Et[:], in1=cmask_s[:, m, j, :], op=ALU.mult),
                               r=[Eb, b_cm], w=[Emb])
                        else:
                            Sm, Smb = St, Sb
                            Em, Emb = Et, Eb
                        it.update(Sm=Sm, Smb=Smb, Em=Em, Emb=Emb)

                    def stB(it=it, hst=hst, gst=gst, h=h, i=i, n=n, kb=kb, nkb=nkb):
                        Sm, Smb, Em, Emb = it["Sm"], it["Smb"], it["Em"], it["Emb"]
                        ra, rab, po, pob = gst["ra"], gst["rab"], gst["po"], gst["pob"]
                        vt, vtb = hst["vt"], hst["vtb"]
                        pc, pcb = psCc.next()
                        op("pe", lambda e: e.matmul(pc[:], triI[:], Sm[:], start=True, stop=(n == 0)), r=[Smb, b_cb], w=[pcb])
                        if n > 0:
                            op("pe", lambda e: e.matmul(pc[:], onesf, ra[:], start=False, stop=True), r=[rab, b_consts], w=[pcb])
                        Xt, Xb = X_r.next()
                        op("act", lambda e: e.activation(out=Xt[:], in_=pc[:], func=AF.Exp, scale=-1.0), r=[pcb], w=[Xb])
                        At, Ab = At_r.next()
                        op("dve", lambda e: e.tensor_tensor(out=At[:], in0=Em[:], in1=Xt[:], op=ALU.mult), r=[Emb, Xb], w=[Ab])
                        if n == 0:
                            op("pool", lambda e: e.tensor_copy(out=ra[:], in_=Sm[:]), r=[Smb], w=[rab])
                        elif n < nkb - 1:
                            op("pool", lambda e: e.tensor_tensor(out=ra[:], in0=ra[:], in1=Sm[:], op=ALU.add), r=[Smb, rab], w=[rab])
                        op("pe", lambda e: e.matmul(po[:], vt[:, kb, :], At[:], start=(n == 0), stop=(n == nkb - 1)),
                           r=[vtb, Ab], w=[pob])
                        if n == nkb - 1:
                            ot, otb = obD_r.next()
                            op("act", lambda e: e.activation(out=ot[:], in_=po[:], func=AF.Copy), r=[pob], w=[otb])
                            dma("sp", lambda e: e.dma_start(out=OB[h, :, i * 512:(i + 1) * 512], in_=ot[:]), r=[otb])

                    items.append((stA, stB))
        pipeline(items, 2)
        sch.barrier()
        sch.emit()
    if upto == "D":
        return finish()

    with ExitStack() as ph:
        wa = ph.enter_context(nc.sbuf_tensor("wa", [128, 8, D], BF16))
        wb_ = ph.enter_context(nc.sbuf_tensor("wb_", [128, 8, D], BF16))
        b_wab = Buf("wab")
        for (wt, src) in ((wa, w_bra), (wb_, w_brb)):
            sv = src.rearrange("(kc p) n -> p kc n", p=128)
            for cq in range(4):
                dma("pool", lambda e, wt=wt, sv=sv, cq=cq: e.dma_start(
                    out=wt[:, :, cq * 512:(cq + 1) * 512], in_=sv[:, :, cq * 512:(cq + 1) * 512]), w=[b_wab])
        zt = ph.enter_context(nc.sbuf_tensor("zt", [128, 4096], BF16))
        b_zt = Buf("zt")
        op("dve", lambda e: e.memset(zt[:], 0.0), w=[b_zt])
        XGf = XG.rearrange("(r p two) c -> r p (two c)", p=128, two=2)
        for r_ in range(NE * CAP // 256):
            dma("sp", lambda e, r_=r_: e.dma_start(out=XGf[r_], in_=zt[:]), r=[b_zt])
        oa_r = Ring(nc, ph, "oaT", [128, 8, 512], BF16, 2)
        ob2_r = Ring(nc, ph, "obT", [128, 8, 512], BF16, 2)
        g_r = Ring(nc, ph, "gt", [128, 2, 512], BF16, 3)
        sg_r = Ring(nc, ph, "sgt", [128, 2, 512], F32, 2)
        t1e_r = Ring(nc, ph, "t1e", [128, 512], F32, 2)
        t2e_r = Ring(nc, ph, "t2e", [128, 512], F32, 2)
        mg_r = Ring(nc, ph, "mgo", [128, 512], BF16, 3)
        psa_r = Ring(nc, ph, "psEa", [128, 512], F32, 3, psum=True)
        psb_r = Ring(nc, ph, "psEb", [128, 512], F32, 3, psum=True)
        for tg in range(So // 512):
            t0 = tg * 512
            oat, oab = oa_r.next()
            obt, obb = ob2_r.next()
            dma("sp", lambda e, oat=oat, t0=t0: e.dma_start(out=oat[:], in_=OA[:, :, t0:t0 + 512].rearrange("h d t -> d h t")), w=[oab])
            dma("sp", lambda e, obt=obt, t0=t0: e.dma_start(out=obt[:], in_=OB[:, :, t0:t0 + 512].rearrange("h d t -> d h t")), w=[obb])
            for fo in range(KC):
                gt, gb_ = g_r.next()
                dma("sp", lambda e, gt=gt, fo=fo, t0=t0: e.dma_start(out=gt[:, 0, :], in_=GA[fo, :, t0:t0 + 512]), w=[gb_])
                dma("sp", lambda e, gt=gt, fo=fo, t0=t0: e.dma_start(out=gt[:, 1, :], in_=GB[fo, :, t0:t0 + 512]), w=[gb_])
                st_, sb_ = sg_r.next()
                op("act", lambda e, st_=st_, gt=gt: e.activation(out=st_[:], in_=gt[:], func=AF.Sigmoid), r=[gb_], w=[sb_])
                pa, pab = psa_r.next()
                mmgroup(pa[:], pab, [(wa[:, kc, fo * 128:(fo + 1) * 128], oat[:, kc, :]) for kc in range(8)], [b_wab, oab])
                pb2, pbb = psb_r.next()
                mmgroup(pb2[:], pbb, [(wb_[:, kc, fo * 128:(fo + 1) * 128], obt[:, kc, :]) for kc in range(8)], [b_wab, obb])
                a1, a1b = t1e_r.next()
                a2, a2b = t2e_r.next()
                op("dve", lambda e, a1=a1, pa=pa, st_=st_: e.tensor_tensor(out=a1[:], in0=pa[:], in1=st_[:, 0, :], op=ALU.mult),
                   r=[pab, sb_], w=[a1b])
                op("dve", lambda e, a2=a2, pb2=pb2, st_=st_: e.tensor_tensor(out=a2[:], in0=pb2[:], in1=st_[:, 1, :], op=ALU.mult),
                   r=[pbb, sb_], w=[a2b])
                mt_, mb_ = mg_r.next()
                op("pool", lambda e, mt_=mt_, a1=a1, a2=a2: e.tensor_tensor(out=mt_[:], in0=a1[:], in1=a2[:], op=ALU.add),
                   r=[a1b, a2b], w=[mb_])
                dma("sp", lambda e, mt_=mt_, fo=fo, t0=t0: e.dma_start(out=MG[fo, :, t0:t0 + 512], in_=mt_[:]), r=[mb_])
        sch.barrier()
        sch.emit()
    if upto == "E1":
        return finish()

    NSLOT = NE * CAP
    NSOK = min(NSLOT, 65535)
    with ExitStack() as ph:
        wo = ph.enter_context(nc.sbuf_tensor("wo", [128, KC, D], BF16))
        b_wo = Buf("wo")
        wo_v = w_out.rearrange("(kc p) n -> p kc n", p=128)
        for cq in range(4):
            dma("pool", lambda e, cq=cq: e.dma_start(out=wo[:, :, cq * 512:(cq + 1) * 512], in_=wo_v[:, :, cq * 512:(cq + 1) * 512]),
                w=[b_wo])
        wr = ph.enter_context(nc.sbuf_tensor("wr", [128, KC, NE], BF16))
        b_wr = Buf("wr")
        dma("pool", lambda e: e.dma_start(out=wr[:], in_=w_router.rearrange("(kc p) n -> p kc n", p=128)), w=[b_wr])
        brow = ph.enter_context(nc.sbuf_tensor("brow", [128, NE], F32))
        dma("sp", lambda e: e.dma_start(out=brow[:], in_=b_router[0:1, :].partition_broadcast(128)), w=[b_wr])
        base = ph.enter_context(nc.sbuf_tensor("base", [128, NE], F32))
        slotbase = ph.enter_context(nc.sbuf_tensor("slotbase", [128, NE], F32))
        b_base = Buf("base")
        op("dve", lambda e: e.memset(base[:], 0.0), w=[b_base])
        op("dve", lambda e: e.tensor_scalar(out=slotbase[:], in0=iota_f[:, 0:NE], scalar1=float(CAP), scalar2=None, op0=ALU.mult),
           r=[b_consts], w=[b_base])
        mg_r = Ring(nc, ph, "mgi", [128, KC, 512], BF16, 1)
        x_r = Ring(nc, ph, "xe", [128, D], F32, 2)
        mix_r = Ring(nc, ph, "mixs", [128, D], F32, 1)
        x1_r = Ring(nc, ph, "x1t", [128, D], F32, 2)
        xn_r = Ring(nc, ph, "xn2", [128, D], BF16, 2)
        h2_r = Ring(nc, ph, "h2T", [128, KC, 128], BF16, 2)
        junk = ph.enter_context(nc.sbuf_tensor("junkE", [128, D], BF16))
        b_junk = Buf("junkE")
        st_r = Ring(nc, ph, "stE", [128, 8], F32, 4)
        rt_r = Ring(nc, ph, "rtE", [128, 8, NE], F32, 2)
        mkb_r = Ring(nc, ph, "mkE", [128, NE], BF16, 2)
        t8_r = Ring(nc, ph, "t8E", [128, 24], F32, 2)
        psm_r = Ring(nc, ph, "psEm", [128, 512], F32, 3, psum=True)
        pst_r = Ring(nc, ph, "psEt", [128, 1024], BF16, 2, psum=True)
        psr_r = Ring(nc, ph, "psEr", [128, 512], F32, 2, psum=True)
        for tg in range(So // 512):
            mgt, mgb = mg_r.next()
            dma("sp", lambda e, mgt=mgt, tg=tg: e.dma_start(
                out=mgt[:], in_=MG[:, :, tg * 512:(tg + 1) * 512].rearrange("f d t -> d f t")), w=[mgb])
            for tt in range(4):
                ti = tg * 4 + tt
                tok0 = ti * 128
                xt, xb = x_r.next()
                dma("sp", lambda e, xt=xt, tok0=tok0: e.dma_start(out=xt[:], in_=x_own[tok0:tok0 + 128, :]), w=[xb])
                mx, mxb = mix_r.next()
                for dc in range(4):
                    pm, pmb = psm_r.next()
                    mmgroup(pm[:], pmb, [(mgt[:, kc, tt * 128:(tt + 1) * 128], wo[:, kc, dc * 512:(dc + 1) * 512])
                                         for kc in range(KC)], [mgb, b_wo])
                    op("act", lambda e, mx=mx, pm=pm, dc=dc: e.activation(out=mx[:, dc * 512:(dc + 1) * 512], in_=pm[:], func=AF.Copy),
                       r=[pmb], w=[mxb])
                st, sb_ = st_r.next()
                op("act", lambda e, st=st, mx=mx: e.activation(out=junk[:], in_=mx[:], func=AF.Square, accum_out=st[:, 0:1]),
                   r=[mxb], w=[b_junk, sb_])
                op("act", lambda e, st=st: e.activation(out=st[:, 1:2], in_=st[:, 0:1], func=AF.Sqrt, scale=1.0 / D, bias=EPS),
                   r=[sb_], w=[sb_])
                op("dve", lambda e, st=st: e.reciprocal(out=st[:, 2:3], in_=st[:, 1:2]), r=[sb_], w=[sb_])
                op("dve", lambda e, st=st, mx=mx: e.scalar_tensor_tensor(out=mx[:], in0=mx[:], scalar=st[:, 2:3], in1=ga1row[:],
                                                                        op0=ALU.mult, op1=ALU.mult),
                   r=[mxb, sb_, b_garow[0]], w=[mxb])
                x1, x1b = x1_r.next()
                op("pool", lambda e, x1=x1, mx=mx, xt=xt: e.tensor_tensor(out=x1[:], in0=mx[:], in1=xt[:], op=ALU.add),
                   r=[mxb, xb], w=[x1b])
                dma("sp", lambda e, x1=x1, tok0=tok0: e.dma_start(out=X1[tok0:tok0 + 128, :], in_=x1[:]), r=[x1b])
                op("act", lambda e, st=st, x1=x1: e.activation(out=junk[:], in_=x1[:], func=AF.Square, accum_out=st[:, 3:4]),
                   r=[x1b], w=[b_junk, sb_])
                op("act", lambda e, st=st: e.activation(out=st[:, 4:5], in_=st[:, 3:4], func=AF.Sqrt, scale=1.0 / D, bias=EPS),
                   r=[sb_], w=[sb_])
                op("dve", lambda e, st=st: e.reciprocal(out=st[:, 5:6], in_=st[:, 4:5]), r=[sb_], w=[sb_])
                xn, xnb = xn_r.next()
                op("dve", lambda e, xn=xn, x1=x1, st=st: e.tensor_scalar(out=xn[:], in0=x1[:], scalar1=st[:, 5:6], scalar2=None,
                                                                       op0=ALU.mult), r=[x1b, sb_], w=[xnb])
                h2, h2b = h2_r.next()
                for half in range(2):
                    pt, pb = pst_r.next()
                    for j in range(8):
                        kc = half * 8 + j
                        op("pe", lambda e, pt=pt, j=j, kc=kc, xn=xn: e.transpose(
                            pt[:, j * 128:(j + 1) * 128], xn[:, kc * 128:(kc + 1) * 128], identb[:]),
                           r=[xnb, b_cb] if j in (0, 7) else (), w=[pb])
                    for j in range(8):
                        kc = half * 8 + j
                        op("act", lambda e, pt=pt, j=j, kc=kc, h2=h2: e.activation(
                            out=h2[:, kc, :], in_=pt[:, j * 128:(j + 1) * 128], func=AF.Identity,
                            scale=modc[:, 2 * KC + kc:2 * KC + kc + 1], bias=modc[:, 3 * KC + kc:3 * KC + kc + 1]),
                           r=[pb, b_modc], w=[h2b])
                pr, prb = psr_r.next()
                mmgroup(pr[:, 0:NE], prb, [(h2[:, kc, :], wr[:, kc, :]) for kc in range(KC)], [h2b, b_wr])
                rt, rtb = rt_r.next()
                t8, t8b = t8_r.next()
                mk, mkb = mkb_r.next()
                lg, pos_, sla, ov, jk = (rt[:, q, :] for q in range(5))
                op("dve", lambda e, lg=lg, pr=pr: e.tensor_tensor(out=lg, in0=pr[:, 0:NE], in1=brow[:], op=ALU.add),
                   r=[prb, b_wr], w=[rtb])
                op("dve", lambda e, lg=lg, t8=t8: e.max(out=t8[:, 0:8], in_=lg), r=[rtb], w=[t8b])
                op("dve", lambda e, t8=t8: e.tensor_scalar(out=t8[:, 8:12], in0=t8[:, 0:4], scalar1=t8[:, 0:1], scalar2=None,
                                                          op0=ALU.subtract), r=[t8b], w=[t8b])
                op("act", lambda e, t8=t8, st=st: e.activation(out=t8[:, 12:16], in_=t8[:, 8:12], func=AF.Exp, accum_out=st[:, 6:7]),
                   r=[t8b], w=[t8b, sb_])
                op("dve", lambda e, st=st: e.reciprocal(out=st[:, 7:8], in_=st[:, 6:7]), r=[sb_], w=[sb_])
                op("dve", lambda e, mk=mk, lg=lg, t8=t8: e.tensor_scalar(out=mk[:], in0=lg, scalar1=t8[:, 3:4], scalar2=None,
                                                                       op0=ALU.is_ge), r=[rtb, t8b], w=[mkb])
                pp, ppb = psr_r.next()
                op("pe", lambda e, pp=pp, mk=mk: e.matmul(pp[:, 0:NE], triS[:], mk[:], start=True, stop=True),
                   r=[mkb, b_cb], w=[ppb])
                op("pe", lambda e, pp=pp, mk=mk: e.matmul(pp[:, 64:64 + NE], onesb[:], mk[:], start=True, stop=True),
                   r=[mkb, b_cb], w=[ppb])
                op("dve", lambda e, pos_=pos_, pp=pp: e.tensor_tensor(out=pos_, in0=pp[:, 0:NE], in1=base[:], op=ALU.add),
                   r=[ppb, b_base], w=[rtb])
                op("dve", lambda e, pp=pp: e.tensor_tensor(out=base[:], in0=pp[:, 64:64 + NE], in1=base[:], op=ALU.add),
                   r=[ppb, b_base], w=[b_base])
                op("dve", lambda e, ov=ov, pos_=pos_: e.tensor_scalar(out=ov, in0=pos_, scalar1=float(CAP), scalar2=1.0e9,
                                                                    op0=ALU.is_ge, op1=ALU.mult), r=[rtb], w=[rtb])
                op("dve", lambda e, sla=sla, pos_=pos_: e.tensor_tensor(out=sla, in0=pos_, in1=slotbase[:], op=ALU.add),
                   r=[rtb, b_base], w=[rtb])
                op("dve", lambda e, sla=sla, ov=ov: e.tensor_tensor(out=sla, in0=sla, in1=ov, op=ALU.add), r=[rtb], w=[rtb])
                for k in range(4):
                    op("dve", lambda e, jk=jk, lg=lg, t8=t8, sla=sla, k=k: e.scalar_tensor_tensor(
                        out=jk, in0=lg, scalar=t8[:, k:k + 1], in1=sla, op0=ALU.is_equal, op1=ALU.mult,
                        accum_out=t8[:, 16 + k:17 + k]), r=[rtb, t8b], w=[rtb, t8b])
                op("dve", lambda e, t8=t8, ti=ti: e.tensor_copy(out=rslot[:, ti * 4:(ti + 1) * 4], in_=t8[:, 16:20]),
                   r=[t8b], w=[b_route])
                op("dve", lambda e, t8=t8: e.tensor_scalar(out=t8[:, 20:24], in0=t8[:, 16:20], scalar1=float(NSOK), scalar2=None,
                                                          op0=ALU.is_lt), r=[t8b], w=[t8b])
                op("dve", lambda e, t8=t8, st=st: e.scalar_tensor_tensor(
                    out=t8[:, 12:16], in0=t8[:, 12:16], scalar=st[:, 7:8], in1=t8[:, 20:24], op0=ALU.mult, op1=ALU.mult),
                   r=[t8b, sb_], w=[t8b])
                op("dve", lambda e, t8=t8, ti=ti: e.tensor_copy(out=rwgt[:, ti * 4:(ti + 1) * 4], in_=t8[:, 12:16]),
                   r=[t8b], w=[b_route])
                for k in range(4):
                    dma("pool", lambda e, xn=xn, ti=ti, k=k: e.indirect_dma_start(
                        out=XG[0:NSOK, :], out_offset=bass.IndirectOffsetOnAxis(ap=rslot[:, ti * 4 + k:ti * 4 + k + 1], axis=0),
                        in_=xn[:, :], in_offset=None, bounds_check=sch.breg(e, NSOK - 1), oob_is_err=False),
                        r=[xnb, b_route])
        sch.barrier()
        sch.emit()
    if upto == "E2":
        return finish()

    NBLK = CAP // 128
    NTG = CAP // 512
    with ExitStack() as ph:
        XT = ph.enter_context(nc.sbuf_tensor("XT", [128, KC, CAP], BF16))
        b_XT = Buf("XT")
        actT = ph.enter_context(nc.sbuf_tensor("actT", [128, KC, CAP], BF16))
        b_act = [Buf(f"act{f}") for f in range(KC)]
        wc_r = Ring(nc, ph, "wcG", [128, KC, 256], BF16, 2)
        gt_r = Ring(nc, ph, "gtG", [128, D], BF16, 2)
        bb_r = Ring(nc, ph, "bbG", [128, 2 * KC], F32, 2)
        b2_r = Ring(nc, ph, "b2G", [1, 256], BF16, 3)
        g_r = Ring(nc, ph, "gG", [128, 512], F32, 2)
        s_r = Ring(nc, ph, "sG", [128, 512], F32, 2)
        l_r = Ring(nc, ph, "lG", [128, 512], F32, 2)
        gs_r = Ring(nc, ph, "gsG", [128, 512], F32, 2)
        ot_r = Ring(nc, ph, "otG", [128, 256], BF16, 3)
        psT_r = Ring(nc, ph, "psGt", [128, 1024], BF16, 2, psum=True)
        psg_r = Ring(nc, ph, "psGg", [128, 512], F32, 2, psum=True)
        psl_r = Ring(nc, ph, "psGl", [128, 512], F32, 2, psum=True)
        pso_r = Ring(nc, ph, "psGo", [128, 512], F32, 2, psum=True)
        for ex in range(NE):
            bt, btb = bb_r.next()
            dma("sp", lambda e, bt=bt, ex=ex: e.dma_start(out=bt[:], in_=b1c[ex]), w=[btb])
            w1v = w1[ex].rearrange("(kc p) n -> p kc n", p=128)
            w2v = w2[ex].rearrange("(kc p) n -> p kc n", p=128)
            for blk in range(NBLK):
                gt, gtb = gt_r.next()
                r0 = ex * CAP + blk * 128
                dma("sp", lambda e, gt=gt, r0=r0: e.dma_start(out=gt[:], in_=XG[r0:r0 + 128, :]), w=[gtb])
                for half in range(2):
                    pt, pb = psT_r.next()
                    for j in range(8):
                        kc = half * 8 + j
                        op("pe", lambda e, pt=pt, j=j, kc=kc, gt=gt: e.transpose(
                            pt[:, j * 128:(j + 1) * 128], gt[:, kc * 128:(kc + 1) * 128], identb[:]),
                           r=[gtb, b_cb] if j in (0, 7) else (), w=[pb])
                    for j in range(8):
                        kc = half * 8 + j
                        if half == 0:
                            op("act", lambda e, pt=pt, j=j, kc=kc, blk=blk: e.activation(
                                out=XT[:, kc, blk * 128:(blk + 1) * 128], in_=pt[:, j * 128:(j + 1) * 128], func=AF.Identity,
                                scale=modc[:, 2 * KC + kc:2 * KC + kc + 1], bias=modc[:, 3 * KC + kc:3 * KC + kc + 1]),
                               r=[pb, b_modc], w=[b_XT])
                        else:
                            op("dve", lambda e, pt=pt, j=j, kc=kc, blk=blk: e.tensor_scalar(
                                out=XT[:, kc, blk * 128:(blk + 1) * 128], in0=pt[:, j * 128:(j + 1) * 128],
                                scalar1=modc[:, 2 * KC + kc:2 * KC + kc + 1], scalar2=modc[:, 3 * KC + kc:3 * KC + kc + 1],
                                op0=ALU.mult, op1=ALU.add), r=[pb, b_modc], w=[b_XT])
            for cq in range(KC):
                wt, wb = wc_r.next()
                dma("pool", lambda e, wt=wt, w1v=w1v, cq=cq: e.dma_start(out=wt[:], in_=w1v[:, :, cq * 256:(cq + 1) * 256]), w=[wb])
                for tg in range(NTG):
                    pg, pgb = psg_r.next()
                    mmgroup(pg[:], pgb, [(wt[:, kc, 0:256:2], XT[:, kc, tg * 512:(tg + 1) * 512]) for kc in range(KC)], [wb, b_XT])
                    pl, plb = psl_r.next()
                    mmgroup(pl[:], plb, [(wt[:, kc, 1:256:2], XT[:, kc, tg * 512:(tg + 1) * 512]) for kc in range(KC)], [wb, b_XT])
                    g_, gb_ = g_r.next()
                    op("dve", lambda e, g_=g_, pg=pg, bt=bt, cq=cq: e.tensor_scalar(
                        out=g_[:], in0=pg[:], scalar1=bt[:, cq:cq + 1], scalar2=LIMIT, op0=ALU.add, op1=ALU.min),
                       r=[pgb, btb], w=[gb_])
                    sg, sgb = s_r.next()
                    op("act", lambda e, sg=sg, g_=g_: e.activation(out=sg[:], in_=g_[:], func=AF.Sigmoid, scale=ALPHA),
                       r=[gb_], w=[sgb])
                    l_, lb_ = l_r.next()
                    op("dve", lambda e, l_=l_, pl=pl, bt=bt, cq=cq: e.tensor_scalar(
                        out=l_[:], in0=pl[:], scalar1=bt[:, KC + cq:KC + cq + 1], scalar2=LIMIT, op0=ALU.add, op1=ALU.min),
                       r=[plb, btb], w=[lb_])
                    op("dve", lambda e, l_=l_: e.tensor_scalar(out=l_[:], in0=l_[:], scalar1=-LIMIT, scalar2=1.0,
                                                               op0=ALU.max, op1=ALU.add), r=[lb_], w=[lb_])
                    gs, gsb = gs_r.next()
                    op("pool", lambda e, gs=gs, g_=g_, sg=sg: e.tensor_tensor(out=gs[:], in0=g_[:], in1=sg[:], op=ALU.mult),
                       r=[gb_, sgb], w=[gsb])
                    op("dve", lambda e, gs=gs, l_=l_, cq=cq, tg=tg: e.tensor_tensor(
                        out=actT[:, cq, tg * 512:(tg + 1) * 512], in0=gs[:], in1=l_[:], op=ALU.mult),
                       r=[gsb, lb_], w=[b_act[cq]])
            for dc in range(D // 256):
                wt, wb = wc_r.next()
                dma("pool", lambda e, wt=wt, w2v=w2v, dc=dc: e.dma_start(out=wt[:], in_=w2v[:, :, dc * 256:(dc + 1) * 256]), w=[wb])
                b2t, b2b = b2_r.next()
                dma("pool", lambda e, b2t=b2t, ex=ex, dc=dc: e.dma_start(out=b2t[:], in_=b2[ex:ex + 1, dc * 256:(dc + 1) * 256]), w=[b2b])
                for blk in range(NBLK):
                    po, pob = pso_r.next()
                    pairs = [(actT[:, fc, blk * 128:(blk + 1) * 128], wt[:, fc, :]) for fc in range(KC)]
                    n = len(pairs)
                    for q, (l, r_) in enumerate(pairs):
                        op("pe", lambda e, po=po, l=l, r_=r_, q=q: e.matmul(po[:, 0:256], l, r_, start=(q == 0), stop=False),
                           r=(b_act + [wb]) if q in (0, n - 1) else (), w=[pob])
                    op("pe", lambda e, po=po, b2t=b2t, dc=dc: e.matmul(
                        po[:, 0:256], onesb[0:1, :], b2t[0:1, :], start=False, stop=True),
                       r=[b2b, b_cb], w=[pob])
                    ot, otb = ot_r.next()
                    op("act", lambda e, ot=ot, po=po: e.activation(out=ot[:], in_=po[:, 0:256], func=AF.Copy), r=[pob], w=[otb])
                    r0 = ex * CAP + blk * 128
                    dma("sp", lambda e, ot=ot, r0=r0, dc=dc: e.dma_start(out=YG[r0:r0 + 128, dc * 256:(dc + 1) * 256], in_=ot[:]),
                        r=[otb])
        sch.barrier()
        sch.emit()
    if upto == "G":
        return finish()

    with ExitStack() as ph:
        y_r = Ring(nc, ph, "yH", [128, D], BF16, 5)
        x1_r = Ring(nc, ph, "x1H", [128, D], F32, 2)
        f_r = Ring(nc, ph, "fH", [128, D], F32, 2)
        o_r = Ring(nc, ph, "oH", [128, D], F32, 2)
        junk = ph.enter_context(nc.sbuf_tensor("junkH", [128, D], BF16))
        b_junk = Buf("junkH")
        st_r = Ring(nc, ph, "stH", [128, 4], F32, 3)
        for (yt, yb) in y_r.tiles:
            op("dve", lambda e, yt=yt: e.memset(yt[:], 0.0), w=[yb])
        for ti in range(NTo):
            tok0 = ti * 128
            ys = []
            for k in range(4):
                yt, yb = y_r.next()
                dma("pool", lambda e, yt=yt, ti=ti, k=k: e.indirect_dma_start(
                    out=yt[:, :], out_offset=None, in_=YG[0:NSOK, :],
                    in_offset=bass.IndirectOffsetOnAxis(ap=rslot[:, ti * 4 + k:ti * 4 + k + 1], axis=0),
                    bounds_check=sch.breg(e, NSOK - 1), oob_is_err=False), r=[b_route], w=[yb])
                ys.append((yt, yb))
            x1, x1b = x1_r.next()
            dma("sp", lambda e, x1=x1, tok0=tok0: e.dma_start(out=x1[:], in_=X1[tok0:tok0 + 128, :]), w=[x1b])
            ft, fb = f_r.next()
            op("dve", lambda e, ft=ft, ti=ti, y0=ys[0][0]: e.tensor_scalar(
                out=ft[:], in0=y0[:], scalar1=rwgt[:, ti * 4:ti * 4 + 1], scalar2=None, op0=ALU.mult),
               r=[ys[0][1], b_route], w=[fb])
            for k in range(1, 4):
                op("dve", lambda e, ft=ft, ti=ti, k=k, yk=ys[k][0]: e.scalar_tensor_tensor(
                    out=ft[:], in0=yk[:], scalar=rwgt[:, ti * 4 + k:ti * 4 + k + 1], in1=ft[:], op0=ALU.mult, op1=ALU.add),
                   r=[ys[k][1], b_route, fb], w=[fb])
            st, sb_ = st_r.next()
            op("act", lambda e, st=st, ft=ft: e.activation(out=junk[:], in_=ft[:], func=AF.Square, accum_out=st[:, 0:1]),
               r=[fb], w=[b_junk, sb_])
            op("act", lambda e, st=st: e.activation(out=st[:, 1:2], in_=st[:, 0:1], func=AF.Sqrt, scale=1.0 / D, bias=EPS),
               r=[sb_], w=[sb_])
            op("dve", lambda e, st=st: e.reciprocal(out=st[:, 2:3], in_=st[:, 1:2]), r=[sb_], w=[sb_])
            op("dve", lambda e, st=st, ft=ft: e.scalar_tensor_tensor(out=ft[:], in0=ft[:], scalar=st[:, 2:3], in1=ga2row[:],
                                                                    op0=ALU.mult, op1=ALU.mult),
               r=[fb, sb_, b_garow[1]], w=[fb])
            ot, otb = o_r.next()
            op("pool", lambda e, ot=ot, ft=ft, x1=x1: e.tensor_tensor(out=ot[:], in0=ft[:], in1=x1[:], op=ALU.add),
               r=[fb, x1b], w=[otb])
            dma("sp", lambda e, ot=ot, tok0=tok0: e.dma_start(out=out_d[tok0:tok0 + 128, :], in_=ot[:]), r=[otb])
        sch.barrier()
        sch.emit()
    return finish()


def make_consts():
    c = np.zeros((128, 8, 128), np.float32)
    c[:, 0, :] = np.eye(128)
    for dp in range(16):
        c[dp + 16, 1, dp] = -1.0
        c[dp, 1, dp + 16] = 1.0
    for o in (0, 64):
        for dp in range(8):
            c[o + dp + 8, 2, o + dp] = -1.0
            c[o + dp, 2, o + dp + 8] = 1.0
    k = np.arange(128)
    c[:, 3, :] = (k[:, None] >= k[None, :])
    c[:, 4, :] = (k[:, None] < k[None, :])
    c[:, 5, :] = 1.0
    invA = THETA ** (-(np.arange(16, dtype=np.float32)) / np.float32(16))
    invI = THETA ** (-(np.arange(8, dtype=np.float32)) / np.float32(8))
    for p in range(128):
        c[p, 6, 0] = invA[p % 16] if p < 32 else 0.0
        c[p, 6, 1] = invI[(p % 64) % 8] if (p % 64) < 16 else 0.0
    c[:, 7, :] = k[None, :]
    return np.ascontiguousarray(c.reshape(128, 8 * 128))


def make_masks(half):
    p = np.arange(128)[:, None]
    tl = np.arange(512)[None, :]
    cm = np.zeros((128, 2, 8, 512), np.float32)
    am = np.zeros((128, 2, 640), np.float32)
    cc = np.arange(640)[None, :]
    for m in range(2):
        delta = (1 if m == 0 else 0) if half == 0 else (0 if m == 0 else 1)
        for j in range(8):
            if delta == 0:
                cm[:, m, j, :] = (128 * (j - 4) + p) < tl
            else:
                cm[:, m, j, :] = (128 * j + p) < tl
        lim = 64 * (p // 64 + 1)
        am[:, m, :] = np.where(512 * (delta - 1) + cc >= lim, NEG, 0.0)
    return (np.ascontiguousarray(cm.reshape(128, -1)).astype(ml_dtypes.bfloat16),
            np.ascontiguousarray(am.reshape(128, -1)))


def col_layout(v):
    return np.ascontiguousarray(v.reshape(-1, 128).T)


def prep(inputs, cfg, batches):
    S, NE = cfg["S"], cfg["NE"]
    G = S // 512
    f32 = np.float32
    x = np.asarray(inputs["x"], f32)
    c = np.asarray(inputs["c"], f32)
    pos = np.asarray(inputs["positions"], np.int32)
    shared = {
        "b_ada": np.ascontiguousarray(np.asarray(inputs["b_ada"], f32)[0][None, :]),
        "badac": col_layout(np.asarray(inputs["b_ada"], f32)[0]),
        "gcols": np.concatenate([col_layout(np.asarray(inputs[k], f32)[0]) for k in
                                 ("g_pre_mix", "g_post_mix", "g_pre_ffn", "g_post_ffn")], axis=1),
        "g_post_mix": np.ascontiguousarray(np.asarray(inputs["g_post_mix"], f32)[0][None, :]),
        "g_post_ffn": np.ascontiguousarray(np.asarray(inputs["g_post_ffn"], f32)[0][None, :]),
        "w_ada": np.ascontiguousarray(np.asarray(inputs["w_ada"], f32)[0]),
        "w_in": np.ascontiguousarray(np.asarray(inputs["w_in"], f32)[0]),
        "w_branch_a": np.ascontiguousarray(np.asarray(inputs["w_branch_a"], f32)[0]),
        "w_branch_b": np.ascontiguousarray(np.asarray(inputs["w_branch_b"], f32)[0]),
        "w_out": np.ascontiguousarray(np.asarray(inputs["w_out"], f32)[0]),
        "w_router": np.ascontiguousarray(np.asarray(inputs["w_router"], f32)[0][:, :NE]),
        "b_router": np.ascontiguousarray(np.asarray(inputs["b_router"], f32)[0][None, :NE]),
        "w1": np.ascontiguousarray(np.asarray(inputs["w1"], f32)[0][:NE]),
        "w2": np.ascontiguousarray(np.asarray(inputs["w2"], f32)[0][:NE]),
        "b2": np.ascontiguousarray(np.asarray(inputs["b2"], f32)[0][:NE]),
        "consts": make_consts(),
    }
    b1 = np.asarray(inputs["b1"], f32)[0][:NE]
    b1g = b1[:, 0::2].reshape(NE, KC, 128).transpose(0, 2, 1)
    b1l = b1[:, 1::2].reshape(NE, KC, 128).transpose(0, 2, 1)
    shared["b1c"] = np.ascontiguousarray(np.concatenate([b1g, b1l], axis=2))
    masks = [make_masks(0), make_masks(1)]
    in_maps, owns = [], []
    for b in batches:
        for half in range(2):
            toks = np.concatenate([np.arange(g * 512, (g + 1) * 512) for g in own_groups(G, half)])
            m = dict(shared)
            m["x_all"] = np.ascontiguousarray(x[b])
            m["x_own"] = np.ascontiguousarray(x[b][toks])
            m["pos_all"] = np.ascontiguousarray(pos[b][None, :])
            m["pos_own"] = np.ascontiguousarray(pos[b][toks][None, :])
            m["cvec"] = col_layout(c[b])
            m["cmask"], m["amask"] = masks[half]
            in_maps.append(m)
            owns.append((b, toks))
    return in_maps, owns


_CACHE = {}


def kernel(**inputs):
    x = np.asarray(inputs["x"])
    B, S, _ = x.shape
    cfg = {"S": S, "NE": 32, "CAP": 2048}
    in_maps, owns = prep(inputs, cfg, list(range(B)))
    nc = build(cfg)
    res = run_bass_kernel_spmd(nc, in_maps, core_ids=list(range(len(in_maps))))
    out = np.empty((B, S, D), np.float32)
    for r, (b, toks) in zip(res.results, owns):
        out[b, toks] = np.asarray(r["out"], np.float32)
    return out
```

```python
import math
from contextlib import ExitStack

import numpy as np
import ml_dtypes

import concourse.bass as bass
import concourse.mybir as mybir
from concourse.bass_utils import run_bass_kernel_spmd

F32 = mybir.dt.float32
BF16 = mybir.dt.bfloat16
I32 = mybir.dt.int32
AF = mybir.ActivationFunctionType
ALU = mybir.AluOpType
AX = mybir.AxisListType

D = 2048
KC = D // 128
A_H, A_KV, HD = 8, 2, 128
IDX_H, IDX_D = 16, 64
B_H = 8
TOPK = 256
THETA = 500000.0
EPS = 1e-6
LIMIT = 7.0
ALPHA = 1.702
C_QA, C_KA, C_VA = 0, 1024, 1280
C_QB, C_KB, C_VB = 1536, 2560, 3584
C_QI, C_KI, C_WI = 4608, 5632, 5696
C_GA, C_GB = 5712, 7760
IN_W = 9808
NEG = -1.0e30
NBIS = 26


class Buf:
    __slots__ = ("w", "r", "name", "excl")

    def __init__(self, name="", excl=False):
        self.w = None
        self.r = []
        self.name = name
        self.excl = excl


class Sched:
    ENGS = ("pe", "act", "dve", "pool", "sp")
    RQ = {"sp": 8, "pool": 2, "act": 4}

    def __init__(self, nc, es):
        self.nc = nc
        self.sems = {}
        for e in self.ENGS:
            self.sems[e] = es.enter_context(nc.semaphore("c_" + e))
        self.dq = {}
        for q in ("sp", "pool", "act"):
            self.dq[q] = [es.enter_context(nc.semaphore(f"d_{q}{i}")) for i in range(self.RQ[q])]
        self.n = {e: 0 for e in self.ENGS}
        self.dn = {q: 0 for q in self.dq}
        self.seen = {e: {} for e in self.ENGS}
        self.streams = {e: [] for e in self.ENGS}
        self.last = {}

    def _sem(self, key):
        return self.sems[key] if isinstance(key, str) else self.dq[key[0]][key[1]]

    def _waits(self, eng, deps):
        out = []
        best = {}
        for d in deps:
            if d is None:
                continue
            k, v = d
            if k == "pe" and eng == "pe":
                continue
            if best.get(k, 0) < v:
                best[k] = v
        for k, v in best.items():
            if self.seen[eng].get(k, 0) >= v:
                continue
            self.seen[eng][k] = v
            out.append((k, v))
        return out

    def _deps(self, r, w):
        deps = []
        for b in r:
            deps.append(b.w)
            if b.excl:
                deps.extend(b.r)
        for b in w:
            deps.append(b.w)
            deps.extend(b.r)
        return deps

    def _commit(self, tok, r, w):
        for b in r:
            b.r.append(tok)
        for b in w:
            b.w = tok
            b.r = []
        self.last[tok[0]] = tok[1]

    def op(self, eng, fn, r=(), w=()):
        waits = self._waits(eng, self._deps(r, w))
        self.n[eng] += 1
        tok = (eng, self.n[eng])
        self.streams[eng].append((waits, fn, self.sems[eng], 1))
        self._commit(tok, r, w)

    def dma(self, q, fn, r=(), w=()):
        i = self.dn[q]
        self.dn[q] += 1
        R = self.RQ[q]
        key = (q, i % R)
        deps = self._deps(r, w)
        if i >= R:
            deps.append((key, 16 * (i // R)))
        waits = self._waits(q, deps)
        tok = (key, 16 * (i // R + 1))
        self.streams[q].append((waits, fn, self._sem(key), 16))
        self._commit(tok, r, w)

    def barrier(self):
        toks = list(self.last.items())
        for e in self.ENGS:
            waits = self._waits(e, [t for t in toks if not (t[0] == e)])
            if waits:
                self.streams[e].append((waits, None, None, 0))

    def breg(self, e, val):
        if val not in self._regs:
            self._regs[val] = e.to_reg(val)
        return self._regs[val]

    def emit(self):
        nc = self.nc
        streams = self.streams
        self._regs = {}
        self.streams = {e: [] for e in self.ENGS}

        def run(e, lst):
            for waits, fn, sem, inc in lst:
                for k, v in waits:
                    e.wait_ge(self._sem(k), v)
                if fn is not None:
                    try:
                        fn(e).then_inc(sem, inc)
                    except Exception:
                        print("EMIT FAIL at stream idx", lst.index((waits, fn, sem, inc)), "of", len(lst), flush=True)
                        raise

        with nc.Block() as block:
            @block.tensor
            def _(e):
                run(e, streams["pe"])

            @block.scalar
            def _(e):
                run(e, streams["act"])

            @block.vector
            def _(e):
                run(e, streams["dve"])

            @block.gpsimd
            def _(e):
                run(e, streams["pool"])

            @block.sync
            def _(e):
                run(e, streams["sp"])


class Ring:
    def __init__(self, nc, es, name, shape, dtype, n, psum=False):
        self.tiles = []
        for i in range(n):
            if psum:
                t = es.enter_context(nc.psum_tensor(f"{name}{i}", shape, dtype))
            else:
                t = es.enter_context(nc.sbuf_tensor(f"{name}{i}", shape, dtype))
            self.tiles.append((t, Buf(f"{name}{i}", excl=psum)))
        self.i = 0

    def next(self):
        t = self.tiles[self.i % len(self.tiles)]
        self.i += 1
        return t


def own_groups(G, half):
    return [g for g in range(G) if ((g % 4) in (0, 3)) == (half == 0)]


def build(cfg):
    S = cfg["S"]
    NE = cfg["NE"]
    CAP = cfg["CAP"]
    dbg = cfg.get("dbg", False)
    upto = cfg.get("upto", "Z")
    So = S // 2
    G = S // 512
    Go = G // 2
    NT = S // 128
    NTo = So // 128
    SG = min(2048, So)
    nc = bass.Bass("TRN2", target_bir_lowering=False)
    es = ExitStack()
    scratch_kind = "ExternalOutput" if dbg else "Internal"

    def din(name, shape, dt):
        return nc.dram_tensor(name, list(shape), dt, kind="ExternalInput").ap()

    def dscr(name, shape, dt):
        return nc.dram_tensor(name, list(shape), dt, kind=scratch_kind).ap()

    x_all = din("x_all", [S, D], F32)
    x_own = din("x_own", [So, D], F32)
    pos_all = din("pos_all", [1, S], I32)
    pos_own = din("pos_own", [1, So], I32)
    cvec = din("cvec", [128, KC], F32)
    badac = din("badac", [128, 6 * KC], F32)
    gcols = din("gcols", [128, 4 * KC], F32)
    b_ada = din("b_ada", [1, 6 * D], F32)
    g_post_mix = din("g_post_mix", [1, D], F32)
    g_post_ffn = din("g_post_ffn", [1, D], F32)
    w_ada = din("w_ada", [D, 6 * D], F32)
    w_in = din("w_in", [D, IN_W], F32)
    w_bra = din("w_branch_a", [A_H * HD, D], F32)
    w_brb = din("w_branch_b", [B_H * HD, D], F32)
    w_out = din("w_out", [D, D], F32)
    w_router = din("w_router", [D, NE], F32)
    b_router = din("b_router", [1, NE], F32)
    w1 = din("w1", [NE, D, 2 * D], F32)
    b1c = din("b1c", [NE, 128, 2 * KC], F32)
    w2 = din("w2", [NE, D, D], F32)
    b2 = din("b2", [NE, D], F32)
    cmask_d = din("cmask", [128, 2 * 8 * 512], BF16)
    amask_d = din("amask", [128, 2 * 640], F32)
    consts_d = din("consts", [128, 8 * 128], F32)
    out_d = nc.dram_tensor("out", [So, D], F32, kind="ExternalOutput").ap()

    KA = dscr("s_ka", [A_KV, 128, S], BF16)
    KB = dscr("s_kb", [B_H, 128, S], BF16)
    KI = dscr("s_ki", [IDX_D, S], BF16)
    VA = dscr("s_va", [S, A_KV * HD], BF16)
    VB = dscr("s_vb", [S, B_H * HD], BF16)
    QA = dscr("s_qa", [A_H, 128, So], BF16)
    QB = dscr("s_qb", [B_H, 128, So], BF16)
    QI = dscr("s_qi", [IDX_H // 2, 128, So], BF16)
    WI = dscr("s_wi", [So, IDX_H], F32)
    GA = dscr("s_ga", [KC, 128, So], BF16)
    GB = dscr("s_gb", [KC, 128, So], BF16)
    MK = dscr("s_mk", [NTo, 128, S], BF16)
    OA = dscr("s_oa", [A_H, 128, So], BF16)
    OB = dscr("s_ob", [B_H, 128, So], BF16)
    MG = dscr("s_mg", [KC, 128, So], BF16)
    X1 = dscr("s_x1", [So, D], F32)
    XG = dscr("s_xg", [NE * CAP, D], BF16)
    YG = dscr("s_yg", [NE * CAP, D], BF16)

    sch = Sched(nc, es)
    op, dma = sch.op, sch.dma
    dbg_outs = {}

    def finish():
        if dbg:
            d1 = nc.dram_tensor("dbg_modc", [128, 4 * KC], F32, kind="ExternalOutput").ap()
            d2 = nc.dram_tensor("dbg_ga", [128, 2 * D], F32, kind="ExternalOutput").ap()
            dma("sp", lambda e: e.dma_start(out=d1[:, :], in_=modc[:]), r=[b_modc])
            dma("sp", lambda e: e.dma_start(out=d2[:, 0:D], in_=ga1row[:]), r=[b_garow[0]])
            dma("sp", lambda e: e.dma_start(out=d2[:, D:2 * D], in_=ga2row[:]), r=[b_garow[1]])
            d3 = nc.dram_tensor("dbg_rslot", [128, NTo * 4], I32, kind="ExternalOutput").ap()
            d4 = nc.dram_tensor("dbg_rwgt", [128, NTo * 4], F32, kind="ExternalOutput").ap()
            dma("sp", lambda e: e.dma_start(out=d3[:, :], in_=rslot[:]), r=[b_route])
            dma("sp", lambda e: e.dma_start(out=d4[:, :], in_=rwgt[:]), r=[b_route])
        sch.barrier()
        sch.emit()
        es.close()
        return nc

    def sbp(name, shape, dt):
        return es.enter_context(nc.sbuf_tensor(name, shape, dt))

    consts = sbp("consts_s", [128, 8 * 128], F32)
    b_consts = Buf("consts")
    identb = sbp("identb", [128, 128], BF16)
    rtA = sbp("rtA", [128, 128], BF16)
    rtI = sbp("rtI", [128, 128], BF16)
    triI = sbp("triI", [128, 128], BF16)
    triS = sbp("triS", [128, 128], BF16)
    onesb = sbp("onesb", [128, 128], BF16)
    onesf = consts[:, 5 * 128:6 * 128]
    invfA = consts[:, 6 * 128:6 * 128 + 1]
    invfI = consts[:, 6 * 128 + 1:6 * 128 + 2]
    iota_f = consts[:, 7 * 128:8 * 128]
    b_cb = Buf("constsb")
    modc = sbp("modc", [128, 4 * KC], F32)
    b_modc = Buf("modc")
    ga1row = sbp("ga1row", [128, D], F32)
    ga2row = sbp("ga2row", [128, D], F32)
    b_garow = [Buf("ga1row"), Buf("ga2row")]
    rslot = sbp("rslot", [128, NTo * 4], I32)
    rwgt = sbp("rwgt", [128, NTo * 4], F32)
    b_route = Buf("route")

    dma("sp", lambda e: e.dma_start(out=consts[:], in_=consts_d[:, :]), w=[b_consts])
    for i, t in enumerate((identb, rtA, rtI, triI, triS, onesb)):
        op("dve", lambda e, i=i, t=t: e.tensor_copy(out=t[:], in_=consts[:, i * 128:(i + 1) * 128]),
           r=[b_consts], w=[b_cb])

    def mmgroup(out_ap, out_buf, pairs, rbufs):
        n = len(pairs)
        for i, (l, r_) in enumerate(pairs):
            rb = rbufs if (i == 0 or i == n - 1) else ()
            op("pe", lambda e, l=l, r_=r_, i=i: e.matmul(out_ap, l, r_, start=(i == 0), stop=(i == n - 1)),
               r=rb, w=[out_buf])

    with ExitStack() as ph:
        cc = ph.enter_context(nc.sbuf_tensor("cc", [128, KC], F32))
        sg_ = ph.enter_context(nc.sbuf_tensor("sg_", [128, KC], F32))
        siluc = ph.enter_context(nc.sbuf_tensor("siluc", [128, KC], F32))
        silurep = ph.enter_context(nc.sbuf_tensor("silurep", [128, KC, 128], F32))
        badac_s = ph.enter_context(nc.sbuf_tensor("badac_s", [128, 6 * KC], F32))
        gcols_s = ph.enter_context(nc.sbuf_tensor("gcols_s", [128, 4 * KC], F32))
        rawc = ph.enter_context(nc.sbuf_tensor("rawc", [128, 4 * KC], F32))
        b_small = Buf("small")
        b_rawc = Buf("rawc")
        wch = Ring(nc, ph, "wch", [128, KC, 512], F32, 2)
        rowt = Ring(nc, ph, "rowt", [128, 2, 512], F32, 2)
        tmpr = Ring(nc, ph, "tmpr", [128, 512], F32, 2)
        psA = Ring(nc, ph, "psA", [128, 512], F32, 2, psum=True)
        psC = ph.enter_context(nc.psum_tensor("psC", [128, 512], F32))
        b_psC = Buf("psC", excl=True)

        dma("sp", lambda e: e.dma_start(out=cc[:], in_=cvec[:, :]), w=[b_small])
        dma("sp", lambda e: e.dma_start(out=badac_s[:], in_=badac[:, :]), w=[b_small])
        dma("sp", lambda e: e.dma_start(out=gcols_s[:], in_=gcols[:, :]), w=[b_small])
        op("act", lambda e: e.activation(out=sg_[:], in_=cc[:], func=AF.Sigmoid), r=[b_small], w=[b_small])
        op("dve", lambda e: e.tensor_tensor(out=siluc[:], in0=cc[:], in1=sg_[:], op=ALU.mult), r=[b_small], w=[b_small])
        op("dve", lambda e: e.tensor_copy(out=silurep[:], in_=siluc[:].unsqueeze(2).broadcast_to([128, KC, 128])),
           r=[b_small], w=[b_small])
        w_ada_v = w_ada.rearrange("(kc p) n -> p kc n", p=128)
        for idx, which in enumerate((1, 0, 4, 3)):
            for cq in range(4):
                wt, wb = wch.next()
                c0 = which * D + cq * 512
                dma("sp", lambda e, wt=wt, c0=c0: e.dma_start(out=wt[:], in_=w_ada_v[:, :, c0:c0 + 512]), w=[wb])
                for j in range(4):
                    col = idx * KC + cq * 4 + j
                    mmgroup(psC[:, col:col + 1], b_psC,
                            [(wt[:, kc, j * 128:(j + 1) * 128], siluc[:, kc:kc + 1]) for kc in range(KC)],
                            [wb, b_small])
        op("dve", lambda e: e.tensor_copy(out=rawc[:], in_=psC[:, 0:4 * KC]), r=[b_psC], w=[b_rawc])
        for idx, which in enumerate((1, 0, 4, 3)):
            op("dve", lambda e, idx=idx, which=which: e.tensor_tensor(
                out=rawc[:, idx * KC:(idx + 1) * KC], in0=rawc[:, idx * KC:(idx + 1) * KC],
                in1=badac_s[:, which * KC:(which + 1) * KC], op=ALU.add), r=[b_small, b_rawc], w=[b_rawc])
        op("dve", lambda e: e.scalar_tensor_tensor(out=modc[:, 0:KC], in0=rawc[:, 0:KC], scalar=1.0,
                                                   in1=gcols_s[:, 0:KC], op0=ALU.add, op1=ALU.mult),
           r=[b_rawc, b_small], w=[b_modc])
        op("dve", lambda e: e.tensor_copy(out=modc[:, KC:2 * KC], in_=rawc[:, KC:2 * KC]), r=[b_rawc], w=[b_modc])
        op("dve", lambda e: e.scalar_tensor_tensor(out=modc[:, 2 * KC:3 * KC], in0=rawc[:, 2 * KC:3 * KC], scalar=1.0,
                                                   in1=gcols_s[:, 2 * KC:3 * KC], op0=ALU.add, op1=ALU.mult),
           r=[b_rawc, b_small], w=[b_modc])
        op("dve", lambda e: e.tensor_copy(out=modc[:, 3 * KC:4 * KC], in_=rawc[:, 3 * KC:4 * KC]), r=[b_rawc], w=[b_modc])
        for gi, (which, grow, gpost) in enumerate(((2, ga1row, g_post_mix), (5, ga2row, g_post_ffn))):
            for cq in range(4):
                wt, wb = wch.next()
                c0 = which * D + cq * 512
                dma("sp", lambda e, wt=wt, c0=c0: e.dma_start(out=wt[:], in_=w_ada_v[:, :, c0:c0 + 512]), w=[wb])
                rt, rb = rowt.next()
                dma("sp", lambda e, rt=rt, c0=c0: e.dma_start(out=rt[:, 0, :], in_=b_ada[0:1, c0:c0 + 512].partition_broadcast(128)), w=[rb])
                dma("sp", lambda e, rt=rt, cq=cq, gpost=gpost: e.dma_start(
                    out=rt[:, 1, :], in_=gpost[0:1, cq * 512:(cq + 1) * 512].partition_broadcast(128)), w=[rb])
                pt, pb = psA.next()
                mmgroup(pt[:], pb, [(silurep[:, kc, :], wt[:, kc, :]) for kc in range(KC)], [wb, b_small])
                tt, tb = tmpr.next()
                op("dve", lambda e, tt=tt, pt=pt, rt=rt: e.tensor_tensor(out=tt[:], in0=pt[:], in1=rt[:, 0, :], op=ALU.add),
                   r=[pb, rb], w=[tb])
                op("dve", lambda e, tt=tt, rt=rt, grow=grow, cq=cq: e.tensor_tensor(
                    out=grow[:, cq * 512:(cq + 1) * 512], in0=tt[:], in1=rt[:, 1, :], op=ALU.mult),
                   r=[tb, rb], w=[b_garow[gi]])
        sch.barrier()
        sch.emit()
    if upto == "A":
        return finish()

    w_in_v = w_in.rearrange("(kc p) n -> p kc n", p=128)
    TWO_PI = 2.0 * math.pi
    BL = cfg.get('blevel', 9)
    ROPE_ADD_ENG = cfg.get('rope_add', 'dve')
    CW1 = 6.28125
    CW2 = TWO_PI - CW1

    def norm_transpose(x_src, tok0, hT, b_hT, hcol0, sc_off, xt_r, xn_r, junk, b_junk, st_r, psT_r, alt):
        xt, xb = xt_r.next()
        dma("sp", lambda e: e.dma_start(out=xt[:], in_=x_src[tok0:tok0 + 128, :]), w=[xb])
        st, sb_ = st_r.next()
        op("act", lambda e: e.activation(out=junk[:], in_=xt[:], func=AF.Square, accum_out=st[:, 0:1]),
           r=[xb], w=[b_junk, sb_])
        op("act", lambda e: e.activation(out=st[:, 1:2], in_=st[:, 0:1], func=AF.Sqrt, scale=1.0 / D, bias=EPS),
           r=[sb_], w=[sb_])
        op("dve", lambda e: e.reciprocal(out=st[:, 2:3], in_=st[:, 1:2]), r=[sb_], w=[sb_])
        xn, xnb = xn_r.next()
        op("dve", lambda e: e.tensor_scalar(out=xn[:], in0=xt[:], scalar1=st[:, 2:3], scalar2=None, op0=ALU.mult),
           r=[xb, sb_], w=[xnb])
        for half in range(2):
            pt, pb = psT_r.next()
            for j in range(8):
                kc = half * 8 + j
                op("pe", lambda e, pt=pt, j=j, kc=kc: e.transpose(pt[:, j * 128:(j + 1) * 128],
                                                                   xn[:, kc * 128:(kc + 1) * 128], identb[:]),
                   r=[xnb, b_cb] if j in (0, 7) else (), w=[pb])
            for j in range(8):
                kc = half * 8 + j
                eng = "act" if (half + alt) % 2 == 0 else "dve"
                if eng == "act":
                    op("act", lambda e, pt=pt, j=j, kc=kc: e.activation(
                        out=hT[:, kc, hcol0:hcol0 + 128], in_=pt[:, j * 128:(j + 1) * 128], func=AF.Identity,
                        scale=modc[:, sc_off + kc:sc_off + kc + 1], bias=modc[:, sc_off + KC + kc:sc_off + KC + kc + 1]),
                       r=[pb, b_modc], w=[b_hT])
                else:
                    op("dve", lambda e, pt=pt, j=j, kc=kc: e.tensor_scalar(
                        out=hT[:, kc, hcol0:hcol0 + 128], in0=pt[:, j * 128:(j + 1) * 128],
                        scalar1=modc[:, sc_off + kc:sc_off + kc + 1], scalar2=modc[:, sc_off + KC + kc:sc_off + KC + kc + 1],
                        op0=ALU.mult, op1=ALU.add), r=[pb, b_modc], w=[b_hT])

    def proj_pass(pname, x_src, pos_src, T, fm_blocks, tm_blocks):
        with ExitStack() as ph:
            sg = min(SG, T)
            ngr = sg // 512
            hT = ph.enter_context(nc.sbuf_tensor(pname + "hT", [128, KC, sg], BF16))
            b_hT = Buf("hT")
            xt_r = Ring(nc, ph, pname + "xt", [128, D], F32, 2)
            xn_r = Ring(nc, ph, pname + "xn", [128, D], BF16, 1)
            junk = ph.enter_context(nc.sbuf_tensor(pname + "junk", [128, D], BF16))
            b_junk = Buf("junk")
            st_r = Ring(nc, ph, pname + "st", [128, 4], F32, 4)
            psT_r = Ring(nc, ph, pname + "psT", [128, 1024], BF16, 2, psum=True)
            psM_r = Ring(nc, ph, pname + "psM", [128, 512], F32, 4, psum=True)
            psR_r = Ring(nc, ph, pname + "psR", [128, 512], F32, 2, psum=True)
            wfm_r = Ring(nc, ph, pname + "wfm", [128, KC, 128], BF16, 3)
            wtm_r = Ring(nc, ph, pname + "wtm", [128, KC, 512], BF16, 1)
            zb_r = Ring(nc, ph, pname + "zb", [128, 512], BF16, 2)
            t1_r = Ring(nc, ph, pname + "t1", [128, 512], F32, 2)
            t2_r = Ring(nc, ph, pname + "t2", [128, 512], F32, 2)
            ob_r = Ring(nc, ph, pname + "ob", [128, 512], BF16, 3)
            obf_r = Ring(nc, ph, pname + "obf", [128, 512], F32, 2)
            posi = ph.enter_context(nc.sbuf_tensor(pname + "posi", [128, 512], I32))
            posf = ph.enter_context(nc.sbuf_tensor(pname + "posf", [128, 512], F32))
            ang = ph.enter_context(nc.sbuf_tensor(pname + "ang", [128, 512], F32))
            kfi = ph.enter_context(nc.sbuf_tensor(pname + "kfi", [128, 512], I32))
            kff = ph.enter_context(nc.sbuf_tensor(pname + "kff", [128, 512], F32))
            msk = ph.enter_context(nc.sbuf_tensor(pname + "msk", [128, 512], F32))
            b_tmp = Buf("ropetmp")
            tabs = ph.enter_context(nc.sbuf_tensor(pname + "tabs", [128, ngr, 4, 512], F32))
            b_tabs = Buf("tabs")
            for s0 in range(0, T, sg):
                for ti in range(sg // 128):
                    norm_transpose(x_src, s0 + ti * 128, hT, b_hT, ti * 128, 0, xt_r, xn_r, junk, b_junk, st_r, psT_r, ti)
                for g in range(ngr if BL >= 2 else 0):
                    t0 = s0 + g * 512
                    dma("sp", lambda e, t0=t0: e.dma_start(out=posi[:], in_=pos_src[0:1, t0:t0 + 512].partition_broadcast(128)),
                        w=[b_tmp])
                    op("dve", lambda e: e.tensor_copy(out=posf[:], in_=posi[:]), r=[b_tmp], w=[b_tmp])
                    for ti_, (invf, shift) in enumerate(((invfA, math.pi / 2), (invfA, 0.0), (invfI, math.pi / 2), (invfI, 0.0))):
                        R_, W_ = [b_tmp, b_consts], [b_tmp]
                        op("dve", lambda e, invf=invf, shift=shift: e.tensor_scalar(
                            out=ang[:], in0=posf[:], scalar1=invf, scalar2=shift, op0=ALU.mult, op1=ALU.add), r=R_, w=W_)
                        op("dve", lambda e: e.tensor_scalar(out=kfi[:], in0=ang[:], scalar1=1.0 / TWO_PI, scalar2=None,
                                                            op0=ALU.mult), r=R_, w=W_)
                        op("dve", lambda e: e.tensor_copy(out=kff[:], in_=kfi[:]), r=R_, w=W_)
                        op("dve", lambda e: e.scalar_tensor_tensor(out=ang[:], in0=kff[:], scalar=-CW1, in1=ang[:],
                                                                   op0=ALU.mult, op1=ALU.add), r=R_, w=W_)
                        op("dve", lambda e: e.scalar_tensor_tensor(out=ang[:], in0=kff[:], scalar=-CW2, in1=ang[:],
                                                                   op0=ALU.mult, op1=ALU.add), r=R_, w=W_)
                        op("dve", lambda e: e.tensor_scalar(out=msk[:], in0=ang[:], scalar1=math.pi, scalar2=None,
                                                            op0=ALU.is_gt), r=R_, w=W_)
                        op("dve", lambda e: e.scalar_tensor_tensor(out=ang[:], in0=msk[:], scalar=-TWO_PI, in1=ang[:],
                                                                   op0=ALU.mult, op1=ALU.add), r=R_, w=W_)
                        op("dve", lambda e: e.tensor_scalar(out=msk[:], in0=ang[:], scalar1=-math.pi, scalar2=None,
                                                            op0=ALU.is_lt), r=R_, w=W_)
                        op("dve", lambda e: e.scalar_tensor_tensor(out=ang[:], in0=msk[:], scalar=TWO_PI, in1=ang[:],
                                                                   op0=ALU.mult, op1=ALU.add), r=R_, w=W_)
                        op("dve", lambda e: e.tensor_scalar(out=ang[:], in0=ang[:], scalar1=math.pi, scalar2=-math.pi,
                                                            op0=ALU.min, op1=ALU.max), r=R_, w=W_)
                        op("act", lambda e, g=g, ti_=ti_: e.activation(out=tabs[:, g, ti_, :], in_=ang[:], func=AF.Sin),
                           r=[b_tmp], w=[b_tabs, b_tmp])
                for (col0, ncol, dst, rope) in fm_blocks:
                    if BL < 3 or (BL < 4 and rope is not None):
                        continue
                    if rope is not None and cfg.get('ropeonly') and (rope, ncol) != cfg.get('ropeonly'):
                        continue
                    wt, wb = wfm_r.next()
                    dma("pool", lambda e, wt=wt, col0=col0, ncol=ncol: e.dma_start(
                        out=wt[:, :, 0:ncol], in_=w_in_v[:, :, col0:col0 + ncol]), w=[wb])
                    for g in range(ngr):
                        t0 = s0 + g * 512
                        pt, pb = psM_r.next()
                        mmgroup(pt[0:ncol, :], pb,
                                [(wt[:, kc, 0:ncol], hT[:, kc, g * 512:(g + 1) * 512]) for kc in range(KC)], [wb, b_hT])
                        ot, obb = ob_r.next()
                        if rope is None:
                            op("act", lambda e, ot=ot, pt=pt, ncol=ncol: e.activation(out=ot[0:ncol, :], in_=pt[0:ncol, :], func=AF.Copy),
                               r=[pb], w=[obb])
                        else:
                            ci = 0 if rope == "A" else 2
                            rt = rtA if rope == "A" else rtI
                            zt, zbb = zb_r.next()
                            op("act", lambda e, zt=zt, pt=pt, ncol=ncol: e.activation(out=zt[0:ncol, :], in_=pt[0:ncol, :], func=AF.Copy),
                               r=[pb], w=[zbb])
                            p2, p2b = psR_r.next()
                            if cfg.get('ropevar', 0) != 1:
                                op("pe", lambda e, p2=p2, zt=zt, rt=rt, ncol=ncol: e.matmul(
                                    p2[0:ncol, :], rt[0:ncol, 0:ncol], zt[0:ncol, :], start=True, stop=True),
                                   r=[zbb, b_cb], w=[p2b])
                            else:
                                p2, p2b = pt, pb
                            a1, a1b = t1_r.next()
                            a2, a2b = t2_r.next()
                            if cfg.get('ropevar', 0) == 2:
                                op("act", lambda e, ot=ot, pt=pt, ncol=ncol: e.activation(out=ot[0:ncol, :], in_=pt[0:ncol, :], func=AF.Copy),
                                   r=[pb], w=[obb])
                                dma("sp", lambda e, ot=ot, dst=dst, t0=t0, ncol=ncol: e.dma_start(
                                    out=dst[0:ncol, t0:t0 + 512], in_=ot[0:ncol, :]), r=[obb])
                                continue
                            if cfg.get('ropevar', 0) == 3:
                                op("dve", lambda e, ot=ot, pt=pt, g=g, ci=ci, ncol=ncol: e.tensor_tensor(
                                    out=ot[0:ncol, :], in0=pt[0:ncol, :], in1=tabs[0:ncol, g, ci, :], op=ALU.mult),
                                   r=[pb, b_tabs], w=[obb])
                                dma("sp", lambda e, ot=ot, dst=dst, t0=t0, ncol=ncol: e.dma_start(
                                    out=dst[0:ncol, t0:t0 + 512], in_=ot[0:ncol, :]), r=[obb])
                                continue
                            op("dve", lambda e, a1=a1, pt=pt, g=g, ci=ci, ncol=ncol: e.tensor_tensor(
                                out=a1[0:ncol, :], in0=pt[0:ncol, :], in1=tabs[0:ncol, g, ci, :], op=ALU.mult),
                               r=[pb, b_tabs], w=[a1b])
                            op("dve", lambda e, a2=a2, p2=p2, g=g, ci=ci, ncol=ncol: e.tensor_tensor(
                                out=a2[0:ncol, :], in0=p2[0:ncol, :], in1=tabs[0:ncol, g, ci + 1, :], op=ALU.mult),
                               r=[p2b, b_tabs], w=[a2b])
                            op(ROPE_ADD_ENG, lambda e, ot=ot, a1=a1, a2=a2, ncol=ncol: e.tensor_tensor(
                                out=ot[0:ncol, :], in0=a1[0:ncol, :], in1=a2[0:ncol, :], op=ALU.add),
                               r=[a1b, a2b], w=[obb])
                        dma("sp", lambda e, ot=ot, dst=dst, t0=t0, ncol=ncol: e.dma_start(
                            out=dst[0:ncol, t0:t0 + 512], in_=ot[0:ncol, :]), r=[obb])
                for (col0, ncol, dst, dcol0, isf32) in tm_blocks:
                    if BL < 5:
                        continue
                    wt, wb = wtm_r.next()
                    dma("pool", lambda e, wt=wt, col0=col0, ncol=ncol: e.dma_start(
                        out=wt[:, :, 0:ncol], in_=w_in_v[:, :, col0:col0 + ncol]), w=[wb])
                    for ti in range(sg // 128):
                        t0 = s0 + ti * 128
                        pt, pb = psM_r.next()
                        mmgroup(pt[:, 0:ncol], pb,
                                [(hT[:, kc, ti * 128:(ti + 1) * 128], wt[:, kc, 0:ncol]) for kc in range(KC)], [wb, b_hT])
                        ot, obb = (obf_r if isf32 else ob_r).next()
                        op("act", lambda e, ot=ot, pt=pt, ncol=ncol: e.activation(out=ot[:, 0:ncol], in_=pt[:, 0:ncol], func=AF.Copy),
                           r=[pb], w=[obb])
                        dma("sp", lambda e, ot=ot, dst=dst, t0=t0, ncol=ncol, dcol0=dcol0: e.dma_start(
                            out=dst[t0:t0 + 128, dcol0:dcol0 + ncol], in_=ot[:, 0:ncol]), r=[obb])
            sch.barrier()
            sch.emit()

    fm1 = [(C_KA + j * 128, 128, KA[j], "A") for j in range(A_KV)]
    fm1 += [(C_KB + h * 128, 128, KB[h], None) for h in range(B_H)]
    fm1 += [(C_KI, 64, KI, "I")]
    tm1 = [(C_VA, 256, VA, 0, False), (C_VB, 512, VB, 0, False), (C_VB + 512, 512, VB, 512, False)]
    proj_pass("p1", x_all, pos_all, S, fm1, tm1)
    fm2 = [(C_QA + h * 128, 128, QA[h], "A") for h in range(A_H)]
    fm2 += [(C_QB + h * 128, 128, QB[h], None) for h in range(B_H)]
    fm2 += [(C_QI + h * 128, 128, QI[h], "I") for h in range(IDX_H // 2)]
    fm2 += [(C_GA + f * 128, 128, GA[f], None) for f in range(KC)]
    fm2 += [(C_GB + f * 128, 128, GB[f], None) for f in range(KC)]
    tm2 = [(C_WI, IDX_H, WI, 0, True)]
    proj_pass("p2", x_own, pos_own, So, fm2, tm2)
    if upto == "B":
        return finish()

    def pipeline(items, skews):
        if isinstance(skews, int):
            skews = [0, skews]
        n = len(items)
        for idx in range(n + max(skews)):
            for k, sk in enumerate(skews):
                if 0 <= idx - sk < n:
                    items[idx - sk][k]()

    def qblocks():
        for i in range(Go):
            for jj in range(4):
                qb = i * 4 + jj
                yield i, jj, qb, qb * 128, 128 * (4 * (2 * i + 1) + jj + 1), i % 2

    with ExitStack() as ph:
        kiT = ph.enter_context(nc.sbuf_tensor("kiT", [64, S], BF16))
        b_ki = Buf("kiT")
        amask_s = ph.enter_context(nc.sbuf_tensor("amask_s", [128, 2, 640], F32))
        b_am = Buf("amask")
        Ssc = ph.enter_context(nc.sbuf_tensor("Ssc", [128, S], F32))
        b_S = Buf("S")
        Mk = ph.enter_context(nc.sbuf_tensor("Mk", [128, S], BF16))
        b_M = Buf("Mk")
        qi_r = Ring(nc, ph, "qiT", [64, IDX_H, 128], BF16, 2)
        wi_r = Ring(nc, ph, "wit", [128, 3, IDX_H], F32, 2)
        rr_r = Ring(nc, ph, "rr", [128, 512], F32, 3)
        ps_r = Ring(nc, ph, "psI", [128, 512], F32, 4, psum=True)
        bs = ph.enter_context(nc.sbuf_tensor("bs", [128, 8], F32))
        b_bs = Buf("bs")
        dma("sp", lambda e: e.dma_start(out=kiT[:], in_=KI[:, :]), w=[b_ki])
        dma("sp", lambda e: e.dma_start(out=amask_s[:].rearrange("p m c -> p (m c)"), in_=amask_d[:, :]), w=[b_am])
        for (i, jj, qb, tok0, nk, m) in qblocks():
            qt, qbb = qi_r.next()
            for hh in range(2):
                dma("sp", lambda e, qt=qt, hh=hh, tok0=tok0: e.dma_start(
                    out=qt[:, hh::2, :], in_=QI[:, hh * 64:(hh + 1) * 64, tok0:tok0 + 128].rearrange("hp d t -> d hp t")),
                    w=[qbb])
            wt, wbb = wi_r.next()
            dma("sp", lambda e, wt=wt, tok0=tok0: e.dma_start(out=wt[:, 0, :], in_=WI[tok0:tok0 + 128, :]), w=[wbb])
            op("act", lambda e, wt=wt: e.activation(out=wt[:, 1, :], in_=wt[:, 0, :], func=AF.Abs), r=[wbb], w=[wbb])
            op("act", lambda e, wt=wt: e.activation(out=wt[:, 2, :], in_=wt[:, 0, :], func=AF.Sign), r=[wbb], w=[wbb])
            nch = (nk + 511) // 512
            for c in range(nch):
                c0 = c * 512
                cw = min(512, nk - c0)
                for h in range(IDX_H):
                    pt, pb = ps_r.next()
                    op("pe", lambda e, pt=pt, qt=qt, h=h, c0=c0, cw=cw: e.matmul(
                        pt[:, 0:cw], qt[:, h, :], kiT[:, c0:c0 + cw], start=True, stop=True),
                       r=[qbb, b_ki], w=[pb])
                    rt, rb = rr_r.next()
                    op("act", lambda e, rt=rt, pt=pt, wt=wt, h=h, cw=cw: e.activation(
                        out=rt[:, 0:cw], in_=pt[:, 0:cw], func=AF.Relu, scale=wt[:, 1, h:h + 1]),
                       r=[pb, wbb], w=[rb])
                    if h == 0:
                        op("dve", lambda e, rt=rt, wt=wt, h=h, c0=c0, cw=cw: e.tensor_scalar(
                            out=Ssc[:, c0:c0 + cw], in0=rt[:, 0:cw], scalar1=wt[:, 2, h:h + 1], scalar2=None, op0=ALU.mult),
                           r=[rb, wbb], w=[b_S])
                    else:
                        op("dve", lambda e, rt=rt, wt=wt, h=h, c0=c0, cw=cw: e.scalar_tensor_tensor(
                            out=Ssc[:, c0:c0 + cw], in0=rt[:, 0:cw], scalar=wt[:, 2, h:h + 1], in1=Ssc[:, c0:c0 + cw],
                            op0=ALU.mult, op1=ALU.add), r=[rb, wbb, b_S], w=[b_S])
            op("dve", lambda e, nk=nk: e.tensor_reduce(out=bs[:, 5:6], in_=Ssc[:, 0:nk], axis=AX.X, op=ALU.max),
               r=[b_S], w=[b_bs])
            op("dve", lambda e, nk=nk: e.tensor_reduce(out=bs[:, 0:1], in_=Ssc[:, 0:nk], axis=AX.X, op=ALU.min),
               r=[b_S], w=[b_bs])
            op("dve", lambda e: e.tensor_tensor(out=bs[:, 1:2], in0=bs[:, 5:6], in1=bs[:, 0:1], op=ALU.subtract),
               r=[b_bs], w=[b_bs])
            op("dve", lambda e, nk=nk, m=m: e.tensor_tensor(out=Ssc[:, nk - 640:nk], in0=Ssc[:, nk - 640:nk],
                                                             in1=amask_s[:, m, :], op=ALU.add),
               r=[b_S, b_am], w=[b_S])
            for it in range(NBIS):
                op("dve", lambda e: e.tensor_scalar(out=bs[:, 1:2], in0=bs[:, 1:2], scalar1=0.5, scalar2=None, op0=ALU.mult),
                   r=[b_bs], w=[b_bs])
                op("dve", lambda e: e.tensor_tensor(out=bs[:, 2:3], in0=bs[:, 0:1], in1=bs[:, 1:2], op=ALU.add),
                   r=[b_bs], w=[b_bs])
                op("dve", lambda e, nk=nk: e.tensor_scalar(out=Mk[:, 0:nk], in0=Ssc[:, 0:nk], scalar1=bs[:, 2:3], scalar2=None,
                                                           op0=ALU.is_ge, op1=ALU.add, accum_out=bs[:, 3:4]),
                   r=[b_bs, b_S, b_M], w=[b_M, b_bs])
                op("dve", lambda e: e.tensor_scalar(out=bs[:, 4:5], in0=bs[:, 3:4], scalar1=float(TOPK), scalar2=bs[:, 1:2],
                                                    op0=ALU.is_ge, op1=ALU.mult), r=[b_bs], w=[b_bs])
                op("dve", lambda e: e.tensor_tensor(out=bs[:, 0:1], in0=bs[:, 0:1], in1=bs[:, 4:5], op=ALU.add),
                   r=[b_bs], w=[b_bs])
            op("dve", lambda e, nk=nk: e.tensor_scalar(out=Mk[:, 0:nk], in0=Ssc[:, 0:nk], scalar1=bs[:, 0:1], scalar2=None,
                                                       op0=ALU.is_ge), r=[b_bs, b_S, b_M], w=[b_M])
            dma("sp", lambda e, qb=qb, nk=nk: e.dma_start(out=MK[qb, :, 0:nk], in_=Mk[:, 0:nk]), r=[b_M])
        sch.barrier()
        sch.emit()
    if upto == "C1":
        return finish()

    with ExitStack() as ph:
        kaT = ph.enter_context(nc.sbuf_tensor("kaT", [128, A_KV, S], BF16))
        va = ph.enter_context(nc.sbuf_tensor("va_s", [128, NT, A_KV * HD], BF16))
        b_kv = Buf("kv")
        mk_r = Ring(nc, ph, "mk", [128, S], BF16, 2)
        mt_r = Ring(nc, ph, "mt", [128, NT, 128], BF16, 2)
        qa_r = Ring(nc, ph, "qa", [128, A_H * 128], BF16, 2)
        e_r = Ring(nc, ph, "ee", [128, 512], BF16, 3)
        p_r = Ring(nc, ph, "pp", [128, 512], BF16, 4)
        rd_r = Ring(nc, ph, "rd", [128, 512], F32, 2)
        of_r = Ring(nc, ph, "of", [128, 512], F32, 2)
        ob_r = Ring(nc, ph, "obc", [128, 512], BF16, 2)
        psS = Ring(nc, ph, "psS", [128, 512], F32, 3, psum=True)
        psO = Ring(nc, ph, "psO", [128, 512], F32, 2, psum=True)
        psD = Ring(nc, ph, "psD", [128, 512], F32, 2, psum=True)
        psT = Ring(nc, ph, "psTc", [128, 1024], BF16, 1, psum=True)
        for j in range(A_KV):
            dma("sp", lambda e, j=j: e.dma_start(out=kaT[:, j, :], in_=KA[j]), w=[b_kv])
        dma("sp", lambda e: e.dma_start(out=va[:], in_=VA.rearrange("(kb p) c -> p kb c", p=128)), w=[b_kv])
        isq = 1.0 / math.sqrt(HD)
        items = []
        for (i, jj, qb, tok0, nk, m) in qblocks():
            nkb = nk // 128
            qst = {}

            def setup(qst=qst, qb=qb, tok0=tok0, nk=nk, nkb=nkb):
                mkt, mkb = mk_r.next()
                dma("sp", lambda e: e.dma_start(out=mkt[:, 0:nk], in_=MK[qb, :, 0:nk]), w=[mkb])
                qt, qbb = qa_r.next()
                dma("sp", lambda e: e.dma_start(
                    out=qt[:].rearrange("p (h t) -> p h t", h=A_H), in_=QA[:, :, tok0:tok0 + 128].rearrange("h d t -> d h t")),
                    w=[qbb])
                mtt, mtb = mt_r.next()
                for k0 in range(0, nkb, 8):
                    n8 = min(8, nkb - k0)
                    pt, pb = psT.next()
                    for u in range(n8):
                        op("pe", lambda e, pt=pt, u=u, k0=k0: e.transpose(
                            pt[:, u * 128:(u + 1) * 128], mkt[:, (k0 + u) * 128:(k0 + u + 1) * 128], identb[:]),
                           r=[mkb, b_cb] if u in (0, n8 - 1) else (), w=[pb])
                    op("act", lambda e, pt=pt, k0=k0, n8=n8: e.activation(
                        out=mtt[:, k0:k0 + n8, :].rearrange("p k t -> p (k t)"), in_=pt[:, 0:n8 * 128], func=AF.Copy),
                       r=[pb], w=[mtb])
                qst.update(qt=qt, qbb=qbb, mtt=mtt, mtb=mtb)

            for j in range(A_KV):
                jst = {}
                for kb in range(nkb):
                    it = {}

                    def stA(it=it, qst=qst, jst=jst, j=j, kb=kb, first=(j == 0 and kb == 0), setup=setup):
                        if first:
                            setup()
                        if kb == 0:
                            jst["po"], jst["pob"] = psO.next()
                            jst["pd"], jst["pdb"] = psD.next()
                        qt, qbb, mtt, mtb = qst["qt"], qst["qbb"], qst["mtt"], qst["mtb"]
                        ps, psb = psS.next()
                        op("pe", lambda e: e.matmul(ps[:], kaT[:, j, kb * 128:(kb + 1) * 128], qt[:, j * 512:(j + 1) * 512],
                                                    start=True, stop=True), r=[b_kv, qbb], w=[psb])
                        et, eb = e_r.next()
                        op("act", lambda e: e.activation(out=et[:], in_=ps[:], func=AF.Exp, scale=isq), r=[psb], w=[eb])
                        pp, ppb = p_r.next()
                        op("dve", lambda e: e.tensor_tensor(
                            out=pp[:].rearrange("p (g t) -> p g t", g=4), in0=et[:].rearrange("p (g t) -> p g t", g=4),
                            in1=mtt[:, kb, :].unsqueeze(1).broadcast_to([128, 4, 128]), op=ALU.mult), r=[eb, mtb], w=[ppb])
                        it["pp"], it["ppb"] = pp, ppb

                    def stB(it=it, jst=jst, j=j, kb=kb, nkb=nkb, tok0=tok0):
                        pp, ppb = it["pp"], it["ppb"]
                        po, pob, pd, pdb = jst["po"], jst["pob"], jst["pd"], jst["pdb"]
                        op("pe", lambda e: e.matmul(po[:], va[:, kb, j * 128:(j + 1) * 128], pp[:],
                                                    start=(kb == 0), stop=(kb == nkb - 1)), r=[ppb, b_kv], w=[pob])
                        op("pe", lambda e: e.matmul(pd[:], onesb[:], pp[:], start=(kb == 0), stop=(kb == nkb - 1)),
                           r=[ppb, b_cb], w=[pdb])
                        if kb == nkb - 1:
                            rd, rdb = rd_r.next()
                            op("dve", lambda e: e.reciprocal(out=rd[:], in_=pd[:]), r=[pdb], w=[rdb])
                            of_, ofb = of_r.next()
                            op("dve", lambda e: e.tensor_tensor(out=of_[:], in0=po[:], in1=rd[:], op=ALU.mult),
                               r=[pob, rdb], w=[ofb])
                            obt, obb = ob_r.next()
                            op("act", lambda e: e.activation(out=obt[:], in_=of_[:], func=AF.Copy), r=[ofb], w=[obb])
                            dma("sp", lambda e: e.dma_start(
                                out=OA[4 * j:4 * j + 4, :, tok0:tok0 + 128].rearrange("h d t -> d h t"),
                                in_=obt[:].rearrange("p (h t) -> p h t", h=4)), r=[obb])

                    items.append((stA, stB))
        pipeline(items, 2)
        sch.barrier()
        sch.emit()
    if upto == "C2":
        return finish()

    with ExitStack() as ph:
        cmask_s = ph.enter_context(nc.sbuf_tensor("cmask_s", [128, 2, 8, 512], BF16))
        b_cm = Buf("cmask")
        dma("sp", lambda e: e.dma_start(out=cmask_s[:].rearrange("p m j t -> p (m j t)"), in_=cmask_d[:, :]), w=[b_cm])
        kb_r = Ring(nc, ph, "kbT", [128, S], BF16, 2)
        vb_r = Ring(nc, ph, "vbh", [128, NT, 128], BF16, 2)
        qb_r = Ring(nc, ph, "qbT", [128, So], BF16, 2)
        E_r = Ring(nc, ph, "sbE", [128, 512], F32, 4)
        Em_r = Ring(nc, ph, "sbEm", [128, 512], F32, 4)
        SP_r = Ring(nc, ph, "sbSP", [128, 512], BF16, 4)
        SPm_r = Ring(nc, ph, "sbSPm", [128, 512], BF16, 4)
        X_r = Ring(nc, ph, "sbX", [128, 512], F32, 3)
        At_r = Ring(nc, ph, "sbAt", [128, 512], BF16, 4)
        Ra_r = Ring(nc, ph, "sbRa", [128, 512], F32, 2)
        obD_r = Ring(nc, ph, "obD", [128, 512], BF16, 2)
        psZ = Ring(nc, ph, "psZ", [128, 512], F32, 3, psum=True)
        psCc = Ring(nc, ph, "psCc", [128, 512], F32, 3, psum=True)
        psOd = Ring(nc, ph, "psOd", [128, 512], F32, 2, psum=True)
        isq = 1.0 / math.sqrt(HD)
        items = []
        for h in range(B_H):
            hst = {}

            def hsetup(hst=hst, h=h):
                kt, ktb = kb_r.next()
                vt, vtb = vb_r.next()
                qt, qtb = qb_r.next()
                dma("sp", lambda e: e.dma_start(out=kt[:], in_=KB[h]), w=[ktb])
                dma("sp", lambda e: e.dma_start(
                    out=vt[:], in_=VB[:, h * 128:(h + 1) * 128].rearrange("(kb p) d -> p kb d", p=128)), w=[vtb])
                dma("sp", lambda e: e.dma_start(out=qt[:], in_=QB[h]), w=[qtb])
                hst.update(kt=kt, ktb=ktb, vt=vt, vtb=vtb, qt=qt, qtb=qtb)

            for i in range(Go):
                nkb = 4 * (2 * i + 2)
                m = i % 2
                gst = {}
                for n, kb in enumerate(reversed(range(nkb))):
                    j = kb - (nkb - 8)
                    it = {}

                    def stA(it=it, hst=hst, gst=gst, i=i, n=n, kb=kb, j=j, m=m, first=(i == 0 and n == 0), hsetup=hsetup):
                        if first:
                            hsetup()
                        if n == 0:
                            gst["ra"], gst["rab"] = Ra_r.next()
                            gst["po"], gst["pob"] = psOd.next()
                        kt, ktb, qt, qtb = hst["kt"], hst["ktb"], hst["qt"], hst["qtb"]
                        pz, pzb = psZ.next()
                        op("pe", lambda e: e.matmul(pz[:], kt[:, kb * 128:(kb + 1) * 128], qt[:, i * 512:(i + 1) * 512],
                                                    start=True, stop=True), r=[ktb, qtb], w=[pzb])
                        Et, Eb = E_r.next()
                        op("act", lambda e: e.activation(out=Et[:], in_=pz[:], func=AF.Exp, scale=isq), r=[pzb], w=[Eb])
                        St, Sb = SP_r.next()
                        op("act", lambda e: e.activation(out=St[:], in_=Et[:], func=AF.Ln, bias=1.0), r=[Eb], w=[Sb])
                        if j >= 0:
                            Sm, Smb = SPm_r.next()
                            op("dve", lambda e: e.tensor_tensor(out=Sm[:], in0=St[:], in1=cmask_s[:, m, j, :], op=ALU.mult),
                               r=[Sb, b_cm], w=[Smb])
                            Em, Emb = Em_r.next()
                            op("pool", lambda e: e.tensor_tensor(out=Em[:], in0=Et[:], in1=cmask_s[:, m, j, :], op=ALU.mult),
                               r=[Eb, b_cm], w=[Emb])
                        else:
                            Sm, Smb = St, Sb
                            Em, Emb = Et, Eb
                        it.update(Sm=Sm, Smb=Smb, Em=Em, Emb=Emb)

                    def stB1(it=it, gst=gst, n=n, nkb=nkb):
                        Sm, Smb = it["Sm"], it["Smb"]
                        ra, rab = gst["ra"], gst["rab"]
                        pc, pcb = psCc.next()
                        op("pe", lambda e: e.matmul(pc[:], triI[:], Sm[:], start=True, stop=(n == 0)), r=[Smb, b_cb], w=[pcb])
                        if n > 0:
                            op("pe", lambda e: e.matmul(pc[:], onesf, ra[:], start=False, stop=True), r=[rab, b_consts], w=[pcb])
                        if n == 0:
                            op("pool", lambda e: e.tensor_copy(out=ra[:], in_=Sm[:]), r=[Smb], w=[rab])
                        elif n < nkb - 1:
                            op("pool", lambda e: e.tensor_tensor(out=ra[:], in0=ra[:], in1=Sm[:], op=ALU.add), r=[Smb, rab], w=[rab])
                        it.update(pc=pc, pcb=pcb)

                    def stB2(it=it):
                        Em, Emb, pc, pcb = it["Em"], it["Emb"], it["pc"], it["pcb"]
                        Xt, Xb = X_r.next()
                        op("act", lambda e: e.activation(out=Xt[:], in_=pc[:], func=AF.Exp, scale=-1.0), r=[pcb], w=[Xb])
                        At, Ab = At_r.next()
                        op("dve", lambda e: e.tensor_tensor(out=At[:], in0=Em[:], in1=Xt[:], op=ALU.mult), r=[Emb, Xb], w=[Ab])
                        it.update(At=At, Ab=Ab)

                    def stB3(it=it, hst=hst, gst=gst, h=h, i=i, n=n, kb=kb, nkb=nkb):
                        At, Ab = it["At"], it["Ab"]
                        po, pob = gst["po"], gst["pob"]
                        vt, vtb = hst["vt"], hst["vtb"]
                        op("pe", lambda e: e.matmul(po[:], vt[:, kb, :], At[:], start=(n == 0), stop=(n == nkb - 1)),
                           r=[vtb, Ab], w=[pob])
                        if n == nkb - 1:
                            ot, otb = obD_r.next()
                            op("act", lambda e: e.activation(out=ot[:], in_=po[:], func=AF.Copy), r=[pob], w=[otb])
                            dma("sp", lambda e: e.dma_start(out=OB[h, :, i * 512:(i + 1) * 512], in_=ot[:]), r=[otb])

                    items.append((stA, stB1, stB2, stB3))
        pipeline(items, [0, 2, 3, 5])
        sch.barrier()
        sch.emit()
    if upto == "D":
        return finish()

    with ExitStack() as ph:
        wa = ph.enter_context(nc.sbuf_tensor("wa", [128, 8, D], BF16))
        wb_ = ph.enter_context(nc.sbuf_tensor("wb_", [128, 8, D], BF16))
        b_wab = Buf("wab")
        for (wt, src) in ((wa, w_bra), (wb_, w_brb)):
            sv = src.rearrange("(kc p) n -> p kc n", p=128)
            for cq in range(4):
                dma("pool", lambda e, wt=wt, sv=sv, cq=cq: e.dma_start(
                    out=wt[:, :, cq * 512:(cq + 1) * 512], in_=sv[:, :, cq * 512:(cq + 1) * 512]), w=[b_wab])
        zt = ph.enter_context(nc.sbuf_tensor("zt", [128, 4096], BF16))
        b_zt = Buf("zt")
        op("dve", lambda e: e.memset(zt[:], 0.0), w=[b_zt])
        XGf = XG.rearrange("(r p two) c -> r p (two c)", p=128, two=2)
        for r_ in range(NE * CAP // 256):
            dma("sp", lambda e, r_=r_: e.dma_start(out=XGf[r_], in_=zt[:]), r=[b_zt])
        oa_r = Ring(nc, ph, "oaT", [128, 8, 512], BF16, 2)
        ob2_r = Ring(nc, ph, "obT", [128, 8, 512], BF16, 2)
        g_r = Ring(nc, ph, "gt", [128, 2, 512], BF16, 3)
        sg_r = Ring(nc, ph, "sgt", [128, 2, 512], F32, 2)
        t1e_r = Ring(nc, ph, "t1e", [128, 512], F32, 2)
        t2e_r = Ring(nc, ph, "t2e", [128, 512], F32, 2)
        mg_r = Ring(nc, ph, "mgo", [128, 512], BF16, 3)
        psa_r = Ring(nc, ph, "psEa", [128, 512], F32, 3, psum=True)
        psb_r = Ring(nc, ph, "psEb", [128, 512], F32, 3, psum=True)
        for tg in range(So // 512):
            t0 = tg * 512
            oat, oab = oa_r.next()
            obt, obb = ob2_r.next()
            dma("sp", lambda e, oat=oat, t0=t0: e.dma_start(out=oat[:], in_=OA[:, :, t0:t0 + 512].rearrange("h d t -> d h t")), w=[oab])
            dma("sp", lambda e, obt=obt, t0=t0: e.dma_start(out=obt[:], in_=OB[:, :, t0:t0 + 512].rearrange("h d t -> d h t")), w=[obb])
            for fo in range(KC):
                gt, gb_ = g_r.next()
                dma("sp", lambda e, gt=gt, fo=fo, t0=t0: e.dma_start(out=gt[:, 0, :], in_=GA[fo, :, t0:t0 + 512]), w=[gb_])
                dma("sp", lambda e, gt=gt, fo=fo, t0=t0: e.dma_start(out=gt[:, 1, :], in_=GB[fo, :, t0:t0 + 512]), w=[gb_])
                st_, sb_ = sg_r.next()
                op("act", lambda e, st_=st_, gt=gt: e.activation(out=st_[:], in_=gt[:], func=AF.Sigmoid), r=[gb_], w=[sb_])
                pa, pab = psa_r.next()
                mmgroup(pa[:], pab, [(wa[:, kc, fo * 128:(fo + 1) * 128], oat[:, kc, :]) for kc in range(8)], [b_wab, oab])
                pb2, pbb = psb_r.next()
                mmgroup(pb2[:], pbb, [(wb_[:, kc, fo * 128:(fo + 1) * 128], obt[:, kc, :]) for kc in range(8)], [b_wab, obb])
                a1, a1b = t1e_r.next()
                a2, a2b = t2e_r.next()
                op("dve", lambda e, a1=a1, pa=pa, st_=st_: e.tensor_tensor(out=a1[:], in0=pa[:], in1=st_[:, 0, :], op=ALU.mult),
                   r=[pab, sb_], w=[a1b])
                op("dve", lambda e, a2=a2, pb2=pb2, st_=st_: e.tensor_tensor(out=a2[:], in0=pb2[:], in1=st_[:, 1, :], op=ALU.mult),
                   r=[pbb, sb_], w=[a2b])
                mt_, mb_ = mg_r.next()
                op("pool", lambda e, mt_=mt_, a1=a1, a2=a2: e.tensor_tensor(out=mt_[:], in0=a1[:], in1=a2[:], op=ALU.add),
                   r=[a1b, a2b], w=[mb_])
                dma("sp", lambda e, mt_=mt_, fo=fo, t0=t0: e.dma_start(out=MG[fo, :, t0:t0 + 512], in_=mt_[:]), r=[mb_])
        sch.barrier()
        sch.emit()
    if upto == "E1":
        return finish()

    NSLOT = NE * CAP
    NSOK = min(NSLOT, 65535)
    with ExitStack() as ph:
        wo = ph.enter_context(nc.sbuf_tensor("wo", [128, KC, D], BF16))
        b_wo = Buf("wo")
        wo_v = w_out.rearrange("(kc p) n -> p kc n", p=128)
        for cq in range(4):
            dma("pool", lambda e, cq=cq: e.dma_start(out=wo[:, :, cq * 512:(cq + 1) * 512], in_=wo_v[:, :, cq * 512:(cq + 1) * 512]),
                w=[b_wo])
        wr = ph.enter_context(nc.sbuf_tensor("wr", [128, KC, NE], BF16))
        b_wr = Buf("wr")
        dma("pool", lambda e: e.dma_start(out=wr[:], in_=w_router.rearrange("(kc p) n -> p kc n", p=128)), w=[b_wr])
        brow = ph.enter_context(nc.sbuf_tensor("brow", [128, NE], F32))
        dma("sp", lambda e: e.dma_start(out=brow[:], in_=b_router[0:1, :].partition_broadcast(128)), w=[b_wr])
        base = ph.enter_context(nc.sbuf_tensor("base", [128, NE], F32))
        slotbase = ph.enter_context(nc.sbuf_tensor("slotbase", [128, NE], F32))
        b_base = Buf("base")
        op("dve", lambda e: e.memset(base[:], 0.0), w=[b_base])
        op("dve", lambda e: e.tensor_scalar(out=slotbase[:], in0=iota_f[:, 0:NE], scalar1=float(CAP), scalar2=None, op0=ALU.mult),
           r=[b_consts], w=[b_base])
        mg_r = Ring(nc, ph, "mgi", [128, KC, 512], BF16, 1)
        x_r = Ring(nc, ph, "xe", [128, D], F32, 2)
        mix_r = Ring(nc, ph, "mixs", [128, D], F32, 1)
        x1_r = Ring(nc, ph, "x1t", [128, D], F32, 2)
        xn_r = Ring(nc, ph, "xn2", [128, D], BF16, 2)
        h2_r = Ring(nc, ph, "h2T", [128, KC, 128], BF16, 2)
        junk = ph.enter_context(nc.sbuf_tensor("junkE", [128, D], BF16))
        b_junk = Buf("junkE")
        st_r = Ring(nc, ph, "stE", [128, 8], F32, 4)
        rt_r = Ring(nc, ph, "rtE", [128, 8, NE], F32, 2)
        mkb_r = Ring(nc, ph, "mkE", [128, NE], BF16, 2)
        t8_r = Ring(nc, ph, "t8E", [128, 24], F32, 2)
        psm_r = Ring(nc, ph, "psEm", [128, 512], F32, 3, psum=True)
        pst_r = Ring(nc, ph, "psEt", [128, 1024], BF16, 2, psum=True)
        psr_r = Ring(nc, ph, "psEr", [128, 512], F32, 2, psum=True)
        for tg in range(So // 512):
            mgt, mgb = mg_r.next()
            dma("sp", lambda e, mgt=mgt, tg=tg: e.dma_start(
                out=mgt[:], in_=MG[:, :, tg * 512:(tg + 1) * 512].rearrange("f d t -> d f t")), w=[mgb])
            for tt in range(4):
                ti = tg * 4 + tt
                tok0 = ti * 128
                xt, xb = x_r.next()
                dma("sp", lambda e, xt=xt, tok0=tok0: e.dma_start(out=xt[:], in_=x_own[tok0:tok0 + 128, :]), w=[xb])
                mx, mxb = mix_r.next()
                for dc in range(4):
                    pm, pmb = psm_r.next()
                    mmgroup(pm[:], pmb, [(mgt[:, kc, tt * 128:(tt + 1) * 128], wo[:, kc, dc * 512:(dc + 1) * 512])
                                         for kc in range(KC)], [mgb, b_wo])
                    op("act", lambda e, mx=mx, pm=pm, dc=dc: e.activation(out=mx[:, dc * 512:(dc + 1) * 512], in_=pm[:], func=AF.Copy),
                       r=[pmb], w=[mxb])
                st, sb_ = st_r.next()
                op("act", lambda e, st=st, mx=mx: e.activation(out=junk[:], in_=mx[:], func=AF.Square, accum_out=st[:, 0:1]),
                   r=[mxb], w=[b_junk, sb_])
                op("act", lambda e, st=st: e.activation(out=st[:, 1:2], in_=st[:, 0:1], func=AF.Sqrt, scale=1.0 / D, bias=EPS),
                   r=[sb_], w=[sb_])
                op("dve", lambda e, st=st: e.reciprocal(out=st[:, 2:3], in_=st[:, 1:2]), r=[sb_], w=[sb_])
                op("dve", lambda e, st=st, mx=mx: e.scalar_tensor_tensor(out=mx[:], in0=mx[:], scalar=st[:, 2:3], in1=ga1row[:],
                                                                        op0=ALU.mult, op1=ALU.mult),
                   r=[mxb, sb_, b_garow[0]], w=[mxb])
                x1, x1b = x1_r.next()
                op("pool", lambda e, x1=x1, mx=mx, xt=xt: e.tensor_tensor(out=x1[:], in0=mx[:], in1=xt[:], op=ALU.add),
                   r=[mxb, xb], w=[x1b])
                dma("sp", lambda e, x1=x1, tok0=tok0: e.dma_start(out=X1[tok0:tok0 + 128, :], in_=x1[:]), r=[x1b])
                op("act", lambda e, st=st, x1=x1: e.activation(out=junk[:], in_=x1[:], func=AF.Square, accum_out=st[:, 3:4]),
                   r=[x1b], w=[b_junk, sb_])
                op("act", lambda e, st=st: e.activation(out=st[:, 4:5], in_=st[:, 3:4], func=AF.Sqrt, scale=1.0 / D, bias=EPS),
                   r=[sb_], w=[sb_])
                op("dve", lambda e, st=st: e.reciprocal(out=st[:, 5:6], in_=st[:, 4:5]), r=[sb_], w=[sb_])
                xn, xnb = xn_r.next()
                op("dve", lambda e, xn=xn, x1=x1, st=st: e.tensor_scalar(out=xn[:], in0=x1[:], scalar1=st[:, 5:6], scalar2=None,
                                                                       op0=ALU.mult), r=[x1b, sb_], w=[xnb])
                h2, h2b = h2_r.next()
                for half in range(2):
                    pt, pb = pst_r.next()
                    for j in range(8):
                        kc = half * 8 + j
                        op("pe", lambda e, pt=pt, j=j, kc=kc, xn=xn: e.transpose(
                            pt[:, j * 128:(j + 1) * 128], xn[:, kc * 128:(kc + 1) * 128], identb[:]),
                           r=[xnb, b_cb] if j in (0, 7) else (), w=[pb])
                    for j in range(8):
                        kc = half * 8 + j
                        op("act", lambda e, pt=pt, j=j, kc=kc, h2=h2: e.activation(
                            out=h2[:, kc, :], in_=pt[:, j * 128:(j + 1) * 128], func=AF.Identity,
                            scale=modc[:, 2 * KC + kc:2 * KC + kc + 1], bias=modc[:, 3 * KC + kc:3 * KC + kc + 1]),
                           r=[pb, b_modc], w=[h2b])
                pr, prb = psr_r.next()
                mmgroup(pr[:, 0:NE], prb, [(h2[:, kc, :], wr[:, kc, :]) for kc in range(KC)], [h2b, b_wr])
                rt, rtb = rt_r.next()
                t8, t8b = t8_r.next()
                mk, mkb = mkb_r.next()
                lg, pos_, sla, ov, jk = (rt[:, q, :] for q in range(5))
                op("dve", lambda e, lg=lg, pr=pr: e.tensor_tensor(out=lg, in0=pr[:, 0:NE], in1=brow[:], op=ALU.add),
                   r=[prb, b_wr], w=[rtb])
                op("dve", lambda e, lg=lg, t8=t8: e.max(out=t8[:, 0:8], in_=lg), r=[rtb], w=[t8b])
                op("dve", lambda e, t8=t8: e.tensor_scalar(out=t8[:, 8:12], in0=t8[:, 0:4], scalar1=t8[:, 0:1], scalar2=None,
                                                          op0=ALU.subtract), r=[t8b], w=[t8b])
                op("act", lambda e, t8=t8, st=st: e.activation(out=t8[:, 12:16], in_=t8[:, 8:12], func=AF.Exp, accum_out=st[:, 6:7]),
                   r=[t8b], w=[t8b, sb_])
                op("dve", lambda e, st=st: e.reciprocal(out=st[:, 7:8], in_=st[:, 6:7]), r=[sb_], w=[sb_])
                op("dve", lambda e, mk=mk, lg=lg, t8=t8: e.tensor_scalar(out=mk[:], in0=lg, scalar1=t8[:, 3:4], scalar2=None,
                                                                       op0=ALU.is_ge), r=[rtb, t8b], w=[mkb])
                pp, ppb = psr_r.next()
                op("pe", lambda e, pp=pp, mk=mk: e.matmul(pp[:, 0:NE], triS[:], mk[:], start=True, stop=True),
                   r=[mkb, b_cb], w=[ppb])
                op("pe", lambda e, pp=pp, mk=mk: e.matmul(pp[:, 64:64 + NE], onesb[:], mk[:], start=True, stop=True),
                   r=[mkb, b_cb], w=[ppb])
                op("dve", lambda e, pos_=pos_, pp=pp: e.tensor_tensor(out=pos_, in0=pp[:, 0:NE], in1=base[:], op=ALU.add),
                   r=[ppb, b_base], w=[rtb])
                op("dve", lambda e, pp=pp: e.tensor_tensor(out=base[:], in0=pp[:, 64:64 + NE], in1=base[:], op=ALU.add),
                   r=[ppb, b_base], w=[b_base])
                op("dve", lambda e, ov=ov, pos_=pos_: e.tensor_scalar(out=ov, in0=pos_, scalar1=float(CAP), scalar2=1.0e9,
                                                                    op0=ALU.is_ge, op1=ALU.mult), r=[rtb], w=[rtb])
                op("dve", lambda e, sla=sla, pos_=pos_: e.tensor_tensor(out=sla, in0=pos_, in1=slotbase[:], op=ALU.add),
                   r=[rtb, b_base], w=[rtb])
                op("dve", lambda e, sla=sla, ov=ov: e.tensor_tensor(out=sla, in0=sla, in1=ov, op=ALU.add), r=[rtb], w=[rtb])
                for k in range(4):
                    op("dve", lambda e, jk=jk, lg=lg, t8=t8, sla=sla, k=k: e.scalar_tensor_tensor(
                        out=jk, in0=lg, scalar=t8[:, k:k + 1], in1=sla, op0=ALU.is_equal, op1=ALU.mult,
                        accum_out=t8[:, 16 + k:17 + k]), r=[rtb, t8b], w=[rtb, t8b])
                op("dve", lambda e, t8=t8, ti=ti: e.tensor_copy(out=rslot[:, ti * 4:(ti + 1) * 4], in_=t8[:, 16:20]),
                   r=[t8b], w=[b_route])
                op("dve", lambda e, t8=t8: e.tensor_scalar(out=t8[:, 20:24], in0=t8[:, 16:20], scalar1=float(NSOK), scalar2=None,
                                                          op0=ALU.is_lt), r=[t8b], w=[t8b])
                op("dve", lambda e, t8=t8, st=st: e.scalar_tensor_tensor(
                    out=t8[:, 12:16], in0=t8[:, 12:16], scalar=st[:, 7:8], in1=t8[:, 20:24], op0=ALU.mult, op1=ALU.mult),
                   r=[t8b, sb_], w=[t8b])
                op("dve", lambda e, t8=t8, ti=ti: e.tensor_copy(out=rwgt[:, ti * 4:(ti + 1) * 4], in_=t8[:, 12:16]),
                   r=[t8b], w=[b_route])
                for k in range(4):
                    dma("pool", lambda e, xn=xn, ti=ti, k=k: e.indirect_dma_start(
                        out=XG[0:NSOK, :], out_offset=bass.IndirectOffsetOnAxis(ap=rslot[:, ti * 4 + k:ti * 4 + k + 1], axis=0),
                        in_=xn[:, :], in_offset=None, bounds_check=sch.breg(e, NSOK - 1), oob_is_err=False),
                        r=[xnb, b_route])
        sch.barrier()
        sch.emit()
    if upto == "E2":
        return finish()

    NBLK = CAP // 128
    NTG = CAP // 512
    with ExitStack() as ph:
        XT = ph.enter_context(nc.sbuf_tensor("XT", [128, KC, CAP], BF16))
        b_XT = Buf("XT")
        actT = ph.enter_context(nc.sbuf_tensor("actT", [128, KC, CAP], BF16))
        b_act = [Buf(f"act{f}") for f in range(KC)]
        wc_r = Ring(nc, ph, "wcG", [128, KC, 256], BF16, 2)
        gt_r = Ring(nc, ph, "gtG", [128, D], BF16, 2)
        bb_r = Ring(nc, ph, "bbG", [128, 2 * KC], F32, 2)
        b2_r = Ring(nc, ph, "b2G", [1, 256], BF16, 3)
        g_r = Ring(nc, ph, "gG", [128, 512], F32, 2)
        s_r = Ring(nc, ph, "sG", [128, 512], F32, 2)
        l_r = Ring(nc, ph, "lG", [128, 512], F32, 2)
        gs_r = Ring(nc, ph, "gsG", [128, 512], F32, 2)
        ot_r = Ring(nc, ph, "otG", [128, 256], BF16, 3)
        psT_r = Ring(nc, ph, "psGt", [128, 1024], BF16, 2, psum=True)
        psg_r = Ring(nc, ph, "psGg", [128, 512], F32, 2, psum=True)
        psl_r = Ring(nc, ph, "psGl", [128, 512], F32, 2, psum=True)
        pso_r = Ring(nc, ph, "psGo", [128, 512], F32, 2, psum=True)
        def xt_steps(ex):
            steps = []
            for blk in range(NBLK):
                hold = {}
                for half in range(2):
                    def step(ex=ex, blk=blk, half=half, hold=hold):
                        if half == 0:
                            gt, gtb = gt_r.next()
                            r0 = ex * CAP + blk * 128
                            dma("sp", lambda e: e.dma_start(out=gt[:], in_=XG[r0:r0 + 128, :]), w=[gtb])
                            hold["gt"], hold["gtb"] = gt, gtb
                        gt, gtb = hold["gt"], hold["gtb"]
                        pt, pb = psT_r.next()
                        for j in range(8):
                            kc = half * 8 + j
                            op("pe", lambda e, j=j, kc=kc: e.transpose(
                                pt[:, j * 128:(j + 1) * 128], gt[:, kc * 128:(kc + 1) * 128], identb[:]),
                               r=[gtb, b_cb] if j in (0, 7) else (), w=[pb])
                        for j in range(8):
                            kc = half * 8 + j
                            if half == 0:
                                op("act", lambda e, j=j, kc=kc: e.activation(
                                    out=XT[:, kc, blk * 128:(blk + 1) * 128], in_=pt[:, j * 128:(j + 1) * 128], func=AF.Identity,
                                    scale=modc[:, 2 * KC + kc:2 * KC + kc + 1], bias=modc[:, 3 * KC + kc:3 * KC + kc + 1]),
                                   r=[pb, b_modc], w=[b_XT])
                            else:
                                op("dve", lambda e, j=j, kc=kc: e.tensor_scalar(
                                    out=XT[:, kc, blk * 128:(blk + 1) * 128], in0=pt[:, j * 128:(j + 1) * 128],
                                    scalar1=modc[:, 2 * KC + kc:2 * KC + kc + 1], scalar2=modc[:, 3 * KC + kc:3 * KC + kc + 1],
                                    op0=ALU.mult, op1=ALU.add), r=[pb, b_modc], w=[b_XT])
                    steps.append(step)
            return steps

        for ex in range(NE):
            bt, btb = bb_r.next()
            dma("sp", lambda e, bt=bt, ex=ex: e.dma_start(out=bt[:], in_=b1c[ex]), w=[btb])
            w1v = w1[ex].rearrange("(kc p) n -> p kc n", p=128)
            w2v = w2[ex].rearrange("(kc p) n -> p kc n", p=128)
            if ex == 0:
                for st_ in xt_steps(0):
                    st_()
            for cq in range(KC):
                wt, wb = wc_r.next()
                dma("pool", lambda e, wt=wt, w1v=w1v, cq=cq: e.dma_start(out=wt[:], in_=w1v[:, :, cq * 256:(cq + 1) * 256]), w=[wb])
                for tg in range(NTG):
                    pg, pgb = psg_r.next()
                    mmgroup(pg[:], pgb, [(wt[:, kc, 0:256:2], XT[:, kc, tg * 512:(tg + 1) * 512]) for kc in range(KC)], [wb, b_XT])
                    pl, plb = psl_r.next()
                    mmgroup(pl[:], plb, [(wt[:, kc, 1:256:2], XT[:, kc, tg * 512:(tg + 1) * 512]) for kc in range(KC)], [wb, b_XT])
                    g_, gb_ = g_r.next()
                    op("dve", lambda e, g_=g_, pg=pg, bt=bt, cq=cq: e.tensor_scalar(
                        out=g_[:], in0=pg[:], scalar1=bt[:, cq:cq + 1], scalar2=LIMIT, op0=ALU.add, op1=ALU.min),
                       r=[pgb, btb], w=[gb_])
                    sg, sgb = s_r.next()
                    op("act", lambda e, sg=sg, g_=g_: e.activation(out=sg[:], in_=g_[:], func=AF.Sigmoid, scale=ALPHA),
                       r=[gb_], w=[sgb])
                    l_, lb_ = l_r.next()
                    op("dve", lambda e, l_=l_, pl=pl, bt=bt, cq=cq: e.tensor_scalar(
                        out=l_[:], in0=pl[:], scalar1=bt[:, KC + cq:KC + cq + 1], scalar2=LIMIT, op0=ALU.add, op1=ALU.min),
                       r=[plb, btb], w=[lb_])
                    op("dve", lambda e, l_=l_: e.tensor_scalar(out=l_[:], in0=l_[:], scalar1=-LIMIT, scalar2=1.0,
                                                               op0=ALU.max, op1=ALU.add), r=[lb_], w=[lb_])
                    gs, gsb = gs_r.next()
                    op("pool", lambda e, gs=gs, g_=g_, sg=sg: e.tensor_tensor(out=gs[:], in0=g_[:], in1=sg[:], op=ALU.mult),
                       r=[gb_, sgb], w=[gsb])
                    op("dve", lambda e, gs=gs, l_=l_, cq=cq, tg=tg: e.tensor_tensor(
                        out=actT[:, cq, tg * 512:(tg + 1) * 512], in0=gs[:], in1=l_[:], op=ALU.mult),
                       r=[gsb, lb_], w=[b_act[cq]])
            nxt = xt_steps(ex + 1) if ex + 1 < NE else []
            w2step = 0
            for dc in range(D // 256):
                wt, wb = wc_r.next()
                dma("pool", lambda e, wt=wt, w2v=w2v, dc=dc: e.dma_start(out=wt[:], in_=w2v[:, :, dc * 256:(dc + 1) * 256]), w=[wb])
                b2t, b2b = b2_r.next()
                dma("pool", lambda e, b2t=b2t, ex=ex, dc=dc: e.dma_start(out=b2t[:], in_=b2[ex:ex + 1, dc * 256:(dc + 1) * 256]), w=[b2b])
                for blk in range(NBLK):
                    po, pob = pso_r.next()
                    pairs = [(actT[:, fc, blk * 128:(blk + 1) * 128], wt[:, fc, :]) for fc in range(KC)]
                    n = len(pairs)
                    for q, (l, r_) in enumerate(pairs):
                        op("pe", lambda e, po=po, l=l, r_=r_, q=q: e.matmul(po[:, 0:256], l, r_, start=(q == 0), stop=False),
                           r=(b_act + [wb]) if q in (0, n - 1) else (), w=[pob])
                    op("pe", lambda e, po=po, b2t=b2t, dc=dc: e.matmul(
                        po[:, 0:256], onesb[0:1, :], b2t[0:1, :], start=False, stop=True),
                       r=[b2b, b_cb], w=[pob])
                    ot, otb = ot_r.next()
                    op("act", lambda e, ot=ot, po=po: e.activation(out=ot[:], in_=po[:, 0:256], func=AF.Copy), r=[pob], w=[otb])
                    r0 = ex * CAP + blk * 128
                    dma("sp", lambda e, ot=ot, r0=r0, dc=dc: e.dma_start(out=YG[r0:r0 + 128, dc * 256:(dc + 1) * 256], in_=ot[:]),
                        r=[otb])
                    w2step += 1
                    if nxt and w2step % max(1, (D // 256) * NBLK // len(nxt)) == 0:
                        nxt.pop(0)()
            while nxt:
                nxt.pop(0)()
        sch.barrier()
        sch.emit()
    if upto == "G":
        return finish()

    with ExitStack() as ph:
        y_r = Ring(nc, ph, "yH", [128, D], BF16, 5)
        x1_r = Ring(nc, ph, "x1H", [128, D], F32, 2)
        f_r = Ring(nc, ph, "fH", [128, D], F32, 2)
        o_r = Ring(nc, ph, "oH", [128, D], F32, 2)
        junk = ph.enter_context(nc.sbuf_tensor("junkH", [128, D], BF16))
        b_junk = Buf("junkH")
        st_r = Ring(nc, ph, "stH", [128, 4], F32, 3)
        for (yt, yb) in y_r.tiles:
            op("dve", lambda e, yt=yt: e.memset(yt[:], 0.0), w=[yb])
        for ti in range(NTo):
            tok0 = ti * 128
            ys = []
            for k in range(4):
                yt, yb = y_r.next()
                dma("pool", lambda e, yt=yt, ti=ti, k=k: e.indirect_dma_start(
                    out=yt[:, :], out_offset=None, in_=YG[0:NSOK, :],
                    in_offset=bass.IndirectOffsetOnAxis(ap=rslot[:, ti * 4 + k:ti * 4 + k + 1], axis=0),
                    bounds_check=sch.breg(e, NSOK - 1), oob_is_err=False), r=[b_route], w=[yb])
                ys.append((yt, yb))
            x1, x1b = x1_r.next()
            dma("sp", lambda e, x1=x1, tok0=tok0: e.dma_start(out=x1[:], in_=X1[tok0:tok0 + 128, :]), w=[x1b])
            ft, fb = f_r.next()
            op("dve", lambda e, ft=ft, ti=ti, y0=ys[0][0]: e.tensor_scalar(
                out=ft[:], in0=y0[:], scalar1=rwgt[:, ti * 4:ti * 4 + 1], scalar2=None, op0=ALU.mult),
               r=[ys[0][1], b_route], w=[fb])
            for k in range(1, 4):
                op("dve", lambda e, ft=ft, ti=ti, k=k, yk=ys[k][0]: e.scalar_tensor_tensor(
                    out=ft[:], in0=yk[:], scalar=rwgt[:, ti * 4 + k:ti * 4 + k + 1], in1=ft[:], op0=ALU.mult, op1=ALU.add),
                   r=[ys[k][1], b_route, fb], w=[fb])
            st, sb_ = st_r.next()
            op("act", lambda e, st=st, ft=ft: e.activation(out=junk[:], in_=ft[:], func=AF.Square, accum_out=st[:, 0:1]),
               r=[fb], w=[b_junk, sb_])
            op("act", lambda e, st=st: e.activation(out=st[:, 1:2], in_=st[:, 0:1], func=AF.Sqrt, scale=1.0 / D, bias=EPS),
               r=[sb_], w=[sb_])
            op("dve", lambda e, st=st: e.reciprocal(out=st[:, 2:3], in_=st[:, 1:2]), r=[sb_], w=[sb_])
            op("dve", lambda e, st=st, ft=ft: e.scalar_tensor_tensor(out=ft[:], in0=ft[:], scalar=st[:, 2:3], in1=ga2row[:],
                                                                    op0=ALU.mult, op1=ALU.mult),
               r=[fb, sb_, b_garow[1]], w=[fb])
            ot, otb = o_r.next()
            op("pool", lambda e, ot=ot, ft=ft, x1=x1: e.tensor_tensor(out=ot[:], in0=ft[:], in1=x1[:], op=ALU.add),
               r=[fb, x1b], w=[otb])
            dma("sp", lambda e, ot=ot, tok0=tok0: e.dma_start(out=out_d[tok0:tok0 + 128, :], in_=ot[:]), r=[otb])
        sch.barrier()
        sch.emit()
    return finish()


def make_consts():
    c = np.zeros((128, 8, 128), np.float32)
    c[:, 0, :] = np.eye(128)
    for dp in range(16):
        c[dp + 16, 1, dp] = -1.0
        c[dp, 1, dp + 16] = 1.0
    for o in (0, 64):
        for dp in range(8):
            c[o + dp + 8, 2, o + dp] = -1.0
            c[o + dp, 2, o + dp + 8] = 1.0
    k = np.arange(128)
    c[:, 3, :] = (k[:, None] >= k[None, :])
    c[:, 4, :] = (k[:, None] < k[None, :])
    c[:, 5, :] = 1.0
    invA = THETA ** (-(np.arange(16, dtype=np.float32)) / np.float32(16))
    invI = THETA ** (-(np.arange(8, dtype=np.float32)) / np.float32(8))
    for p in range(128):
        c[p, 6, 0] = invA[p % 16] if p < 32 else 0.0
        c[p, 6, 1] = invI[(p % 64) % 8] if (p % 64) < 16 else 0.0
    c[:, 7, :] = k[None, :]
    return np.ascontiguousarray(c.reshape(128, 8 * 128))


def make_masks(half):
    p = np.arange(128)[:, None]
    tl = np.arange(512)[None, :]
    cm = np.zeros((128, 2, 8, 512), np.float32)
    am = np.zeros((128, 2, 640), np.float32)
    cc = np.arange(640)[None, :]
    for m in range(2):
        delta = (1 if m == 0 else 0) if half == 0 else (0 if m == 0 else 1)
        for j in range(8):
            if delta == 0:
                cm[:, m, j, :] = (128 * (j - 4) + p) < tl
            else:
                cm[:, m, j, :] = (128 * j + p) < tl
        lim = 64 * (p // 64 + 1)
        am[:, m, :] = np.where(512 * (delta - 1) + cc >= lim, NEG, 0.0)
    return (np.ascontiguousarray(cm.reshape(128, -1)).astype(ml_dtypes.bfloat16),
            np.ascontiguousarray(am.reshape(128, -1)))


def col_layout(v):
    return np.ascontiguousarray(v.reshape(-1, 128).T)


def prep(inputs, cfg, batches):
    S, NE = cfg["S"], cfg["NE"]
    G = S // 512
    f32 = np.float32
    x = np.asarray(inputs["x"], f32)
    c = np.asarray(inputs["c"], f32)
    pos = np.asarray(inputs["positions"], np.int32)
    shared = {
        "b_ada": np.ascontiguousarray(np.asarray(inputs["b_ada"], f32)[0][None, :]),
        "badac": col_layout(np.asarray(inputs["b_ada"], f32)[0]),
        "gcols": np.concatenate([col_layout(np.asarray(inputs[k], f32)[0]) for k in
                                 ("g_pre_mix", "g_post_mix", "g_pre_ffn", "g_post_ffn")], axis=1),
        "g_post_mix": np.ascontiguousarray(np.asarray(inputs["g_post_mix"], f32)[0][None, :]),
        "g_post_ffn": np.ascontiguousarray(np.asarray(inputs["g_post_ffn"], f32)[0][None, :]),
        "w_ada": np.ascontiguousarray(np.asarray(inputs["w_ada"], f32)[0]),
        "w_in": np.ascontiguousarray(np.asarray(inputs["w_in"], f32)[0]),
        "w_branch_a": np.ascontiguousarray(np.asarray(inputs["w_branch_a"], f32)[0]),
        "w_branch_b": np.ascontiguousarray(np.asarray(inputs["w_branch_b"], f32)[0]),
        "w_out": np.ascontiguousarray(np.asarray(inputs["w_out"], f32)[0]),
        "w_router": np.ascontiguousarray(np.asarray(inputs["w_router"], f32)[0][:, :NE]),
        "b_router": np.ascontiguousarray(np.asarray(inputs["b_router"], f32)[0][None, :NE]),
        "w1": np.ascontiguousarray(np.asarray(inputs["w1"], f32)[0][:NE]),
        "w2": np.ascontiguousarray(np.asarray(inputs["w2"], f32)[0][:NE]),
        "b2": np.ascontiguousarray(np.asarray(inputs["b2"], f32)[0][:NE]),
        "consts": make_consts(),
    }
    b1 = np.asarray(inputs["b1"], f32)[0][:NE]
    b1g = b1[:, 0::2].reshape(NE, KC, 128).transpose(0, 2, 1)
    b1l = b1[:, 1::2].reshape(NE, KC, 128).transpose(0, 2, 1)
    shared["b1c"] = np.ascontiguousarray(np.concatenate([b1g, b1l], axis=2))
    masks = [make_masks(0), make_masks(1)]
    in_maps, owns = [], []
    for b in batches:
        for half in range(2):
            toks = np.concatenate([np.arange(g * 512, (g + 1) * 512) for g in own_groups(G, half)])
            m = dict(shared)
            m["x_all"] = np.ascontiguousarray(x[b])
            m["x_own"] = np.ascontiguousarray(x[b][toks])
            m["pos_all"] = np.ascontiguousarray(pos[b][None, :])
            m["pos_own"] = np.ascontiguousarray(pos[b][toks][None, :])
            m["cvec"] = col_layout(c[b])
            m["cmask"], m["amask"] = masks[half]
            in_maps.append(m)
            owns.append((b, toks))
    return in_maps, owns


_CACHE = {}


def kernel(**inputs):
    x = np.asarray(inputs["x"])
    B, S, _ = x.shape
    cfg = {"S": S, "NE": 32, "CAP": 2048}
    in_maps, owns = prep(inputs, cfg, list(range(B)))
    nc = build(cfg)
    res = run_bass_kernel_spmd(nc, in_maps, core_ids=list(range(len(in_maps))))
    out = np.empty((B, S, D), np.float32)
    for r, (b, toks) in zip(res.results, owns):
        out[b, toks] = np.asarray(r["out"], np.float32)
    return out
```

```python
import math
from contextlib import ExitStack

import numpy as np
import ml_dtypes

import concourse.bass as bass
import concourse.mybir as mybir
from concourse.bass_utils import run_bass_kernel_spmd

F32 = mybir.dt.float32
BF16 = mybir.dt.bfloat16
I32 = mybir.dt.int32
AF = mybir.ActivationFunctionType
ALU = mybir.AluOpType
AX = mybir.AxisListType

D = 2048
KC = D // 128
A_H, A_KV, HD = 8, 2, 128
IDX_H, IDX_D = 16, 64
B_H = 8
TOPK = 256
THETA = 500000.0
EPS = 1e-6
LIMIT = 7.0
ALPHA = 1.702
C_QA, C_KA, C_VA = 0, 1024, 1280
C_QB, C_KB, C_VB = 1536, 2560, 3584
C_QI, C_KI, C_WI = 4608, 5632, 5696
C_GA, C_GB = 5712, 7760
IN_W = 9808
NEG = -1.0e30
NBIS = 26


class Buf:
    __slots__ = ("w", "r", "name", "excl")

    def __init__(self, name="", excl=False):
        self.w = None
        self.r = []
        self.name = name
        self.excl = excl


class Sched:
    ENGS = ("pe", "act", "dve", "pool", "sp")
    RQ = {"sp": 8, "pool": 2, "act": 4}

    def __init__(self, nc, es):
        self.nc = nc
        self.sems = {}
        for e in self.ENGS:
            self.sems[e] = es.enter_context(nc.semaphore("c_" + e))
        self.dq = {}
        for q in ("sp", "pool", "act"):
            self.dq[q] = [es.enter_context(nc.semaphore(f"d_{q}{i}")) for i in range(self.RQ[q])]
        self.n = {e: 0 for e in self.ENGS}
        self.dn = {q: 0 for q in self.dq}
        self.seen = {e: {} for e in self.ENGS}
        self.streams = {e: [] for e in self.ENGS}
        self.last = {}

    def _sem(self, key):
        return self.sems[key] if isinstance(key, str) else self.dq[key[0]][key[1]]

    def _waits(self, eng, deps):
        out = []
        best = {}
        for d in deps:
            if d is None:
                continue
            k, v = d
            if k == "pe" and eng == "pe":
                continue
            if best.get(k, 0) < v:
                best[k] = v
        for k, v in best.items():
            if self.seen[eng].get(k, 0) >= v:
                continue
            self.seen[eng][k] = v
            out.append((k, v))
        return out

    def _deps(self, r, w):
        deps = []
        for b in r:
            deps.append(b.w)
            if b.excl:
                deps.extend(b.r)
        for b in w:
            deps.append(b.w)
            deps.extend(b.r)
        return deps

    def _commit(self, tok, r, w):
        for b in r:
            b.r.append(tok)
        for b in w:
            b.w = tok
            b.r = []
        self.last[tok[0]] = tok[1]

    def op(self, eng, fn, r=(), w=()):
        waits = self._waits(eng, self._deps(r, w))
        self.n[eng] += 1
        tok = (eng, self.n[eng])
        self.streams[eng].append((waits, fn, self.sems[eng], 1))
        self._commit(tok, r, w)

    def dma(self, q, fn, r=(), w=()):
        i = self.dn[q]
        self.dn[q] += 1
        R = self.RQ[q]
        key = (q, i % R)
        deps = self._deps(r, w)
        if i >= R:
            deps.append((key, 16 * (i // R)))
        waits = self._waits(q, deps)
        tok = (key, 16 * (i // R + 1))
        self.streams[q].append((waits, fn, self._sem(key), 16))
        self._commit(tok, r, w)

    def barrier(self):
        toks = list(self.last.items())
        for e in self.ENGS:
            waits = self._waits(e, [t for t in toks if not (t[0] == e)])
            if waits:
                self.streams[e].append((waits, None, None, 0))

    def breg(self, e, val):
        if val not in self._regs:
            self._regs[val] = e.to_reg(val)
        return self._regs[val]

    def emit(self):
        nc = self.nc
        streams = self.streams
        self._regs = {}
        self.streams = {e: [] for e in self.ENGS}

        def run(e, lst):
            for waits, fn, sem, inc in lst:
                for k, v in waits:
                    e.wait_ge(self._sem(k), v)
                if fn is not None:
                    try:
                        fn(e).then_inc(sem, inc)
                    except Exception:
                        print("EMIT FAIL at stream idx", lst.index((waits, fn, sem, inc)), "of", len(lst), flush=True)
                        raise

        with nc.Block() as block:
            @block.tensor
            def _(e):
                run(e, streams["pe"])

            @block.scalar
            def _(e):
                run(e, streams["act"])

            @block.vector
            def _(e):
                run(e, streams["dve"])

            @block.gpsimd
            def _(e):
                run(e, streams["pool"])

            @block.sync
            def _(e):
                run(e, streams["sp"])


class Ring:
    def __init__(self, nc, es, name, shape, dtype, n, psum=False):
        self.tiles = []
        for i in range(n):
            if psum:
                t = es.enter_context(nc.psum_tensor(f"{name}{i}", shape, dtype))
            else:
                t = es.enter_context(nc.sbuf_tensor(f"{name}{i}", shape, dtype))
            self.tiles.append((t, Buf(f"{name}{i}", excl=psum)))
        self.i = 0

    def next(self):
        t = self.tiles[self.i % len(self.tiles)]
        self.i += 1
        return t


def own_groups(G, half):
    return [g for g in range(G) if ((g % 4) in (0, 3)) == (half == 0)]


def build(cfg):
    S = cfg["S"]
    NE = cfg["NE"]
    CAP = cfg["CAP"]
    dbg = cfg.get("dbg", False)
    upto = cfg.get("upto", "Z")
    So = S // 2
    G = S // 512
    Go = G // 2
    NT = S // 128
    NTo = So // 128
    SG = min(2048, So)
    nc = bass.Bass("TRN2", target_bir_lowering=False)
    es = ExitStack()
    scratch_kind = "ExternalOutput" if dbg else "Internal"

    def din(name, shape, dt):
        return nc.dram_tensor(name, list(shape), dt, kind="ExternalInput").ap()

    def dscr(name, shape, dt):
        return nc.dram_tensor(name, list(shape), dt, kind=scratch_kind).ap()

    x_all = din("x_all", [S, D], F32)
    x_own = din("x_own", [So, D], F32)
    pos_all = din("pos_all", [1, S], I32)
    pos_own = din("pos_own", [1, So], I32)
    cvec = din("cvec", [128, KC], F32)
    badac = din("badac", [128, 6 * KC], F32)
    gcols = din("gcols", [128, 4 * KC], F32)
    b_ada = din("b_ada", [1, 6 * D], F32)
    g_post_mix = din("g_post_mix", [1, D], F32)
    g_post_ffn = din("g_post_ffn", [1, D], F32)
    w_ada = din("w_ada", [D, 6 * D], F32)
    w_in = din("w_in", [D, IN_W], F32)
    w_bra = din("w_branch_a", [A_H * HD, D], F32)
    w_brb = din("w_branch_b", [B_H * HD, D], F32)
    w_out = din("w_out", [D, D], F32)
    w_router = din("w_router", [D, NE], F32)
    b_router = din("b_router", [1, NE], F32)
    w1 = din("w1", [NE, D, 2 * D], F32)
    b1c = din("b1c", [NE, 128, 2 * KC], F32)
    w2 = din("w2", [NE, D, D], F32)
    b2 = din("b2", [NE, D], F32)
    cmask_d = din("cmask", [128, 2 * 8 * 512], BF16)
    amask_d = din("amask", [128, 2 * 640], F32)
    consts_d = din("consts", [128, 8 * 128], F32)
    out_d = nc.dram_tensor("out", [So, D], F32, kind="ExternalOutput").ap()

    KA = dscr("s_ka", [A_KV, 128, S], BF16)
    KB = dscr("s_kb", [B_H, 128, S], BF16)
    KI = dscr("s_ki", [IDX_D, S], BF16)
    VA = dscr("s_va", [S, A_KV * HD], BF16)
    VB = dscr("s_vb", [S, B_H * HD], BF16)
    QA = dscr("s_qa", [A_H, 128, So], BF16)
    QB = dscr("s_qb", [B_H, 128, So], BF16)
    QI = dscr("s_qi", [IDX_H // 2, 128, So], BF16)
    WI = dscr("s_wi", [So, IDX_H], F32)
    GA = dscr("s_ga", [KC, 128, So], BF16)
    GB = dscr("s_gb", [KC, 128, So], BF16)
    MK = dscr("s_mk", [NTo, 128, S], BF16)
    OA = dscr("s_oa", [A_H, 128, So], BF16)
    OB = dscr("s_ob", [B_H, 128, So], BF16)
    MG = dscr("s_mg", [KC, 128, So], BF16)
    X1 = dscr("s_x1", [So, D], F32)
    XG = dscr("s_xg", [NE * CAP, D], BF16)
    YG = dscr("s_yg", [NE * CAP, D], BF16)

    sch = Sched(nc, es)
    op, dma = sch.op, sch.dma
    dbg_outs = {}

    def finish():
        if dbg:
            d1 = nc.dram_tensor("dbg_modc", [128, 4 * KC], F32, kind="ExternalOutput").ap()
            d2 = nc.dram_tensor("dbg_ga", [128, 2 * D], F32, kind="ExternalOutput").ap()
            dma("sp", lambda e: e.dma_start(out=d1[:, :], in_=modc[:]), r=[b_modc])
            dma("sp", lambda e: e.dma_start(out=d2[:, 0:D], in_=ga1row[:]), r=[b_garow[0]])
            dma("sp", lambda e: e.dma_start(out=d2[:, D:2 * D], in_=ga2row[:]), r=[b_garow[1]])
            d3 = nc.dram_tensor("dbg_rslot", [128, NTo * 4], I32, kind="ExternalOutput").ap()
            d4 = nc.dram_tensor("dbg_rwgt", [128, NTo * 4], F32, kind="ExternalOutput").ap()
            dma("sp", lambda e: e.dma_start(out=d3[:, :], in_=rslot[:]), r=[b_route])
            dma("sp", lambda e: e.dma_start(out=d4[:, :], in_=rwgt[:]), r=[b_route])
        sch.barrier()
        sch.emit()
        es.close()
        return nc

    def sbp(name, shape, dt):
        return es.enter_context(nc.sbuf_tensor(name, shape, dt))

    consts = sbp("consts_s", [128, 8 * 128], F32)
    b_consts = Buf("consts")
    identb = sbp("identb", [128, 128], BF16)
    rtA = sbp("rtA", [128, 128], BF16)
    rtI = sbp("rtI", [128, 128], BF16)
    triI = sbp("triI", [128, 128], BF16)
    triS = sbp("triS", [128, 128], BF16)
    onesb = sbp("onesb", [128, 128], BF16)
    onesf = consts[:, 5 * 128:6 * 128]
    invfA = consts[:, 6 * 128:6 * 128 + 1]
    invfI = consts[:, 6 * 128 + 1:6 * 128 + 2]
    iota_f = consts[:, 7 * 128:8 * 128]
    b_cb = Buf("constsb")
    modc = sbp("modc", [128, 4 * KC], F32)
    b_modc = Buf("modc")
    ga1row = sbp("ga1row", [128, D], F32)
    ga2row = sbp("ga2row", [128, D], F32)
    b_garow = [Buf("ga1row"), Buf("ga2row")]
    rslot = sbp("rslot", [128, NTo * 4], I32)
    rwgt = sbp("rwgt", [128, NTo * 4], F32)
    b_route = Buf("route")

    dma("sp", lambda e: e.dma_start(out=consts[:], in_=consts_d[:, :]), w=[b_consts])
    for i, t in enumerate((identb, rtA, rtI, triI, triS, onesb)):
        op("dve", lambda e, i=i, t=t: e.tensor_copy(out=t[:], in_=consts[:, i * 128:(i + 1) * 128]),
           r=[b_consts], w=[b_cb])

    def mmgroup(out_ap, out_buf, pairs, rbufs):
        n = len(pairs)
        for i, (l, r_) in enumerate(pairs):
            rb = rbufs if (i == 0 or i == n - 1) else ()
            op("pe", lambda e, l=l, r_=r_, i=i: e.matmul(out_ap, l, r_, start=(i == 0), stop=(i == n - 1)),
               r=rb, w=[out_buf])

    with ExitStack() as ph:
        cc = ph.enter_context(nc.sbuf_tensor("cc", [128, KC], F32))
        sg_ = ph.enter_context(nc.sbuf_tensor("sg_", [128, KC], F32))
        siluc = ph.enter_context(nc.sbuf_tensor("siluc", [128, KC], F32))
        silurep = ph.enter_context(nc.sbuf_tensor("silurep", [128, KC, 128], F32))
        badac_s = ph.enter_context(nc.sbuf_tensor("badac_s", [128, 6 * KC], F32))
        gcols_s = ph.enter_context(nc.sbuf_tensor("gcols_s", [128, 4 * KC], F32))
        rawc = ph.enter_context(nc.sbuf_tensor("rawc", [128, 4 * KC], F32))
        b_small = Buf("small")
        b_rawc = Buf("rawc")
        wch = Ring(nc, ph, "wch", [128, KC, 512], F32, 2)
        rowt = Ring(nc, ph, "rowt", [128, 2, 512], F32, 2)
        tmpr = Ring(nc, ph, "tmpr", [128, 512], F32, 2)
        psA = Ring(nc, ph, "psA", [128, 512], F32, 2, psum=True)
        psC = ph.enter_context(nc.psum_tensor("psC", [128, 512], F32))
        b_psC = Buf("psC", excl=True)

        dma("sp", lambda e: e.dma_start(out=cc[:], in_=cvec[:, :]), w=[b_small])
        dma("sp", lambda e: e.dma_start(out=badac_s[:], in_=badac[:, :]), w=[b_small])
        dma("sp", lambda e: e.dma_start(out=gcols_s[:], in_=gcols[:, :]), w=[b_small])
        op("act", lambda e: e.activation(out=sg_[:], in_=cc[:], func=AF.Sigmoid), r=[b_small], w=[b_small])
        op("dve", lambda e: e.tensor_tensor(out=siluc[:], in0=cc[:], in1=sg_[:], op=ALU.mult), r=[b_small], w=[b_small])
        op("dve", lambda e: e.tensor_copy(out=silurep[:], in_=siluc[:].unsqueeze(2).broadcast_to([128, KC, 128])),
           r=[b_small], w=[b_small])
        w_ada_v = w_ada.rearrange("(kc p) n -> p kc n", p=128)
        for idx, which in enumerate((1, 0, 4, 3)):
            for cq in range(4):
                wt, wb = wch.next()
                c0 = which * D + cq * 512
                dma("sp", lambda e, wt=wt, c0=c0: e.dma_start(out=wt[:], in_=w_ada_v[:, :, c0:c0 + 512]), w=[wb])
                for j in range(4):
                    col = idx * KC + cq * 4 + j
                    mmgroup(psC[:, col:col + 1], b_psC,
                            [(wt[:, kc, j * 128:(j + 1) * 128], siluc[:, kc:kc + 1]) for kc in range(KC)],
                            [wb, b_small])
        op("dve", lambda e: e.tensor_copy(out=rawc[:], in_=psC[:, 0:4 * KC]), r=[b_psC], w=[b_rawc])
        for idx, which in enumerate((1, 0, 4, 3)):
            op("dve", lambda e, idx=idx, which=which: e.tensor_tensor(
                out=rawc[:, idx * KC:(idx + 1) * KC], in0=rawc[:, idx * KC:(idx + 1) * KC],
                in1=badac_s[:, which * KC:(which + 1) * KC], op=ALU.add), r=[b_small, b_rawc], w=[b_rawc])
        op("dve", lambda e: e.scalar_tensor_tensor(out=modc[:, 0:KC], in0=rawc[:, 0:KC], scalar=1.0,
                                                   in1=gcols_s[:, 0:KC], op0=ALU.add, op1=ALU.mult),
           r=[b_rawc, b_small], w=[b_modc])
        op("dve", lambda e: e.tensor_copy(out=modc[:, KC:2 * KC], in_=rawc[:, KC:2 * KC]), r=[b_rawc], w=[b_modc])
        op("dve", lambda e: e.scalar_tensor_tensor(out=modc[:, 2 * KC:3 * KC], in0=rawc[:, 2 * KC:3 * KC], scalar=1.0,
                                                   in1=gcols_s[:, 2 * KC:3 * KC], op0=ALU.add, op1=ALU.mult),
           r=[b_rawc, b_small], w=[b_modc])
        op("dve", lambda e: e.tensor_copy(out=modc[:, 3 * KC:4 * KC], in_=rawc[:, 3 * KC:4 * KC]), r=[b_rawc], w=[b_modc])
        for gi, (which, grow, gpost) in enumerate(((2, ga1row, g_post_mix), (5, ga2row, g_post_ffn))):
            for cq in range(4):
                wt, wb = wch.next()
                c0 = which * D + cq * 512
                dma("sp", lambda e, wt=wt, c0=c0: e.dma_start(out=wt[:], in_=w_ada_v[:, :, c0:c0 + 512]), w=[wb])
                rt, rb = rowt.next()
                dma("sp", lambda e, rt=rt, c0=c0: e.dma_start(out=rt[:, 0, :], in_=b_ada[0:1, c0:c0 + 512].partition_broadcast(128)), w=[rb])
                dma("sp", lambda e, rt=rt, cq=cq, gpost=gpost: e.dma_start(
                    out=rt[:, 1, :], in_=gpost[0:1, cq * 512:(cq + 1) * 512].partition_broadcast(128)), w=[rb])
                pt, pb = psA.next()
                mmgroup(pt[:], pb, [(silurep[:, kc, :], wt[:, kc, :]) for kc in range(KC)], [wb, b_small])
                tt, tb = tmpr.next()
                op("dve", lambda e, tt=tt, pt=pt, rt=rt: e.tensor_tensor(out=tt[:], in0=pt[:], in1=rt[:, 0, :], op=ALU.add),
                   r=[pb, rb], w=[tb])
                op("dve", lambda e, tt=tt, rt=rt, grow=grow, cq=cq: e.tensor_tensor(
                    out=grow[:, cq * 512:(cq + 1) * 512], in0=tt[:], in1=rt[:, 1, :], op=ALU.mult),
                   r=[tb, rb], w=[b_garow[gi]])
        sch.barrier()
        sch.emit()
    if upto == "A":
        return finish()

    w_in_v = w_in.rearrange("(kc p) n -> p kc n", p=128)
    TWO_PI = 2.0 * math.pi
    BL = cfg.get('blevel', 9)
    ROPE_ADD_ENG = cfg.get('rope_add', 'dve')
    CW1 = 6.28125
    CW2 = TWO_PI - CW1

    def norm_transpose(x_src, tok0, hT, b_hT, hcol0, sc_off, xt_r, xn_r, junk, b_junk, st_r, psT_r, alt):
        xt, xb = xt_r.next()
        dma("sp", lambda e: e.dma_start(out=xt[:], in_=x_src[tok0:tok0 + 128, :]), w=[xb])
        st, sb_ = st_r.next()
        op("act", lambda e: e.activation(out=junk[:], in_=xt[:], func=AF.Square, accum_out=st[:, 0:1]),
           r=[xb], w=[b_junk, sb_])
        op("act", lambda e: e.activation(out=st[:, 1:2], in_=st[:, 0:1], func=AF.Sqrt, scale=1.0 / D, bias=EPS),
           r=[sb_], w=[sb_])
        op("dve", lambda e: e.reciprocal(out=st[:, 2:3], in_=st[:, 1:2]), r=[sb_], w=[sb_])
        xn, xnb = xn_r.next()
        op("dve", lambda e: e.tensor_scalar(out=xn[:], in0=xt[:], scalar1=st[:, 2:3], scalar2=None, op0=ALU.mult),
           r=[xb, sb_], w=[xnb])
        for half in range(2):
            pt, pb = psT_r.next()
            for j in range(8):
                kc = half * 8 + j
                op("pe", lambda e, pt=pt, j=j, kc=kc: e.transpose(pt[:, j * 128:(j + 1) * 128],
                                                                   xn[:, kc * 128:(kc + 1) * 128], identb[:]),
                   r=[xnb, b_cb] if j in (0, 7) else (), w=[pb])
            for j in range(8):
                kc = half * 8 + j
                eng = "act" if (half + alt) % 2 == 0 else "dve"
                if eng == "act":
                    op("act", lambda e, pt=pt, j=j, kc=kc: e.activation(
                        out=hT[:, kc, hcol0:hcol0 + 128], in_=pt[:, j * 128:(j + 1) * 128], func=AF.Identity,
                        scale=modc[:, sc_off + kc:sc_off + kc + 1], bias=modc[:, sc_off + KC + kc:sc_off + KC + kc + 1]),
                       r=[pb, b_modc], w=[b_hT])
                else:
                    op("dve", lambda e, pt=pt, j=j, kc=kc: e.tensor_scalar(
                        out=hT[:, kc, hcol0:hcol0 + 128], in0=pt[:, j * 128:(j + 1) * 128],
                        scalar1=modc[:, sc_off + kc:sc_off + kc + 1], scalar2=modc[:, sc_off + KC + kc:sc_off + KC + kc + 1],
                        op0=ALU.mult, op1=ALU.add), r=[pb, b_modc], w=[b_hT])

    def proj_pass(pname, x_src, pos_src, T, fm_blocks, tm_blocks):
        with ExitStack() as ph:
            sg = min(SG, T)
            ngr = sg // 512
            hT = ph.enter_context(nc.sbuf_tensor(pname + "hT", [128, KC, sg], BF16))
            b_hT = Buf("hT")
            xt_r = Ring(nc, ph, pname + "xt", [128, D], F32, 2)
            xn_r = Ring(nc, ph, pname + "xn", [128, D], BF16, 1)
            junk = ph.enter_context(nc.sbuf_tensor(pname + "junk", [128, D], BF16))
            b_junk = Buf("junk")
            st_r = Ring(nc, ph, pname + "st", [128, 4], F32, 4)
            psT_r = Ring(nc, ph, pname + "psT", [128, 1024], BF16, 2, psum=True)
            psM_r = Ring(nc, ph, pname + "psM", [128, 512], F32, 4, psum=True)
            psR_r = Ring(nc, ph, pname + "psR", [128, 512], F32, 2, psum=True)
            wfm_r = Ring(nc, ph, pname + "wfm", [128, KC, 128], BF16, 3)
            wtm_r = Ring(nc, ph, pname + "wtm", [128, KC, 512], BF16, 1)
            zb_r = Ring(nc, ph, pname + "zb", [128, 512], BF16, 2)
            t1_r = Ring(nc, ph, pname + "t1", [128, 512], F32, 2)
            t2_r = Ring(nc, ph, pname + "t2", [128, 512], F32, 2)
            ob_r = Ring(nc, ph, pname + "ob", [128, 512], BF16, 3)
            obf_r = Ring(nc, ph, pname + "obf", [128, 512], F32, 2)
            posi = ph.enter_context(nc.sbuf_tensor(pname + "posi", [128, 512], I32))
            posf = ph.enter_context(nc.sbuf_tensor(pname + "posf", [128, 512], F32))
            ang = ph.enter_context(nc.sbuf_tensor(pname + "ang", [128, 512], F32))
            kfi = ph.enter_context(nc.sbuf_tensor(pname + "kfi", [128, 512], I32))
            kff = ph.enter_context(nc.sbuf_tensor(pname + "kff", [128, 512], F32))
            msk = ph.enter_context(nc.sbuf_tensor(pname + "msk", [128, 512], F32))
            b_tmp = Buf("ropetmp")
            tabs = ph.enter_context(nc.sbuf_tensor(pname + "tabs", [128, ngr, 4, 512], F32))
            b_tabs = Buf("tabs")
            for s0 in range(0, T, sg):
                for ti in range(sg // 128):
                    norm_transpose(x_src, s0 + ti * 128, hT, b_hT, ti * 128, 0, xt_r, xn_r, junk, b_junk, st_r, psT_r, ti)
                for g in range(ngr if BL >= 2 else 0):
                    t0 = s0 + g * 512
                    dma("sp", lambda e, t0=t0: e.dma_start(out=posi[:], in_=pos_src[0:1, t0:t0 + 512].partition_broadcast(128)),
                        w=[b_tmp])
                    op("dve", lambda e: e.tensor_copy(out=posf[:], in_=posi[:]), r=[b_tmp], w=[b_tmp])
                    for ti_, (invf, shift) in enumerate(((invfA, math.pi / 2), (invfA, 0.0), (invfI, math.pi / 2), (invfI, 0.0))):
                        R_, W_ = [b_tmp, b_consts], [b_tmp]
                        op("dve", lambda e, invf=invf, shift=shift: e.tensor_scalar(
                            out=ang[:], in0=posf[:], scalar1=invf, scalar2=shift, op0=ALU.mult, op1=ALU.add), r=R_, w=W_)
                        op("dve", lambda e: e.tensor_scalar(out=kfi[:], in0=ang[:], scalar1=1.0 / TWO_PI, scalar2=None,
                                                            op0=ALU.mult), r=R_, w=W_)
                        op("dve", lambda e: e.tensor_copy(out=kff[:], in_=kfi[:]), r=R_, w=W_)
                        op("dve", lambda e: e.scalar_tensor_tensor(out=ang[:], in0=kff[:], scalar=-CW1, in1=ang[:],
                                                                   op0=ALU.mult, op1=ALU.add), r=R_, w=W_)
                        op("dve", lambda e: e.scalar_tensor_tensor(out=ang[:], in0=kff[:], scalar=-CW2, in1=ang[:],
                                                                   op0=ALU.mult, op1=ALU.add), r=R_, w=W_)
                        op("dve", lambda e: e.tensor_scalar(out=msk[:], in0=ang[:], scalar1=math.pi, scalar2=None,
                                                            op0=ALU.is_gt), r=R_, w=W_)
                        op("dve", lambda e: e.scalar_tensor_tensor(out=ang[:], in0=msk[:], scalar=-TWO_PI, in1=ang[:],
                                                                   op0=ALU.mult, op1=ALU.add), r=R_, w=W_)
                        op("dve", lambda e: e.tensor_scalar(out=msk[:], in0=ang[:], scalar1=-math.pi, scalar2=None,
                                                            op0=ALU.is_lt), r=R_, w=W_)
                        op("dve", lambda e: e.scalar_tensor_tensor(out=ang[:], in0=msk[:], scalar=TWO_PI, in1=ang[:],
                                                                   op0=ALU.mult, op1=ALU.add), r=R_, w=W_)
                        op("dve", lambda e: e.tensor_scalar(out=ang[:], in0=ang[:], scalar1=math.pi, scalar2=-math.pi,
                                                            op0=ALU.min, op1=ALU.max), r=R_, w=W_)
                        op("act", lambda e, g=g, ti_=ti_: e.activation(out=tabs[:, g, ti_, :], in_=ang[:], func=AF.Sin),
                           r=[b_tmp], w=[b_tabs, b_tmp])
                for (col0, ncol, dst, rope) in fm_blocks:
                    if BL < 3 or (BL < 4 and rope is not None):
                        continue
                    if rope is not None and cfg.get('ropeonly') and (rope, ncol) != cfg.get('ropeonly'):
                        continue
                    wt, wb = wfm_r.next()
                    dma("pool", lambda e, wt=wt, col0=col0, ncol=ncol: e.dma_start(
                        out=wt[:, :, 0:ncol], in_=w_in_v[:, :, col0:col0 + ncol]), w=[wb])
                    for g in range(ngr):
                        t0 = s0 + g * 512
                        pt, pb = psM_r.next()
                        mmgroup(pt[0:ncol, :], pb,
                                [(wt[:, kc, 0:ncol], hT[:, kc, g * 512:(g + 1) * 512]) for kc in range(KC)], [wb, b_hT])
                        ot, obb = ob_r.next()
                        if rope is None:
                            op("act", lambda e, ot=ot, pt=pt, ncol=ncol: e.activation(out=ot[0:ncol, :], in_=pt[0:ncol, :], func=AF.Copy),
                               r=[pb], w=[obb])
                        else:
                            ci = 0 if rope == "A" else 2
                            rt = rtA if rope == "A" else rtI
                            zt, zbb = zb_r.next()
                            op("act", lambda e, zt=zt, pt=pt, ncol=ncol: e.activation(out=zt[0:ncol, :], in_=pt[0:ncol, :], func=AF.Copy),
                               r=[pb], w=[zbb])
                            p2, p2b = psR_r.next()
                            if cfg.get('ropevar', 0) != 1:
                                op("pe", lambda e, p2=p2, zt=zt, rt=rt, ncol=ncol: e.matmul(
                                    p2[0:ncol, :], rt[0:ncol, 0:ncol], zt[0:ncol, :], start=True, stop=True),
                                   r=[zbb, b_cb], w=[p2b])
                            else:
                                p2, p2b = pt, pb
                            a1, a1b = t1_r.next()
                            a2, a2b = t2_r.next()
                            if cfg.get('ropevar', 0) == 2:
                                op("act", lambda e, ot=ot, pt=pt, ncol=ncol: e.activation(out=ot[0:ncol, :], in_=pt[0:ncol, :], func=AF.Copy),
                                   r=[pb], w=[obb])
                                dma("sp", lambda e, ot=ot, dst=dst, t0=t0, ncol=ncol: e.dma_start(
                                    out=dst[0:ncol, t0:t0 + 512], in_=ot[0:ncol, :]), r=[obb])
                                continue
                            if cfg.get('ropevar', 0) == 3:
                                op("dve", lambda e, ot=ot, pt=pt, g=g, ci=ci, ncol=ncol: e.tensor_tensor(
                                    out=ot[0:ncol, :], in0=pt[0:ncol, :], in1=tabs[0:ncol, g, ci, :], op=ALU.mult),
                                   r=[pb, b_tabs], w=[obb])
                                dma("sp", lambda e, ot=ot, dst=dst, t0=t0, ncol=ncol: e.dma_start(
                                    out=dst[0:ncol, t0:t0 + 512], in_=ot[0:ncol, :]), r=[obb])
                                continue
                            op("dve", lambda e, a1=a1, pt=pt, g=g, ci=ci, ncol=ncol: e.tensor_tensor(
                                out=a1[0:ncol, :], in0=pt[0:ncol, :], in1=tabs[0:ncol, g, ci, :], op=ALU.mult),
                               r=[pb, b_tabs], w=[a1b])
                            op("dve", lambda e, a2=a2, p2=p2, g=g, ci=ci, ncol=ncol: e.tensor_tensor(
                                out=a2[0:ncol, :], in0=p2[0:ncol, :], in1=tabs[0:ncol, g, ci + 1, :], op=ALU.mult),
                               r=[p2b, b_tabs], w=[a2b])
                            op(ROPE_ADD_ENG, lambda e, ot=ot, a1=a1, a2=a2, ncol=ncol: e.tensor_tensor(
                                out=ot[0:ncol, :], in0=a1[0:ncol, :], in1=a2[0:ncol, :], op=ALU.add),
                               r=[a1b, a2b], w=[obb])
                        dma("sp", lambda e, ot=ot, dst=dst, t0=t0, ncol=ncol: e.dma_start(
                            out=dst[0:ncol, t0:t0 + 512], in_=ot[0:ncol, :]), r=[obb])
                for (col0, ncol, dst, dcol0, isf32) in tm_blocks:
                    if BL < 5:
                        continue
                    wt, wb = wtm_r.next()
                    dma("pool", lambda e, wt=wt, col0=col0, ncol=ncol: e.dma_start(
                        out=wt[:, :, 0:ncol], in_=w_in_v[:, :, col0:col0 + ncol]), w=[wb])
                    for ti in range(sg // 128):
                        t0 = s0 + ti * 128
                        pt, pb = psM_r.next()
                        mmgroup(pt[:, 0:ncol], pb,
                                [(hT[:, kc, ti * 128:(ti + 1) * 128], wt[:, kc, 0:ncol]) for kc in range(KC)], [wb, b_hT])
                        ot, obb = (obf_r if isf32 else ob_r).next()
                        op("act", lambda e, ot=ot, pt=pt, ncol=ncol: e.activation(out=ot[:, 0:ncol], in_=pt[:, 0:ncol], func=AF.Copy),
                           r=[pb], w=[obb])
                        dma("sp", lambda e, ot=ot, dst=dst, t0=t0, ncol=ncol, dcol0=dcol0: e.dma_start(
                            out=dst[t0:t0 + 128, dcol0:dcol0 + ncol], in_=ot[:, 0:ncol]), r=[obb])
            sch.barrier()
            sch.emit()

    fm1 = [(C_KA + j * 128, 128, KA[j], "A") for j in range(A_KV)]
    fm1 += [(C_KB + h * 128, 128, KB[h], None) for h in range(B_H)]
    fm1 += [(C_KI, 64, KI, "I")]
    tm1 = [(C_VA, 256, VA, 0, False), (C_VB, 512, VB, 0, False), (C_VB + 512, 512, VB, 512, False)]
    proj_pass("p1", x_all, pos_all, S, fm1, tm1)
    fm2 = [(C_QA + h * 128, 128, QA[h], "A") for h in range(A_H)]
    fm2 += [(C_QB + h * 128, 128, QB[h], None) for h in range(B_H)]
    fm2 += [(C_QI + h * 128, 128, QI[h], "I") for h in range(IDX_H // 2)]
    fm2 += [(C_GA + f * 128, 128, GA[f], None) for f in range(KC)]
    fm2 += [(C_GB + f * 128, 128, GB[f], None) for f in range(KC)]
    tm2 = [(C_WI, IDX_H, WI, 0, True)]
    proj_pass("p2", x_own, pos_own, So, fm2, tm2)
    if upto == "B":
        return finish()

    def pipeline(items, skews):
        if isinstance(skews, int):
            skews = [0, skews]
        n = len(items)
        for idx in range(n + max(skews)):
            for k, sk in enumerate(skews):
                if 0 <= idx - sk < n:
                    items[idx - sk][k]()

    def qblocks():
        for i in range(Go):
            for jj in range(4):
                qb = i * 4 + jj
                yield i, jj, qb, qb * 128, 128 * (4 * (2 * i + 1) + jj + 1), i % 2

    with ExitStack() as ph:
        kiT = ph.enter_context(nc.sbuf_tensor("kiT", [64, S], BF16))
        b_ki = Buf("kiT")
        amask_s = ph.enter_context(nc.sbuf_tensor("amask_s", [128, 2, 640], F32))
        b_am = Buf("amask")
        Ssc = ph.enter_context(nc.sbuf_tensor("Ssc", [128, S], F32))
        b_S = Buf("S")
        Mk = ph.enter_context(nc.sbuf_tensor("Mk", [128, S], BF16))
        b_M = Buf("Mk")
        qi_r = Ring(nc, ph, "qiT", [64, IDX_H, 128], BF16, 2)
        wi_r = Ring(nc, ph, "wit", [128, 3, IDX_H], F32, 2)
        rr_r = Ring(nc, ph, "rr", [128, 512], F32, 3)
        ps_r = Ring(nc, ph, "psI", [128, 512], F32, 4, psum=True)
        bs = ph.enter_context(nc.sbuf_tensor("bs", [128, 8], F32))
        b_bs = Buf("bs")
        dma("sp", lambda e: e.dma_start(out=kiT[:], in_=KI[:, :]), w=[b_ki])
        dma("sp", lambda e: e.dma_start(out=amask_s[:].rearrange("p m c -> p (m c)"), in_=amask_d[:, :]), w=[b_am])
        for (i, jj, qb, tok0, nk, m) in qblocks():
            qt, qbb = qi_r.next()
            for hh in range(2):
                dma("sp", lambda e, qt=qt, hh=hh, tok0=tok0: e.dma_start(
                    out=qt[:, hh::2, :], in_=QI[:, hh * 64:(hh + 1) * 64, tok0:tok0 + 128].rearrange("hp d t -> d hp t")),
                    w=[qbb])
            wt, wbb = wi_r.next()
            dma("sp", lambda e, wt=wt, tok0=tok0: e.dma_start(out=wt[:, 0, :], in_=WI[tok0:tok0 + 128, :]), w=[wbb])
            op("act", lambda e, wt=wt: e.activation(out=wt[:, 1, :], in_=wt[:, 0, :], func=AF.Abs), r=[wbb], w=[wbb])
            op("act", lambda e, wt=wt: e.activation(out=wt[:, 2, :], in_=wt[:, 0, :], func=AF.Sign), r=[wbb], w=[wbb])
            nch = (nk + 511) // 512
            for c in range(nch):
                c0 = c * 512
                cw = min(512, nk - c0)
                for h in range(IDX_H):
                    pt, pb = ps_r.next()
                    op("pe", lambda e, pt=pt, qt=qt, h=h, c0=c0, cw=cw: e.matmul(
                        pt[:, 0:cw], qt[:, h, :], kiT[:, c0:c0 + cw], start=True, stop=True),
                       r=[qbb, b_ki], w=[pb])
                    rt, rb = rr_r.next()
                    op("act", lambda e, rt=rt, pt=pt, wt=wt, h=h, cw=cw: e.activation(
                        out=rt[:, 0:cw], in_=pt[:, 0:cw], func=AF.Relu, scale=wt[:, 1, h:h + 1]),
                       r=[pb, wbb], w=[rb])
                    if h == 0:
                        op("dve", lambda e, rt=rt, wt=wt, h=h, c0=c0, cw=cw: e.tensor_scalar(
                            out=Ssc[:, c0:c0 + cw], in0=rt[:, 0:cw], scalar1=wt[:, 2, h:h + 1], scalar2=None, op0=ALU.mult),
                           r=[rb, wbb], w=[b_S])
                    else:
                        op("dve", lambda e, rt=rt, wt=wt, h=h, c0=c0, cw=cw: e.scalar_tensor_tensor(
                            out=Ssc[:, c0:c0 + cw], in0=rt[:, 0:cw], scalar=wt[:, 2, h:h + 1], in1=Ssc[:, c0:c0 + cw],
                            op0=ALU.mult, op1=ALU.add), r=[rb, wbb, b_S], w=[b_S])
            op("dve", lambda e, nk=nk: e.tensor_reduce(out=bs[:, 5:6], in_=Ssc[:, 0:nk], axis=AX.X, op=ALU.max),
               r=[b_S], w=[b_bs])
            op("dve", lambda e, nk=nk: e.tensor_reduce(out=bs[:, 0:1], in_=Ssc[:, 0:nk], axis=AX.X, op=ALU.min),
               r=[b_S], w=[b_bs])
            op("dve", lambda e: e.tensor_tensor(out=bs[:, 1:2], in0=bs[:, 5:6], in1=bs[:, 0:1], op=ALU.subtract),
               r=[b_bs], w=[b_bs])
            op("dve", lambda e, nk=nk, m=m: e.tensor_tensor(out=Ssc[:, nk - 640:nk], in0=Ssc[:, nk - 640:nk],
                                                             in1=amask_s[:, m, :], op=ALU.add),
               r=[b_S, b_am], w=[b_S])
            for it in range(NBIS):
                op("dve", lambda e: e.tensor_scalar(out=bs[:, 1:2], in0=bs[:, 1:2], scalar1=0.5, scalar2=None, op0=ALU.mult),
                   r=[b_bs], w=[b_bs])
                op("dve", lambda e: e.tensor_tensor(out=bs[:, 2:3], in0=bs[:, 0:1], in1=bs[:, 1:2], op=ALU.add),
                   r=[b_bs], w=[b_bs])
                op("dve", lambda e, nk=nk: e.tensor_scalar(out=Mk[:, 0:nk], in0=Ssc[:, 0:nk], scalar1=bs[:, 2:3], scalar2=None,
                                                           op0=ALU.is_ge, op1=ALU.add, accum_out=bs[:, 3:4]),
                   r=[b_bs, b_S, b_M], w=[b_M, b_bs])
                op("dve", lambda e: e.tensor_scalar(out=bs[:, 4:5], in0=bs[:, 3:4], scalar1=float(TOPK), scalar2=bs[:, 1:2],
                                                    op0=ALU.is_ge, op1=ALU.mult), r=[b_bs], w=[b_bs])
                op("dve", lambda e: e.tensor_tensor(out=bs[:, 0:1], in0=bs[:, 0:1], in1=bs[:, 4:5], op=ALU.add),
                   r=[b_bs], w=[b_bs])
            op("dve", lambda e, nk=nk: e.tensor_scalar(out=Mk[:, 0:nk], in0=Ssc[:, 0:nk], scalar1=bs[:, 0:1], scalar2=None,
                                                       op0=ALU.is_ge), r=[b_bs, b_S, b_M], w=[b_M])
            dma("sp", lambda e, qb=qb, nk=nk: e.dma_start(out=MK[qb, :, 0:nk], in_=Mk[:, 0:nk]), r=[b_M])
        sch.barrier()
        sch.emit()
    if upto == "C1":
        return finish()

    with ExitStack() as ph:
        kaT = ph.enter_context(nc.sbuf_tensor("kaT", [128, A_KV, S], BF16))
        va = ph.enter_context(nc.sbuf_tensor("va_s", [128, NT, A_KV * HD], BF16))
        b_kv = Buf("kv")
        mk_r = Ring(nc, ph, "mk", [128, S], BF16, 2)
        mt_r = Ring(nc, ph, "mt", [128, NT, 128], BF16, 2)
        qa_r = Ring(nc, ph, "qa", [128, A_H * 128], BF16, 2)
        e_r = Ring(nc, ph, "ee", [128, 512], BF16, 3)
        p_r = Ring(nc, ph, "pp", [128, 512], BF16, 4)
        rd_r = Ring(nc, ph, "rd", [128, 512], F32, 2)
        of_r = Ring(nc, ph, "of", [128, 512], F32, 2)
        ob_r = Ring(nc, ph, "obc", [128, 512], BF16, 2)
        psS = Ring(nc, ph, "psS", [128, 512], F32, 3, psum=True)
        psO = Ring(nc, ph, "psO", [128, 512], F32, 2, psum=True)
        psD = Ring(nc, ph, "psD", [128, 512], F32, 2, psum=True)
        psT = Ring(nc, ph, "psTc", [128, 1024], BF16, 1, psum=True)
        for j in range(A_KV):
            dma("sp", lambda e, j=j: e.dma_start(out=kaT[:, j, :], in_=KA[j]), w=[b_kv])
        dma("sp", lambda e: e.dma_start(out=va[:], in_=VA.rearrange("(kb p) c -> p kb c", p=128)), w=[b_kv])
        isq = 1.0 / math.sqrt(HD)
        items = []
        for (i, jj, qb, tok0, nk, m) in qblocks():
            nkb = nk // 128
            qst = {}

            def setup(qst=qst, qb=qb, tok0=tok0, nk=nk, nkb=nkb):
                mkt, mkb = mk_r.next()
                dma("sp", lambda e: e.dma_start(out=mkt[:, 0:nk], in_=MK[qb, :, 0:nk]), w=[mkb])
                qt, qbb = qa_r.next()
                dma("sp", lambda e: e.dma_start(
                    out=qt[:].rearrange("p (h t) -> p h t", h=A_H), in_=QA[:, :, tok0:tok0 + 128].rearrange("h d t -> d h t")),
                    w=[qbb])
                mtt, mtb = mt_r.next()
                for k0 in range(0, nkb, 8):
                    n8 = min(8, nkb - k0)
                    pt, pb = psT.next()
                    for u in range(n8):
                        op("pe", lambda e, pt=pt, u=u, k0=k0: e.transpose(
                            pt[:, u * 128:(u + 1) * 128], mkt[:, (k0 + u) * 128:(k0 + u + 1) * 128], identb[:]),
                           r=[mkb, b_cb] if u in (0, n8 - 1) else (), w=[pb])
                    op("act", lambda e, pt=pt, k0=k0, n8=n8: e.activation(
                        out=mtt[:, k0:k0 + n8, :].rearrange("p k t -> p (k t)"), in_=pt[:, 0:n8 * 128], func=AF.Copy),
                       r=[pb], w=[mtb])
                qst.update(qt=qt, qbb=qbb, mtt=mtt, mtb=mtb)

            for j in range(A_KV):
                jst = {}
                for kb in range(nkb):
                    it = {}

                    def stA(it=it, qst=qst, jst=jst, j=j, kb=kb, first=(j == 0 and kb == 0), setup=setup):
                        if first:
                            setup()
                        if kb == 0:
                            jst["po"], jst["pob"] = psO.next()
                            jst["pd"], jst["pdb"] = psD.next()
                        qt, qbb, mtt, mtb = qst["qt"], qst["qbb"], qst["mtt"], qst["mtb"]
                        ps, psb = psS.next()
                        op("pe", lambda e: e.matmul(ps[:], kaT[:, j, kb * 128:(kb + 1) * 128], qt[:, j * 512:(j + 1) * 512],
                                                    start=True, stop=True), r=[b_kv, qbb], w=[psb])
                        et, eb = e_r.next()
                        op("act", lambda e: e.activation(out=et[:], in_=ps[:], func=AF.Exp, scale=isq), r=[psb], w=[eb])
                        pp, ppb = p_r.next()
                        op("dve", lambda e: e.tensor_tensor(
                            out=pp[:].rearrange("p (g t) -> p g t", g=4), in0=et[:].rearrange("p (g t) -> p g t", g=4),
                            in1=mtt[:, kb, :].unsqueeze(1).broadcast_to([128, 4, 128]), op=ALU.mult), r=[eb, mtb], w=[ppb])
                        it["pp"], it["ppb"] = pp, ppb

                    def stB(it=it, jst=jst, j=j, kb=kb, nkb=nkb, tok0=tok0):
                        pp, ppb = it["pp"], it["ppb"]
                        po, pob, pd, pdb = jst["po"], jst["pob"], jst["pd"], jst["pdb"]
                        op("pe", lambda e: e.matmul(po[:], va[:, kb, j * 128:(j + 1) * 128], pp[:],
                                                    start=(kb == 0), stop=(kb == nkb - 1)), r=[ppb, b_kv], w=[pob])
                        op("pe", lambda e: e.matmul(pd[:], onesb[:], pp[:], start=(kb == 0), stop=(kb == nkb - 1)),
                           r=[ppb, b_cb], w=[pdb])
                        if kb == nkb - 1:
                            rd, rdb = rd_r.next()
                            op("dve", lambda e: e.reciprocal(out=rd[:], in_=pd[:]), r=[pdb], w=[rdb])
                            of_, ofb = of_r.next()
                            op("dve", lambda e: e.tensor_tensor(out=of_[:], in0=po[:], in1=rd[:], op=ALU.mult),
                               r=[pob, rdb], w=[ofb])
                            obt, obb = ob_r.next()
                            op("act", lambda e: e.activation(out=obt[:], in_=of_[:], func=AF.Copy), r=[ofb], w=[obb])
                            dma("sp", lambda e: e.dma_start(
                                out=OA[4 * j:4 * j + 4, :, tok0:tok0 + 128].rearrange("h d t -> d h t"),
                                in_=obt[:].rearrange("p (h t) -> p h t", h=4)), r=[obb])

                    items.append((stA, stB))
        pipeline(items, 2)
        sch.barrier()
        sch.emit()
    if upto == "C2":
        return finish()

    with ExitStack() as ph:
        cmask_s = ph.enter_context(nc.sbuf_tensor("cmask_s", [128, 2, 8, 512], BF16))
        b_cm = Buf("cmask")
        dma("sp", lambda e: e.dma_start(out=cmask_s[:].rearrange("p m j t -> p (m j t)"), in_=cmask_d[:, :]), w=[b_cm])
        kb_r = Ring(nc, ph, "kbT", [128, S], BF16, 2)
        vb_r = Ring(nc, ph, "vbh", [128, NT, 128], BF16, 2)
        qb_r = Ring(nc, ph, "qbT", [128, So], BF16, 2)
        E_r = Ring(nc, ph, "sbE", [128, 512], F32, 4)
        Em_r = Ring(nc, ph, "sbEm", [128, 512], F32, 4)
        SP_r = Ring(nc, ph, "sbSP", [128, 512], BF16, 4)
        SPm_r = Ring(nc, ph, "sbSPm", [128, 512], BF16, 4)
        X_r = Ring(nc, ph, "sbX", [128, 512], F32, 3)
        At_r = Ring(nc, ph, "sbAt", [128, 512], BF16, 4)
        Ra_r = Ring(nc, ph, "sbRa", [128, 512], F32, 2)
        obD_r = Ring(nc, ph, "obD", [128, 512], BF16, 2)
        psZ = Ring(nc, ph, "psZ", [128, 512], F32, 3, psum=True)
        psCc = Ring(nc, ph, "psCc", [128, 512], F32, 3, psum=True)
        psOd = Ring(nc, ph, "psOd", [128, 512], F32, 2, psum=True)
        isq = 1.0 / math.sqrt(HD)
        items = []
        for h in range(B_H):
            hst = {}

            def hsetup(hst=hst, h=h):
                kt, ktb = kb_r.next()
                vt, vtb = vb_r.next()
                qt, qtb = qb_r.next()
                dma("sp", lambda e: e.dma_start(out=kt[:], in_=KB[h]), w=[ktb])
                dma("sp", lambda e: e.dma_start(
                    out=vt[:], in_=VB[:, h * 128:(h + 1) * 128].rearrange("(kb p) d -> p kb d", p=128)), w=[vtb])
                dma("sp", lambda e: e.dma_start(out=qt[:], in_=QB[h]), w=[qtb])
                hst.update(kt=kt, ktb=ktb, vt=vt, vtb=vtb, qt=qt, qtb=qtb)

            for i in range(Go):
                nkb = 4 * (2 * i + 2)
                m = i % 2
                gst = {}
                for n, kb in enumerate(reversed(range(nkb))):
                    j = kb - (nkb - 8)
                    it = {}

                    def stA(it=it, hst=hst, gst=gst, i=i, n=n, kb=kb, j=j, m=m, first=(i == 0 and n == 0), hsetup=hsetup):
                        if first:
                            hsetup()
                        if n == 0:
                            gst["ra"], gst["rab"] = Ra_r.next()
                            gst["po"], gst["pob"] = psOd.next()
                        kt, ktb, qt, qtb = hst["kt"], hst["ktb"], hst["qt"], hst["qtb"]
                        pz, pzb = psZ.next()
                        op("pe", lambda e: e.matmul(pz[:], kt[:, kb * 128:(kb + 1) * 128], qt[:, i * 512:(i + 1) * 512],
                                                    start=True, stop=True), r=[ktb, qtb], w=[pzb])
                        Et, Eb = E_r.next()
                        op("act", lambda e: e.activation(out=Et[:], in_=pz[:], func=AF.Exp, scale=isq), r=[pzb], w=[Eb])
                        St, Sb = SP_r.next()
                        op("act", lambda e: e.activation(out=St[:], in_=Et[:], func=AF.Ln, bias=1.0), r=[Eb], w=[Sb])
                        if j >= 0:
                            Sm, Smb = SPm_r.next()
                            op("dve", lambda e: e.tensor_tensor(out=Sm[:], in0=St[:], in1=cmask_s[:, m, j, :], op=ALU.mult),
                               r=[Sb, b_cm], w=[Smb])
                            Em, Emb = Em_r.next()
                            op("pool", lambda e: e.tensor_tensor(out=Em[:], in0=Et[:], in1=cmask_s[:, m, j, :], op=ALU.mult),
                               r=[Eb, b_cm], w=[Emb])
                        else:
                            Sm, Smb = St, Sb
                            Em, Emb = Et, Eb
                        it.update(Sm=Sm, Smb=Smb, Em=Em, Emb=Emb)

                    def stB1(it=it, gst=gst, n=n, nkb=nkb):
                        Sm, Smb = it["Sm"], it["Smb"]
                        ra, rab = gst["ra"], gst["rab"]
                        pc, pcb = psCc.next()
                        op("pe", lambda e: e.matmul(pc[:], triI[:], Sm[:], start=True, stop=(n == 0)), r=[Smb, b_cb], w=[pcb])
                        if n > 0:
                            op("pe", lambda e: e.matmul(pc[:], onesf, ra[:], start=False, stop=True), r=[rab, b_consts], w=[pcb])
                        if n == 0:
                            op("pool", lambda e: e.tensor_copy(out=ra[:], in_=Sm[:]), r=[Smb], w=[rab])
                        elif n < nkb - 1:
                            op("pool", lambda e: e.tensor_tensor(out=ra[:], in0=ra[:], in1=Sm[:], op=ALU.add), r=[Smb, rab], w=[rab])
                        it.update(pc=pc, pcb=pcb)

                    def stB2(it=it):
                        Em, Emb, pc, pcb = it["Em"], it["Emb"], it["pc"], it["pcb"]
                        Xt, Xb = X_r.next()
                        op("act", lambda e: e.activation(out=Xt[:], in_=pc[:], func=AF.Exp, scale=-1.0), r=[pcb], w=[Xb])
                        At, Ab = At_r.next()
                        op("dve", lambda e: e.tensor_tensor(out=At[:], in0=Em[:], in1=Xt[:], op=ALU.mult), r=[Emb, Xb], w=[Ab])
                        it.update(At=At, Ab=Ab)

                    def stB3(it=it, hst=hst, gst=gst, h=h, i=i, n=n, kb=kb, nkb=nkb):
                        At, Ab = it["At"], it["Ab"]
                        po, pob = gst["po"], gst["pob"]
                        vt, vtb = hst["vt"], hst["vtb"]
                        op("pe", lambda e: e.matmul(po[:], vt[:, kb, :], At[:], start=(n == 0), stop=(n == nkb - 1)),
                           r=[vtb, Ab], w=[pob])
                        if n == nkb - 1:
                            ot, otb = obD_r.next()
                            op("act", lambda e: e.activation(out=ot[:], in_=po[:], func=AF.Copy), r=[pob], w=[otb])
                            dma("sp", lambda e: e.dma_start(out=OB[h, :, i * 512:(i + 1) * 512], in_=ot[:]), r=[otb])

                    items.append((stA, stB1, stB2, stB3))
        pipeline(items, [0, 2, 3, 5])
        sch.barrier()
        sch.emit()
    if upto == "D":
        return finish()

    with ExitStack() as ph:
        wa = ph.enter_context(nc.sbuf_tensor("wa", [128, 8, D], BF16))
        wb_ = ph.enter_context(nc.sbuf_tensor("wb_", [128, 8, D], BF16))
        b_wab = Buf("wab")
        for (wt, src) in ((wa, w_bra), (wb_, w_brb)):
            sv = src.rearrange("(kc p) n -> p kc n", p=128)
            for cq in range(4):
                dma("pool", lambda e, wt=wt, sv=sv, cq=cq: e.dma_start(
                    out=wt[:, :, cq * 512:(cq + 1) * 512], in_=sv[:, :, cq * 512:(cq + 1) * 512]), w=[b_wab])
        zt = ph.enter_context(nc.sbuf_tensor("zt", [128, 4096], BF16))
        b_zt = Buf("zt")
        op("dve", lambda e: e.memset(zt[:], 0.0), w=[b_zt])
        XGf = XG.rearrange("(r p two) c -> r p (two c)", p=128, two=2)
        for r_ in range(NE * CAP // 256):
            dma("sp", lambda e, r_=r_: e.dma_start(out=XGf[r_], in_=zt[:]), r=[b_zt])
        oa_r = Ring(nc, ph, "oaT", [128, 8, 512], BF16, 2)
        ob2_r = Ring(nc, ph, "obT", [128, 8, 512], BF16, 2)
        g_r = Ring(nc, ph, "gt", [128, 2, 512], BF16, 3)
        sg_r = Ring(nc, ph, "sgt", [128, 2, 512], F32, 2)
        t1e_r = Ring(nc, ph, "t1e", [128, 512], F32, 2)
        t2e_r = Ring(nc, ph, "t2e", [128, 512], F32, 2)
        mg_r = Ring(nc, ph, "mgo", [128, 512], BF16, 3)
        psa_r = Ring(nc, ph, "psEa", [128, 512], F32, 3, psum=True)
        psb_r = Ring(nc, ph, "psEb", [128, 512], F32, 3, psum=True)
        for tg in range(So // 512):
            t0 = tg * 512
            oat, oab = oa_r.next()
            obt, obb = ob2_r.next()
            dma("sp", lambda e, oat=oat, t0=t0: e.dma_start(out=oat[:], in_=OA[:, :, t0:t0 + 512].rearrange("h d t -> d h t")), w=[oab])
            dma("sp", lambda e, obt=obt, t0=t0: e.dma_start(out=obt[:], in_=OB[:, :, t0:t0 + 512].rearrange("h d t -> d h t")), w=[obb])
            for fo in range(KC):
                gt, gb_ = g_r.next()
                dma("sp", lambda e, gt=gt, fo=fo, t0=t0: e.dma_start(out=gt[:, 0, :], in_=GA[fo, :, t0:t0 + 512]), w=[gb_])
                dma("sp", lambda e, gt=gt, fo=fo, t0=t0: e.dma_start(out=gt[:, 1, :], in_=GB[fo, :, t0:t0 + 512]), w=[gb_])
                st_, sb_ = sg_r.next()
                op("act", lambda e, st_=st_, gt=gt: e.activation(out=st_[:], in_=gt[:], func=AF.Sigmoid), r=[gb_], w=[sb_])
                pa, pab = psa_r.next()
                mmgroup(pa[:], pab, [(wa[:, kc, fo * 128:(fo + 1) * 128], oat[:, kc, :]) for kc in range(8)], [b_wab, oab])
                pb2, pbb = psb_r.next()
                mmgroup(pb2[:], pbb, [(wb_[:, kc, fo * 128:(fo + 1) * 128], obt[:, kc, :]) for kc in range(8)], [b_wab, obb])
                a1, a1b = t1e_r.next()
                a2, a2b = t2e_r.next()
                op("dve", lambda e, a1=a1, pa=pa, st_=st_: e.tensor_tensor(out=a1[:], in0=pa[:], in1=st_[:, 0, :], op=ALU.mult),
                   r=[pab, sb_], w=[a1b])
                op("dve", lambda e, a2=a2, pb2=pb2, st_=st_: e.tensor_tensor(out=a2[:], in0=pb2[:], in1=st_[:, 1, :], op=ALU.mult),
                   r=[pbb, sb_], w=[a2b])
                mt_, mb_ = mg_r.next()
                op("pool", lambda e, mt_=mt_, a1=a1, a2=a2: e.tensor_tensor(out=mt_[:], in0=a1[:], in1=a2[:], op=ALU.add),
                   r=[a1b, a2b], w=[mb_])
                dma("sp", lambda e, mt_=mt_, fo=fo, t0=t0: e.dma_start(out=MG[fo, :, t0:t0 + 512], in_=mt_[:]), r=[mb_])
        sch.barrier()
        sch.emit()
    if upto == "E1":
        return finish()

    NSLOT = NE * CAP
    NSOK = min(NSLOT, 65535)
    with ExitStack() as ph:
        wo = ph.enter_context(nc.sbuf_tensor("wo", [128, KC, D], BF16))
        b_wo = Buf("wo")
        wo_v = w_out.rearrange("(kc p) n -> p kc n", p=128)
        for cq in range(4):
            dma("pool", lambda e, cq=cq: e.dma_start(out=wo[:, :, cq * 512:(cq + 1) * 512], in_=wo_v[:, :, cq * 512:(cq + 1) * 512]),
                w=[b_wo])
        wr = ph.enter_context(nc.sbuf_tensor("wr", [128, KC, NE], BF16))
        b_wr = Buf("wr")
        dma("pool", lambda e: e.dma_start(out=wr[:], in_=w_router.rearrange("(kc p) n -> p kc n", p=128)), w=[b_wr])
        brow = ph.enter_context(nc.sbuf_tensor("brow", [128, NE], F32))
        dma("sp", lambda e: e.dma_start(out=brow[:], in_=b_router[0:1, :].partition_broadcast(128)), w=[b_wr])
        base = ph.enter_context(nc.sbuf_tensor("base", [128, NE], F32))
        slotbase = ph.enter_context(nc.sbuf_tensor("slotbase", [128, NE], F32))
        b_base = Buf("base")
        op("dve", lambda e: e.memset(base[:], 0.0), w=[b_base])
        op("dve", lambda e: e.tensor_scalar(out=slotbase[:], in0=iota_f[:, 0:NE], scalar1=float(CAP), scalar2=None, op0=ALU.mult),
           r=[b_consts], w=[b_base])
        mg_r = Ring(nc, ph, "mgi", [128, KC, 512], BF16, 1)
        x_r = Ring(nc, ph, "xe", [128, D], F32, 2)
        mix_r = Ring(nc, ph, "mixs", [128, D], F32, 1)
        x1_r = Ring(nc, ph, "x1t", [128, D], F32, 2)
        xn_r = Ring(nc, ph, "xn2", [128, D], BF16, 2)
        h2_r = Ring(nc, ph, "h2T", [128, KC, 128], BF16, 2)
        junk = ph.enter_context(nc.sbuf_tensor("junkE", [128, D], BF16))
        b_junk = Buf("junkE")
        st_r = Ring(nc, ph, "stE", [128, 8], F32, 4)
        rt_r = Ring(nc, ph, "rtE", [128, 8, NE], F32, 2)
        mkb_r = Ring(nc, ph, "mkE", [128, NE], BF16, 2)
        t8_r = Ring(nc, ph, "t8E", [128, 24], F32, 2)
        psm_r = Ring(nc, ph, "psEm", [128, 512], F32, 3, psum=True)
        pst_r = Ring(nc, ph, "psEt", [128, 1024], BF16, 2, psum=True)
        psr_r = Ring(nc, ph, "psEr", [128, 512], F32, 2, psum=True)
        for tg in range(So // 512):
            mgt, mgb = mg_r.next()
            dma("sp", lambda e, mgt=mgt, tg=tg: e.dma_start(
                out=mgt[:], in_=MG[:, :, tg * 512:(tg + 1) * 512].rearrange("f d t -> d f t")), w=[mgb])
            for tt in range(4):
                ti = tg * 4 + tt
                tok0 = ti * 128
                xt, xb = x_r.next()
                dma("sp", lambda e, xt=xt, tok0=tok0: e.dma_start(out=xt[:], in_=x_own[tok0:tok0 + 128, :]), w=[xb])
                mx, mxb = mix_r.next()
                for dc in range(4):
                    pm, pmb = psm_r.next()
                    mmgroup(pm[:], pmb, [(mgt[:, kc, tt * 128:(tt + 1) * 128], wo[:, kc, dc * 512:(dc + 1) * 512])
                                         for kc in range(KC)], [mgb, b_wo])
                    op("act", lambda e, mx=mx, pm=pm, dc=dc: e.activation(out=mx[:, dc * 512:(dc + 1) * 512], in_=pm[:], func=AF.Copy),
                       r=[pmb], w=[mxb])
                st, sb_ = st_r.next()
                op("act", lambda e, st=st, mx=mx: e.activation(out=junk[:], in_=mx[:], func=AF.Square, accum_out=st[:, 0:1]),
                   r=[mxb], w=[b_junk, sb_])
                op("act", lambda e, st=st: e.activation(out=st[:, 1:2], in_=st[:, 0:1], func=AF.Sqrt, scale=1.0 / D, bias=EPS),
                   r=[sb_], w=[sb_])
                op("dve", lambda e, st=st: e.reciprocal(out=st[:, 2:3], in_=st[:, 1:2]), r=[sb_], w=[sb_])
                op("dve", lambda e, st=st, mx=mx: e.scalar_tensor_tensor(out=mx[:], in0=mx[:], scalar=st[:, 2:3], in1=ga1row[:],
                                                                        op0=ALU.mult, op1=ALU.mult),
                   r=[mxb, sb_, b_garow[0]], w=[mxb])
                x1, x1b = x1_r.next()
                op("pool", lambda e, x1=x1, mx=mx, xt=xt: e.tensor_tensor(out=x1[:], in0=mx[:], in1=xt[:], op=ALU.add),
                   r=[mxb, xb], w=[x1b])
                dma("sp", lambda e, x1=x1, tok0=tok0: e.dma_start(out=X1[tok0:tok0 + 128, :], in_=x1[:]), r=[x1b])
                op("act", lambda e, st=st, x1=x1: e.activation(out=junk[:], in_=x1[:], func=AF.Square, accum_out=st[:, 3:4]),
                   r=[x1b], w=[b_junk, sb_])
                op("act", lambda e, st=st: e.activation(out=st[:, 4:5], in_=st[:, 3:4], func=AF.Sqrt, scale=1.0 / D, bias=EPS),
                   r=[sb_], w=[sb_])
                op("dve", lambda e, st=st: e.reciprocal(out=st[:, 5:6], in_=st[:, 4:5]), r=[sb_], w=[sb_])
                xn, xnb = xn_r.next()
                op("dve", lambda e, xn=xn, x1=x1, st=st: e.tensor_scalar(out=xn[:], in0=x1[:], scalar1=st[:, 5:6], scalar2=None,
                                                                       op0=ALU.mult), r=[x1b, sb_], w=[xnb])
                h2, h2b = h2_r.next()
                for half in range(2):
                    pt, pb = pst_r.next()
                    for j in range(8):
                        kc = half * 8 + j
                        op("pe", lambda e, pt=pt, j=j, kc=kc, xn=xn: e.transpose(
                            pt[:, j * 128:(j + 1) * 128], xn[:, kc * 128:(kc + 1) * 128], identb[:]),
                           r=[xnb, b_cb] if j in (0, 7) else (), w=[pb])
                    for j in range(8):
                        kc = half * 8 + j
                        op("act", lambda e, pt=pt, j=j, kc=kc, h2=h2: e.activation(
                            out=h2[:, kc, :], in_=pt[:, j * 128:(j + 1) * 128], func=AF.Identity,
                            scale=modc[:, 2 * KC + kc:2 * KC + kc + 1], bias=modc[:, 3 * KC + kc:3 * KC + kc + 1]),
                           r=[pb, b_modc], w=[h2b])
                pr, prb = psr_r.next()
                mmgroup(pr[:, 0:NE], prb, [(h2[:, kc, :], wr[:, kc, :]) for kc in range(KC)], [h2b, b_wr])
                rt, rtb = rt_r.next()
                t8, t8b = t8_r.next()
                mk, mkb = mkb_r.next()
                lg, pos_, sla, ov, jk = (rt[:, q, :] for q in range(5))
                op("dve", lambda e, lg=lg, pr=pr: e.tensor_tensor(out=lg, in0=pr[:, 0:NE], in1=brow[:], op=ALU.add),
                   r=[prb, b_wr], w=[rtb])
                op("dve", lambda e, lg=lg, t8=t8: e.max(out=t8[:, 0:8], in_=lg), r=[rtb], w=[t8b])
                op("dve", lambda e, t8=t8: e.tensor_scalar(out=t8[:, 8:12], in0=t8[:, 0:4], scalar1=t8[:, 0:1], scalar2=None,
                                                          op0=ALU.subtract), r=[t8b], w=[t8b])
                op("act", lambda e, t8=t8, st=st: e.activation(out=t8[:, 12:16], in_=t8[:, 8:12], func=AF.Exp, accum_out=st[:, 6:7]),
                   r=[t8b], w=[t8b, sb_])
                op("dve", lambda e, st=st: e.reciprocal(out=st[:, 7:8], in_=st[:, 6:7]), r=[sb_], w=[sb_])
                op("dve", lambda e, mk=mk, lg=lg, t8=t8: e.tensor_scalar(out=mk[:], in0=lg, scalar1=t8[:, 3:4], scalar2=None,
                                                                       op0=ALU.is_ge), r=[rtb, t8b], w=[mkb])
                pp, ppb = psr_r.next()
                op("pe", lambda e, pp=pp, mk=mk: e.matmul(pp[:, 0:NE], triS[:], mk[:], start=True, stop=True),
                   r=[mkb, b_cb], w=[ppb])
                op("pe", lambda e, pp=pp, mk=mk: e.matmul(pp[:, 64:64 + NE], onesb[:], mk[:], start=True, stop=True),
                   r=[mkb, b_cb], w=[ppb])
                op("dve", lambda e, pos_=pos_, pp=pp: e.tensor_tensor(out=pos_, in0=pp[:, 0:NE], in1=base[:], op=ALU.add),
                   r=[ppb, b_base], w=[rtb])
                op("dve", lambda e, pp=pp: e.tensor_tensor(out=base[:], in0=pp[:, 64:64 + NE], in1=base[:], op=ALU.add),
                   r=[ppb, b_base], w=[b_base])
                op("dve", lambda e, ov=ov, pos_=pos_: e.tensor_scalar(out=ov, in0=pos_, scalar1=float(CAP), scalar2=1.0e9,
                                                                    op0=ALU.is_ge, op1=ALU.mult), r=[rtb], w=[rtb])
                op("dve", lambda e, sla=sla, pos_=pos_: e.tensor_tensor(out=sla, in0=pos_, in1=slotbase[:], op=ALU.add),
                   r=[rtb, b_base], w=[rtb])
                op("dve", lambda e, sla=sla, ov=ov: e.tensor_tensor(out=sla, in0=sla, in1=ov, op=ALU.add), r=[rtb], w=[rtb])
                for k in range(4):
                    op("dve", lambda e, jk=jk, lg=lg, t8=t8, sla=sla, k=k: e.scalar_tensor_tensor(
                        out=jk, in0=lg, scalar=t8[:, k:k + 1], in1=sla, op0=ALU.is_equal, op1=ALU.mult,
                        accum_out=t8[:, 16 + k:17 + k]), r=[rtb, t8b], w=[rtb, t8b])
                op("dve", lambda e, t8=t8, ti=ti: e.tensor_copy(out=rslot[:, ti * 4:(ti + 1) * 4], in_=t8[:, 16:20]),
                   r=[t8b], w=[b_route])
                op("dve", lambda e, t8=t8: e.tensor_scalar(out=t8[:, 20:24], in0=t8[:, 16:20], scalar1=float(NSOK), scalar2=None,
                                                          op0=ALU.is_lt), r=[t8b], w=[t8b])
                op("dve", lambda e, t8=t8, st=st: e.scalar_tensor_tensor(
                    out=t8[:, 12:16], in0=t8[:, 12:16], scalar=st[:, 7:8], in1=t8[:, 20:24], op0=ALU.mult, op1=ALU.mult),
                   r=[t8b, sb_], w=[t8b])
                op("dve", lambda e, t8=t8, ti=ti: e.tensor_copy(out=rwgt[:, ti * 4:(ti + 1) * 4], in_=t8[:, 12:16]),
                   r=[t8b], w=[b_route])
                for k in range(4):
                    dma("pool", lambda e, xn=xn, ti=ti, k=k: e.indirect_dma_start(
                        out=XG[0:NSOK, :], out_offset=bass.IndirectOffsetOnAxis(ap=rslot[:, ti * 4 + k:ti * 4 + k + 1], axis=0),
                        in_=xn[:, :], in_offset=None, bounds_check=sch.breg(e, NSOK - 1), oob_is_err=False),
                        r=[xnb, b_route])
        sch.barrier()
        sch.emit()
    if upto == "E2":
        return finish()

    NBLK = CAP // 128
    NTG = CAP // 512
    with ExitStack() as ph:
        XT = ph.enter_context(nc.sbuf_tensor("XT", [128, KC, CAP], BF16))
        b_XT = Buf("XT")
        actT = ph.enter_context(nc.sbuf_tensor("actT", [128, KC, CAP], BF16))
        b_act = [Buf(f"act{f}") for f in range(KC)]
        wc_r = Ring(nc, ph, "wcG", [128, KC, 256], BF16, 2)
        gt_r = Ring(nc, ph, "gtG", [128, D], BF16, 3)
        bb_r = Ring(nc, ph, "bbG", [128, 2 * KC], F32, 2)
        b2_r = Ring(nc, ph, "b2G", [1, 256], BF16, 3)
        g_r = Ring(nc, ph, "gG", [128, 512], F32, 2)
        s_r = Ring(nc, ph, "sG", [128, 512], F32, 2)
        l_r = Ring(nc, ph, "lG", [128, 512], F32, 2)
        gs_r = Ring(nc, ph, "gsG", [128, 512], F32, 2)
        ot_r = Ring(nc, ph, "otG", [128, 256], BF16, 3)
        psT_r = Ring(nc, ph, "psGt", [128, 1024], BF16, 2, psum=True)
        psg_r = Ring(nc, ph, "psGg", [128, 512], F32, 2, psum=True)
        psl_r = Ring(nc, ph, "psGl", [128, 512], F32, 2, psum=True)
        pso_r = Ring(nc, ph, "psGo", [128, 512], F32, 2, psum=True)
        def xt_steps(ex):
            steps = []
            holds = [dict() for _ in range(NBLK)]
            for blk in range(NBLK):
                hold = holds[blk]
                for half in range(2):
                    def step(ex=ex, blk=blk, half=half, hold=hold, holds=holds):
                        def fetch(b):
                            if b < NBLK and "gt" not in holds[b]:
                                gt, gtb = gt_r.next()
                                r0 = ex * CAP + b * 128
                                dma("sp", lambda e: e.dma_start(out=gt[:], in_=XG[r0:r0 + 128, :]), w=[gtb])
                                holds[b]["gt"], holds[b]["gtb"] = gt, gtb
                        if half == 0:
                            fetch(blk)
                            fetch(blk + 1)
                        gt, gtb = hold["gt"], hold["gtb"]
                        pt, pb = psT_r.next()
                        for j in range(8):
                            kc = half * 8 + j
                            op("pe", lambda e, j=j, kc=kc: e.transpose(
                                pt[:, j * 128:(j + 1) * 128], gt[:, kc * 128:(kc + 1) * 128], identb[:]),
                               r=[gtb, b_cb] if j in (0, 7) else (), w=[pb])
                        for j in range(8):
                            kc = half * 8 + j
                            if half == 0:
                                op("act", lambda e, j=j, kc=kc: e.activation(
                                    out=XT[:, kc, blk * 128:(blk + 1) * 128], in_=pt[:, j * 128:(j + 1) * 128], func=AF.Identity,
                                    scale=modc[:, 2 * KC + kc:2 * KC + kc + 1], bias=modc[:, 3 * KC + kc:3 * KC + kc + 1]),
                                   r=[pb, b_modc], w=[b_XT])
                            else:
                                op("dve", lambda e, j=j, kc=kc: e.tensor_scalar(
                                    out=XT[:, kc, blk * 128:(blk + 1) * 128], in0=pt[:, j * 128:(j + 1) * 128],
                                    scalar1=modc[:, 2 * KC + kc:2 * KC + kc + 1], scalar2=modc[:, 3 * KC + kc:3 * KC + kc + 1],
                                    op0=ALU.mult, op1=ALU.add), r=[pb, b_modc], w=[b_XT])
                    steps.append(step)
            return steps

        chunks = [(ex_, kind, c) for ex_ in range(NE) for (kind, c) in
                  ([("w1", c) for c in range(KC)] + [("w2", c) for c in range(D // 256)])]
        wslots = {}

        def issue_chunk(ci):
            if ci >= len(chunks) or ci in wslots:
                return
            ex_, kind, c = chunks[ci]
            src = (w1 if kind == "w1" else w2)[ex_].rearrange("(kc p) n -> p kc n", p=128)
            wt, wb = wc_r.next()
            dma("pool", lambda e: e.dma_start(out=wt[:], in_=src[:, :, c * 256:(c + 1) * 256]), w=[wb])
            wslots[ci] = (wt, wb)

        def get_chunk(ex_, kind, c):
            ci = ex_ * (KC + D // 256) + (c if kind == "w1" else KC + c)
            issue_chunk(ci)
            issue_chunk(ci + 1)
            return wslots.pop(ci)

        for ex in range(NE):
            bt, btb = bb_r.next()
            dma("sp", lambda e, bt=bt, ex=ex: e.dma_start(out=bt[:], in_=b1c[ex]), w=[btb])
            w1v = w1[ex].rearrange("(kc p) n -> p kc n", p=128)
            w2v = w2[ex].rearrange("(kc p) n -> p kc n", p=128)
            if ex == 0:
                for st_ in xt_steps(0):
                    st_()
            for cq in range(KC):
                wt, wb = get_chunk(ex, "w1", cq)
                for tg in range(NTG):
                    pg, pgb = psg_r.next()
                    mmgroup(pg[:], pgb, [(wt[:, kc, 0:256:2], XT[:, kc, tg * 512:(tg + 1) * 512]) for kc in range(KC)], [wb, b_XT])
                    pl, plb = psl_r.next()
                    mmgroup(pl[:], plb, [(wt[:, kc, 1:256:2], XT[:, kc, tg * 512:(tg + 1) * 512]) for kc in range(KC)], [wb, b_XT])
                    g_, gb_ = g_r.next()
                    op("dve", lambda e, g_=g_, pg=pg, bt=bt, cq=cq: e.tensor_scalar(
                        out=g_[:], in0=pg[:], scalar1=bt[:, cq:cq + 1], scalar2=LIMIT, op0=ALU.add, op1=ALU.min),
                       r=[pgb, btb], w=[gb_])
                    sg, sgb = s_r.next()
                    op("act", lambda e, sg=sg, g_=g_: e.activation(out=sg[:], in_=g_[:], func=AF.Sigmoid, scale=ALPHA),
                       r=[gb_], w=[sgb])
                    l_, lb_ = l_r.next()
                    op("dve", lambda e, l_=l_, pl=pl, bt=bt, cq=cq: e.tensor_scalar(
                        out=l_[:], in0=pl[:], scalar1=bt[:, KC + cq:KC + cq + 1], scalar2=LIMIT, op0=ALU.add, op1=ALU.min),
                       r=[plb, btb], w=[lb_])
                    op("dve", lambda e, l_=l_: e.tensor_scalar(out=l_[:], in0=l_[:], scalar1=-LIMIT, scalar2=1.0,
                                                               op0=ALU.max, op1=ALU.add), r=[lb_], w=[lb_])
                    gs, gsb = gs_r.next()
                    op("pool", lambda e, gs=gs, g_=g_, sg=sg: e.tensor_tensor(out=gs[:], in0=g_[:], in1=sg[:], op=ALU.mult),
                       r=[gb_, sgb], w=[gsb])
                    op("dve", lambda e, gs=gs, l_=l_, cq=cq, tg=tg: e.tensor_tensor(
                        out=actT[:, cq, tg * 512:(tg + 1) * 512], in0=gs[:], in1=l_[:], op=ALU.mult),
                       r=[gsb, lb_], w=[b_act[cq]])
            nxt = xt_steps(ex + 1) if ex + 1 < NE else []
            w2step = 0
            for dc in range(D // 256):
                wt, wb = get_chunk(ex, "w2", dc)
                b2t, b2b = b2_r.next()
                dma("pool", lambda e, b2t=b2t, ex=ex, dc=dc: e.dma_start(out=b2t[:], in_=b2[ex:ex + 1, dc * 256:(dc + 1) * 256]), w=[b2b])
                for blk in range(NBLK):
                    po, pob = pso_r.next()
                    pairs = [(actT[:, fc, blk * 128:(blk + 1) * 128], wt[:, fc, :]) for fc in range(KC)]
                    n = len(pairs)
                    for q, (l, r_) in enumerate(pairs):
                        op("pe", lambda e, po=po, l=l, r_=r_, q=q: e.matmul(po[:, 0:256], l, r_, start=(q == 0), stop=False),
                           r=(b_act + [wb]) if q in (0, n - 1) else (), w=[pob])
                    op("pe", lambda e, po=po, b2t=b2t, dc=dc: e.matmul(
                        po[:, 0:256], onesb[0:1, :], b2t[0:1, :], start=False, stop=True),
                       r=[b2b, b_cb], w=[pob])
                    ot, otb = ot_r.next()
                    op("act", lambda e, ot=ot, po=po: e.activation(out=ot[:], in_=po[:, 0:256], func=AF.Copy), r=[pob], w=[otb])
                    r0 = ex * CAP + blk * 128
                    dma("sp", lambda e, ot=ot, r0=r0, dc=dc: e.dma_start(out=YG[r0:r0 + 128, dc * 256:(dc + 1) * 256], in_=ot[:]),
                        r=[otb])
                    w2step += 1
                    if nxt and w2step % max(1, (D // 256) * NBLK // len(nxt)) == 0:
                        nxt.pop(0)()
            while nxt:
                nxt.pop(0)()
        sch.barrier()
        sch.emit()
    if upto == "G":
        return finish()

    with ExitStack() as ph:
        y_r = Ring(nc, ph, "yH", [128, D], BF16, 5)
        x1_r = Ring(nc, ph, "x1H", [128, D], F32, 2)
        f_r = Ring(nc, ph, "fH", [128, D], F32, 2)
        o_r = Ring(nc, ph, "oH", [128, D], F32, 2)
        junk = ph.enter_context(nc.sbuf_tensor("junkH", [128, D], BF16))
        b_junk = Buf("junkH")
        st_r = Ring(nc, ph, "stH", [128, 4], F32, 3)
        for (yt, yb) in y_r.tiles:
            op("dve", lambda e, yt=yt: e.memset(yt[:], 0.0), w=[yb])
        for ti in range(NTo):
            tok0 = ti * 128
            ys = []
            for k in range(4):
                yt, yb = y_r.next()
                dma("pool", lambda e, yt=yt, ti=ti, k=k: e.indirect_dma_start(
                    out=yt[:, :], out_offset=None, in_=YG[0:NSOK, :],
                    in_offset=bass.IndirectOffsetOnAxis(ap=rslot[:, ti * 4 + k:ti * 4 + k + 1], axis=0),
                    bounds_check=sch.breg(e, NSOK - 1), oob_is_err=False), r=[b_route], w=[yb])
                ys.append((yt, yb))
            x1, x1b = x1_r.next()
            dma("sp", lambda e, x1=x1, tok0=tok0: e.dma_start(out=x1[:], in_=X1[tok0:tok0 + 128, :]), w=[x1b])
            ft, fb = f_r.next()
            op("dve", lambda e, ft=ft, ti=ti, y0=ys[0][0]: e.tensor_scalar(
                out=ft[:], in0=y0[:], scalar1=rwgt[:, ti * 4:ti * 4 + 1], scalar2=None, op0=ALU.mult),
               r=[ys[0][1], b_route], w=[fb])
            for k in range(1, 4):
                op("dve", lambda e, ft=ft, ti=ti, k=k, yk=ys[k][0]: e.scalar_tensor_tensor(
                    out=ft[:], in0=yk[:], scalar=rwgt[:, ti * 4 + k:ti * 4 + k + 1], in1=ft[:], op0=ALU.mult, op1=ALU.add),
                   r=[ys[k][1], b_route, fb], w=[fb])
            st, sb_ = st_r.next()
            op("act", lambda e, st=st, ft=ft: e.activation(out=junk[:], in_=ft[:], func=AF.Square, accum_out=st[:, 0:1]),
               r=[fb], w=[b_junk, sb_])
            op("act", lambda e, st=st: e.activation(out=st[:, 1:2], in_=st[:, 0:1], func=AF.Sqrt, scale=1.0 / D, bias=EPS),
               r=[sb_], w=[sb_])
            op("dve", lambda e, st=st: e.reciprocal(out=st[:, 2:3], in_=st[:, 1:2]), r=[sb_], w=[sb_])
            op("dve", lambda e, st=st, ft=ft: e.scalar_tensor_tensor(out=ft[:], in0=ft[:], scalar=st[:, 2:3], in1=ga2row[:],
                                                                    op0=ALU.mult, op1=ALU.mult),
               r=[fb, sb_, b_garow[1]], w=[fb])
            ot, otb = o_r.next()
            op("pool", lambda e, ot=ot, ft=ft, x1=x1: e.tensor_tensor(out=ot[:], in0=ft[:], in1=x1[:], op=ALU.add),
               r=[fb, x1b], w=[otb])
            dma("sp", lambda e, ot=ot, tok0=tok0: e.dma_start(out=out_d[tok0:tok0 + 128, :], in_=ot[:]), r=[otb])
        sch.barrier()
        sch.emit()
    return finish()


def make_consts():
    c = np.zeros((128, 8, 128), np.float32)
    c[:, 0, :] = np.eye(128)
    for dp in range(16):
        c[dp + 16, 1, dp] = -1.0
        c[dp, 1, dp + 16] = 1.0
    for o in (0, 64):
        for dp in range(8):
            c[o + dp + 8, 2, o + dp] = -1.0
            c[o + dp, 2, o + dp + 8] = 1.0
    k = np.arange(128)
    c[:, 3, :] = (k[:, None] >= k[None, :])
    c[:, 4, :] = (k[:, None] < k[None, :])
    c[:, 5, :] = 1.0
    invA = THETA ** (-(np.arange(16, dtype=np.float32)) / np.float32(16))
    invI = THETA ** (-(np.arange(8, dtype=np.float32)) / np.float32(8))
    for p in range(128):
        c[p, 6, 0] = invA[p % 16] if p < 32 else 0.0
        c[p, 6, 1] = invI[(p % 64) % 8] if (p % 64) < 16 else 0.0
    c[:, 7, :] = k[None, :]
    return np.ascontiguousarray(c.reshape(128, 8 * 128))


def make_masks(half):
    p = np.arange(128)[:, None]
    tl = np.arange(512)[None, :]
    cm = np.zeros((128, 2, 8, 512), np.float32)
    am = np.zeros((128, 2, 640), np.float32)
    cc = np.arange(640)[None, :]
    for m in range(2):
        delta = (1 if m == 0 else 0) if half == 0 else (0 if m == 0 else 1)
        for j in range(8):
            if delta == 0:
                cm[:, m, j, :] = (128 * (j - 4) + p) < tl
            else:
                cm[:, m, j, :] = (128 * j + p) < tl
        lim = 64 * (p // 64 + 1)
        am[:, m, :] = np.where(512 * (delta - 1) + cc >= lim, NEG, 0.0)
    return (np.ascontiguousarray(cm.reshape(128, -1)).astype(ml_dtypes.bfloat16),
            np.ascontiguousarray(am.reshape(128, -1)))


def col_layout(v):
    return np.ascontiguousarray(v.reshape(-1, 128).T)


def prep(inputs, cfg, batches):
    S, NE = cfg["S"], cfg["NE"]
    G = S // 512
    f32 = np.float32
    x = np.asarray(inputs["x"], f32)
    c = np.asarray(inputs["c"], f32)
    pos = np.asarray(inputs["positions"], np.int32)
    shared = {
        "b_ada": np.ascontiguousarray(np.asarray(inputs["b_ada"], f32)[0][None, :]),
        "badac": col_layout(np.asarray(inputs["b_ada"], f32)[0]),
        "gcols": np.concatenate([col_layout(np.asarray(inputs[k], f32)[0]) for k in
                                 ("g_pre_mix", "g_post_mix", "g_pre_ffn", "g_post_ffn")], axis=1),
        "g_post_mix": np.ascontiguousarray(np.asarray(inputs["g_post_mix"], f32)[0][None, :]),
        "g_post_ffn": np.ascontiguousarray(np.asarray(inputs["g_post_ffn"], f32)[0][None, :]),
        "w_ada": np.ascontiguousarray(np.asarray(inputs["w_ada"], f32)[0]),
        "w_in": np.ascontiguousarray(np.asarray(inputs["w_in"], f32)[0]),
        "w_branch_a": np.ascontiguousarray(np.asarray(inputs["w_branch_a"], f32)[0]),
        "w_branch_b": np.ascontiguousarray(np.asarray(inputs["w_branch_b"], f32)[0]),
        "w_out": np.ascontiguousarray(np.asarray(inputs["w_out"], f32)[0]),
        "w_router": np.ascontiguousarray(np.asarray(inputs["w_router"], f32)[0][:, :NE]),
        "b_router": np.ascontiguousarray(np.asarray(inputs["b_router"], f32)[0][None, :NE]),
        "w1": np.ascontiguousarray(np.asarray(inputs["w1"], f32)[0][:NE]),
        "w2": np.ascontiguousarray(np.asarray(inputs["w2"], f32)[0][:NE]),
        "b2": np.ascontiguousarray(np.asarray(inputs["b2"], f32)[0][:NE]),
        "consts": make_consts(),
    }
    b1 = np.asarray(inputs["b1"], f32)[0][:NE]
    b1g = b1[:, 0::2].reshape(NE, KC, 128).transpose(0, 2, 1)
    b1l = b1[:, 1::2].reshape(NE, KC, 128).transpose(0, 2, 1)
    shared["b1c"] = np.ascontiguousarray(np.concatenate([b1g, b1l], axis=2))
    masks = [make_masks(0), make_masks(1)]
    in_maps, owns = [], []
    for b in batches:
        for half in range(2):
            toks = np.concatenate([np.arange(g * 512, (g + 1) * 512) for g in own_groups(G, half)])
            m = dict(shared)
            m["x_all"] = np.ascontiguousarray(x[b])
            m["x_own"] = np.ascontiguousarray(x[b][toks])
            m["pos_all"] = np.ascontiguousarray(pos[b][None, :])
            m["pos_own"] = np.ascontiguousarray(pos[b][toks][None, :])
            m["cvec"] = col_layout(c[b])
            m["cmask"], m["amask"] = masks[half]
            in_maps.append(m)
            owns.append((b, toks))
    return in_maps, owns


_CACHE = {}


def kernel(**inputs):
    x = np.asarray(inputs["x"])
    B, S, _ = x.shape
    cfg = {"S": S, "NE": 32, "CAP": 2048}
    in_maps, owns = prep(inputs, cfg, list(range(B)))
    nc = build(cfg)
    res = run_bass_kernel_spmd(nc, in_maps, core_ids=list(range(len(in_maps))))
    out = np.empty((B, S, D), np.float32)
    for r, (b, toks) in zip(res.results, owns):
        out[b, toks] = np.asarray(r["out"], np.float32)
    return out
```

```python
import math
from contextlib import ExitStack

import numpy as np
import ml_dtypes

import concourse.bass as bass
import concourse.mybir as mybir
from concourse.bass_utils import run_bass_kernel_spmd

F32 = mybir.dt.float32
BF16 = mybir.dt.bfloat16
I32 = mybir.dt.int32
AF = mybir.ActivationFunctionType
ALU = mybir.AluOpType
AX = mybir.AxisListType

D = 2048
KC = D // 128
A_H, A_KV, HD = 8, 2, 128
IDX_H, IDX_D = 16, 64
B_H = 8
TOPK = 256
THETA = 500000.0
EPS = 1e-6
LIMIT = 7.0
ALPHA = 1.702
C_QA, C_KA, C_VA = 0, 1024, 1280
C_QB, C_KB, C_VB = 1536, 2560, 3584
C_QI, C_KI, C_WI = 4608, 5632, 5696
C_GA, C_GB = 5712, 7760
IN_W = 9808
NEG = -1.0e30
NBIS = 26


class Buf:
    __slots__ = ("w", "r", "name", "excl")

    def __init__(self, name="", excl=False):
        self.w = None
        self.r = []
        self.name = name
        self.excl = excl


class Sched:
    ENGS = ("pe", "act", "dve", "pool", "sp")
    RQ = {"sp": 8, "pool": 2, "act": 4}

    def __init__(self, nc, es):
        self.nc = nc
        self.sems = {}
        for e in self.ENGS:
            self.sems[e] = es.enter_context(nc.semaphore("c_" + e))
        self.dq = {}
        for q in ("sp", "pool", "act"):
            self.dq[q] = [es.enter_context(nc.semaphore(f"d_{q}{i}")) for i in range(self.RQ[q])]
        self.n = {e: 0 for e in self.ENGS}
        self.dn = {q: 0 for q in self.dq}
        self.seen = {e: {} for e in self.ENGS}
        self.streams = {e: [] for e in self.ENGS}
        self.last = {}

    def _sem(self, key):
        return self.sems[key] if isinstance(key, str) else self.dq[key[0]][key[1]]

    def _waits(self, eng, deps):
        out = []
        best = {}
        for d in deps:
            if d is None:
                continue
            k, v = d
            if k == "pe" and eng == "pe":
                continue
            if best.get(k, 0) < v:
                best[k] = v
        for k, v in best.items():
            if self.seen[eng].get(k, 0) >= v:
                continue
            self.seen[eng][k] = v
            out.append((k, v))
        return out

    def _deps(self, r, w):
        deps = []
        for b in r:
            deps.append(b.w)
            if b.excl:
                deps.extend(b.r)
        for b in w:
            deps.append(b.w)
            deps.extend(b.r)
        return deps

    def _commit(self, tok, r, w):
        for b in r:
            b.r.append(tok)
        for b in w:
            b.w = tok
            b.r = []
        self.last[tok[0]] = tok[1]

    def op(self, eng, fn, r=(), w=()):
        waits = self._waits(eng, self._deps(r, w))
        self.n[eng] += 1
        tok = (eng, self.n[eng])
        self.streams[eng].append((waits, fn, self.sems[eng], 1))
        self._commit(tok, r, w)

    def dma(self, q, fn, r=(), w=()):
        i = self.dn[q]
        self.dn[q] += 1
        R = self.RQ[q]
        key = (q, i % R)
        deps = self._deps(r, w)
        if i >= R:
            deps.append((key, 16 * (i // R)))
        waits = self._waits(q, deps)
        tok = (key, 16 * (i // R + 1))
        self.streams[q].append((waits, fn, self._sem(key), 16))
        self._commit(tok, r, w)

    def barrier(self):
        toks = list(self.last.items())
        for e in self.ENGS:
            waits = self._waits(e, [t for t in toks if not (t[0] == e)])
            if waits:
                self.streams[e].append((waits, None, None, 0))

    def breg(self, e, val):
        if val not in self._regs:
            self._regs[val] = e.to_reg(val)
        return self._regs[val]

    def emit(self):
        nc = self.nc
        streams = self.streams
        self._regs = {}
        self.streams = {e: [] for e in self.ENGS}

        def run(e, lst):
            for waits, fn, sem, inc in lst:
                for k, v in waits:
                    e.wait_ge(self._sem(k), v)
                if fn is not None:
                    try:
                        fn(e).then_inc(sem, inc)
                    except Exception:
                        print("EMIT FAIL at stream idx", lst.index((waits, fn, sem, inc)), "of", len(lst), flush=True)
                        raise

        with nc.Block() as block:
            @block.tensor
            def _(e):
                run(e, streams["pe"])

            @block.scalar
            def _(e):
                run(e, streams["act"])

            @block.vector
            def _(e):
                run(e, streams["dve"])

            @block.gpsimd
            def _(e):
                run(e, streams["pool"])

            @block.sync
            def _(e):
                run(e, streams["sp"])


class Ring:
    def __init__(self, nc, es, name, shape, dtype, n, psum=False):
        self.tiles = []
        for i in range(n):
            if psum:
                t = es.enter_context(nc.psum_tensor(f"{name}{i}", shape, dtype))
            else:
                t = es.enter_context(nc.sbuf_tensor(f"{name}{i}", shape, dtype))
            self.tiles.append((t, Buf(f"{name}{i}", excl=psum)))
        self.i = 0

    def next(self):
        t = self.tiles[self.i % len(self.tiles)]
        self.i += 1
        return t


def own_groups(G, half):
    return [g for g in range(G) if ((g % 4) in (0, 3)) == (half == 0)]


def build(cfg):
    S = cfg["S"]
    NE = cfg["NE"]
    CAP = cfg["CAP"]
    dbg = cfg.get("dbg", False)
    upto = cfg.get("upto", "Z")
    So = S // 2
    G = S // 512
    Go = G // 2
    NT = S // 128
    NTo = So // 128
    SG = min(2048, So)
    nc = bass.Bass("TRN2", target_bir_lowering=False)
    es = ExitStack()
    scratch_kind = "ExternalOutput" if dbg else "Internal"

    def din(name, shape, dt):
        return nc.dram_tensor(name, list(shape), dt, kind="ExternalInput").ap()

    def dscr(name, shape, dt):
        return nc.dram_tensor(name, list(shape), dt, kind=scratch_kind).ap()

    x_all = din("x_all", [S, D], F32)
    x_own = din("x_own", [So, D], F32)
    pos_all = din("pos_all", [1, S], I32)
    pos_own = din("pos_own", [1, So], I32)
    cvec = din("cvec", [128, KC], F32)
    badac = din("badac", [128, 6 * KC], F32)
    gcols = din("gcols", [128, 4 * KC], F32)
    b_ada = din("b_ada", [1, 6 * D], F32)
    g_post_mix = din("g_post_mix", [1, D], F32)
    g_post_ffn = din("g_post_ffn", [1, D], F32)
    w_ada = din("w_ada", [D, 6 * D], F32)
    w_in = din("w_in", [D, IN_W], F32)
    w_bra = din("w_branch_a", [A_H * HD, D], F32)
    w_brb = din("w_branch_b", [B_H * HD, D], F32)
    w_out = din("w_out", [D, D], F32)
    w_router = din("w_router", [D, NE], F32)
    b_router = din("b_router", [1, NE], F32)
    w1 = din("w1", [NE, D, 2 * D], F32)
    b1c = din("b1c", [NE, 128, 2 * KC], F32)
    w2 = din("w2", [NE, D, D], F32)
    b2 = din("b2", [NE, D], F32)
    cmask_d = din("cmask", [128, 2 * 8 * 512], BF16)
    amask_d = din("amask", [128, 2 * 640], F32)
    consts_d = din("consts", [128, 8 * 128], F32)
    out_d = nc.dram_tensor("out", [So, D], F32, kind="ExternalOutput").ap()

    KA = dscr("s_ka", [A_KV, 128, S], BF16)
    KB = dscr("s_kb", [B_H, 128, S], BF16)
    KI = dscr("s_ki", [IDX_D, S], BF16)
    VA = dscr("s_va", [S, A_KV * HD], BF16)
    VB = dscr("s_vb", [S, B_H * HD], BF16)
    QA = dscr("s_qa", [A_H, 128, So], BF16)
    QB = dscr("s_qb", [B_H, 128, So], BF16)
    QI = dscr("s_qi", [IDX_H // 2, 128, So], BF16)
    WI = dscr("s_wi", [So, IDX_H], F32)
    GA = dscr("s_ga", [KC, 128, So], BF16)
    GB = dscr("s_gb", [KC, 128, So], BF16)
    MK = dscr("s_mk", [NTo, 128, S], BF16)
    OA = dscr("s_oa", [A_H, 128, So], BF16)
    OB = dscr("s_ob", [B_H, 128, So], BF16)
    MG = dscr("s_mg", [KC, 128, So], BF16)
    X1 = dscr("s_x1", [So, D], F32)
    XG = dscr("s_xg", [NE * CAP, D], BF16)
    YG = dscr("s_yg", [NE * CAP, D], BF16)

    sch = Sched(nc, es)
    op, dma = sch.op, sch.dma
    dbg_outs = {}

    def finish():
        if dbg:
            d1 = nc.dram_tensor("dbg_modc", [128, 4 * KC], F32, kind="ExternalOutput").ap()
            d2 = nc.dram_tensor("dbg_ga", [128, 2 * D], F32, kind="ExternalOutput").ap()
            dma("sp", lambda e: e.dma_start(out=d1[:, :], in_=modc[:]), r=[b_modc])
            dma("sp", lambda e: e.dma_start(out=d2[:, 0:D], in_=ga1row[:]), r=[b_garow[0]])
            dma("sp", lambda e: e.dma_start(out=d2[:, D:2 * D], in_=ga2row[:]), r=[b_garow[1]])
            d3 = nc.dram_tensor("dbg_rslot", [128, NTo * 4], I32, kind="ExternalOutput").ap()
            d4 = nc.dram_tensor("dbg_rwgt", [128, NTo * 4], F32, kind="ExternalOutput").ap()
            dma("sp", lambda e: e.dma_start(out=d3[:, :], in_=rslot[:]), r=[b_route])
            dma("sp", lambda e: e.dma_start(out=d4[:, :], in_=rwgt[:]), r=[b_route])
        sch.barrier()
        sch.emit()
        es.close()
        return nc

    def sbp(name, shape, dt):
        return es.enter_context(nc.sbuf_tensor(name, shape, dt))

    consts = sbp("consts_s", [128, 8 * 128], F32)
    b_consts = Buf("consts")
    identb = sbp("identb", [128, 128], BF16)
    rtA = sbp("rtA", [128, 128], BF16)
    rtI = sbp("rtI", [128, 128], BF16)
    triI = sbp("triI", [128, 128], BF16)
    triS = sbp("triS", [128, 128], BF16)
    onesb = sbp("onesb", [128, 128], BF16)
    onesf = consts[:, 5 * 128:6 * 128]
    invfA = consts[:, 6 * 128:6 * 128 + 1]
    invfI = consts[:, 6 * 128 + 1:6 * 128 + 2]
    iota_f = consts[:, 7 * 128:8 * 128]
    b_cb = Buf("constsb")
    modc = sbp("modc", [128, 4 * KC], F32)
    b_modc = Buf("modc")
    ga1row = sbp("ga1row", [128, D], F32)
    ga2row = sbp("ga2row", [128, D], F32)
    b_garow = [Buf("ga1row"), Buf("ga2row")]
    rslot = sbp("rslot", [128, NTo * 4], I32)
    rwgt = sbp("rwgt", [128, NTo * 4], F32)
    b_route = Buf("route")

    dma("sp", lambda e: e.dma_start(out=consts[:], in_=consts_d[:, :]), w=[b_consts])
    for i, t in enumerate((identb, rtA, rtI, triI, triS, onesb)):
        op("dve", lambda e, i=i, t=t: e.tensor_copy(out=t[:], in_=consts[:, i * 128:(i + 1) * 128]),
           r=[b_consts], w=[b_cb])

    def mmgroup(out_ap, out_buf, pairs, rbufs):
        n = len(pairs)
        for i, (l, r_) in enumerate(pairs):
            rb = rbufs if (i == 0 or i == n - 1) else ()
            op("pe", lambda e, l=l, r_=r_, i=i: e.matmul(out_ap, l, r_, start=(i == 0), stop=(i == n - 1)),
               r=rb, w=[out_buf])

    with ExitStack() as ph:
        cc = ph.enter_context(nc.sbuf_tensor("cc", [128, KC], F32))
        sg_ = ph.enter_context(nc.sbuf_tensor("sg_", [128, KC], F32))
        siluc = ph.enter_context(nc.sbuf_tensor("siluc", [128, KC], F32))
        silurep = ph.enter_context(nc.sbuf_tensor("silurep", [128, KC, 128], F32))
        badac_s = ph.enter_context(nc.sbuf_tensor("badac_s", [128, 6 * KC], F32))
        gcols_s = ph.enter_context(nc.sbuf_tensor("gcols_s", [128, 4 * KC], F32))
        rawc = ph.enter_context(nc.sbuf_tensor("rawc", [128, 4 * KC], F32))
        b_small = Buf("small")
        b_rawc = Buf("rawc")
        wch = Ring(nc, ph, "wch", [128, KC, 512], F32, 2)
        rowt = Ring(nc, ph, "rowt", [128, 2, 512], F32, 2)
        tmpr = Ring(nc, ph, "tmpr", [128, 512], F32, 2)
        psA = Ring(nc, ph, "psA", [128, 512], F32, 2, psum=True)
        psC = ph.enter_context(nc.psum_tensor("psC", [128, 512], F32))
        b_psC = Buf("psC", excl=True)

        dma("sp", lambda e: e.dma_start(out=cc[:], in_=cvec[:, :]), w=[b_small])
        dma("sp", lambda e: e.dma_start(out=badac_s[:], in_=badac[:, :]), w=[b_small])
        dma("sp", lambda e: e.dma_start(out=gcols_s[:], in_=gcols[:, :]), w=[b_small])
        op("act", lambda e: e.activation(out=sg_[:], in_=cc[:], func=AF.Sigmoid), r=[b_small], w=[b_small])
        op("dve", lambda e: e.tensor_tensor(out=siluc[:], in0=cc[:], in1=sg_[:], op=ALU.mult), r=[b_small], w=[b_small])
        op("dve", lambda e: e.tensor_copy(out=silurep[:], in_=siluc[:].unsqueeze(2).broadcast_to([128, KC, 128])),
           r=[b_small], w=[b_small])
        w_ada_v = w_ada.rearrange("(kc p) n -> p kc n", p=128)
        for idx, which in enumerate((1, 0, 4, 3)):
            for cq in range(4):
                wt, wb = wch.next()
                c0 = which * D + cq * 512
                dma("sp", lambda e, wt=wt, c0=c0: e.dma_start(out=wt[:], in_=w_ada_v[:, :, c0:c0 + 512]), w=[wb])
                for j in range(4):
                    col = idx * KC + cq * 4 + j
                    mmgroup(psC[:, col:col + 1], b_psC,
                            [(wt[:, kc, j * 128:(j + 1) * 128], siluc[:, kc:kc + 1]) for kc in range(KC)],
                            [wb, b_small])
        op("dve", lambda e: e.tensor_copy(out=rawc[:], in_=psC[:, 0:4 * KC]), r=[b_psC], w=[b_rawc])
        for idx, which in enumerate((1, 0, 4, 3)):
            op("dve", lambda e, idx=idx, which=which: e.tensor_tensor(
                out=rawc[:, idx * KC:(idx + 1) * KC], in0=rawc[:, idx * KC:(idx + 1) * KC],
                in1=badac_s[:, which * KC:(which + 1) * KC], op=ALU.add), r=[b_small, b_rawc], w=[b_rawc])
        op("dve", lambda e: e.scalar_tensor_tensor(out=modc[:, 0:KC], in0=rawc[:, 0:KC], scalar=1.0,
                                                   in1=gcols_s[:, 0:KC], op0=ALU.add, op1=ALU.mult),
           r=[b_rawc, b_small], w=[b_modc])
        op("dve", lambda e: e.tensor_copy(out=modc[:, KC:2 * KC], in_=rawc[:, KC:2 * KC]), r=[b_rawc], w=[b_modc])
        op("dve", lambda e: e.scalar_tensor_tensor(out=modc[:, 2 * KC:3 * KC], in0=rawc[:, 2 * KC:3 * KC], scalar=1.0,
                                                   in1=gcols_s[:, 2 * KC:3 * KC], op0=ALU.add, op1=ALU.mult),
           r=[b_rawc, b_small], w=[b_modc])
        op("dve", lambda e: e.tensor_copy(out=modc[:, 3 * KC:4 * KC], in_=rawc[:, 3 * KC:4 * KC]), r=[b_rawc], w=[b_modc])
        for gi, (which, grow, gpost) in enumerate(((2, ga1row, g_post_mix), (5, ga2row, g_post_ffn))):
            for cq in range(4):
                wt, wb = wch.next()
                c0 = which * D + cq * 512
                dma("sp", lambda e, wt=wt, c0=c0: e.dma_start(out=wt[:], in_=w_ada_v[:, :, c0:c0 + 512]), w=[wb])
                rt, rb = rowt.next()
                dma("sp", lambda e, rt=rt, c0=c0: e.dma_start(out=rt[:, 0, :], in_=b_ada[0:1, c0:c0 + 512].partition_broadcast(128)), w=[rb])
                dma("sp", lambda e, rt=rt, cq=cq, gpost=gpost: e.dma_start(
                    out=rt[:, 1, :], in_=gpost[0:1, cq * 512:(cq + 1) * 512].partition_broadcast(128)), w=[rb])
                pt, pb = psA.next()
                mmgroup(pt[:], pb, [(silurep[:, kc, :], wt[:, kc, :]) for kc in range(KC)], [wb, b_small])
                tt, tb = tmpr.next()
                op("dve", lambda e, tt=tt, pt=pt, rt=rt: e.tensor_tensor(out=tt[:], in0=pt[:], in1=rt[:, 0, :], op=ALU.add),
                   r=[pb, rb], w=[tb])
                op("dve", lambda e, tt=tt, rt=rt, grow=grow, cq=cq: e.tensor_tensor(
                    out=grow[:, cq * 512:(cq + 1) * 512], in0=tt[:], in1=rt[:, 1, :], op=ALU.mult),
                   r=[tb, rb], w=[b_garow[gi]])
        sch.barrier()
        sch.emit()
    if upto == "A":
        return finish()

    w_in_v = w_in.rearrange("(kc p) n -> p kc n", p=128)
    TWO_PI = 2.0 * math.pi
    BL = cfg.get('blevel', 9)
    ROPE_ADD_ENG = cfg.get('rope_add', 'dve')
    CW1 = 6.28125
    CW2 = TWO_PI - CW1

    def norm_transpose(x_src, tok0, hT, b_hT, hcol0, sc_off, xt_r, xn_r, junk, b_junk, st_r, psT_r, alt):
        xt, xb = xt_r.next()
        dma("sp", lambda e: e.dma_start(out=xt[:], in_=x_src[tok0:tok0 + 128, :]), w=[xb])
        st, sb_ = st_r.next()
        op("act", lambda e: e.activation(out=junk[:], in_=xt[:], func=AF.Square, accum_out=st[:, 0:1]),
           r=[xb], w=[b_junk, sb_])
        op("act", lambda e: e.activation(out=st[:, 1:2], in_=st[:, 0:1], func=AF.Sqrt, scale=1.0 / D, bias=EPS),
           r=[sb_], w=[sb_])
        op("dve", lambda e: e.reciprocal(out=st[:, 2:3], in_=st[:, 1:2]), r=[sb_], w=[sb_])
        xn, xnb = xn_r.next()
        op("dve", lambda e: e.tensor_scalar(out=xn[:], in0=xt[:], scalar1=st[:, 2:3], scalar2=None, op0=ALU.mult),
           r=[xb, sb_], w=[xnb])
        for half in range(2):
            pt, pb = psT_r.next()
            for j in range(8):
                kc = half * 8 + j
                op("pe", lambda e, pt=pt, j=j, kc=kc: e.transpose(pt[:, j * 128:(j + 1) * 128],
                                                                   xn[:, kc * 128:(kc + 1) * 128], identb[:]),
                   r=[xnb, b_cb] if j in (0, 7) else (), w=[pb])
            for j in range(8):
                kc = half * 8 + j
                eng = "act" if (half + alt) % 2 == 0 else "dve"
                if eng == "act":
                    op("act", lambda e, pt=pt, j=j, kc=kc: e.activation(
                        out=hT[:, kc, hcol0:hcol0 + 128], in_=pt[:, j * 128:(j + 1) * 128], func=AF.Identity,
                        scale=modc[:, sc_off + kc:sc_off + kc + 1], bias=modc[:, sc_off + KC + kc:sc_off + KC + kc + 1]),
                       r=[pb, b_modc], w=[b_hT])
                else:
                    op("dve", lambda e, pt=pt, j=j, kc=kc: e.tensor_scalar(
                        out=hT[:, kc, hcol0:hcol0 + 128], in0=pt[:, j * 128:(j + 1) * 128],
                        scalar1=modc[:, sc_off + kc:sc_off + kc + 1], scalar2=modc[:, sc_off + KC + kc:sc_off + KC + kc + 1],
                        op0=ALU.mult, op1=ALU.add), r=[pb, b_modc], w=[b_hT])

    def proj_pass(pname, x_src, pos_src, T, fm_blocks, tm_blocks):
        with ExitStack() as ph:
            sg = min(SG, T)
            ngr = sg // 512
            hT = ph.enter_context(nc.sbuf_tensor(pname + "hT", [128, KC, sg], BF16))
            b_hT = Buf("hT")
            xt_r = Ring(nc, ph, pname + "xt", [128, D], F32, 2)
            xn_r = Ring(nc, ph, pname + "xn", [128, D], BF16, 1)
            junk = ph.enter_context(nc.sbuf_tensor(pname + "junk", [128, D], BF16))
            b_junk = Buf("junk")
            st_r = Ring(nc, ph, pname + "st", [128, 4], F32, 4)
            psT_r = Ring(nc, ph, pname + "psT", [128, 1024], BF16, 2, psum=True)
            psM_r = Ring(nc, ph, pname + "psM", [128, 512], F32, 4, psum=True)
            psR_r = Ring(nc, ph, pname + "psR", [128, 512], F32, 2, psum=True)
            wfm_r = Ring(nc, ph, pname + "wfm", [128, KC, 128], BF16, 3)
            wtm_r = Ring(nc, ph, pname + "wtm", [128, KC, 512], BF16, 1)
            zb_r = Ring(nc, ph, pname + "zb", [128, 512], BF16, 2)
            t1_r = Ring(nc, ph, pname + "t1", [128, 512], F32, 2)
            t2_r = Ring(nc, ph, pname + "t2", [128, 512], F32, 2)
            ob_r = Ring(nc, ph, pname + "ob", [128, 512], BF16, 3)
            obf_r = Ring(nc, ph, pname + "obf", [128, 512], F32, 2)
            posi = ph.enter_context(nc.sbuf_tensor(pname + "posi", [128, 512], I32))
            posf = ph.enter_context(nc.sbuf_tensor(pname + "posf", [128, 512], F32))
            ang = ph.enter_context(nc.sbuf_tensor(pname + "ang", [128, 512], F32))
            kfi = ph.enter_context(nc.sbuf_tensor(pname + "kfi", [128, 512], I32))
            kff = ph.enter_context(nc.sbuf_tensor(pname + "kff", [128, 512], F32))
            msk = ph.enter_context(nc.sbuf_tensor(pname + "msk", [128, 512], F32))
            b_tmp = Buf("ropetmp")
            tabs = ph.enter_context(nc.sbuf_tensor(pname + "tabs", [128, ngr, 4, 512], F32))
            b_tabs = Buf("tabs")
            for s0 in range(0, T, sg):
                for ti in range(sg // 128):
                    norm_transpose(x_src, s0 + ti * 128, hT, b_hT, ti * 128, 0, xt_r, xn_r, junk, b_junk, st_r, psT_r, ti)
                for g in range(ngr if BL >= 2 else 0):
                    t0 = s0 + g * 512
                    dma("sp", lambda e, t0=t0: e.dma_start(out=posi[:], in_=pos_src[0:1, t0:t0 + 512].partition_broadcast(128)),
                        w=[b_tmp])
                    op("dve", lambda e: e.tensor_copy(out=posf[:], in_=posi[:]), r=[b_tmp], w=[b_tmp])
                    for ti_, (invf, shift) in enumerate(((invfA, math.pi / 2), (invfA, 0.0), (invfI, math.pi / 2), (invfI, 0.0))):
                        R_, W_ = [b_tmp, b_consts], [b_tmp]
                        op("dve", lambda e, invf=invf, shift=shift: e.tensor_scalar(
                            out=ang[:], in0=posf[:], scalar1=invf, scalar2=shift, op0=ALU.mult, op1=ALU.add), r=R_, w=W_)
                        op("dve", lambda e: e.tensor_scalar(out=kfi[:], in0=ang[:], scalar1=1.0 / TWO_PI, scalar2=None,
                                                            op0=ALU.mult), r=R_, w=W_)
                        op("dve", lambda e: e.tensor_copy(out=kff[:], in_=kfi[:]), r=R_, w=W_)
                        op("dve", lambda e: e.scalar_tensor_tensor(out=ang[:], in0=kff[:], scalar=-CW1, in1=ang[:],
                                                                   op0=ALU.mult, op1=ALU.add), r=R_, w=W_)
                        op("dve", lambda e: e.scalar_tensor_tensor(out=ang[:], in0=kff[:], scalar=-CW2, in1=ang[:],
                                                                   op0=ALU.mult, op1=ALU.add), r=R_, w=W_)
                        op("dve", lambda e: e.tensor_scalar(out=msk[:], in0=ang[:], scalar1=math.pi, scalar2=None,
                                                            op0=ALU.is_gt), r=R_, w=W_)
                        op("dve", lambda e: e.scalar_tensor_tensor(out=ang[:], in0=msk[:], scalar=-TWO_PI, in1=ang[:],
                                                                   op0=ALU.mult, op1=ALU.add), r=R_, w=W_)
                        op("dve", lambda e: e.tensor_scalar(out=msk[:], in0=ang[:], scalar1=-math.pi, scalar2=None,
                                                            op0=ALU.is_lt), r=R_, w=W_)
                        op("dve", lambda e: e.scalar_tensor_tensor(out=ang[:], in0=msk[:], scalar=TWO_PI, in1=ang[:],
                                                                   op0=ALU.mult, op1=ALU.add), r=R_, w=W_)
                        op("dve", lambda e: e.tensor_scalar(out=ang[:], in0=ang[:], scalar1=math.pi, scalar2=-math.pi,
                                                            op0=ALU.min, op1=ALU.max), r=R_, w=W_)
                        op("act", lambda e, g=g, ti_=ti_: e.activation(out=tabs[:, g, ti_, :], in_=ang[:], func=AF.Sin),
                           r=[b_tmp], w=[b_tabs, b_tmp])
                fmw = {}

                def fm_issue(bi):
                    if bi < len(fm_blocks) and bi not in fmw:
                        c0_, nc_ = fm_blocks[bi][0], fm_blocks[bi][1]
                        wt_, wb_ = wfm_r.next()
                        dma("pool", lambda e: e.dma_start(out=wt_[:, :, 0:nc_], in_=w_in_v[:, :, c0_:c0_ + nc_]), w=[wb_])
                        fmw[bi] = (wt_, wb_)

                for bi, (col0, ncol, dst, rope) in enumerate(fm_blocks):
                    fm_issue(bi)
                    fm_issue(bi + 1)
                    wt, wb = fmw.pop(bi)
                    for g in range(ngr):
                        t0 = s0 + g * 512
                        pt, pb = psM_r.next()
                        mmgroup(pt[0:ncol, :], pb,
                                [(wt[:, kc, 0:ncol], hT[:, kc, g * 512:(g + 1) * 512]) for kc in range(KC)], [wb, b_hT])
                        ot, obb = ob_r.next()
                        if rope is None:
                            op("act", lambda e, ot=ot, pt=pt, ncol=ncol: e.activation(out=ot[0:ncol, :], in_=pt[0:ncol, :], func=AF.Copy),
                               r=[pb], w=[obb])
                        else:
                            ci = 0 if rope == "A" else 2
                            rt = rtA if rope == "A" else rtI
                            zt, zbb = zb_r.next()
                            op("act", lambda e, zt=zt, pt=pt, ncol=ncol: e.activation(out=zt[0:ncol, :], in_=pt[0:ncol, :], func=AF.Copy),
                               r=[pb], w=[zbb])
                            p2, p2b = psR_r.next()
                            if cfg.get('ropevar', 0) != 1:
                                op("pe", lambda e, p2=p2, zt=zt, rt=rt, ncol=ncol: e.matmul(
                                    p2[0:ncol, :], rt[0:ncol, 0:ncol], zt[0:ncol, :], start=True, stop=True),
                                   r=[zbb, b_cb], w=[p2b])
                            else:
                                p2, p2b = pt, pb
                            a1, a1b = t1_r.next()
                            a2, a2b = t2_r.next()
                            if cfg.get('ropevar', 0) == 2:
                                op("act", lambda e, ot=ot, pt=pt, ncol=ncol: e.activation(out=ot[0:ncol, :], in_=pt[0:ncol, :], func=AF.Copy),
                                   r=[pb], w=[obb])
                                dma("sp", lambda e, ot=ot, dst=dst, t0=t0, ncol=ncol: e.dma_start(
                                    out=dst[0:ncol, t0:t0 + 512], in_=ot[0:ncol, :]), r=[obb])
                                continue
                            if cfg.get('ropevar', 0) == 3:
                                op("dve", lambda e, ot=ot, pt=pt, g=g, ci=ci, ncol=ncol: e.tensor_tensor(
                                    out=ot[0:ncol, :], in0=pt[0:ncol, :], in1=tabs[0:ncol, g, ci, :], op=ALU.mult),
                                   r=[pb, b_tabs], w=[obb])
                                dma("sp", lambda e, ot=ot, dst=dst, t0=t0, ncol=ncol: e.dma_start(
                                    out=dst[0:ncol, t0:t0 + 512], in_=ot[0:ncol, :]), r=[obb])
                                continue
                            op("dve", lambda e, a1=a1, pt=pt, g=g, ci=ci, ncol=ncol: e.tensor_tensor(
                                out=a1[0:ncol, :], in0=pt[0:ncol, :], in1=tabs[0:ncol, g, ci, :], op=ALU.mult),
                               r=[pb, b_tabs], w=[a1b])
                            op("dve", lambda e, a2=a2, p2=p2, g=g, ci=ci, ncol=ncol: e.tensor_tensor(
                                out=a2[0:ncol, :], in0=p2[0:ncol, :], in1=tabs[0:ncol, g, ci + 1, :], op=ALU.mult),
                               r=[p2b, b_tabs], w=[a2b])
                            op(ROPE_ADD_ENG, lambda e, ot=ot, a1=a1, a2=a2, ncol=ncol: e.tensor_tensor(
                                out=ot[0:ncol, :], in0=a1[0:ncol, :], in1=a2[0:ncol, :], op=ALU.add),
                               r=[a1b, a2b], w=[obb])
                        dma("sp", lambda e, ot=ot, dst=dst, t0=t0, ncol=ncol: e.dma_start(
                            out=dst[0:ncol, t0:t0 + 512], in_=ot[0:ncol, :]), r=[obb])
                for (col0, ncol, dst, dcol0, isf32) in tm_blocks:
                    if BL < 5:
                        continue
                    wt, wb = wtm_r.next()
                    dma("pool", lambda e, wt=wt, col0=col0, ncol=ncol: e.dma_start(
                        out=wt[:, :, 0:ncol], in_=w_in_v[:, :, col0:col0 + ncol]), w=[wb])
                    for ti in range(sg // 128):
                        t0 = s0 + ti * 128
                        pt, pb = psM_r.next()
                        mmgroup(pt[:, 0:ncol], pb,
                                [(hT[:, kc, ti * 128:(ti + 1) * 128], wt[:, kc, 0:ncol]) for kc in range(KC)], [wb, b_hT])
                        ot, obb = (obf_r if isf32 else ob_r).next()
                        op("act", lambda e, ot=ot, pt=pt, ncol=ncol: e.activation(out=ot[:, 0:ncol], in_=pt[:, 0:ncol], func=AF.Copy),
                           r=[pb], w=[obb])
                        dma("sp", lambda e, ot=ot, dst=dst, t0=t0, ncol=ncol, dcol0=dcol0: e.dma_start(
                            out=dst[t0:t0 + 128, dcol0:dcol0 + ncol], in_=ot[:, 0:ncol]), r=[obb])
            sch.barrier()
            sch.emit()

    fm1 = [(C_KA + j * 128, 128, KA[j], "A") for j in range(A_KV)]
    fm1 += [(C_KB + h * 128, 128, KB[h], None) for h in range(B_H)]
    fm1 += [(C_KI, 64, KI, "I")]
    tm1 = [(C_VA, 256, VA, 0, False), (C_VB, 512, VB, 0, False), (C_VB + 512, 512, VB, 512, False)]
    proj_pass("p1", x_all, pos_all, S, fm1, tm1)
    fm2 = [(C_QA + h * 128, 128, QA[h], "A") for h in range(A_H)]
    fm2 += [(C_QB + h * 128, 128, QB[h], None) for h in range(B_H)]
    fm2 += [(C_QI + h * 128, 128, QI[h], "I") for h in range(IDX_H // 2)]
    fm2 += [(C_GA + f * 128, 128, GA[f], None) for f in range(KC)]
    fm2 += [(C_GB + f * 128, 128, GB[f], None) for f in range(KC)]
    tm2 = [(C_WI, IDX_H, WI, 0, True)]
    proj_pass("p2", x_own, pos_own, So, fm2, tm2)
    if upto == "B":
        return finish()

    def pipeline(items, skews):
        if isinstance(skews, int):
            skews = [0, skews]
        n = len(items)
        for idx in range(n + max(skews)):
            for k, sk in enumerate(skews):
                if 0 <= idx - sk < n:
                    items[idx - sk][k]()

    def qblocks():
        for i in range(Go):
            for jj in range(4):
                qb = i * 4 + jj
                yield i, jj, qb, qb * 128, 128 * (4 * (2 * i + 1) + jj + 1), i % 2

    with ExitStack() as ph:
        kiT = ph.enter_context(nc.sbuf_tensor("kiT", [64, S], BF16))
        b_ki = Buf("kiT")
        amask_s = ph.enter_context(nc.sbuf_tensor("amask_s", [128, 2, 640], F32))
        b_am = Buf("amask")
        Ssc = ph.enter_context(nc.sbuf_tensor("Ssc", [128, S], F32))
        b_S = Buf("S")
        Mk = ph.enter_context(nc.sbuf_tensor("Mk", [128, S], BF16))
        b_M = Buf("Mk")
        qi_r = Ring(nc, ph, "qiT", [64, IDX_H, 128], BF16, 2)
        wi_r = Ring(nc, ph, "wit", [128, 3, IDX_H], F32, 2)
        rr_r = Ring(nc, ph, "rr", [128, 512], F32, 3)
        ps_r = Ring(nc, ph, "psI", [128, 512], F32, 4, psum=True)
        bs = ph.enter_context(nc.sbuf_tensor("bs", [128, 8], F32))
        b_bs = Buf("bs")
        dma("sp", lambda e: e.dma_start(out=kiT[:], in_=KI[:, :]), w=[b_ki])
        dma("sp", lambda e: e.dma_start(out=amask_s[:].rearrange("p m c -> p (m c)"), in_=amask_d[:, :]), w=[b_am])
        for (i, jj, qb, tok0, nk, m) in qblocks():
            qt, qbb = qi_r.next()
            for hh in range(2):
                dma("sp", lambda e, qt=qt, hh=hh, tok0=tok0: e.dma_start(
                    out=qt[:, hh::2, :], in_=QI[:, hh * 64:(hh + 1) * 64, tok0:tok0 + 128].rearrange("hp d t -> d hp t")),
                    w=[qbb])
            wt, wbb = wi_r.next()
            dma("sp", lambda e, wt=wt, tok0=tok0: e.dma_start(out=wt[:, 0, :], in_=WI[tok0:tok0 + 128, :]), w=[wbb])
            op("act", lambda e, wt=wt: e.activation(out=wt[:, 1, :], in_=wt[:, 0, :], func=AF.Abs), r=[wbb], w=[wbb])
            op("act", lambda e, wt=wt: e.activation(out=wt[:, 2, :], in_=wt[:, 0, :], func=AF.Sign), r=[wbb], w=[wbb])
            nch = (nk + 511) // 512
            for c in range(nch):
                c0 = c * 512
                cw = min(512, nk - c0)
                for h in range(IDX_H):
                    pt, pb = ps_r.next()
                    op("pe", lambda e, pt=pt, qt=qt, h=h, c0=c0, cw=cw: e.matmul(
                        pt[:, 0:cw], qt[:, h, :], kiT[:, c0:c0 + cw], start=True, stop=True),
                       r=[qbb, b_ki], w=[pb])
                    rt, rb = rr_r.next()
                    op("act", lambda e, rt=rt, pt=pt, wt=wt, h=h, cw=cw: e.activation(
                        out=rt[:, 0:cw], in_=pt[:, 0:cw], func=AF.Relu, scale=wt[:, 1, h:h + 1]),
                       r=[pb, wbb], w=[rb])
                    if h == 0:
                        op("dve", lambda e, rt=rt, wt=wt, h=h, c0=c0, cw=cw: e.tensor_scalar(
                            out=Ssc[:, c0:c0 + cw], in0=rt[:, 0:cw], scalar1=wt[:, 2, h:h + 1], scalar2=None, op0=ALU.mult),
                           r=[rb, wbb], w=[b_S])
                    else:
                        op("dve", lambda e, rt=rt, wt=wt, h=h, c0=c0, cw=cw: e.scalar_tensor_tensor(
                            out=Ssc[:, c0:c0 + cw], in0=rt[:, 0:cw], scalar=wt[:, 2, h:h + 1], in1=Ssc[:, c0:c0 + cw],
                            op0=ALU.mult, op1=ALU.add), r=[rb, wbb, b_S], w=[b_S])
            op("dve", lambda e, nk=nk: e.tensor_reduce(out=bs[:, 5:6], in_=Ssc[:, 0:nk], axis=AX.X, op=ALU.max),
               r=[b_S], w=[b_bs])
            op("dve", lambda e, nk=nk: e.tensor_reduce(out=bs[:, 0:1], in_=Ssc[:, 0:nk], axis=AX.X, op=ALU.min),
               r=[b_S], w=[b_bs])
            op("dve", lambda e: e.tensor_tensor(out=bs[:, 1:2], in0=bs[:, 5:6], in1=bs[:, 0:1], op=ALU.subtract),
               r=[b_bs], w=[b_bs])
            op("dve", lambda e, nk=nk, m=m: e.tensor_tensor(out=Ssc[:, nk - 640:nk], in0=Ssc[:, nk - 640:nk],
                                                             in1=amask_s[:, m, :], op=ALU.add),
               r=[b_S, b_am], w=[b_S])
            for it in range(NBIS):
                op("dve", lambda e: e.tensor_scalar(out=bs[:, 1:2], in0=bs[:, 1:2], scalar1=0.5, scalar2=None, op0=ALU.mult),
                   r=[b_bs], w=[b_bs])
                op("dve", lambda e: e.tensor_tensor(out=bs[:, 2:3], in0=bs[:, 0:1], in1=bs[:, 1:2], op=ALU.add),
                   r=[b_bs], w=[b_bs])
                op("dve", lambda e, nk=nk: e.tensor_scalar(out=Mk[:, 0:nk], in0=Ssc[:, 0:nk], scalar1=bs[:, 2:3], scalar2=None,
                                                           op0=ALU.is_ge, op1=ALU.add, accum_out=bs[:, 3:4]),
                   r=[b_bs, b_S, b_M], w=[b_M, b_bs])
                op("dve", lambda e: e.tensor_scalar(out=bs[:, 4:5], in0=bs[:, 3:4], scalar1=float(TOPK), scalar2=bs[:, 1:2],
                                                    op0=ALU.is_ge, op1=ALU.mult), r=[b_bs], w=[b_bs])
                op("dve", lambda e: e.tensor_tensor(out=bs[:, 0:1], in0=bs[:, 0:1], in1=bs[:, 4:5], op=ALU.add),
                   r=[b_bs], w=[b_bs])
            op("dve", lambda e, nk=nk: e.tensor_scalar(out=Mk[:, 0:nk], in0=Ssc[:, 0:nk], scalar1=bs[:, 0:1], scalar2=None,
                                                       op0=ALU.is_ge), r=[b_bs, b_S, b_M], w=[b_M])
            dma("sp", lambda e, qb=qb, nk=nk: e.dma_start(out=MK[qb, :, 0:nk], in_=Mk[:, 0:nk]), r=[b_M])
        sch.barrier()
        sch.emit()
    if upto == "C1":
        return finish()

    with ExitStack() as ph:
        kaT = ph.enter_context(nc.sbuf_tensor("kaT", [128, A_KV, S], BF16))
        va = ph.enter_context(nc.sbuf_tensor("va_s", [128, NT, A_KV * HD], BF16))
        b_kv = Buf("kv")
        mk_r = Ring(nc, ph, "mk", [128, S], BF16, 2)
        mt_r = Ring(nc, ph, "mt", [128, NT, 128], BF16, 2)
        qa_r = Ring(nc, ph, "qa", [128, A_H * 128], BF16, 2)
        e_r = Ring(nc, ph, "ee", [128, 512], BF16, 3)
        p_r = Ring(nc, ph, "pp", [128, 512], BF16, 4)
        rd_r = Ring(nc, ph, "rd", [128, 512], F32, 2)
        of_r = Ring(nc, ph, "of", [128, 512], F32, 2)
        ob_r = Ring(nc, ph, "obc", [128, 512], BF16, 2)
        psS = Ring(nc, ph, "psS", [128, 512], F32, 3, psum=True)
        psO = Ring(nc, ph, "psO", [128, 512], F32, 2, psum=True)
        psD = Ring(nc, ph, "psD", [128, 512], F32, 2, psum=True)
        psT = Ring(nc, ph, "psTc", [128, 1024], BF16, 1, psum=True)
        for j in range(A_KV):
            dma("sp", lambda e, j=j: e.dma_start(out=kaT[:, j, :], in_=KA[j]), w=[b_kv])
        dma("sp", lambda e: e.dma_start(out=va[:], in_=VA.rearrange("(kb p) c -> p kb c", p=128)), w=[b_kv])
        isq = 1.0 / math.sqrt(HD)
        items = []
        for (i, jj, qb, tok0, nk, m) in qblocks():
            nkb = nk // 128
            qst = {}

            def setup(qst=qst, qb=qb, tok0=tok0, nk=nk, nkb=nkb):
                mkt, mkb = mk_r.next()
                dma("sp", lambda e: e.dma_start(out=mkt[:, 0:nk], in_=MK[qb, :, 0:nk]), w=[mkb])
                qt, qbb = qa_r.next()
                dma("sp", lambda e: e.dma_start(
                    out=qt[:].rearrange("p (h t) -> p h t", h=A_H), in_=QA[:, :, tok0:tok0 + 128].rearrange("h d t -> d h t")),
                    w=[qbb])
                mtt, mtb = mt_r.next()
                for k0 in range(0, nkb, 8):
                    n8 = min(8, nkb - k0)
                    pt, pb = psT.next()
                    for u in range(n8):
                        op("pe", lambda e, pt=pt, u=u, k0=k0: e.transpose(
                            pt[:, u * 128:(u + 1) * 128], mkt[:, (k0 + u) * 128:(k0 + u + 1) * 128], identb[:]),
                           r=[mkb, b_cb] if u in (0, n8 - 1) else (), w=[pb])
                    op("act", lambda e, pt=pt, k0=k0, n8=n8: e.activation(
                        out=mtt[:, k0:k0 + n8, :].rearrange("p k t -> p (k t)"), in_=pt[:, 0:n8 * 128], func=AF.Copy),
                       r=[pb], w=[mtb])
                qst.update(qt=qt, qbb=qbb, mtt=mtt, mtb=mtb)

            for j in range(A_KV):
                jst = {}
                for kb in range(nkb):
                    it = {}

                    def stA(it=it, qst=qst, jst=jst, j=j, kb=kb, first=(j == 0 and kb == 0), setup=setup):
                        if first:
                            setup()
                        if kb == 0:
                            jst["po"], jst["pob"] = psO.next()
                            jst["pd"], jst["pdb"] = psD.next()
                        qt, qbb, mtt, mtb = qst["qt"], qst["qbb"], qst["mtt"], qst["mtb"]
                        ps, psb = psS.next()
                        op("pe", lambda e: e.matmul(ps[:], kaT[:, j, kb * 128:(kb + 1) * 128], qt[:, j * 512:(j + 1) * 512],
                                                    start=True, stop=True), r=[b_kv, qbb], w=[psb])
                        et, eb = e_r.next()
                        op("act", lambda e: e.activation(out=et[:], in_=ps[:], func=AF.Exp, scale=isq), r=[psb], w=[eb])
                        pp, ppb = p_r.next()
                        op("dve", lambda e: e.tensor_tensor(
                            out=pp[:].rearrange("p (g t) -> p g t", g=4), in0=et[:].rearrange("p (g t) -> p g t", g=4),
                            in1=mtt[:, kb, :].unsqueeze(1).broadcast_to([128, 4, 128]), op=ALU.mult), r=[eb, mtb], w=[ppb])
                        it["pp"], it["ppb"] = pp, ppb

                    def stB(it=it, jst=jst, j=j, kb=kb, nkb=nkb, tok0=tok0):
                        pp, ppb = it["pp"], it["ppb"]
                        po, pob, pd, pdb = jst["po"], jst["pob"], jst["pd"], jst["pdb"]
                        op("pe", lambda e: e.matmul(po[:], va[:, kb, j * 128:(j + 1) * 128], pp[:],
                                                    start=(kb == 0), stop=(kb == nkb - 1)), r=[ppb, b_kv], w=[pob])
                        op("pe", lambda e: e.matmul(pd[:], onesb[:], pp[:], start=(kb == 0), stop=(kb == nkb - 1)),
                           r=[ppb, b_cb], w=[pdb])
                        if kb == nkb - 1:
                            rd, rdb = rd_r.next()
                            op("dve", lambda e: e.reciprocal(out=rd[:], in_=pd[:]), r=[pdb], w=[rdb])
                            of_, ofb = of_r.next()
                            op("dve", lambda e: e.tensor_tensor(out=of_[:], in0=po[:], in1=rd[:], op=ALU.mult),
                               r=[pob, rdb], w=[ofb])
                            obt, obb = ob_r.next()
                            op("act", lambda e: e.activation(out=obt[:], in_=of_[:], func=AF.Copy), r=[ofb], w=[obb])
                            dma("sp", lambda e: e.dma_start(
                                out=OA[4 * j:4 * j + 4, :, tok0:tok0 + 128].rearrange("h d t -> d h t"),
                                in_=obt[:].rearrange("p (h t) -> p h t", h=4)), r=[obb])

                    items.append((stA, stB))
        pipeline(items, 2)
        sch.barrier()
        sch.emit()
    if upto == "C2":
        return finish()

    with ExitStack() as ph:
        cmask_s = ph.enter_context(nc.sbuf_tensor("cmask_s", [128, 2, 8, 512], BF16))
        b_cm = Buf("cmask")
        dma("sp", lambda e: e.dma_start(out=cmask_s[:].rearrange("p m j t -> p (m j t)"), in_=cmask_d[:, :]), w=[b_cm])
        kb_r = Ring(nc, ph, "kbT", [128, S], BF16, 2)
        vb_r = Ring(nc, ph, "vbh", [128, NT, 128], BF16, 2)
        qb_r = Ring(nc, ph, "qbT", [128, So], BF16, 2)
        E_r = Ring(nc, ph, "sbE", [128, 512], F32, 4)
        Em_r = Ring(nc, ph, "sbEm", [128, 512], F32, 4)
        SP_r = Ring(nc, ph, "sbSP", [128, 512], BF16, 4)
        SPm_r = Ring(nc, ph, "sbSPm", [128, 512], BF16, 4)
        X_r = Ring(nc, ph, "sbX", [128, 512], F32, 3)
        At_r = Ring(nc, ph, "sbAt", [128, 512], BF16, 4)
        Ra_r = Ring(nc, ph, "sbRa", [128, 512], F32, 2)
        obD_r = Ring(nc, ph, "obD", [128, 512], BF16, 2)
        psZ = Ring(nc, ph, "psZ", [128, 512], F32, 3, psum=True)
        psCc = Ring(nc, ph, "psCc", [128, 512], F32, 3, psum=True)
        psOd = Ring(nc, ph, "psOd", [128, 512], F32, 2, psum=True)
        isq = 1.0 / math.sqrt(HD)
        items = []
        for h in range(B_H):
            hst = {}

            def hsetup(hst=hst, h=h):
                kt, ktb = kb_r.next()
                vt, vtb = vb_r.next()
                qt, qtb = qb_r.next()
                dma("sp", lambda e: e.dma_start(out=kt[:], in_=KB[h]), w=[ktb])
                dma("sp", lambda e: e.dma_start(
                    out=vt[:], in_=VB[:, h * 128:(h + 1) * 128].rearrange("(kb p) d -> p kb d", p=128)), w=[vtb])
                dma("sp", lambda e: e.dma_start(out=qt[:], in_=QB[h]), w=[qtb])
                hst.update(kt=kt, ktb=ktb, vt=vt, vtb=vtb, qt=qt, qtb=qtb)

            for i in range(Go):
                nkb = 4 * (2 * i + 2)
                m = i % 2
                gst = {}
                for n, kb in enumerate(reversed(range(nkb))):
                    j = kb - (nkb - 8)
                    it = {}

                    def stA(it=it, hst=hst, gst=gst, i=i, n=n, kb=kb, j=j, m=m, first=(i == 0 and n == 0), hsetup=hsetup):
                        if first:
                            hsetup()
                        if n == 0:
                            gst["ra"], gst["rab"] = Ra_r.next()
                            gst["po"], gst["pob"] = psOd.next()
                        kt, ktb, qt, qtb = hst["kt"], hst["ktb"], hst["qt"], hst["qtb"]
                        pz, pzb = psZ.next()
                        op("pe", lambda e: e.matmul(pz[:], kt[:, kb * 128:(kb + 1) * 128], qt[:, i * 512:(i + 1) * 512],
                                                    start=True, stop=True), r=[ktb, qtb], w=[pzb])
                        Et, Eb = E_r.next()
                        op("act", lambda e: e.activation(out=Et[:], in_=pz[:], func=AF.Exp, scale=isq), r=[pzb], w=[Eb])
                        St, Sb = SP_r.next()
                        op("act", lambda e: e.activation(out=St[:], in_=Et[:], func=AF.Ln, bias=1.0), r=[Eb], w=[Sb])
                        if j >= 0:
                            Sm, Smb = SPm_r.next()
                            op("dve", lambda e: e.tensor_tensor(out=Sm[:], in0=St[:], in1=cmask_s[:, m, j, :], op=ALU.mult),
                               r=[Sb, b_cm], w=[Smb])
                            Em, Emb = Em_r.next()
                            op("pool", lambda e: e.tensor_tensor(out=Em[:], in0=Et[:], in1=cmask_s[:, m, j, :], op=ALU.mult),
                               r=[Eb, b_cm], w=[Emb])
                        else:
                            Sm, Smb = St, Sb
                            Em, Emb = Et, Eb
                        it.update(Sm=Sm, Smb=Smb, Em=Em, Emb=Emb)

                    def stB1(it=it, gst=gst, n=n, nkb=nkb):
                        Sm, Smb = it["Sm"], it["Smb"]
                        ra, rab = gst["ra"], gst["rab"]
                        pc, pcb = psCc.next()
                        op("pe", lambda e: e.matmul(pc[:], triI[:], Sm[:], start=True, stop=(n == 0)), r=[Smb, b_cb], w=[pcb])
                        if n > 0:
                            op("pe", lambda e: e.matmul(pc[:], onesf, ra[:], start=False, stop=True), r=[rab, b_consts], w=[pcb])
                        if n == 0:
                            op("pool", lambda e: e.tensor_copy(out=ra[:], in_=Sm[:]), r=[Smb], w=[rab])
                        elif n < nkb - 1:
                            op("pool", lambda e: e.tensor_tensor(out=ra[:], in0=ra[:], in1=Sm[:], op=ALU.add), r=[Smb, rab], w=[rab])
                        it.update(pc=pc, pcb=pcb)

                    def stB2(it=it):
                        Em, Emb, pc, pcb = it["Em"], it["Emb"], it["pc"], it["pcb"]
                        Xt, Xb = X_r.next()
                        op("act", lambda e: e.activation(out=Xt[:], in_=pc[:], func=AF.Exp, scale=-1.0), r=[pcb], w=[Xb])
                        At, Ab = At_r.next()
                        op("dve", lambda e: e.tensor_tensor(out=At[:], in0=Em[:], in1=Xt[:], op=ALU.mult), r=[Emb, Xb], w=[Ab])
                        it.update(At=At, Ab=Ab)

                    def stB3(it=it, hst=hst, gst=gst, h=h, i=i, n=n, kb=kb, nkb=nkb):
                        At, Ab = it["At"], it["Ab"]
                        po, pob = gst["po"], gst["pob"]
                        vt, vtb = hst["vt"], hst["vtb"]
                        op("pe", lambda e: e.matmul(po[:], vt[:, kb, :], At[:], start=(n == 0), stop=(n == nkb - 1)),
                           r=[vtb, Ab], w=[pob])
                        if n == nkb - 1:
                            ot, otb = obD_r.next()
                            op("act", lambda e: e.activation(out=ot[:], in_=po[:], func=AF.Copy), r=[pob], w=[otb])
                            dma("sp", lambda e: e.dma_start(out=OB[h, :, i * 512:(i + 1) * 512], in_=ot[:]), r=[otb])

                    items.append((stA, stB1, stB2, stB3))
        pipeline(items, [0, 2, 3, 5])
        sch.barrier()
        sch.emit()
    if upto == "D":
        return finish()

    with ExitStack() as ph:
        wa = ph.enter_context(nc.sbuf_tensor("wa", [128, 8, D], BF16))
        wb_ = ph.enter_context(nc.sbuf_tensor("wb_", [128, 8, D], BF16))
        b_wab = Buf("wab")
        for (wt, src) in ((wa, w_bra), (wb_, w_brb)):
            sv = src.rearrange("(kc p) n -> p kc n", p=128)
            for cq in range(4):
                dma("pool", lambda e, wt=wt, sv=sv, cq=cq: e.dma_start(
                    out=wt[:, :, cq * 512:(cq + 1) * 512], in_=sv[:, :, cq * 512:(cq + 1) * 512]), w=[b_wab])
        zt = ph.enter_context(nc.sbuf_tensor("zt", [128, 4096], BF16))
        b_zt = Buf("zt")
        op("dve", lambda e: e.memset(zt[:], 0.0), w=[b_zt])
        XGf = XG.rearrange("(r p two) c -> r p (two c)", p=128, two=2)
        for r_ in range(NE * CAP // 256):
            dma("sp", lambda e, r_=r_: e.dma_start(out=XGf[r_], in_=zt[:]), r=[b_zt])
        oa_r = Ring(nc, ph, "oaT", [128, 8, 512], BF16, 2)
        ob2_r = Ring(nc, ph, "obT", [128, 8, 512], BF16, 2)
        g_r = Ring(nc, ph, "gt", [128, 2, 512], BF16, 3)
        sg_r = Ring(nc, ph, "sgt", [128, 2, 512], F32, 2)
        t1e_r = Ring(nc, ph, "t1e", [128, 512], F32, 2)
        t2e_r = Ring(nc, ph, "t2e", [128, 512], F32, 2)
        mg_r = Ring(nc, ph, "mgo", [128, 512], BF16, 3)
        psa_r = Ring(nc, ph, "psEa", [128, 512], F32, 3, psum=True)
        psb_r = Ring(nc, ph, "psEb", [128, 512], F32, 3, psum=True)
        for tg in range(So // 512):
            t0 = tg * 512
            oat, oab = oa_r.next()
            obt, obb = ob2_r.next()
            dma("sp", lambda e, oat=oat, t0=t0: e.dma_start(out=oat[:], in_=OA[:, :, t0:t0 + 512].rearrange("h d t -> d h t")), w=[oab])
            dma("sp", lambda e, obt=obt, t0=t0: e.dma_start(out=obt[:], in_=OB[:, :, t0:t0 + 512].rearrange("h d t -> d h t")), w=[obb])
            for fo in range(KC):
                gt, gb_ = g_r.next()
                dma("sp", lambda e, gt=gt, fo=fo, t0=t0: e.dma_start(out=gt[:, 0, :], in_=GA[fo, :, t0:t0 + 512]), w=[gb_])
                dma("sp", lambda e, gt=gt, fo=fo, t0=t0: e.dma_start(out=gt[:, 1, :], in_=GB[fo, :, t0:t0 + 512]), w=[gb_])
                st_, sb_ = sg_r.next()
                op("act", lambda e, st_=st_, gt=gt: e.activation(out=st_[:], in_=gt[:], func=AF.Sigmoid), r=[gb_], w=[sb_])
                pa, pab = psa_r.next()
                mmgroup(pa[:], pab, [(wa[:, kc, fo * 128:(fo + 1) * 128], oat[:, kc, :]) for kc in range(8)], [b_wab, oab])
                pb2, pbb = psb_r.next()
                mmgroup(pb2[:], pbb, [(wb_[:, kc, fo * 128:(fo + 1) * 128], obt[:, kc, :]) for kc in range(8)], [b_wab, obb])
                a1, a1b = t1e_r.next()
                a2, a2b = t2e_r.next()
                op("dve", lambda e, a1=a1, pa=pa, st_=st_: e.tensor_tensor(out=a1[:], in0=pa[:], in1=st_[:, 0, :], op=ALU.mult),
                   r=[pab, sb_], w=[a1b])
                op("dve", lambda e, a2=a2, pb2=pb2, st_=st_: e.tensor_tensor(out=a2[:], in0=pb2[:], in1=st_[:, 1, :], op=ALU.mult),
                   r=[pbb, sb_], w=[a2b])
                mt_, mb_ = mg_r.next()
                op("pool", lambda e, mt_=mt_, a1=a1, a2=a2: e.tensor_tensor(out=mt_[:], in0=a1[:], in1=a2[:], op=ALU.add),
                   r=[a1b, a2b], w=[mb_])
                dma("sp", lambda e, mt_=mt_, fo=fo, t0=t0: e.dma_start(out=MG[fo, :, t0:t0 + 512], in_=mt_[:]), r=[mb_])
        sch.barrier()
        sch.emit()
    if upto == "E1":
        return finish()

    NSLOT = NE * CAP
    NSOK = min(NSLOT, 65535)
    with ExitStack() as ph:
        wo = ph.enter_context(nc.sbuf_tensor("wo", [128, KC, D], BF16))
        b_wo = Buf("wo")
        wo_v = w_out.rearrange("(kc p) n -> p kc n", p=128)
        for cq in range(4):
            dma("pool", lambda e, cq=cq: e.dma_start(out=wo[:, :, cq * 512:(cq + 1) * 512], in_=wo_v[:, :, cq * 512:(cq + 1) * 512]),
                w=[b_wo])
        wr = ph.enter_context(nc.sbuf_tensor("wr", [128, KC, NE], BF16))
        b_wr = Buf("wr")
        dma("pool", lambda e: e.dma_start(out=wr[:], in_=w_router.rearrange("(kc p) n -> p kc n", p=128)), w=[b_wr])
        brow = ph.enter_context(nc.sbuf_tensor("brow", [128, NE], F32))
        dma("sp", lambda e: e.dma_start(out=brow[:], in_=b_router[0:1, :].partition_broadcast(128)), w=[b_wr])
        base = ph.enter_context(nc.sbuf_tensor("base", [128, NE], F32))
        slotbase = ph.enter_context(nc.sbuf_tensor("slotbase", [128, NE], F32))
        b_base = Buf("base")
        op("dve", lambda e: e.memset(base[:], 0.0), w=[b_base])
        op("dve", lambda e: e.tensor_scalar(out=slotbase[:], in0=iota_f[:, 0:NE], scalar1=float(CAP), scalar2=None, op0=ALU.mult),
           r=[b_consts], w=[b_base])
        mg_r = Ring(nc, ph, "mgi", [128, KC, 512], BF16, 1)
        x_r = Ring(nc, ph, "xe", [128, D], F32, 2)
        mix_r = Ring(nc, ph, "mixs", [128, D], F32, 1)
        x1_r = Ring(nc, ph, "x1t", [128, D], F32, 2)
        xn_r = Ring(nc, ph, "xn2", [128, D], BF16, 2)
        h2_r = Ring(nc, ph, "h2T", [128, KC, 128], BF16, 2)
        junk = ph.enter_context(nc.sbuf_tensor("junkE", [128, D], BF16))
        b_junk = Buf("junkE")
        st_r = Ring(nc, ph, "stE", [128, 8], F32, 4)
        rt_r = Ring(nc, ph, "rtE", [128, 8, NE], F32, 2)
        mkb_r = Ring(nc, ph, "mkE", [128, NE], BF16, 2)
        t8_r = Ring(nc, ph, "t8E", [128, 24], F32, 2)
        psm_r = Ring(nc, ph, "psEm", [128, 512], F32, 3, psum=True)
        pst_r = Ring(nc, ph, "psEt", [128, 1024], BF16, 2, psum=True)
        psr_r = Ring(nc, ph, "psEr", [128, 512], F32, 2, psum=True)
        for tg in range(So // 512):
            mgt, mgb = mg_r.next()
            dma("sp", lambda e, mgt=mgt, tg=tg: e.dma_start(
                out=mgt[:], in_=MG[:, :, tg * 512:(tg + 1) * 512].rearrange("f d t -> d f t")), w=[mgb])
            for tt in range(4):
                ti = tg * 4 + tt
                tok0 = ti * 128
                xt, xb = x_r.next()
                dma("sp", lambda e, xt=xt, tok0=tok0: e.dma_start(out=xt[:], in_=x_own[tok0:tok0 + 128, :]), w=[xb])
                mx, mxb = mix_r.next()
                for dc in range(4):
                    pm, pmb = psm_r.next()
                    mmgroup(pm[:], pmb, [(mgt[:, kc, tt * 128:(tt + 1) * 128], wo[:, kc, dc * 512:(dc + 1) * 512])
                                         for kc in range(KC)], [mgb, b_wo])
                    op("act", lambda e, mx=mx, pm=pm, dc=dc: e.activation(out=mx[:, dc * 512:(dc + 1) * 512], in_=pm[:], func=AF.Copy),
                       r=[pmb], w=[mxb])
                st, sb_ = st_r.next()
                op("act", lambda e, st=st, mx=mx: e.activation(out=junk[:], in_=mx[:], func=AF.Square, accum_out=st[:, 0:1]),
                   r=[mxb], w=[b_junk, sb_])
                op("act", lambda e, st=st: e.activation(out=st[:, 1:2], in_=st[:, 0:1], func=AF.Sqrt, scale=1.0 / D, bias=EPS),
                   r=[sb_], w=[sb_])
                op("dve", lambda e, st=st: e.reciprocal(out=st[:, 2:3], in_=st[:, 1:2]), r=[sb_], w=[sb_])
                op("dve", lambda e, st=st, mx=mx: e.scalar_tensor_tensor(out=mx[:], in0=mx[:], scalar=st[:, 2:3], in1=ga1row[:],
                                                                        op0=ALU.mult, op1=ALU.mult),
                   r=[mxb, sb_, b_garow[0]], w=[mxb])
                x1, x1b = x1_r.next()
                op("pool", lambda e, x1=x1, mx=mx, xt=xt: e.tensor_tensor(out=x1[:], in0=mx[:], in1=xt[:], op=ALU.add),
                   r=[mxb, xb], w=[x1b])
                dma("sp", lambda e, x1=x1, tok0=tok0: e.dma_start(out=X1[tok0:tok0 + 128, :], in_=x1[:]), r=[x1b])
                op("act", lambda e, st=st, x1=x1: e.activation(out=junk[:], in_=x1[:], func=AF.Square, accum_out=st[:, 3:4]),
                   r=[x1b], w=[b_junk, sb_])
                op("act", lambda e, st=st: e.activation(out=st[:, 4:5], in_=st[:, 3:4], func=AF.Sqrt, scale=1.0 / D, bias=EPS),
                   r=[sb_], w=[sb_])
                op("dve", lambda e, st=st: e.reciprocal(out=st[:, 5:6], in_=st[:, 4:5]), r=[sb_], w=[sb_])
                xn, xnb = xn_r.next()
                op("dve", lambda e, xn=xn, x1=x1, st=st: e.tensor_scalar(out=xn[:], in0=x1[:], scalar1=st[:, 5:6], scalar2=None,
                                                                       op0=ALU.mult), r=[x1b, sb_], w=[xnb])
                h2, h2b = h2_r.next()
                for half in range(2):
                    pt, pb = pst_r.next()
                    for j in range(8):
                        kc = half * 8 + j
                        op("pe", lambda e, pt=pt, j=j, kc=kc, xn=xn: e.transpose(
                            pt[:, j * 128:(j + 1) * 128], xn[:, kc * 128:(kc + 1) * 128], identb[:]),
                           r=[xnb, b_cb] if j in (0, 7) else (), w=[pb])
                    for j in range(8):
                        kc = half * 8 + j
                        op("act", lambda e, pt=pt, j=j, kc=kc, h2=h2: e.activation(
                            out=h2[:, kc, :], in_=pt[:, j * 128:(j + 1) * 128], func=AF.Identity,
                            scale=modc[:, 2 * KC + kc:2 * KC + kc + 1], bias=modc[:, 3 * KC + kc:3 * KC + kc + 1]),
                           r=[pb, b_modc], w=[h2b])
                pr, prb = psr_r.next()
                mmgroup(pr[:, 0:NE], prb, [(h2[:, kc, :], wr[:, kc, :]) for kc in range(KC)], [h2b, b_wr])
                rt, rtb = rt_r.next()
                t8, t8b = t8_r.next()
                mk, mkb = mkb_r.next()
                lg, pos_, sla, ov, jk = (rt[:, q, :] for q in range(5))
                op("dve", lambda e, lg=lg, pr=pr: e.tensor_tensor(out=lg, in0=pr[:, 0:NE], in1=brow[:], op=ALU.add),
                   r=[prb, b_wr], w=[rtb])
                op("dve", lambda e, lg=lg, t8=t8: e.max(out=t8[:, 0:8], in_=lg), r=[rtb], w=[t8b])
                op("dve", lambda e, t8=t8: e.tensor_scalar(out=t8[:, 8:12], in0=t8[:, 0:4], scalar1=t8[:, 0:1], scalar2=None,
                                                          op0=ALU.subtract), r=[t8b], w=[t8b])
                op("act", lambda e, t8=t8, st=st: e.activation(out=t8[:, 12:16], in_=t8[:, 8:12], func=AF.Exp, accum_out=st[:, 6:7]),
                   r=[t8b], w=[t8b, sb_])
                op("dve", lambda e, st=st: e.reciprocal(out=st[:, 7:8], in_=st[:, 6:7]), r=[sb_], w=[sb_])
                op("dve", lambda e, mk=mk, lg=lg, t8=t8: e.tensor_scalar(out=mk[:], in0=lg, scalar1=t8[:, 3:4], scalar2=None,
                                                                       op0=ALU.is_ge), r=[rtb, t8b], w=[mkb])
                pp, ppb = psr_r.next()
                op("pe", lambda e, pp=pp, mk=mk: e.matmul(pp[:, 0:NE], triS[:], mk[:], start=True, stop=True),
                   r=[mkb, b_cb], w=[ppb])
                op("pe", lambda e, pp=pp, mk=mk: e.matmul(pp[:, 64:64 + NE], onesb[:], mk[:], start=True, stop=True),
                   r=[mkb, b_cb], w=[ppb])
                op("dve", lambda e, pos_=pos_, pp=pp: e.tensor_tensor(out=pos_, in0=pp[:, 0:NE], in1=base[:], op=ALU.add),
                   r=[ppb, b_base], w=[rtb])
                op("dve", lambda e, pp=pp: e.tensor_tensor(out=base[:], in0=pp[:, 64:64 + NE], in1=base[:], op=ALU.add),
                   r=[ppb, b_base], w=[b_base])
                op("dve", lambda e, ov=ov, pos_=pos_: e.tensor_scalar(out=ov, in0=pos_, scalar1=float(CAP), scalar2=1.0e9,
                                                                    op0=ALU.is_ge, op1=ALU.mult), r=[rtb], w=[rtb])
                op("dve", lambda e, sla=sla, pos_=pos_: e.tensor_tensor(out=sla, in0=pos_, in1=slotbase[:], op=ALU.add),
                   r=[rtb, b_base], w=[rtb])
                op("dve", lambda e, sla=sla, ov=ov: e.tensor_tensor(out=sla, in0=sla, in1=ov, op=ALU.add), r=[rtb], w=[rtb])
                for k in range(4):
                    op("dve", lambda e, jk=jk, lg=lg, t8=t8, sla=sla, k=k: e.scalar_tensor_tensor(
                        out=jk, in0=lg, scalar=t8[:, k:k + 1], in1=sla, op0=ALU.is_equal, op1=ALU.mult,
                        accum_out=t8[:, 16 + k:17 + k]), r=[rtb, t8b], w=[rtb, t8b])
                op("dve", lambda e, t8=t8, ti=ti: e.tensor_copy(out=rslot[:, ti * 4:(ti + 1) * 4], in_=t8[:, 16:20]),
                   r=[t8b], w=[b_route])
                op("dve", lambda e, t8=t8: e.tensor_scalar(out=t8[:, 20:24], in0=t8[:, 16:20], scalar1=float(NSOK), scalar2=None,
                                                          op0=ALU.is_lt), r=[t8b], w=[t8b])
                op("dve", lambda e, t8=t8, st=st: e.scalar_tensor_tensor(
                    out=t8[:, 12:16], in0=t8[:, 12:16], scalar=st[:, 7:8], in1=t8[:, 20:24], op0=ALU.mult, op1=ALU.mult),
                   r=[t8b, sb_], w=[t8b])
                op("dve", lambda e, t8=t8, ti=ti: e.tensor_copy(out=rwgt[:, ti * 4:(ti + 1) * 4], in_=t8[:, 12:16]),
                   r=[t8b], w=[b_route])
                for k in range(4):
                    dma("pool", lambda e, xn=xn, ti=ti, k=k: e.indirect_dma_start(
                        out=XG[0:NSOK, :], out_offset=bass.IndirectOffsetOnAxis(ap=rslot[:, ti * 4 + k:ti * 4 + k + 1], axis=0),
                        in_=xn[:, :], in_offset=None, bounds_check=sch.breg(e, NSOK - 1), oob_is_err=False),
                        r=[xnb, b_route])
        sch.barrier()
        sch.emit()
    if upto == "E2":
        return finish()

    NBLK = CAP // 128
    NTG = CAP // 512
    with ExitStack() as ph:
        XT = ph.enter_context(nc.sbuf_tensor("XT", [128, KC, CAP], BF16))
        b_XT = Buf("XT")
        actT = ph.enter_context(nc.sbuf_tensor("actT", [128, KC, CAP], BF16))
        b_act = [Buf(f"act{f}") for f in range(KC)]
        wc_r = Ring(nc, ph, "wcG", [128, KC, 256], BF16, 2)
        gt_r = Ring(nc, ph, "gtG", [128, D], BF16, 3)
        bb_r = Ring(nc, ph, "bbG", [128, 2 * KC], F32, 2)
        b2_r = Ring(nc, ph, "b2G", [1, 256], BF16, 3)
        g_r = Ring(nc, ph, "gG", [128, 512], F32, 2)
        s_r = Ring(nc, ph, "sG", [128, 512], F32, 2)
        l_r = Ring(nc, ph, "lG", [128, 512], F32, 2)
        gs_r = Ring(nc, ph, "gsG", [128, 512], F32, 2)
        ot_r = Ring(nc, ph, "otG", [128, 256], BF16, 3)
        psT_r = Ring(nc, ph, "psGt", [128, 1024], BF16, 2, psum=True)
        psg_r = Ring(nc, ph, "psGg", [128, 512], F32, 2, psum=True)
        psl_r = Ring(nc, ph, "psGl", [128, 512], F32, 2, psum=True)
        pso_r = Ring(nc, ph, "psGo", [128, 512], F32, 2, psum=True)
        def xt_steps(ex):
            steps = []
            holds = [dict() for _ in range(NBLK)]
            for blk in range(NBLK):
                hold = holds[blk]
                for half in range(2):
                    def step(ex=ex, blk=blk, half=half, hold=hold, holds=holds):
                        def fetch(b):
                            if b < NBLK and "gt" not in holds[b]:
                                gt, gtb = gt_r.next()
                                r0 = ex * CAP + b * 128
                                dma("sp", lambda e: e.dma_start(out=gt[:], in_=XG[r0:r0 + 128, :]), w=[gtb])
                                holds[b]["gt"], holds[b]["gtb"] = gt, gtb
                        if half == 0:
                            fetch(blk)
                            fetch(blk + 1)
                        gt, gtb = hold["gt"], hold["gtb"]
                        pt, pb = psT_r.next()
                        for j in range(8):
                            kc = half * 8 + j
                            op("pe", lambda e, j=j, kc=kc: e.transpose(
                                pt[:, j * 128:(j + 1) * 128], gt[:, kc * 128:(kc + 1) * 128], identb[:]),
                               r=[gtb, b_cb] if j in (0, 7) else (), w=[pb])
                        for j in range(8):
                            kc = half * 8 + j
                            if half == 0:
                                op("act", lambda e, j=j, kc=kc: e.activation(
                                    out=XT[:, kc, blk * 128:(blk + 1) * 128], in_=pt[:, j * 128:(j + 1) * 128], func=AF.Identity,
                                    scale=modc[:, 2 * KC + kc:2 * KC + kc + 1], bias=modc[:, 3 * KC + kc:3 * KC + kc + 1]),
                                   r=[pb, b_modc], w=[b_XT])
                            else:
                                op("dve", lambda e, j=j, kc=kc: e.tensor_scalar(
                                    out=XT[:, kc, blk * 128:(blk + 1) * 128], in0=pt[:, j * 128:(j + 1) * 128],
                                    scalar1=modc[:, 2 * KC + kc:2 * KC + kc + 1], scalar2=modc[:, 3 * KC + kc:3 * KC + kc + 1],
                                    op0=ALU.mult, op1=ALU.add), r=[pb, b_modc], w=[b_XT])
                    steps.append(step)
            return steps

        chunks = [(ex_, kind, c) for ex_ in range(NE) for (kind, c) in
                  ([("w1", c) for c in range(KC)] + [("w2", c) for c in range(D // 256)])]
        wslots = {}
        b2slots = {}

        def issue_chunk(ci):
            if ci >= len(chunks) or ci in wslots:
                return
            ex_, kind, c = chunks[ci]
            src = (w1 if kind == "w1" else w2)[ex_].rearrange("(kc p) n -> p kc n", p=128)
            wt, wb = wc_r.next()
            dma("pool", lambda e: e.dma_start(out=wt[:], in_=src[:, :, c * 256:(c + 1) * 256]), w=[wb])
            if kind == "w2":
                b2t, b2b = b2_r.next()
                dma("pool", lambda e: e.dma_start(out=b2t[:], in_=b2[ex_:ex_ + 1, c * 256:(c + 1) * 256]), w=[b2b])
                b2slots[ci] = (b2t, b2b)
            wslots[ci] = (wt, wb)

        def get_chunk(ex_, kind, c):
            ci = ex_ * (KC + D // 256) + (c if kind == "w1" else KC + c)
            issue_chunk(ci)
            issue_chunk(ci + 1)
            return wslots.pop(ci)

        for ex in range(NE):
            bt, btb = bb_r.next()
            dma("sp", lambda e, bt=bt, ex=ex: e.dma_start(out=bt[:], in_=b1c[ex]), w=[btb])
            w1v = w1[ex].rearrange("(kc p) n -> p kc n", p=128)
            w2v = w2[ex].rearrange("(kc p) n -> p kc n", p=128)
            if ex == 0:
                for st_ in xt_steps(0):
                    st_()
            for cq in range(KC):
                wt, wb = get_chunk(ex, "w1", cq)
                for tg in range(NTG):
                    pg, pgb = psg_r.next()
                    mmgroup(pg[:], pgb, [(wt[:, kc, 0:256:2], XT[:, kc, tg * 512:(tg + 1) * 512]) for kc in range(KC)], [wb, b_XT])
                    pl, plb = psl_r.next()
                    mmgroup(pl[:], plb, [(wt[:, kc, 1:256:2], XT[:, kc, tg * 512:(tg + 1) * 512]) for kc in range(KC)], [wb, b_XT])
                    g_, gb_ = g_r.next()
                    op("dve", lambda e, g_=g_, pg=pg, bt=bt, cq=cq: e.tensor_scalar(
                        out=g_[:], in0=pg[:], scalar1=bt[:, cq:cq + 1], scalar2=LIMIT, op0=ALU.add, op1=ALU.min),
                       r=[pgb, btb], w=[gb_])
                    sg, sgb = s_r.next()
                    op("act", lambda e, sg=sg, g_=g_: e.activation(out=sg[:], in_=g_[:], func=AF.Sigmoid, scale=ALPHA),
                       r=[gb_], w=[sgb])
                    l_, lb_ = l_r.next()
                    op("dve", lambda e, l_=l_, pl=pl, bt=bt, cq=cq: e.tensor_scalar(
                        out=l_[:], in0=pl[:], scalar1=bt[:, KC + cq:KC + cq + 1], scalar2=LIMIT, op0=ALU.add, op1=ALU.min),
                       r=[plb, btb], w=[lb_])
                    op("dve", lambda e, l_=l_: e.tensor_scalar(out=l_[:], in0=l_[:], scalar1=-LIMIT, scalar2=1.0,
                                                               op0=ALU.max, op1=ALU.add), r=[lb_], w=[lb_])
                    gs, gsb = gs_r.next()
                    op("pool", lambda e, gs=gs, g_=g_, sg=sg: e.tensor_tensor(out=gs[:], in0=g_[:], in1=sg[:], op=ALU.mult),
                       r=[gb_, sgb], w=[gsb])
                    op("dve", lambda e, gs=gs, l_=l_, cq=cq, tg=tg: e.tensor_tensor(
                        out=actT[:, cq, tg * 512:(tg + 1) * 512], in0=gs[:], in1=l_[:], op=ALU.mult),
                       r=[gsb, lb_], w=[b_act[cq]])
            nxt = xt_steps(ex + 1) if ex + 1 < NE else []
            w2step = 0
            for dc in range(D // 256):
                wt, wb = get_chunk(ex, "w2", dc)
                b2t, b2b = b2slots.pop(ex * (KC + D // 256) + KC + dc)
                for blk in range(NBLK):
                    po, pob = pso_r.next()
                    pairs = [(actT[:, fc, blk * 128:(blk + 1) * 128], wt[:, fc, :]) for fc in range(KC)]
                    n = len(pairs)
                    for q, (l, r_) in enumerate(pairs):
                        op("pe", lambda e, po=po, l=l, r_=r_, q=q: e.matmul(po[:, 0:256], l, r_, start=(q == 0), stop=False),
                           r=(b_act + [wb]) if q in (0, n - 1) else (), w=[pob])
                    op("pe", lambda e, po=po, b2t=b2t, dc=dc: e.matmul(
                        po[:, 0:256], onesb[0:1, :], b2t[0:1, :], start=False, stop=True),
                       r=[b2b, b_cb], w=[pob])
                    ot, otb = ot_r.next()
                    op("act", lambda e, ot=ot, po=po: e.activation(out=ot[:], in_=po[:, 0:256], func=AF.Copy), r=[pob], w=[otb])
                    r0 = ex * CAP + blk * 128
                    dma("sp", lambda e, ot=ot, r0=r0, dc=dc: e.dma_start(out=YG[r0:r0 + 128, dc * 256:(dc + 1) * 256], in_=ot[:]),
                        r=[otb])
                    w2step += 1
                    if nxt and w2step % max(1, (D // 256) * NBLK // len(nxt)) == 0:
                        nxt.pop(0)()
            while nxt:
                nxt.pop(0)()
        sch.barrier()
        sch.emit()
    if upto == "G":
        return finish()

    with ExitStack() as ph:
        y_r = Ring(nc, ph, "yH", [128, D], BF16, 5)
        x1_r = Ring(nc, ph, "x1H", [128, D], F32, 2)
        f_r = Ring(nc, ph, "fH", [128, D], F32, 2)
        o_r = Ring(nc, ph, "oH", [128, D], F32, 2)
        junk = ph.enter_context(nc.sbuf_tensor("junkH", [128, D], BF16))
        b_junk = Buf("junkH")
        st_r = Ring(nc, ph, "stH", [128, 4], F32, 3)
        for (yt, yb) in y_r.tiles:
            op("dve", lambda e, yt=yt: e.memset(yt[:], 0.0), w=[yb])
        for ti in range(NTo):
            tok0 = ti * 128
            ys = []
            for k in range(4):
                yt, yb = y_r.next()
                dma("pool", lambda e, yt=yt, ti=ti, k=k: e.indirect_dma_start(
                    out=yt[:, :], out_offset=None, in_=YG[0:NSOK, :],
                    in_offset=bass.IndirectOffsetOnAxis(ap=rslot[:, ti * 4 + k:ti * 4 + k + 1], axis=0),
                    bounds_check=sch.breg(e, NSOK - 1), oob_is_err=False), r=[b_route], w=[yb])
                ys.append((yt, yb))
            x1, x1b = x1_r.next()
            dma("sp", lambda e, x1=x1, tok0=tok0: e.dma_start(out=x1[:], in_=X1[tok0:tok0 + 128, :]), w=[x1b])
            ft, fb = f_r.next()
            op("dve", lambda e, ft=ft, ti=ti, y0=ys[0][0]: e.tensor_scalar(
                out=ft[:], in0=y0[:], scalar1=rwgt[:, ti * 4:ti * 4 + 1], scalar2=None, op0=ALU.mult),
               r=[ys[0][1], b_route], w=[fb])
            for k in range(1, 4):
                op("dve", lambda e, ft=ft, ti=ti, k=k, yk=ys[k][0]: e.scalar_tensor_tensor(
                    out=ft[:], in0=yk[:], scalar=rwgt[:, ti * 4 + k:ti * 4 + k + 1], in1=ft[:], op0=ALU.mult, op1=ALU.add),
                   r=[ys[k][1], b_route, fb], w=[fb])
            st, sb_ = st_r.next()
            op("act", lambda e, st=st, ft=ft: e.activation(out=junk[:], in_=ft[:], func=AF.Square, accum_out=st[:, 0:1]),
               r=[fb], w=[b_junk, sb_])
            op("act", lambda e, st=st: e.activation(out=st[:, 1:2], in_=st[:, 0:1], func=AF.Sqrt, scale=1.0 / D, bias=EPS),
               r=[sb_], w=[sb_])
            op("dve", lambda e, st=st: e.reciprocal(out=st[:, 2:3], in_=st[:, 1:2]), r=[sb_], w=[sb_])
            op("dve", lambda e, st=st, ft=ft: e.scalar_tensor_tensor(out=ft[:], in0=ft[:], scalar=st[:, 2:3], in1=ga2row[:],
                                                                    op0=ALU.mult, op1=ALU.mult),
               r=[fb, sb_, b_garow[1]], w=[fb])
            ot, otb = o_r.next()
            op("pool", lambda e, ot=ot, ft=ft, x1=x1: e.tensor_tensor(out=ot[:], in0=ft[:], in1=x1[:], op=ALU.add),
               r=[fb, x1b], w=[otb])
            dma("sp", lambda e, ot=ot, tok0=tok0: e.dma_start(out=out_d[tok0:tok0 + 128, :], in_=ot[:]), r=[otb])
        sch.barrier()
        sch.emit()
    return finish()


def make_consts():
    c = np.zeros((128, 8, 128), np.float32)
    c[:, 0, :] = np.eye(128)
    for dp in range(16):
        c[dp + 16, 1, dp] = -1.0
        c[dp, 1, dp + 16] = 1.0
    for o in (0, 64):
        for dp in range(8):
            c[o + dp + 8, 2, o + dp] = -1.0
            c[o + dp, 2, o + dp + 8] = 1.0
    k = np.arange(128)
    c[:, 3, :] = (k[:, None] >= k[None, :])
    c[:, 4, :] = (k[:, None] < k[None, :])
    c[:, 5, :] = 1.0
    invA = THETA ** (-(np.arange(16, dtype=np.float32)) / np.float32(16))
    invI = THETA ** (-(np.arange(8, dtype=np.float32)) / np.float32(8))
    for p in range(128):
        c[p, 6, 0] = invA[p % 16] if p < 32 else 0.0
        c[p, 6, 1] = invI[(p % 64) % 8] if (p % 64) < 16 else 0.0
    c[:, 7, :] = k[None, :]
    return np.ascontiguousarray(c.reshape(128, 8 * 128))


def make_masks(half):
    p = np.arange(128)[:, None]
    tl = np.arange(512)[None, :]
    cm = np.zeros((128, 2, 8, 512), np.float32)
    am = np.zeros((128, 2, 640), np.float32)
    cc = np.arange(640)[None, :]
    for m in range(2):
        delta = (1 if m == 0 else 0) if half == 0 else (0 if m == 0 else 1)
        for j in range(8):
            if delta == 0:
                cm[:, m, j, :] = (128 * (j - 4) + p) < tl
            else:
                cm[:, m, j, :] = (128 * j + p) < tl
        lim = 64 * (p // 64 + 1)
        am[:, m, :] = np.where(512 * (delta - 1) + cc >= lim, NEG, 0.0)
    return (np.ascontiguousarray(cm.reshape(128, -1)).astype(ml_dtypes.bfloat16),
            np.ascontiguousarray(am.reshape(128, -1)))


def col_layout(v):
    return np.ascontiguousarray(v.reshape(-1, 128).T)


def prep(inputs, cfg, batches):
    S, NE = cfg["S"], cfg["NE"]
    G = S // 512
    f32 = np.float32
    x = np.asarray(inputs["x"], f32)
    c = np.asarray(inputs["c"], f32)
    pos = np.asarray(inputs["positions"], np.int32)
    shared = {
        "b_ada": np.ascontiguousarray(np.asarray(inputs["b_ada"], f32)[0][None, :]),
        "badac": col_layout(np.asarray(inputs["b_ada"], f32)[0]),
        "gcols": np.concatenate([col_layout(np.asarray(inputs[k], f32)[0]) for k in
                                 ("g_pre_mix", "g_post_mix", "g_pre_ffn", "g_post_ffn")], axis=1),
        "g_post_mix": np.ascontiguousarray(np.asarray(inputs["g_post_mix"], f32)[0][None, :]),
        "g_post_ffn": np.ascontiguousarray(np.asarray(inputs["g_post_ffn"], f32)[0][None, :]),
        "w_ada": np.ascontiguousarray(np.asarray(inputs["w_ada"], f32)[0]),
        "w_in": np.ascontiguousarray(np.asarray(inputs["w_in"], f32)[0]),
        "w_branch_a": np.ascontiguousarray(np.asarray(inputs["w_branch_a"], f32)[0]),
        "w_branch_b": np.ascontiguousarray(np.asarray(inputs["w_branch_b"], f32)[0]),
        "w_out": np.ascontiguousarray(np.asarray(inputs["w_out"], f32)[0]),
        "w_router": np.ascontiguousarray(np.asarray(inputs["w_router"], f32)[0][:, :NE]),
        "b_router": np.ascontiguousarray(np.asarray(inputs["b_router"], f32)[0][None, :NE]),
        "w1": np.ascontiguousarray(np.asarray(inputs["w1"], f32)[0][:NE]),
        "w2": np.ascontiguousarray(np.asarray(inputs["w2"], f32)[0][:NE]),
        "b2": np.ascontiguousarray(np.asarray(inputs["b2"], f32)[0][:NE]),
        "consts": make_consts(),
    }
    b1 = np.asarray(inputs["b1"], f32)[0][:NE]
    b1g = b1[:, 0::2].reshape(NE, KC, 128).transpose(0, 2, 1)
    b1l = b1[:, 1::2].reshape(NE, KC, 128).transpose(0, 2, 1)
    shared["b1c"] = np.ascontiguousarray(np.concatenate([b1g, b1l], axis=2))
    masks = [make_masks(0), make_masks(1)]
    in_maps, owns = [], []
    for b in batches:
        for half in range(2):
            toks = np.concatenate([np.arange(g * 512, (g + 1) * 512) for g in own_groups(G, half)])
            m = dict(shared)
            m["x_all"] = np.ascontiguousarray(x[b])
            m["x_own"] = np.ascontiguousarray(x[b][toks])
            m["pos_all"] = np.ascontiguousarray(pos[b][None, :])
            m["pos_own"] = np.ascontiguousarray(pos[b][toks][None, :])
            m["cvec"] = col_layout(c[b])
            m["cmask"], m["amask"] = masks[half]
            in_maps.append(m)
            owns.append((b, toks))
    return in_maps, owns


_CACHE = {}


def kernel(**inputs):
    x = np.asarray(inputs["x"])
    B, S, _ = x.shape
    cfg = {"S": S, "NE": 32, "CAP": 2048}
    in_maps, owns = prep(inputs, cfg, list(range(B)))
    nc = build(cfg)
    res = run_bass_kernel_spmd(nc, in_maps, core_ids=list(range(len(in_maps))))
    out = np.empty((B, S, D), np.float32)
    for r, (b, toks) in zip(res.results, owns):
        out[b, toks] = np.asarray(r["out"], np.float32)
    return out
```

```python
import math
from contextlib import ExitStack

import numpy as np
import ml_dtypes

import concourse.bass as bass
import concourse.mybir as mybir
from concourse.bass_utils import run_bass_kernel_spmd

F32 = mybir.dt.float32
BF16 = mybir.dt.bfloat16
I32 = mybir.dt.int32
AF = mybir.ActivationFunctionType
ALU = mybir.AluOpType
AX = mybir.AxisListType

D = 2048
KC = D // 128
A_H, A_KV, HD = 8, 2, 128
IDX_H, IDX_D = 16, 64
B_H = 8
TOPK = 256
THETA = 500000.0
EPS = 1e-6
LIMIT = 7.0
ALPHA = 1.702
C_QA, C_KA, C_VA = 0, 1024, 1280
C_QB, C_KB, C_VB = 1536, 2560, 3584
C_QI, C_KI, C_WI = 4608, 5632, 5696
C_GA, C_GB = 5712, 7760
IN_W = 9808
NEG = -1.0e30
NBIS = 24


class Buf:
    __slots__ = ("w", "r", "name", "excl")

    def __init__(self, name="", excl=False):
        self.w = None
        self.r = []
        self.name = name
        self.excl = excl


class Sched:
    ENGS = ("pe", "act", "dve", "pool", "sp")
    RQ = {"sp": 8, "pool": 2, "act": 4}

    def __init__(self, nc, es):
        self.nc = nc
        self.sems = {}
        for e in self.ENGS:
            self.sems[e] = es.enter_context(nc.semaphore("c_" + e))
        self.dq = {}
        for q in ("sp", "pool", "act"):
            self.dq[q] = [es.enter_context(nc.semaphore(f"d_{q}{i}")) for i in range(self.RQ[q])]
        self.n = {e: 0 for e in self.ENGS}
        self.dn = {q: 0 for q in self.dq}
        self.seen = {e: {} for e in self.ENGS}
        self.streams = {e: [] for e in self.ENGS}
        self.last = {}

    def _sem(self, key):
        return self.sems[key] if isinstance(key, str) else self.dq[key[0]][key[1]]

    def _waits(self, eng, deps):
        out = []
        best = {}
        for d in deps:
            if d is None:
                continue
            k, v = d
            if k == "pe" and eng == "pe":
                continue
            if best.get(k, 0) < v:
                best[k] = v
        for k, v in best.items():
            if self.seen[eng].get(k, 0) >= v:
                continue
            self.seen[eng][k] = v
            out.append((k, v))
        return out

    def _deps(self, r, w):
        deps = []
        for b in r:
            deps.append(b.w)
            if b.excl:
                deps.extend(b.r)
        for b in w:
            deps.append(b.w)
            deps.extend(b.r)
        return deps

    def _commit(self, tok, r, w):
        for b in r:
            b.r.append(tok)
        for b in w:
            b.w = tok
            b.r = []
        self.last[tok[0]] = tok[1]

    def op(self, eng, fn, r=(), w=()):
        waits = self._waits(eng, self._deps(r, w))
        self.n[eng] += 1
        tok = (eng, self.n[eng])
        self.streams[eng].append((waits, fn, self.sems[eng], 1))
        self._commit(tok, r, w)

    def dma(self, q, fn, r=(), w=()):
        i = self.dn[q]
        self.dn[q] += 1
        R = self.RQ[q]
        key = (q, i % R)
        deps = self._deps(r, w)
        if i >= R:
            deps.append((key, 16 * (i // R)))
        waits = self._waits(q, deps)
        tok = (key, 16 * (i // R + 1))
        self.streams[q].append((waits, fn, self._sem(key), 16))
        self._commit(tok, r, w)

    def barrier(self):
        toks = list(self.last.items())
        for e in self.ENGS:
            waits = self._waits(e, [t for t in toks if not (t[0] == e)])
            if waits:
                self.streams[e].append((waits, None, None, 0))

    def breg(self, e, val):
        if val not in self._regs:
            self._regs[val] = e.to_reg(val)
        return self._regs[val]

    def emit(self):
        nc = self.nc
        streams = self.streams
        self._regs = {}
        self.streams = {e: [] for e in self.ENGS}

        def run(e, lst):
            for waits, fn, sem, inc in lst:
                for k, v in waits:
                    e.wait_ge(self._sem(k), v)
                if fn is not None:
                    try:
                        fn(e).then_inc(sem, inc)
                    except Exception:
                        print("EMIT FAIL at stream idx", lst.index((waits, fn, sem, inc)), "of", len(lst), flush=True)
                        raise

        with nc.Block() as block:
            @block.tensor
            def _(e):
                run(e, streams["pe"])

            @block.scalar
            def _(e):
                run(e, streams["act"])

            @block.vector
            def _(e):
                run(e, streams["dve"])

            @block.gpsimd
            def _(e):
                run(e, streams["pool"])

            @block.sync
            def _(e):
                run(e, streams["sp"])


class Ring:
    def __init__(self, nc, es, name, shape, dtype, n, psum=False):
        self.tiles = []
        for i in range(n):
            if psum:
                t = es.enter_context(nc.psum_tensor(f"{name}{i}", shape, dtype))
            else:
                t = es.enter_context(nc.sbuf_tensor(f"{name}{i}", shape, dtype))
            self.tiles.append((t, Buf(f"{name}{i}", excl=psum)))
        self.i = 0

    def next(self):
        t = self.tiles[self.i % len(self.tiles)]
        self.i += 1
        return t


def own_groups(G, half):
    return [g for g in range(G) if ((g % 4) in (0, 3)) == (half == 0)]


def build(cfg):
    S = cfg["S"]
    NE = cfg["NE"]
    CAP = cfg["CAP"]
    dbg = cfg.get("dbg", False)
    upto = cfg.get("upto", "Z")
    So = S // 2
    G = S // 512
    Go = G // 2
    NT = S // 128
    NTo = So // 128
    SG = min(2048, So)
    nc = bass.Bass("TRN2", target_bir_lowering=False)
    es = ExitStack()
    scratch_kind = "ExternalOutput" if dbg else "Internal"

    def din(name, shape, dt):
        return nc.dram_tensor(name, list(shape), dt, kind="ExternalInput").ap()

    def dscr(name, shape, dt):
        return nc.dram_tensor(name, list(shape), dt, kind=scratch_kind).ap()

    x_all = din("x_all", [S, D], F32)
    x_own = din("x_own", [So, D], F32)
    pos_all = din("pos_all", [1, S], I32)
    pos_own = din("pos_own", [1, So], I32)
    cvec = din("cvec", [128, KC], F32)
    badac = din("badac", [128, 6 * KC], F32)
    gcols = din("gcols", [128, 4 * KC], F32)
    b_ada = din("b_ada", [1, 6 * D], F32)
    g_post_mix = din("g_post_mix", [1, D], F32)
    g_post_ffn = din("g_post_ffn", [1, D], F32)
    w_ada = din("w_ada", [D, 6 * D], F32)
    w_in = din("w_in", [D, IN_W], F32)
    w_bra = din("w_branch_a", [A_H * HD, D], F32)
    w_brb = din("w_branch_b", [B_H * HD, D], F32)
    w_out = din("w_out", [D, D], F32)
    w_router = din("w_router", [D, NE], F32)
    b_router = din("b_router", [1, NE], F32)
    w1 = din("w1", [NE, D, 2 * D], F32)
    b1c = din("b1c", [NE, 128, 2 * KC], F32)
    w2 = din("w2", [NE, D, D], F32)
    b2 = din("b2", [NE, D], F32)
    cmask_d = din("cmask", [128, 2 * 8 * 512], BF16)
    amask_d = din("amask", [128, 2 * 640], F32)
    consts_d = din("consts", [128, 8 * 128], F32)
    out_d = nc.dram_tensor("out", [So, D], F32, kind="ExternalOutput").ap()

    KA = dscr("s_ka", [A_KV, 128, S], BF16)
    KB = dscr("s_kb", [B_H, 128, S], BF16)
    KI = dscr("s_ki", [IDX_D, S], BF16)
    VA = dscr("s_va", [S, A_KV * HD], BF16)
    VB = dscr("s_vb", [S, B_H * HD], BF16)
    QA = dscr("s_qa", [A_H, 128, So], BF16)
    QB = dscr("s_qb", [B_H, 128, So], BF16)
    QI = dscr("s_qi", [IDX_H // 2, 128, So], BF16)
    WI = dscr("s_wi", [So, IDX_H], F32)
    GA = dscr("s_ga", [KC, 128, So], BF16)
    GB = dscr("s_gb", [KC, 128, So], BF16)
    MK = dscr("s_mk", [NTo, 128, S], BF16)
    OA = dscr("s_oa", [A_H, 128, So], BF16)
    OB = dscr("s_ob", [B_H, 128, So], BF16)
    MG = dscr("s_mg", [KC, 128, So], BF16)
    X1 = dscr("s_x1", [So, D], F32)
    XG = dscr("s_xg", [NE * CAP, D], BF16)
    YG = dscr("s_yg", [NE * CAP, D], BF16)

    sch = Sched(nc, es)
    op, dma = sch.op, sch.dma
    dbg_outs = {}

    def finish():
        if dbg:
            d1 = nc.dram_tensor("dbg_modc", [128, 4 * KC], F32, kind="ExternalOutput").ap()
            d2 = nc.dram_tensor("dbg_ga", [128, 2 * D], F32, kind="ExternalOutput").ap()
            dma("sp", lambda e: e.dma_start(out=d1[:, :], in_=modc[:]), r=[b_modc])
            dma("sp", lambda e: e.dma_start(out=d2[:, 0:D], in_=ga1row[:]), r=[b_garow[0]])
            dma("sp", lambda e: e.dma_start(out=d2[:, D:2 * D], in_=ga2row[:]), r=[b_garow[1]])
            d3 = nc.dram_tensor("dbg_rslot", [128, NTo * 4], I32, kind="ExternalOutput").ap()
            d4 = nc.dram_tensor("dbg_rwgt", [128, NTo * 4], F32, kind="ExternalOutput").ap()
            dma("sp", lambda e: e.dma_start(out=d3[:, :], in_=rslot[:]), r=[b_route])
            dma("sp", lambda e: e.dma_start(out=d4[:, :], in_=rwgt[:]), r=[b_route])
        sch.barrier()
        sch.emit()
        es.close()
        return nc

    def sbp(name, shape, dt):
        return es.enter_context(nc.sbuf_tensor(name, shape, dt))

    consts = sbp("consts_s", [128, 8 * 128], F32)
    b_consts = Buf("consts")
    identb = sbp("identb", [128, 128], BF16)
    rtA = sbp("rtA", [128, 128], BF16)
    rtI = sbp("rtI", [128, 128], BF16)
    triI = sbp("triI", [128, 128], BF16)
    triS = sbp("triS", [128, 128], BF16)
    onesb = sbp("onesb", [128, 128], BF16)
    onesf = consts[:, 5 * 128:6 * 128]
    invfA = consts[:, 6 * 128:6 * 128 + 1]
    invfI = consts[:, 6 * 128 + 1:6 * 128 + 2]
    iota_f = consts[:, 7 * 128:8 * 128]
    b_cb = Buf("constsb")
    modc = sbp("modc", [128, 4 * KC], F32)
    b_modc = Buf("modc")
    ga1row = sbp("ga1row", [128, D], F32)
    ga2row = sbp("ga2row", [128, D], F32)
    b_garow = [Buf("ga1row"), Buf("ga2row")]
    rslot = sbp("rslot", [128, NTo * 4], I32)
    rwgt = sbp("rwgt", [128, NTo * 4], F32)
    b_route = Buf("route")

    dma("sp", lambda e: e.dma_start(out=consts[:], in_=consts_d[:, :]), w=[b_consts])
    for i, t in enumerate((identb, rtA, rtI, triI, triS, onesb)):
        op("dve", lambda e, i=i, t=t: e.tensor_copy(out=t[:], in_=consts[:, i * 128:(i + 1) * 128]),
           r=[b_consts], w=[b_cb])

    def mmgroup(out_ap, out_buf, pairs, rbufs):
        n = len(pairs)
        for i, (l, r_) in enumerate(pairs):
            rb = rbufs if (i == 0 or i == n - 1) else ()
            op("pe", lambda e, l=l, r_=r_, i=i: e.matmul(out_ap, l, r_, start=(i == 0), stop=(i == n - 1)),
               r=rb, w=[out_buf])

    with ExitStack() as ph:
        cc = ph.enter_context(nc.sbuf_tensor("cc", [128, KC], F32))
        sg_ = ph.enter_context(nc.sbuf_tensor("sg_", [128, KC], F32))
        siluc = ph.enter_context(nc.sbuf_tensor("siluc", [128, KC], F32))
        silurep = ph.enter_context(nc.sbuf_tensor("silurep", [128, KC, 128], F32))
        badac_s = ph.enter_context(nc.sbuf_tensor("badac_s", [128, 6 * KC], F32))
        gcols_s = ph.enter_context(nc.sbuf_tensor("gcols_s", [128, 4 * KC], F32))
        rawc = ph.enter_context(nc.sbuf_tensor("rawc", [128, 4 * KC], F32))
        b_small = Buf("small")
        b_rawc = Buf("rawc")
        wch = Ring(nc, ph, "wch", [128, KC, 512], F32, 2)
        rowt = Ring(nc, ph, "rowt", [128, 2, 512], F32, 2)
        tmpr = Ring(nc, ph, "tmpr", [128, 512], F32, 2)
        psA = Ring(nc, ph, "psA", [128, 512], F32, 2, psum=True)
        psC = ph.enter_context(nc.psum_tensor("psC", [128, 512], F32))
        b_psC = Buf("psC", excl=True)

        dma("sp", lambda e: e.dma_start(out=cc[:], in_=cvec[:, :]), w=[b_small])
        dma("sp", lambda e: e.dma_start(out=badac_s[:], in_=badac[:, :]), w=[b_small])
        dma("sp", lambda e: e.dma_start(out=gcols_s[:], in_=gcols[:, :]), w=[b_small])
        op("act", lambda e: e.activation(out=sg_[:], in_=cc[:], func=AF.Sigmoid), r=[b_small], w=[b_small])
        op("dve", lambda e: e.tensor_tensor(out=siluc[:], in0=cc[:], in1=sg_[:], op=ALU.mult), r=[b_small], w=[b_small])
        op("dve", lambda e: e.tensor_copy(out=silurep[:], in_=siluc[:].unsqueeze(2).broadcast_to([128, KC, 128])),
           r=[b_small], w=[b_small])
        w_ada_v = w_ada.rearrange("(kc p) n -> p kc n", p=128)
        for idx, which in enumerate((1, 0, 4, 3)):
            for cq in range(4):
                wt, wb = wch.next()
                c0 = which * D + cq * 512
                dma("sp", lambda e, wt=wt, c0=c0: e.dma_start(out=wt[:], in_=w_ada_v[:, :, c0:c0 + 512]), w=[wb])
                for j in range(4):
                    col = idx * KC + cq * 4 + j
                    mmgroup(psC[:, col:col + 1], b_psC,
                            [(wt[:, kc, j * 128:(j + 1) * 128], siluc[:, kc:kc + 1]) for kc in range(KC)],
                            [wb, b_small])
        op("dve", lambda e: e.tensor_copy(out=rawc[:], in_=psC[:, 0:4 * KC]), r=[b_psC], w=[b_rawc])
        for idx, which in enumerate((1, 0, 4, 3)):
            op("dve", lambda e, idx=idx, which=which: e.tensor_tensor(
                out=rawc[:, idx * KC:(idx + 1) * KC], in0=rawc[:, idx * KC:(idx + 1) * KC],
                in1=badac_s[:, which * KC:(which + 1) * KC], op=ALU.add), r=[b_small, b_rawc], w=[b_rawc])
        op("dve", lambda e: e.scalar_tensor_tensor(out=modc[:, 0:KC], in0=rawc[:, 0:KC], scalar=1.0,
                                                   in1=gcols_s[:, 0:KC], op0=ALU.add, op1=ALU.mult),
           r=[b_rawc, b_small], w=[b_modc])
        op("dve", lambda e: e.tensor_copy(out=modc[:, KC:2 * KC], in_=rawc[:, KC:2 * KC]), r=[b_rawc], w=[b_modc])
        op("dve", lambda e: e.scalar_tensor_tensor(out=modc[:, 2 * KC:3 * KC], in0=rawc[:, 2 * KC:3 * KC], scalar=1.0,
                                                   in1=gcols_s[:, 2 * KC:3 * KC], op0=ALU.add, op1=ALU.mult),
           r=[b_rawc, b_small], w=[b_modc])
        op("dve", lambda e: e.tensor_copy(out=modc[:, 3 * KC:4 * KC], in_=rawc[:, 3 * KC:4 * KC]), r=[b_rawc], w=[b_modc])
        for gi, (which, grow, gpost) in enumerate(((2, ga1row, g_post_mix), (5, ga2row, g_post_ffn))):
            for cq in range(4):
                wt, wb = wch.next()
                c0 = which * D + cq * 512
                dma("sp", lambda e, wt=wt, c0=c0: e.dma_start(out=wt[:], in_=w_ada_v[:, :, c0:c0 + 512]), w=[wb])
                rt, rb = rowt.next()
                dma("sp", lambda e, rt=rt, c0=c0: e.dma_start(out=rt[:, 0, :], in_=b_ada[0:1, c0:c0 + 512].partition_broadcast(128)), w=[rb])
                dma("sp", lambda e, rt=rt, cq=cq, gpost=gpost: e.dma_start(
                    out=rt[:, 1, :], in_=gpost[0:1, cq * 512:(cq + 1) * 512].partition_broadcast(128)), w=[rb])
                pt, pb = psA.next()
                mmgroup(pt[:], pb, [(silurep[:, kc, :], wt[:, kc, :]) for kc in range(KC)], [wb, b_small])
                tt, tb = tmpr.next()
                op("dve", lambda e, tt=tt, pt=pt, rt=rt: e.tensor_tensor(out=tt[:], in0=pt[:], in1=rt[:, 0, :], op=ALU.add),
                   r=[pb, rb], w=[tb])
                op("dve", lambda e, tt=tt, rt=rt, grow=grow, cq=cq: e.tensor_tensor(
                    out=grow[:, cq * 512:(cq + 1) * 512], in0=tt[:], in1=rt[:, 1, :], op=ALU.mult),
                   r=[tb, rb], w=[b_garow[gi]])
        sch.barrier()
        sch.emit()
    if upto == "A":
        return finish()

    w_in_v = w_in.rearrange("(kc p) n -> p kc n", p=128)
    TWO_PI = 2.0 * math.pi
    BL = cfg.get('blevel', 9)
    ROPE_ADD_ENG = cfg.get('rope_add', 'dve')
    CW1 = 6.28125
    CW2 = TWO_PI - CW1

    def norm_transpose(x_src, tok0, hT, b_hT, hcol0, sc_off, xt_r, xn_r, junk, b_junk, st_r, psT_r, alt):
        xt, xb = xt_r.next()
        dma("sp", lambda e: e.dma_start(out=xt[:], in_=x_src[tok0:tok0 + 128, :]), w=[xb])
        st, sb_ = st_r.next()
        op("act", lambda e: e.activation(out=junk[:], in_=xt[:], func=AF.Square, accum_out=st[:, 0:1]),
           r=[xb], w=[b_junk, sb_])
        op("act", lambda e: e.activation(out=st[:, 1:2], in_=st[:, 0:1], func=AF.Sqrt, scale=1.0 / D, bias=EPS),
           r=[sb_], w=[sb_])
        op("dve", lambda e: e.reciprocal(out=st[:, 2:3], in_=st[:, 1:2]), r=[sb_], w=[sb_])
        xn, xnb = xn_r.next()
        op("dve", lambda e: e.tensor_scalar(out=xn[:], in0=xt[:], scalar1=st[:, 2:3], scalar2=None, op0=ALU.mult),
           r=[xb, sb_], w=[xnb])
        for half in range(2):
            pt, pb = psT_r.next()
            for j in range(8):
                kc = half * 8 + j
                op("pe", lambda e, pt=pt, j=j, kc=kc: e.transpose(pt[:, j * 128:(j + 1) * 128],
                                                                   xn[:, kc * 128:(kc + 1) * 128], identb[:]),
                   r=[xnb, b_cb] if j in (0, 7) else (), w=[pb])
            for j in range(8):
                kc = half * 8 + j
                eng = "act" if (half + alt) % 2 == 0 else "dve"
                if eng == "act":
                    op("act", lambda e, pt=pt, j=j, kc=kc: e.activation(
                        out=hT[:, kc, hcol0:hcol0 + 128], in_=pt[:, j * 128:(j + 1) * 128], func=AF.Identity,
                        scale=modc[:, sc_off + kc:sc_off + kc + 1], bias=modc[:, sc_off + KC + kc:sc_off + KC + kc + 1]),
                       r=[pb, b_modc], w=[b_hT])
                else:
                    op("dve", lambda e, pt=pt, j=j, kc=kc: e.tensor_scalar(
                        out=hT[:, kc, hcol0:hcol0 + 128], in0=pt[:, j * 128:(j + 1) * 128],
                        scalar1=modc[:, sc_off + kc:sc_off + kc + 1], scalar2=modc[:, sc_off + KC + kc:sc_off + KC + kc + 1],
                        op0=ALU.mult, op1=ALU.add), r=[pb, b_modc], w=[b_hT])

    def proj_pass(pname, x_src, pos_src, T, fm_blocks, tm_blocks):
        with ExitStack() as ph:
            sg = min(SG, T)
            ngr = sg // 512
            hT = ph.enter_context(nc.sbuf_tensor(pname + "hT", [128, KC, sg], BF16))
            b_hT = Buf("hT")
            xt_r = Ring(nc, ph, pname + "xt", [128, D], F32, 2)
            xn_r = Ring(nc, ph, pname + "xn", [128, D], BF16, 1)
            junk = ph.enter_context(nc.sbuf_tensor(pname + "junk", [128, D], BF16))
            b_junk = Buf("junk")
            st_r = Ring(nc, ph, pname + "st", [128, 4], F32, 4)
            psT_r = Ring(nc, ph, pname + "psT", [128, 1024], BF16, 2, psum=True)
            psM_r = Ring(nc, ph, pname + "psM", [128, 512], F32, 4, psum=True)
            psR_r = Ring(nc, ph, pname + "psR", [128, 512], F32, 2, psum=True)
            wfm_r = Ring(nc, ph, pname + "wfm", [128, KC, 128], BF16, 3)
            wtm_r = Ring(nc, ph, pname + "wtm", [128, KC, 512], BF16, 1)
            zb_r = Ring(nc, ph, pname + "zb", [128, 512], BF16, 2)
            t1_r = Ring(nc, ph, pname + "t1", [128, 512], F32, 2)
            t2_r = Ring(nc, ph, pname + "t2", [128, 512], F32, 2)
            ob_r = Ring(nc, ph, pname + "ob", [128, 512], BF16, 3)
            obf_r = Ring(nc, ph, pname + "obf", [128, 512], F32, 2)
            posi = ph.enter_context(nc.sbuf_tensor(pname + "posi", [128, 512], I32))
            posf = ph.enter_context(nc.sbuf_tensor(pname + "posf", [128, 512], F32))
            ang = ph.enter_context(nc.sbuf_tensor(pname + "ang", [128, 512], F32))
            kfi = ph.enter_context(nc.sbuf_tensor(pname + "kfi", [128, 512], I32))
            kff = ph.enter_context(nc.sbuf_tensor(pname + "kff", [128, 512], F32))
            msk = ph.enter_context(nc.sbuf_tensor(pname + "msk", [128, 512], F32))
            b_tmp = Buf("ropetmp")
            tabs = ph.enter_context(nc.sbuf_tensor(pname + "tabs", [128, ngr, 4, 512], F32))
            b_tabs = Buf("tabs")
            for s0 in range(0, T, sg):
                for ti in range(sg // 128):
                    norm_transpose(x_src, s0 + ti * 128, hT, b_hT, ti * 128, 0, xt_r, xn_r, junk, b_junk, st_r, psT_r, ti)
                for g in range(ngr if BL >= 2 else 0):
                    t0 = s0 + g * 512
                    dma("sp", lambda e, t0=t0: e.dma_start(out=posi[:], in_=pos_src[0:1, t0:t0 + 512].partition_broadcast(128)),
                        w=[b_tmp])
                    op("dve", lambda e: e.tensor_copy(out=posf[:], in_=posi[:]), r=[b_tmp], w=[b_tmp])
                    for ti_, (invf, shift) in enumerate(((invfA, math.pi / 2), (invfA, 0.0), (invfI, math.pi / 2), (invfI, 0.0))):
                        R_, W_ = [b_tmp, b_consts], [b_tmp]
                        op("dve", lambda e, invf=invf, shift=shift: e.tensor_scalar(
                            out=ang[:], in0=posf[:], scalar1=invf, scalar2=shift, op0=ALU.mult, op1=ALU.add), r=R_, w=W_)
                        op("dve", lambda e: e.tensor_scalar(out=kfi[:], in0=ang[:], scalar1=1.0 / TWO_PI, scalar2=None,
                                                            op0=ALU.mult), r=R_, w=W_)
                        op("dve", lambda e: e.tensor_copy(out=kff[:], in_=kfi[:]), r=R_, w=W_)
                        op("dve", lambda e: e.scalar_tensor_tensor(out=ang[:], in0=kff[:], scalar=-CW1, in1=ang[:],
                                                                   op0=ALU.mult, op1=ALU.add), r=R_, w=W_)
                        op("dve", lambda e: e.scalar_tensor_tensor(out=ang[:], in0=kff[:], scalar=-CW2, in1=ang[:],
                                                                   op0=ALU.mult, op1=ALU.add), r=R_, w=W_)
                        op("dve", lambda e: e.tensor_scalar(out=msk[:], in0=ang[:], scalar1=math.pi, scalar2=None,
                                                            op0=ALU.is_gt), r=R_, w=W_)
                        op("dve", lambda e: e.scalar_tensor_tensor(out=ang[:], in0=msk[:], scalar=-TWO_PI, in1=ang[:],
                                                                   op0=ALU.mult, op1=ALU.add), r=R_, w=W_)
                        op("dve", lambda e: e.tensor_scalar(out=msk[:], in0=ang[:], scalar1=-math.pi, scalar2=None,
                                                            op0=ALU.is_lt), r=R_, w=W_)
                        op("dve", lambda e: e.scalar_tensor_tensor(out=ang[:], in0=msk[:], scalar=TWO_PI, in1=ang[:],
                                                                   op0=ALU.mult, op1=ALU.add), r=R_, w=W_)
                        op("dve", lambda e: e.tensor_scalar(out=ang[:], in0=ang[:], scalar1=math.pi, scalar2=-math.pi,
                                                            op0=ALU.min, op1=ALU.max), r=R_, w=W_)
                        op("act", lambda e, g=g, ti_=ti_: e.activation(out=tabs[:, g, ti_, :], in_=ang[:], func=AF.Sin),
                           r=[b_tmp], w=[b_tabs, b_tmp])
                fmw = {}

                def fm_issue(bi):
                    if bi < len(fm_blocks) and bi not in fmw:
                        c0_, nc_ = fm_blocks[bi][0], fm_blocks[bi][1]
                        wt_, wb_ = wfm_r.next()
                        dma("pool", lambda e: e.dma_start(out=wt_[:, :, 0:nc_], in_=w_in_v[:, :, c0_:c0_ + nc_]), w=[wb_])
                        fmw[bi] = (wt_, wb_)

                for bi, (col0, ncol, dst, rope) in enumerate(fm_blocks):
                    fm_issue(bi)
                    fm_issue(bi + 1)
                    wt, wb = fmw.pop(bi)
                    for g in range(ngr):
                        t0 = s0 + g * 512
                        pt, pb = psM_r.next()
                        mmgroup(pt[0:ncol, :], pb,
                                [(wt[:, kc, 0:ncol], hT[:, kc, g * 512:(g + 1) * 512]) for kc in range(KC)], [wb, b_hT])
                        ot, obb = ob_r.next()
                        if rope is None:
                            op("act", lambda e, ot=ot, pt=pt, ncol=ncol: e.activation(out=ot[0:ncol, :], in_=pt[0:ncol, :], func=AF.Copy),
                               r=[pb], w=[obb])
                        else:
                            ci = 0 if rope == "A" else 2
                            rt = rtA if rope == "A" else rtI
                            zt, zbb = zb_r.next()
                            op("act", lambda e, zt=zt, pt=pt, ncol=ncol: e.activation(out=zt[0:ncol, :], in_=pt[0:ncol, :], func=AF.Copy),
                               r=[pb], w=[zbb])
                            p2, p2b = psR_r.next()
                            if cfg.get('ropevar', 0) != 1:
                                op("pe", lambda e, p2=p2, zt=zt, rt=rt, ncol=ncol: e.matmul(
                                    p2[0:ncol, :], rt[0:ncol, 0:ncol], zt[0:ncol, :], start=True, stop=True),
                                   r=[zbb, b_cb], w=[p2b])
                            else:
                                p2, p2b = pt, pb
                            a1, a1b = t1_r.next()
                            a2, a2b = t2_r.next()
                            if cfg.get('ropevar', 0) == 2:
                                op("act", lambda e, ot=ot, pt=pt, ncol=ncol: e.activation(out=ot[0:ncol, :], in_=pt[0:ncol, :], func=AF.Copy),
                                   r=[pb], w=[obb])
                                dma("sp", lambda e, ot=ot, dst=dst, t0=t0, ncol=ncol: e.dma_start(
                                    out=dst[0:ncol, t0:t0 + 512], in_=ot[0:ncol, :]), r=[obb])
                                continue
                            if cfg.get('ropevar', 0) == 3:
                                op("dve", lambda e, ot=ot, pt=pt, g=g, ci=ci, ncol=ncol: e.tensor_tensor(
                                    out=ot[0:ncol, :], in0=pt[0:ncol, :], in1=tabs[0:ncol, g, ci, :], op=ALU.mult),
                                   r=[pb, b_tabs], w=[obb])
                                dma("sp", lambda e, ot=ot, dst=dst, t0=t0, ncol=ncol: e.dma_start(
                                    out=dst[0:ncol, t0:t0 + 512], in_=ot[0:ncol, :]), r=[obb])
                                continue
                            op("dve", lambda e, a1=a1, pt=pt, g=g, ci=ci, ncol=ncol: e.tensor_tensor(
                                out=a1[0:ncol, :], in0=pt[0:ncol, :], in1=tabs[0:ncol, g, ci, :], op=ALU.mult),
                               r=[pb, b_tabs], w=[a1b])
                            op("dve", lambda e, a2=a2, p2=p2, g=g, ci=ci, ncol=ncol: e.tensor_tensor(
                                out=a2[0:ncol, :], in0=p2[0:ncol, :], in1=tabs[0:ncol, g, ci + 1, :], op=ALU.mult),
                               r=[p2b, b_tabs], w=[a2b])
                            op(ROPE_ADD_ENG, lambda e, ot=ot, a1=a1, a2=a2, ncol=ncol: e.tensor_tensor(
                                out=ot[0:ncol, :], in0=a1[0:ncol, :], in1=a2[0:ncol, :], op=ALU.add),
                               r=[a1b, a2b], w=[obb])
                        dma("sp", lambda e, ot=ot, dst=dst, t0=t0, ncol=ncol: e.dma_start(
                            out=dst[0:ncol, t0:t0 + 512], in_=ot[0:ncol, :]), r=[obb])
                for (col0, ncol, dst, dcol0, isf32) in tm_blocks:
                    if BL < 5:
                        continue
                    wt, wb = wtm_r.next()
                    dma("pool", lambda e, wt=wt, col0=col0, ncol=ncol: e.dma_start(
                        out=wt[:, :, 0:ncol], in_=w_in_v[:, :, col0:col0 + ncol]), w=[wb])
                    for ti in range(sg // 128):
                        t0 = s0 + ti * 128
                        pt, pb = psM_r.next()
                        mmgroup(pt[:, 0:ncol], pb,
                                [(hT[:, kc, ti * 128:(ti + 1) * 128], wt[:, kc, 0:ncol]) for kc in range(KC)], [wb, b_hT])
                        ot, obb = (obf_r if isf32 else ob_r).next()
                        op("act", lambda e, ot=ot, pt=pt, ncol=ncol: e.activation(out=ot[:, 0:ncol], in_=pt[:, 0:ncol], func=AF.Copy),
                           r=[pb], w=[obb])
                        dma("sp", lambda e, ot=ot, dst=dst, t0=t0, ncol=ncol, dcol0=dcol0: e.dma_start(
                            out=dst[t0:t0 + 128, dcol0:dcol0 + ncol], in_=ot[:, 0:ncol]), r=[obb])
            sch.barrier()
            sch.emit()

    fm1 = [(C_KA + j * 128, 128, KA[j], "A") for j in range(A_KV)]
    fm1 += [(C_KB + h * 128, 128, KB[h], None) for h in range(B_H)]
    fm1 += [(C_KI, 64, KI, "I")]
    tm1 = [(C_VA, 256, VA, 0, False), (C_VB, 512, VB, 0, False), (C_VB + 512, 512, VB, 512, False)]
    proj_pass("p1", x_all, pos_all, S, fm1, tm1)
    fm2 = [(C_QA + h * 128, 128, QA[h], "A") for h in range(A_H)]
    fm2 += [(C_QB + h * 128, 128, QB[h], None) for h in range(B_H)]
    fm2 += [(C_QI + h * 128, 128, QI[h], "I") for h in range(IDX_H // 2)]
    fm2 += [(C_GA + f * 128, 128, GA[f], None) for f in range(KC)]
    fm2 += [(C_GB + f * 128, 128, GB[f], None) for f in range(KC)]
    tm2 = [(C_WI, IDX_H, WI, 0, True)]
    proj_pass("p2", x_own, pos_own, So, fm2, tm2)
    if upto == "B":
        return finish()

    def pipeline(items, skews):
        if isinstance(skews, int):
            skews = [0, skews]
        n = len(items)
        for idx in range(n + max(skews)):
            for k, sk in enumerate(skews):
                if 0 <= idx - sk < n:
                    items[idx - sk][k]()

    def qblocks():
        for i in range(Go):
            for jj in range(4):
                qb = i * 4 + jj
                yield i, jj, qb, qb * 128, 128 * (4 * (2 * i + 1) + jj + 1), i % 2

    with ExitStack() as ph:
        kiT = ph.enter_context(nc.sbuf_tensor("kiT", [64, S], BF16))
        b_ki = Buf("kiT")
        amask_s = ph.enter_context(nc.sbuf_tensor("amask_s", [128, 2, 640], F32))
        b_am = Buf("amask")
        Ssc = ph.enter_context(nc.sbuf_tensor("Ssc", [128, S], F32))
        b_S = Buf("S")
        Mk = ph.enter_context(nc.sbuf_tensor("Mk", [128, S], BF16))
        b_M = Buf("Mk")
        qi_r = Ring(nc, ph, "qiT", [64, IDX_H, 128], BF16, 2)
        wi_r = Ring(nc, ph, "wit", [128, 3, IDX_H], F32, 2)
        rr_r = Ring(nc, ph, "rr", [128, 512], F32, 3)
        ps_r = Ring(nc, ph, "psI", [128, 512], F32, 4, psum=True)
        bs = ph.enter_context(nc.sbuf_tensor("bs", [128, 8], F32))
        b_bs = Buf("bs")
        dma("sp", lambda e: e.dma_start(out=kiT[:], in_=KI[:, :]), w=[b_ki])
        dma("sp", lambda e: e.dma_start(out=amask_s[:].rearrange("p m c -> p (m c)"), in_=amask_d[:, :]), w=[b_am])
        for (i, jj, qb, tok0, nk, m) in qblocks():
            qt, qbb = qi_r.next()
            for hh in range(2):
                dma("sp", lambda e, qt=qt, hh=hh, tok0=tok0: e.dma_start(
                    out=qt[:, hh::2, :], in_=QI[:, hh * 64:(hh + 1) * 64, tok0:tok0 + 128].rearrange("hp d t -> d hp t")),
                    w=[qbb])
            wt, wbb = wi_r.next()
            dma("sp", lambda e, wt=wt, tok0=tok0: e.dma_start(out=wt[:, 0, :], in_=WI[tok0:tok0 + 128, :]), w=[wbb])
            op("act", lambda e, wt=wt: e.activation(out=wt[:, 1, :], in_=wt[:, 0, :], func=AF.Abs), r=[wbb], w=[wbb])
            op("act", lambda e, wt=wt: e.activation(out=wt[:, 2, :], in_=wt[:, 0, :], func=AF.Sign), r=[wbb], w=[wbb])
            nch = (nk + 511) // 512
            for c in range(nch):
                c0 = c * 512
                cw = min(512, nk - c0)
                for h in range(IDX_H):
                    pt, pb = ps_r.next()
                    op("pe", lambda e, pt=pt, qt=qt, h=h, c0=c0, cw=cw: e.matmul(
                        pt[:, 0:cw], qt[:, h, :], kiT[:, c0:c0 + cw], start=True, stop=True),
                       r=[qbb, b_ki], w=[pb])
                    rt, rb = rr_r.next()
                    op("act", lambda e, rt=rt, pt=pt, wt=wt, h=h, cw=cw: e.activation(
                        out=rt[:, 0:cw], in_=pt[:, 0:cw], func=AF.Relu, scale=wt[:, 1, h:h + 1]),
                       r=[pb, wbb], w=[rb])
                    if h == 0:
                        op("dve", lambda e, rt=rt, wt=wt, h=h, c0=c0, cw=cw: e.tensor_scalar(
                            out=Ssc[:, c0:c0 + cw], in0=rt[:, 0:cw], scalar1=wt[:, 2, h:h + 1], scalar2=None, op0=ALU.mult),
                           r=[rb, wbb], w=[b_S])
                    else:
                        op("dve", lambda e, rt=rt, wt=wt, h=h, c0=c0, cw=cw: e.scalar_tensor_tensor(
                            out=Ssc[:, c0:c0 + cw], in0=rt[:, 0:cw], scalar=wt[:, 2, h:h + 1], in1=Ssc[:, c0:c0 + cw],
                            op0=ALU.mult, op1=ALU.add), r=[rb, wbb, b_S], w=[b_S])
            op("dve", lambda e, nk=nk: e.tensor_reduce(out=bs[:, 5:6], in_=Ssc[:, 0:nk], axis=AX.X, op=ALU.max),
               r=[b_S], w=[b_bs])
            op("dve", lambda e, nk=nk: e.tensor_reduce(out=bs[:, 0:1], in_=Ssc[:, 0:nk], axis=AX.X, op=ALU.min),
               r=[b_S], w=[b_bs])
            op("dve", lambda e: e.tensor_tensor(out=bs[:, 1:2], in0=bs[:, 5:6], in1=bs[:, 0:1], op=ALU.subtract),
               r=[b_bs], w=[b_bs])
            op("dve", lambda e, nk=nk, m=m: e.tensor_tensor(out=Ssc[:, nk - 640:nk], in0=Ssc[:, nk - 640:nk],
                                                             in1=amask_s[:, m, :], op=ALU.add),
               r=[b_S, b_am], w=[b_S])
            for it in range(NBIS):
                op("dve", lambda e: e.tensor_scalar(out=bs[:, 1:2], in0=bs[:, 1:2], scalar1=0.5, scalar2=None, op0=ALU.mult),
                   r=[b_bs], w=[b_bs])
                op("dve", lambda e: e.tensor_tensor(out=bs[:, 2:3], in0=bs[:, 0:1], in1=bs[:, 1:2], op=ALU.add),
                   r=[b_bs], w=[b_bs])
                op("dve", lambda e, nk=nk: e.tensor_scalar(out=Mk[:, 0:nk], in0=Ssc[:, 0:nk], scalar1=bs[:, 2:3], scalar2=None,
                                                           op0=ALU.is_ge, op1=ALU.add, accum_out=bs[:, 3:4]),
                   r=[b_bs, b_S, b_M], w=[b_M, b_bs])
                op("dve", lambda e: e.tensor_scalar(out=bs[:, 4:5], in0=bs[:, 3:4], scalar1=float(TOPK), scalar2=bs[:, 1:2],
                                                    op0=ALU.is_ge, op1=ALU.mult), r=[b_bs], w=[b_bs])
                op("dve", lambda e: e.tensor_tensor(out=bs[:, 0:1], in0=bs[:, 0:1], in1=bs[:, 4:5], op=ALU.add),
                   r=[b_bs], w=[b_bs])
            op("dve", lambda e, nk=nk: e.tensor_scalar(out=Mk[:, 0:nk], in0=Ssc[:, 0:nk], scalar1=bs[:, 0:1], scalar2=None,
                                                       op0=ALU.is_ge), r=[b_bs, b_S, b_M], w=[b_M])
            dma("sp", lambda e, qb=qb, nk=nk: e.dma_start(out=MK[qb, :, 0:nk], in_=Mk[:, 0:nk]), r=[b_M])
        sch.barrier()
        sch.emit()
    if upto == "C1":
        return finish()

    with ExitStack() as ph:
        kaT = ph.enter_context(nc.sbuf_tensor("kaT", [128, A_KV, S], BF16))
        va = ph.enter_context(nc.sbuf_tensor("va_s", [128, NT, A_KV * HD], BF16))
        b_kv = Buf("kv")
        mk_r = Ring(nc, ph, "mk", [128, S], BF16, 2)
        mt_r = Ring(nc, ph, "mt", [128, NT, 128], BF16, 2)
        qa_r = Ring(nc, ph, "qa", [128, A_H * 128], BF16, 2)
        e_r = Ring(nc, ph, "ee", [128, 512], BF16, 3)
        p_r = Ring(nc, ph, "pp", [128, 512], BF16, 4)
        rd_r = Ring(nc, ph, "rd", [128, 512], F32, 2)
        of_r = Ring(nc, ph, "of", [128, 512], F32, 2)
        ob_r = Ring(nc, ph, "obc", [128, 512], BF16, 2)
        psS = Ring(nc, ph, "psS", [128, 512], F32, 3, psum=True)
        psO = Ring(nc, ph, "psO", [128, 512], F32, 2, psum=True)
        psD = Ring(nc, ph, "psD", [128, 512], F32, 2, psum=True)
        psT = Ring(nc, ph, "psTc", [128, 1024], BF16, 1, psum=True)
        for j in range(A_KV):
            dma("sp", lambda e, j=j: e.dma_start(out=kaT[:, j, :], in_=KA[j]), w=[b_kv])
        dma("sp", lambda e: e.dma_start(out=va[:], in_=VA.rearrange("(kb p) c -> p kb c", p=128)), w=[b_kv])
        isq = 1.0 / math.sqrt(HD)
        items = []
        for (i, jj, qb, tok0, nk, m) in qblocks():
            nkb = nk // 128
            qst = {}

            def setup(qst=qst, qb=qb, tok0=tok0, nk=nk, nkb=nkb):
                mkt, mkb = mk_r.next()
                dma("sp", lambda e: e.dma_start(out=mkt[:, 0:nk], in_=MK[qb, :, 0:nk]), w=[mkb])
                qt, qbb = qa_r.next()
                dma("sp", lambda e: e.dma_start(
                    out=qt[:].rearrange("p (h t) -> p h t", h=A_H), in_=QA[:, :, tok0:tok0 + 128].rearrange("h d t -> d h t")),
                    w=[qbb])
                mtt, mtb = mt_r.next()
                for k0 in range(0, nkb, 8):
                    n8 = min(8, nkb - k0)
                    pt, pb = psT.next()
                    for u in range(n8):
                        op("pe", lambda e, pt=pt, u=u, k0=k0: e.transpose(
                            pt[:, u * 128:(u + 1) * 128], mkt[:, (k0 + u) * 128:(k0 + u + 1) * 128], identb[:]),
                           r=[mkb, b_cb] if u in (0, n8 - 1) else (), w=[pb])
                    op("act", lambda e, pt=pt, k0=k0, n8=n8: e.activation(
                        out=mtt[:, k0:k0 + n8, :].rearrange("p k t -> p (k t)"), in_=pt[:, 0:n8 * 128], func=AF.Copy),
                       r=[pb], w=[mtb])
                qst.update(qt=qt, qbb=qbb, mtt=mtt, mtb=mtb)

            for j in range(A_KV):
                jst = {}
                for kb in range(nkb):
                    it = {}

                    def stA(it=it, qst=qst, jst=jst, j=j, kb=kb, first=(j == 0 and kb == 0), setup=setup):
                        if first:
                            setup()
                        if kb == 0:
                            jst["po"], jst["pob"] = psO.next()
                            jst["pd"], jst["pdb"] = psD.next()
                        qt, qbb, mtt, mtb = qst["qt"], qst["qbb"], qst["mtt"], qst["mtb"]
                        ps, psb = psS.next()
                        op("pe", lambda e: e.matmul(ps[:], kaT[:, j, kb * 128:(kb + 1) * 128], qt[:, j * 512:(j + 1) * 512],
                                                    start=True, stop=True), r=[b_kv, qbb], w=[psb])
                        et, eb = e_r.next()
                        op("act", lambda e: e.activation(out=et[:], in_=ps[:], func=AF.Exp, scale=isq), r=[psb], w=[eb])
                        pp, ppb = p_r.next()
                        op("dve", lambda e: e.tensor_tensor(
                            out=pp[:].rearrange("p (g t) -> p g t", g=4), in0=et[:].rearrange("p (g t) -> p g t", g=4),
                            in1=mtt[:, kb, :].unsqueeze(1).broadcast_to([128, 4, 128]), op=ALU.mult), r=[eb, mtb], w=[ppb])
                        it["pp"], it["ppb"] = pp, ppb

                    def stB(it=it, jst=jst, j=j, kb=kb, nkb=nkb, tok0=tok0):
                        pp, ppb = it["pp"], it["ppb"]
                        po, pob, pd, pdb = jst["po"], jst["pob"], jst["pd"], jst["pdb"]
                        op("pe", lambda e: e.matmul(po[:], va[:, kb, j * 128:(j + 1) * 128], pp[:],
                                                    start=(kb == 0), stop=(kb == nkb - 1)), r=[ppb, b_kv], w=[pob])
                        op("pe", lambda e: e.matmul(pd[:], onesb[:], pp[:], start=(kb == 0), stop=(kb == nkb - 1)),
                           r=[ppb, b_cb], w=[pdb])
                        if kb == nkb - 1:
                            rd, rdb = rd_r.next()
                            op("dve", lambda e: e.reciprocal(out=rd[:], in_=pd[:]), r=[pdb], w=[rdb])
                            of_, ofb = of_r.next()
                            op("dve", lambda e: e.tensor_tensor(out=of_[:], in0=po[:], in1=rd[:], op=ALU.mult),
                               r=[pob, rdb], w=[ofb])
                            obt, obb = ob_r.next()
                            op("act", lambda e: e.activation(out=obt[:], in_=of_[:], func=AF.Copy), r=[ofb], w=[obb])
                            dma("sp", lambda e: e.dma_start(
                                out=OA[4 * j:4 * j + 4, :, tok0:tok0 + 128].rearrange("h d t -> d h t"),
                                in_=obt[:].rearrange("p (h t) -> p h t", h=4)), r=[obb])

                    items.append((stA, stB))
        pipeline(items, 2)
        sch.barrier()
        sch.emit()
    if upto == "C2":
        return finish()

    with ExitStack() as ph:
        cmask_s = ph.enter_context(nc.sbuf_tensor("cmask_s", [128, 2, 8, 512], BF16))
        b_cm = Buf("cmask")
        dma("sp", lambda e: e.dma_start(out=cmask_s[:].rearrange("p m j t -> p (m j t)"), in_=cmask_d[:, :]), w=[b_cm])
        kb_r = Ring(nc, ph, "kbT", [128, S], BF16, 2)
        vb_r = Ring(nc, ph, "vbh", [128, NT, 128], BF16, 2)
        qb_r = Ring(nc, ph, "qbT", [128, So], BF16, 2)
        E_r = Ring(nc, ph, "sbE", [128, 512], F32, 4)
        Em_r = Ring(nc, ph, "sbEm", [128, 512], F32, 4)
        SP_r = Ring(nc, ph, "sbSP", [128, 512], BF16, 4)
        SPm_r = Ring(nc, ph, "sbSPm", [128, 512], BF16, 4)
        X_r = Ring(nc, ph, "sbX", [128, 512], F32, 3)
        At_r = Ring(nc, ph, "sbAt", [128, 512], BF16, 4)
        Ra_r = Ring(nc, ph, "sbRa", [128, 512], F32, 2)
        obD_r = Ring(nc, ph, "obD", [128, 512], BF16, 2)
        psZ = Ring(nc, ph, "psZ", [128, 512], F32, 3, psum=True)
        psCc = Ring(nc, ph, "psCc", [128, 512], F32, 3, psum=True)
        psOd = Ring(nc, ph, "psOd", [128, 512], F32, 2, psum=True)
        isq = 1.0 / math.sqrt(HD)
        items = []
        for h in range(B_H):
            hst = {}

            def hsetup(hst=hst, h=h):
                kt, ktb = kb_r.next()
                vt, vtb = vb_r.next()
                qt, qtb = qb_r.next()
                dma("sp", lambda e: e.dma_start(out=kt[:], in_=KB[h]), w=[ktb])
                dma("sp", lambda e: e.dma_start(
                    out=vt[:], in_=VB[:, h * 128:(h + 1) * 128].rearrange("(kb p) d -> p kb d", p=128)), w=[vtb])
                dma("sp", lambda e: e.dma_start(out=qt[:], in_=QB[h]), w=[qtb])
                hst.update(kt=kt, ktb=ktb, vt=vt, vtb=vtb, qt=qt, qtb=qtb)

            for i in range(Go):
                nkb = 4 * (2 * i + 2)
                m = i % 2
                gst = {}
                for n, kb in enumerate(reversed(range(nkb))):
                    j = kb - (nkb - 8)
                    it = {}

                    def stA(it=it, hst=hst, gst=gst, i=i, n=n, kb=kb, j=j, m=m, first=(i == 0 and n == 0), hsetup=hsetup):
                        if first:
                            hsetup()
                        if n == 0:
                            gst["ra"], gst["rab"] = Ra_r.next()
                            gst["po"], gst["pob"] = psOd.next()
                        kt, ktb, qt, qtb = hst["kt"], hst["ktb"], hst["qt"], hst["qtb"]
                        pz, pzb = psZ.next()
                        op("pe", lambda e: e.matmul(pz[:], kt[:, kb * 128:(kb + 1) * 128], qt[:, i * 512:(i + 1) * 512],
                                                    start=True, stop=True), r=[ktb, qtb], w=[pzb])
                        Et, Eb = E_r.next()
                        op("act", lambda e: e.activation(out=Et[:], in_=pz[:], func=AF.Exp, scale=isq), r=[pzb], w=[Eb])
                        St, Sb = SP_r.next()
                        op("act", lambda e: e.activation(out=St[:], in_=Et[:], func=AF.Ln, bias=1.0), r=[Eb], w=[Sb])
                        if j >= 0:
                            Sm, Smb = SPm_r.next()
                            op("dve", lambda e: e.tensor_tensor(out=Sm[:], in0=St[:], in1=cmask_s[:, m, j, :], op=ALU.mult),
                               r=[Sb, b_cm], w=[Smb])
                            Em, Emb = Em_r.next()
                            op("pool", lambda e: e.tensor_tensor(out=Em[:], in0=Et[:], in1=cmask_s[:, m, j, :], op=ALU.mult),
                               r=[Eb, b_cm], w=[Emb])
                        else:
                            Sm, Smb = St, Sb
                            Em, Emb = Et, Eb
                        it.update(Sm=Sm, Smb=Smb, Em=Em, Emb=Emb)

                    def stB1(it=it, gst=gst, n=n, nkb=nkb):
                        Sm, Smb = it["Sm"], it["Smb"]
                        ra, rab = gst["ra"], gst["rab"]
                        pc, pcb = psCc.next()
                        op("pe", lambda e: e.matmul(pc[:], triI[:], Sm[:], start=True, stop=(n == 0)), r=[Smb, b_cb], w=[pcb])
                        if n > 0:
                            op("pe", lambda e: e.matmul(pc[:], onesf, ra[:], start=False, stop=True), r=[rab, b_consts], w=[pcb])
                        if n == 0:
                            op("pool", lambda e: e.tensor_copy(out=ra[:], in_=Sm[:]), r=[Smb], w=[rab])
                        elif n < nkb - 1:
                            op("pool", lambda e: e.tensor_tensor(out=ra[:], in0=ra[:], in1=Sm[:], op=ALU.add), r=[Smb, rab], w=[rab])
                        it.update(pc=pc, pcb=pcb)

                    def stB2(it=it):
                        Em, Emb, pc, pcb = it["Em"], it["Emb"], it["pc"], it["pcb"]
                        Xt, Xb = X_r.next()
                        op("act", lambda e: e.activation(out=Xt[:], in_=pc[:], func=AF.Exp, scale=-1.0), r=[pcb], w=[Xb])
                        At, Ab = At_r.next()
                        op("dve", lambda e: e.tensor_tensor(out=At[:], in0=Em[:], in1=Xt[:], op=ALU.mult), r=[Emb, Xb], w=[Ab])
                        it.update(At=At, Ab=Ab)

                    def stB3(it=it, hst=hst, gst=gst, h=h, i=i, n=n, kb=kb, nkb=nkb):
                        At, Ab = it["At"], it["Ab"]
                        po, pob = gst["po"], gst["pob"]
                        vt, vtb = hst["vt"], hst["vtb"]
                        op("pe", lambda e: e.matmul(po[:], vt[:, kb, :], At[:], start=(n == 0), stop=(n == nkb - 1)),
                           r=[vtb, Ab], w=[pob])
                        if n == nkb - 1:
                            ot, otb = obD_r.next()
                            op("act", lambda e: e.activation(out=ot[:], in_=po[:], func=AF.Copy), r=[pob], w=[otb])
                            dma("sp", lambda e: e.dma_start(out=OB[h, :, i * 512:(i + 1) * 512], in_=ot[:]), r=[otb])

                    items.append((stA, stB1, stB2, stB3))
        pipeline(items, [0, 2, 3, 5])
        sch.barrier()
        sch.emit()
    if upto == "D":
        return finish()

    with ExitStack() as ph:
        wa = ph.enter_context(nc.sbuf_tensor("wa", [128, 8, D], BF16))
        wb_ = ph.enter_context(nc.sbuf_tensor("wb_", [128, 8, D], BF16))
        b_wab = Buf("wab")
        for (wt, src) in ((wa, w_bra), (wb_, w_brb)):
            sv = src.rearrange("(kc p) n -> p kc n", p=128)
            for cq in range(4):
                dma("pool", lambda e, wt=wt, sv=sv, cq=cq: e.dma_start(
                    out=wt[:, :, cq * 512:(cq + 1) * 512], in_=sv[:, :, cq * 512:(cq + 1) * 512]), w=[b_wab])
        zt = ph.enter_context(nc.sbuf_tensor("zt", [128, 4096], BF16))
        b_zt = Buf("zt")
        op("dve", lambda e: e.memset(zt[:], 0.0), w=[b_zt])
        XGf = XG.rearrange("(r p two) c -> r p (two c)", p=128, two=2)
        for r_ in range(NE * CAP // 256):
            dma("sp", lambda e, r_=r_: e.dma_start(out=XGf[r_], in_=zt[:]), r=[b_zt])
        oa_r = Ring(nc, ph, "oaT", [128, 8, 512], BF16, 2)
        ob2_r = Ring(nc, ph, "obT", [128, 8, 512], BF16, 2)
        g_r = Ring(nc, ph, "gt", [128, 2, 512], BF16, 3)
        sg_r = Ring(nc, ph, "sgt", [128, 2, 512], F32, 2)
        t1e_r = Ring(nc, ph, "t1e", [128, 512], F32, 2)
        t2e_r = Ring(nc, ph, "t2e", [128, 512], F32, 2)
        mg_r = Ring(nc, ph, "mgo", [128, 512], BF16, 3)
        psa_r = Ring(nc, ph, "psEa", [128, 512], F32, 3, psum=True)
        psb_r = Ring(nc, ph, "psEb", [128, 512], F32, 3, psum=True)
        for tg in range(So // 512):
            t0 = tg * 512
            oat, oab = oa_r.next()
            obt, obb = ob2_r.next()
            dma("sp", lambda e, oat=oat, t0=t0: e.dma_start(out=oat[:], in_=OA[:, :, t0:t0 + 512].rearrange("h d t -> d h t")), w=[oab])
            dma("sp", lambda e, obt=obt, t0=t0: e.dma_start(out=obt[:], in_=OB[:, :, t0:t0 + 512].rearrange("h d t -> d h t")), w=[obb])
            for fo in range(KC):
                gt, gb_ = g_r.next()
                dma("sp", lambda e, gt=gt, fo=fo, t0=t0: e.dma_start(out=gt[:, 0, :], in_=GA[fo, :, t0:t0 + 512]), w=[gb_])
                dma("sp", lambda e, gt=gt, fo=fo, t0=t0: e.dma_start(out=gt[:, 1, :], in_=GB[fo, :, t0:t0 + 512]), w=[gb_])
                st_, sb_ = sg_r.next()
                op("act", lambda e, st_=st_, gt=gt: e.activation(out=st_[:], in_=gt[:], func=AF.Sigmoid), r=[gb_], w=[sb_])
                pa, pab = psa_r.next()
                mmgroup(pa[:], pab, [(wa[:, kc, fo * 128:(fo + 1) * 128], oat[:, kc, :]) for kc in range(8)], [b_wab, oab])
                pb2, pbb = psb_r.next()
                mmgroup(pb2[:], pbb, [(wb_[:, kc, fo * 128:(fo + 1) * 128], obt[:, kc, :]) for kc in range(8)], [b_wab, obb])
                a1, a1b = t1e_r.next()
                a2, a2b = t2e_r.next()
                op("dve", lambda e, a1=a1, pa=pa, st_=st_: e.tensor_tensor(out=a1[:], in0=pa[:], in1=st_[:, 0, :], op=ALU.mult),
                   r=[pab, sb_], w=[a1b])
                op("dve", lambda e, a2=a2, pb2=pb2, st_=st_: e.tensor_tensor(out=a2[:], in0=pb2[:], in1=st_[:, 1, :], op=ALU.mult),
                   r=[pbb, sb_], w=[a2b])
                mt_, mb_ = mg_r.next()
                op("pool", lambda e, mt_=mt_, a1=a1, a2=a2: e.tensor_tensor(out=mt_[:], in0=a1[:], in1=a2[:], op=ALU.add),
                   r=[a1b, a2b], w=[mb_])
                dma("sp", lambda e, mt_=mt_, fo=fo, t0=t0: e.dma_start(out=MG[fo, :, t0:t0 + 512], in_=mt_[:]), r=[mb_])
        sch.barrier()
        sch.emit()
    if upto == "E1":
        return finish()

    NSLOT = NE * CAP
    NSOK = min(NSLOT, 65535)
    with ExitStack() as ph:
        wo = ph.enter_context(nc.sbuf_tensor("wo", [128, KC, D], BF16))
        b_wo = Buf("wo")
        wo_v = w_out.rearrange("(kc p) n -> p kc n", p=128)
        for cq in range(4):
            dma("pool", lambda e, cq=cq: e.dma_start(out=wo[:, :, cq * 512:(cq + 1) * 512], in_=wo_v[:, :, cq * 512:(cq + 1) * 512]),
                w=[b_wo])
        wr = ph.enter_context(nc.sbuf_tensor("wr", [128, KC, NE], BF16))
        b_wr = Buf("wr")
        dma("pool", lambda e: e.dma_start(out=wr[:], in_=w_router.rearrange("(kc p) n -> p kc n", p=128)), w=[b_wr])
        brow = ph.enter_context(nc.sbuf_tensor("brow", [128, NE], F32))
        dma("sp", lambda e: e.dma_start(out=brow[:], in_=b_router[0:1, :].partition_broadcast(128)), w=[b_wr])
        base = ph.enter_context(nc.sbuf_tensor("base", [128, NE], F32))
        slotbase = ph.enter_context(nc.sbuf_tensor("slotbase", [128, NE], F32))
        b_base = Buf("base")
        op("dve", lambda e: e.memset(base[:], 0.0), w=[b_base])
        op("dve", lambda e: e.tensor_scalar(out=slotbase[:], in0=iota_f[:, 0:NE], scalar1=float(CAP), scalar2=None, op0=ALU.mult),
           r=[b_consts], w=[b_base])
        mg_r = Ring(nc, ph, "mgi", [128, KC, 512], BF16, 1)
        x_r = Ring(nc, ph, "xe", [128, D], F32, 2)
        mix_r = Ring(nc, ph, "mixs", [128, D], F32, 1)
        x1_r = Ring(nc, ph, "x1t", [128, D], F32, 2)
        xn_r = Ring(nc, ph, "xn2", [128, D], BF16, 2)
        h2_r = Ring(nc, ph, "h2T", [128, KC, 128], BF16, 2)
        junk = ph.enter_context(nc.sbuf_tensor("junkE", [128, D], BF16))
        b_junk = Buf("junkE")
        st_r = Ring(nc, ph, "stE", [128, 8], F32, 4)
        rt_r = Ring(nc, ph, "rtE", [128, 8, NE], F32, 2)
        mkb_r = Ring(nc, ph, "mkE", [128, NE], BF16, 2)
        t8_r = Ring(nc, ph, "t8E", [128, 24], F32, 2)
        psm_r = Ring(nc, ph, "psEm", [128, 512], F32, 3, psum=True)
        pst_r = Ring(nc, ph, "psEt", [128, 1024], BF16, 2, psum=True)
        psr_r = Ring(nc, ph, "psEr", [128, 512], F32, 2, psum=True)
        for tg in range(So // 512):
            mgt, mgb = mg_r.next()
            dma("sp", lambda e, mgt=mgt, tg=tg: e.dma_start(
                out=mgt[:], in_=MG[:, :, tg * 512:(tg + 1) * 512].rearrange("f d t -> d f t")), w=[mgb])
            for tt in range(4):
                ti = tg * 4 + tt
                tok0 = ti * 128
                xt, xb = x_r.next()
                dma("sp", lambda e, xt=xt, tok0=tok0: e.dma_start(out=xt[:], in_=x_own[tok0:tok0 + 128, :]), w=[xb])
                mx, mxb = mix_r.next()
                for dc in range(4):
                    pm, pmb = psm_r.next()
                    mmgroup(pm[:], pmb, [(mgt[:, kc, tt * 128:(tt + 1) * 128], wo[:, kc, dc * 512:(dc + 1) * 512])
                                         for kc in range(KC)], [mgb, b_wo])
                    op("act", lambda e, mx=mx, pm=pm, dc=dc: e.activation(out=mx[:, dc * 512:(dc + 1) * 512], in_=pm[:], func=AF.Copy),
                       r=[pmb], w=[mxb])
                st, sb_ = st_r.next()
                op("act", lambda e, st=st, mx=mx: e.activation(out=junk[:], in_=mx[:], func=AF.Square, accum_out=st[:, 0:1]),
                   r=[mxb], w=[b_junk, sb_])
                op("act", lambda e, st=st: e.activation(out=st[:, 1:2], in_=st[:, 0:1], func=AF.Sqrt, scale=1.0 / D, bias=EPS),
                   r=[sb_], w=[sb_])
                op("dve", lambda e, st=st: e.reciprocal(out=st[:, 2:3], in_=st[:, 1:2]), r=[sb_], w=[sb_])
                op("dve", lambda e, st=st, mx=mx: e.scalar_tensor_tensor(out=mx[:], in0=mx[:], scalar=st[:, 2:3], in1=ga1row[:],
                                                                        op0=ALU.mult, op1=ALU.mult),
                   r=[mxb, sb_, b_garow[0]], w=[mxb])
                x1, x1b = x1_r.next()
                op("pool", lambda e, x1=x1, mx=mx, xt=xt: e.tensor_tensor(out=x1[:], in0=mx[:], in1=xt[:], op=ALU.add),
                   r=[mxb, xb], w=[x1b])
                dma("sp", lambda e, x1=x1, tok0=tok0: e.dma_start(out=X1[tok0:tok0 + 128, :], in_=x1[:]), r=[x1b])
                op("act", lambda e, st=st, x1=x1: e.activation(out=junk[:], in_=x1[:], func=AF.Square, accum_out=st[:, 3:4]),
                   r=[x1b], w=[b_junk, sb_])
                op("act", lambda e, st=st: e.activation(out=st[:, 4:5], in_=st[:, 3:4], func=AF.Sqrt, scale=1.0 / D, bias=EPS),
                   r=[sb_], w=[sb_])
                op("dve", lambda e, st=st: e.reciprocal(out=st[:, 5:6], in_=st[:, 4:5]), r=[sb_], w=[sb_])
                xn, xnb = xn_r.next()
                op("dve", lambda e, xn=xn, x1=x1, st=st: e.tensor_scalar(out=xn[:], in0=x1[:], scalar1=st[:, 5:6], scalar2=None,
                                                                       op0=ALU.mult), r=[x1b, sb_], w=[xnb])
                h2, h2b = h2_r.next()
                for half in range(2):
                    pt, pb = pst_r.next()
                    for j in range(8):
                        kc = half * 8 + j
                        op("pe", lambda e, pt=pt, j=j, kc=kc, xn=xn: e.transpose(
                            pt[:, j * 128:(j + 1) * 128], xn[:, kc * 128:(kc + 1) * 128], identb[:]),
                           r=[xnb, b_cb] if j in (0, 7) else (), w=[pb])
                    for j in range(8):
                        kc = half * 8 + j
                        op("act", lambda e, pt=pt, j=j, kc=kc, h2=h2: e.activation(
                            out=h2[:, kc, :], in_=pt[:, j * 128:(j + 1) * 128], func=AF.Identity,
                            scale=modc[:, 2 * KC + kc:2 * KC + kc + 1], bias=modc[:, 3 * KC + kc:3 * KC + kc + 1]),
                           r=[pb, b_modc], w=[h2b])
                pr, prb = psr_r.next()
                mmgroup(pr[:, 0:NE], prb, [(h2[:, kc, :], wr[:, kc, :]) for kc in range(KC)], [h2b, b_wr])
                rt, rtb = rt_r.next()
                t8, t8b = t8_r.next()
                mk, mkb = mkb_r.next()
                lg, pos_, sla, ov, jk = (rt[:, q, :] for q in range(5))
                op("dve", lambda e, lg=lg, pr=pr: e.tensor_tensor(out=lg, in0=pr[:, 0:NE], in1=brow[:], op=ALU.add),
                   r=[prb, b_wr], w=[rtb])
                op("dve", lambda e, lg=lg, t8=t8: e.max(out=t8[:, 0:8], in_=lg), r=[rtb], w=[t8b])
                op("dve", lambda e, t8=t8: e.tensor_scalar(out=t8[:, 8:12], in0=t8[:, 0:4], scalar1=t8[:, 0:1], scalar2=None,
                                                          op0=ALU.subtract), r=[t8b], w=[t8b])
                op("act", lambda e, t8=t8, st=st: e.activation(out=t8[:, 12:16], in_=t8[:, 8:12], func=AF.Exp, accum_out=st[:, 6:7]),
                   r=[t8b], w=[t8b, sb_])
                op("dve", lambda e, st=st: e.reciprocal(out=st[:, 7:8], in_=st[:, 6:7]), r=[sb_], w=[sb_])
                op("dve", lambda e, mk=mk, lg=lg, t8=t8: e.tensor_scalar(out=mk[:], in0=lg, scalar1=t8[:, 3:4], scalar2=None,
                                                                       op0=ALU.is_ge), r=[rtb, t8b], w=[mkb])
                pp, ppb = psr_r.next()
                op("pe", lambda e, pp=pp, mk=mk: e.matmul(pp[:, 0:NE], triS[:], mk[:], start=True, stop=True),
                   r=[mkb, b_cb], w=[ppb])
                op("pe", lambda e, pp=pp, mk=mk: e.matmul(pp[:, 64:64 + NE], onesb[:], mk[:], start=True, stop=True),
                   r=[mkb, b_cb], w=[ppb])
                op("dve", lambda e, pos_=pos_, pp=pp: e.tensor_tensor(out=pos_, in0=pp[:, 0:NE], in1=base[:], op=ALU.add),
                   r=[ppb, b_base], w=[rtb])
                op("dve", lambda e, pp=pp: e.tensor_tensor(out=base[:], in0=pp[:, 64:64 + NE], in1=base[:], op=ALU.add),
                   r=[ppb, b_base], w=[b_base])
                op("dve", lambda e, ov=ov, pos_=pos_: e.tensor_scalar(out=ov, in0=pos_, scalar1=float(CAP), scalar2=1.0e9,
                                                                    op0=ALU.is_ge, op1=ALU.mult), r=[rtb], w=[rtb])
                op("dve", lambda e, sla=sla, pos_=pos_: e.tensor_tensor(out=sla, in0=pos_, in1=slotbase[:], op=ALU.add),
                   r=[rtb, b_base], w=[rtb])
                op("dve", lambda e, sla=sla, ov=ov: e.tensor_tensor(out=sla, in0=sla, in1=ov, op=ALU.add), r=[rtb], w=[rtb])
                for k in range(4):
                    op("dve", lambda e, jk=jk, lg=lg, t8=t8, sla=sla, k=k: e.scalar_tensor_tensor(
                        out=jk, in0=lg, scalar=t8[:, k:k + 1], in1=sla, op0=ALU.is_equal, op1=ALU.mult,
                        accum_out=t8[:, 16 + k:17 + k]), r=[rtb, t8b], w=[rtb, t8b])
                op("dve", lambda e, t8=t8, ti=ti: e.tensor_copy(out=rslot[:, ti * 4:(ti + 1) * 4], in_=t8[:, 16:20]),
                   r=[t8b], w=[b_route])
                op("dve", lambda e, t8=t8: e.tensor_scalar(out=t8[:, 20:24], in0=t8[:, 16:20], scalar1=float(NSOK), scalar2=None,
                                                          op0=ALU.is_lt), r=[t8b], w=[t8b])
                op("dve", lambda e, t8=t8, st=st: e.scalar_tensor_tensor(
                    out=t8[:, 12:16], in0=t8[:, 12:16], scalar=st[:, 7:8], in1=t8[:, 20:24], op0=ALU.mult, op1=ALU.mult),
                   r=[t8b, sb_], w=[t8b])
                op("dve", lambda e, t8=t8, ti=ti: e.tensor_copy(out=rwgt[:, ti * 4:(ti + 1) * 4], in_=t8[:, 12:16]),
                   r=[t8b], w=[b_route])
                for k in range(4):
                    dma("pool", lambda e, xn=xn, ti=ti, k=k: e.indirect_dma_start(
                        out=XG[0:NSOK, :], out_offset=bass.IndirectOffsetOnAxis(ap=rslot[:, ti * 4 + k:ti * 4 + k + 1], axis=0),
                        in_=xn[:, :], in_offset=None, bounds_check=sch.breg(e, NSOK - 1), oob_is_err=False),
                        r=[xnb, b_route])
        sch.barrier()
        sch.emit()
    if upto == "E2":
        return finish()

    NBLK = CAP // 128
    NTG = CAP // 512
    with ExitStack() as ph:
        XT = ph.enter_context(nc.sbuf_tensor("XT", [128, KC, CAP], BF16))
        b_XT = Buf("XT")
        actT = ph.enter_context(nc.sbuf_tensor("actT", [128, KC, CAP], BF16))
        b_act = [Buf(f"act{f}") for f in range(KC)]
        wc_r = Ring(nc, ph, "wcG", [128, KC, 256], BF16, 2)
        gt_r = Ring(nc, ph, "gtG", [128, D], BF16, 3)
        bb_r = Ring(nc, ph, "bbG", [128, 2 * KC], F32, 2)
        b2_r = Ring(nc, ph, "b2G", [1, 256], BF16, 3)
        g_r = Ring(nc, ph, "gG", [128, 512], F32, 2)
        s_r = Ring(nc, ph, "sG", [128, 512], F32, 2)
        l_r = Ring(nc, ph, "lG", [128, 512], F32, 2)
        gs_r = Ring(nc, ph, "gsG", [128, 512], F32, 2)
        ot_r = Ring(nc, ph, "otG", [128, 256], BF16, 3)
        psT_r = Ring(nc, ph, "psGt", [128, 1024], BF16, 2, psum=True)
        psg_r = Ring(nc, ph, "psGg", [128, 512], F32, 2, psum=True)
        psl_r = Ring(nc, ph, "psGl", [128, 512], F32, 2, psum=True)
        pso_r = Ring(nc, ph, "psGo", [128, 512], F32, 2, psum=True)
        def xt_steps(ex):
            steps = []
            holds = [dict() for _ in range(NBLK)]
            for blk in range(NBLK):
                hold = holds[blk]
                for half in range(2):
                    def step(ex=ex, blk=blk, half=half, hold=hold, holds=holds):
                        def fetch(b):
                            if b < NBLK and "gt" not in holds[b]:
                                gt, gtb = gt_r.next()
                                r0 = ex * CAP + b * 128
                                dma("sp", lambda e: e.dma_start(out=gt[:], in_=XG[r0:r0 + 128, :]), w=[gtb])
                                holds[b]["gt"], holds[b]["gtb"] = gt, gtb
                        if half == 0:
                            fetch(blk)
                            fetch(blk + 1)
                        gt, gtb = hold["gt"], hold["gtb"]
                        pt, pb = psT_r.next()
                        for j in range(8):
                            kc = half * 8 + j
                            op("pe", lambda e, j=j, kc=kc: e.transpose(
                                pt[:, j * 128:(j + 1) * 128], gt[:, kc * 128:(kc + 1) * 128], identb[:]),
                               r=[gtb, b_cb] if j in (0, 7) else (), w=[pb])
                        for j in range(8):
                            kc = half * 8 + j
                            if half == 0:
                                op("act", lambda e, j=j, kc=kc: e.activation(
                                    out=XT[:, kc, blk * 128:(blk + 1) * 128], in_=pt[:, j * 128:(j + 1) * 128], func=AF.Identity,
                                    scale=modc[:, 2 * KC + kc:2 * KC + kc + 1], bias=modc[:, 3 * KC + kc:3 * KC + kc + 1]),
                                   r=[pb, b_modc], w=[b_XT])
                            else:
                                op("dve", lambda e, j=j, kc=kc: e.tensor_scalar(
                                    out=XT[:, kc, blk * 128:(blk + 1) * 128], in0=pt[:, j * 128:(j + 1) * 128],
                                    scalar1=modc[:, 2 * KC + kc:2 * KC + kc + 1], scalar2=modc[:, 3 * KC + kc:3 * KC + kc + 1],
                                    op0=ALU.mult, op1=ALU.add), r=[pb, b_modc], w=[b_XT])
                    steps.append(step)
            return steps

        chunks = [(ex_, kind, c) for ex_ in range(NE) for (kind, c) in
                  ([("w1", c) for c in range(KC)] + [("w2", c) for c in range(D // 256)])]
        wslots = {}
        b2slots = {}

        def issue_chunk(ci):
            if ci >= len(chunks) or ci in wslots:
                return
            ex_, kind, c = chunks[ci]
            src = (w1 if kind == "w1" else w2)[ex_].rearrange("(kc p) n -> p kc n", p=128)
            wt, wb = wc_r.next()
            dma("pool", lambda e: e.dma_start(out=wt[:], in_=src[:, :, c * 256:(c + 1) * 256]), w=[wb])
            if kind == "w2":
                b2t, b2b = b2_r.next()
                dma("pool", lambda e: e.dma_start(out=b2t[:], in_=b2[ex_:ex_ + 1, c * 256:(c + 1) * 256]), w=[b2b])
                b2slots[ci] = (b2t, b2b)
            wslots[ci] = (wt, wb)

        def get_chunk(ex_, kind, c):
            ci = ex_ * (KC + D // 256) + (c if kind == "w1" else KC + c)
            issue_chunk(ci)
            issue_chunk(ci + 1)
            return wslots.pop(ci)

        for ex in range(NE):
            bt, btb = bb_r.next()
            dma("sp", lambda e, bt=bt, ex=ex: e.dma_start(out=bt[:], in_=b1c[ex]), w=[btb])
            w1v = w1[ex].rearrange("(kc p) n -> p kc n", p=128)
            w2v = w2[ex].rearrange("(kc p) n -> p kc n", p=128)
            if ex == 0:
                for st_ in xt_steps(0):
                    st_()
            for cq in range(KC):
                wt, wb = get_chunk(ex, "w1", cq)
                for tg in range(NTG):
                    pg, pgb = psg_r.next()
                    mmgroup(pg[:], pgb, [(wt[:, kc, 0:256:2], XT[:, kc, tg * 512:(tg + 1) * 512]) for kc in range(KC)], [wb, b_XT])
                    pl, plb = psl_r.next()
                    mmgroup(pl[:], plb, [(wt[:, kc, 1:256:2], XT[:, kc, tg * 512:(tg + 1) * 512]) for kc in range(KC)], [wb, b_XT])
                    g_, gb_ = g_r.next()
                    op("dve", lambda e, g_=g_, pg=pg, bt=bt, cq=cq: e.tensor_scalar(
                        out=g_[:], in0=pg[:], scalar1=bt[:, cq:cq + 1], scalar2=LIMIT, op0=ALU.add, op1=ALU.min),
                       r=[pgb, btb], w=[gb_])
                    sg, sgb = s_r.next()
                    op("act", lambda e, sg=sg, g_=g_: e.activation(out=sg[:], in_=g_[:], func=AF.Sigmoid, scale=ALPHA),
                       r=[gb_], w=[sgb])
                    l_, lb_ = l_r.next()
                    op("dve", lambda e, l_=l_, pl=pl, bt=bt, cq=cq: e.tensor_scalar(
                        out=l_[:], in0=pl[:], scalar1=bt[:, KC + cq:KC + cq + 1], scalar2=LIMIT, op0=ALU.add, op1=ALU.min),
                       r=[plb, btb], w=[lb_])
                    op("dve", lambda e, l_=l_: e.tensor_scalar(out=l_[:], in0=l_[:], scalar1=-LIMIT, scalar2=1.0,
                                                               op0=ALU.max, op1=ALU.add), r=[lb_], w=[lb_])
                    gs, gsb = gs_r.next()
                    op("pool", lambda e, gs=gs, g_=g_, sg=sg: e.tensor_tensor(out=gs[:], in0=g_[:], in1=sg[:], op=ALU.mult),
                       r=[gb_, sgb], w=[gsb])
                    op("dve", lambda e, gs=gs, l_=l_, cq=cq, tg=tg: e.tensor_tensor(
                        out=actT[:, cq, tg * 512:(tg + 1) * 512], in0=gs[:], in1=l_[:], op=ALU.mult),
                       r=[gsb, lb_], w=[b_act[cq]])
            nxt = xt_steps(ex + 1) if ex + 1 < NE else []
            w2step = 0
            for dc in range(D // 256):
                wt, wb = get_chunk(ex, "w2", dc)
                b2t, b2b = b2slots.pop(ex * (KC + D // 256) + KC + dc)
                for blk in range(NBLK):
                    po, pob = pso_r.next()
                    pairs = [(actT[:, fc, blk * 128:(blk + 1) * 128], wt[:, fc, :]) for fc in range(KC)]
                    n = len(pairs)
                    for q, (l, r_) in enumerate(pairs):
                        op("pe", lambda e, po=po, l=l, r_=r_, q=q: e.matmul(po[:, 0:256], l, r_, start=(q == 0), stop=False),
                           r=(b_act + [wb]) if q in (0, n - 1) else (), w=[pob])
                    op("pe", lambda e, po=po, b2t=b2t, dc=dc: e.matmul(
                        po[:, 0:256], onesb[0:1, :], b2t[0:1, :], start=False, stop=True),
                       r=[b2b, b_cb], w=[pob])
                    ot, otb = ot_r.next()
                    op("act", lambda e, ot=ot, po=po: e.activation(out=ot[:], in_=po[:, 0:256], func=AF.Copy), r=[pob], w=[otb])
                    r0 = ex * CAP + blk * 128
                    dma("sp", lambda e, ot=ot, r0=r0, dc=dc: e.dma_start(out=YG[r0:r0 + 128, dc * 256:(dc + 1) * 256], in_=ot[:]),
                        r=[otb])
                    w2step += 1
                    if nxt and w2step % max(1, (D // 256) * NBLK // len(nxt)) == 0:
                        nxt.pop(0)()
            while nxt:
                nxt.pop(0)()
        sch.barrier()
        sch.emit()
    if upto == "G":
        return finish()

    with ExitStack() as ph:
        y_r = Ring(nc, ph, "yH", [128, D], BF16, 5)
        x1_r = Ring(nc, ph, "x1H", [128, D], F32, 2)
        f_r = Ring(nc, ph, "fH", [128, D], F32, 2)
        o_r = Ring(nc, ph, "oH", [128, D], F32, 2)
        junk = ph.enter_context(nc.sbuf_tensor("junkH", [128, D], BF16))
        b_junk = Buf("junkH")
        st_r = Ring(nc, ph, "stH", [128, 4], F32, 3)
        for (yt, yb) in y_r.tiles:
            op("dve", lambda e, yt=yt: e.memset(yt[:], 0.0), w=[yb])
        for ti in range(NTo):
            tok0 = ti * 128
            ys = []
            for k in range(4):
                yt, yb = y_r.next()
                dma("pool", lambda e, yt=yt, ti=ti, k=k: e.indirect_dma_start(
                    out=yt[:, :], out_offset=None, in_=YG[0:NSOK, :],
                    in_offset=bass.IndirectOffsetOnAxis(ap=rslot[:, ti * 4 + k:ti * 4 + k + 1], axis=0),
                    bounds_check=sch.breg(e, NSOK - 1), oob_is_err=False), r=[b_route], w=[yb])
                ys.append((yt, yb))
            x1, x1b = x1_r.next()
            dma("sp", lambda e, x1=x1, tok0=tok0: e.dma_start(out=x1[:], in_=X1[tok0:tok0 + 128, :]), w=[x1b])
            ft, fb = f_r.next()
            op("dve", lambda e, ft=ft, ti=ti, y0=ys[0][0]: e.tensor_scalar(
                out=ft[:], in0=y0[:], scalar1=rwgt[:, ti * 4:ti * 4 + 1], scalar2=None, op0=ALU.mult),
               r=[ys[0][1], b_route], w=[fb])
            for k in range(1, 4):
                op("dve", lambda e, ft=ft, ti=ti, k=k, yk=ys[k][0]: e.scalar_tensor_tensor(
                    out=ft[:], in0=yk[:], scalar=rwgt[:, ti * 4 + k:ti * 4 + k + 1], in1=ft[:], op0=ALU.mult, op1=ALU.add),
                   r=[ys[k][1], b_route, fb], w=[fb])
            st, sb_ = st_r.next()
            op("act", lambda e, st=st, ft=ft: e.activation(out=junk[:], in_=ft[:], func=AF.Square, accum_out=st[:, 0:1]),
               r=[fb], w=[b_junk, sb_])
            op("act", lambda e, st=st: e.activation(out=st[:, 1:2], in_=st[:, 0:1], func=AF.Sqrt, scale=1.0 / D, bias=EPS),
               r=[sb_], w=[sb_])
            op("dve", lambda e, st=st: e.reciprocal(out=st[:, 2:3], in_=st[:, 1:2]), r=[sb_], w=[sb_])
            op("dve", lambda e, st=st, ft=ft: e.scalar_tensor_tensor(out=ft[:], in0=ft[:], scalar=st[:, 2:3], in1=ga2row[:],
                                                                    op0=ALU.mult, op1=ALU.mult),
               r=[fb, sb_, b_garow[1]], w=[fb])
            ot, otb = o_r.next()
            op("pool", lambda e, ot=ot, ft=ft, x1=x1: e.tensor_tensor(out=ot[:], in0=ft[:], in1=x1[:], op=ALU.add),
               r=[fb, x1b], w=[otb])
            dma("sp", lambda e, ot=ot, tok0=tok0: e.dma_start(out=out_d[tok0:tok0 + 128, :], in_=ot[:]), r=[otb])
        sch.barrier()
        sch.emit()
    return finish()


def make_consts():
    c = np.zeros((128, 8, 128), np.float32)
    c[:, 0, :] = np.eye(128)
    for dp in range(16):
        c[dp + 16, 1, dp] = -1.0
        c[dp, 1, dp + 16] = 1.0
    for o in (0, 64):
        for dp in range(8):
            c[o + dp + 8, 2, o + dp] = -1.0
            c[o + dp, 2, o + dp + 8] = 1.0
    k = np.arange(128)
    c[:, 3, :] = (k[:, None] >= k[None, :])
    c[:, 4, :] = (k[:, None] < k[None, :])
    c[:, 5, :] = 1.0
    invA = THETA ** (-(np.arange(16, dtype=np.float32)) / np.float32(16))
    invI = THETA ** (-(np.arange(8, dtype=np.float32)) / np.float32(8))
    for p in range(128):
        c[p, 6, 0] = invA[p % 16] if p < 32 else 0.0
        c[p, 6, 1] = invI[(p % 64) % 8] if (p % 64) < 16 else 0.0
    c[:, 7, :] = k[None, :]
    return np.ascontiguousarray(c.reshape(128, 8 * 128))


def make_masks(half):
    p = np.arange(128)[:, None]
    tl = np.arange(512)[None, :]
    cm = np.zeros((128, 2, 8, 512), np.float32)
    am = np.zeros((128, 2, 640), np.float32)
    cc = np.arange(640)[None, :]
    for m in range(2):
        delta = (1 if m == 0 else 0) if half == 0 else (0 if m == 0 else 1)
        for j in range(8):
            if delta == 0:
                cm[:, m, j, :] = (128 * (j - 4) + p) < tl
            else:
                cm[:, m, j, :] = (128 * j + p) < tl
        lim = 64 * (p // 64 + 1)
        am[:, m, :] = np.where(512 * (delta - 1) + cc >= lim, NEG, 0.0)
    return (np.ascontiguousarray(cm.reshape(128, -1)).astype(ml_dtypes.bfloat16),
            np.ascontiguousarray(am.reshape(128, -1)))


def col_layout(v):
    return np.ascontiguousarray(v.reshape(-1, 128).T)


def prep(inputs, cfg, batches):
    S, NE = cfg["S"], cfg["NE"]
    G = S // 512
    f32 = np.float32
    x = np.asarray(inputs["x"], f32)
    c = np.asarray(inputs["c"], f32)
    pos = np.asarray(inputs["positions"], np.int32)
    shared = {
        "b_ada": np.ascontiguousarray(np.asarray(inputs["b_ada"], f32)[0][None, :]),
        "badac": col_layout(np.asarray(inputs["b_ada"], f32)[0]),
        "gcols": np.concatenate([col_layout(np.asarray(inputs[k], f32)[0]) for k in
                                 ("g_pre_mix", "g_post_mix", "g_pre_ffn", "g_post_ffn")], axis=1),
        "g_post_mix": np.ascontiguousarray(np.asarray(inputs["g_post_mix"], f32)[0][None, :]),
        "g_post_ffn": np.ascontiguousarray(np.asarray(inputs["g_post_ffn"], f32)[0][None, :]),
        "w_ada": np.ascontiguousarray(np.asarray(inputs["w_ada"], f32)[0]),
        "w_in": np.ascontiguousarray(np.asarray(inputs["w_in"], f32)[0]),
        "w_branch_a": np.ascontiguousarray(np.asarray(inputs["w_branch_a"], f32)[0]),
        "w_branch_b": np.ascontiguousarray(np.asarray(inputs["w_branch_b"], f32)[0]),
        "w_out": np.ascontiguousarray(np.asarray(inputs["w_out"], f32)[0]),
        "w_router": np.ascontiguousarray(np.asarray(inputs["w_router"], f32)[0][:, :NE]),
        "b_router": np.ascontiguousarray(np.asarray(inputs["b_router"], f32)[0][None, :NE]),
        "w1": np.ascontiguousarray(np.asarray(inputs["w1"], f32)[0][:NE]),
        "w2": np.ascontiguousarray(np.asarray(inputs["w2"], f32)[0][:NE]),
        "b2": np.ascontiguousarray(np.asarray(inputs["b2"], f32)[0][:NE]),
        "consts": make_consts(),
    }
    b1 = np.asarray(inputs["b1"], f32)[0][:NE]
    b1g = b1[:, 0::2].reshape(NE, KC, 128).transpose(0, 2, 1)
    b1l = b1[:, 1::2].reshape(NE, KC, 128).transpose(0, 2, 1)
    shared["b1c"] = np.ascontiguousarray(np.concatenate([b1g, b1l], axis=2))
    masks = [make_masks(0), make_masks(1)]
    in_maps, owns = [], []
    for b in batches:
        for half in range(2):
            toks = np.concatenate([np.arange(g * 512, (g + 1) * 512) for g in own_groups(G, half)])
            m = dict(shared)
            m["x_all"] = np.ascontiguousarray(x[b])
            m["x_own"] = np.ascontiguousarray(x[b][toks])
            m["pos_all"] = np.ascontiguousarray(pos[b][None, :])
            m["pos_own"] = np.ascontiguousarray(pos[b][toks][None, :])
            m["cvec"] = col_layout(c[b])
            m["cmask"], m["amask"] = masks[half]
            in_maps.append(m)
            owns.append((b, toks))
    return in_maps, owns


_CACHE = {}


def kernel(**inputs):
    x = np.asarray(inputs["x"])
    B, S, _ = x.shape
    cfg = {"S": S, "NE": 32, "CAP": 2048}
    in_maps, owns = prep(inputs, cfg, list(range(B)))
    nc = build(cfg)
    res = run_bass_kernel_spmd(nc, in_maps, core_ids=list(range(len(in_maps))))
    out = np.empty((B, S, D), np.float32)
    for r, (b, toks) in zip(res.results, owns):
        out[b, toks] = np.asarray(r["out"], np.float32)
    return out
```
